# Optimizing a Trainium2 kernel written in Bass

```python
import math
import jax
import jax.numpy as jnp
from jax import lax
import numpy as np

D_MODEL = 1024
BATCH = 8
SEQ = 2048
DEPTH = 4

GRID_W = 64
CTX_LEN = 256
MIX_WIDTH = D_MODEL
A_WIDTH = MIX_WIDTH // 2
B_WIDTH = MIX_WIDTH - A_WIDTH
SHORT_CONV_W = 3
CONFORMER_CONV_W = 31
AB_IN = 3 * A_WIDTH + 2 * B_WIDTH
C_HEAD_DIM = 64
C_V_DIM = 2 * C_HEAD_DIM
C_HEADS = (MIX_WIDTH // 2) // C_V_DIM
C_QK = C_HEADS * 2 * C_HEAD_DIM
C_V = C_HEADS * C_V_DIM
HY_WIDTH = MIX_WIDTH - C_V
CD_IN = 2 * C_QK + C_V + 3 * HY_WIDTH
Q_BLOCK = 128
ROPE_THETA = 10000.0
HY_BANDS = 16
HY_EMB = 2 * HY_BANDS + 1
HY_ORDER = 64
HY_MIN_DECAY = math.log(1e-2) / 1.5
HY_MAX_DECAY = math.log(1e-2) / 0.3
N_EXPERTS = 16
EXPERT_FF = 1024
EC_CAPACITY = 2
NORM_EPS = 1e-6
SUBLN_EPS = 1e-5

kernel_name = 'hybrid_diffusion_conv_diffattn_hyena_ec_trunk'


def rms_norm(x, g, eps=NORM_EPS):
    xf = x.astype(jnp.float32)
    y = xf * lax.rsqrt(jnp.mean(jnp.square(xf), axis=-1, keepdims=True) + eps)
    return (y * g.astype(jnp.float32)).astype(x.dtype)


def layer_norm(x, g, b, eps=NORM_EPS):
    xf = x.astype(jnp.float32)
    mu = jnp.mean(xf, axis=-1, keepdims=True)
    var = jnp.mean(jnp.square(xf - mu), axis=-1, keepdims=True)
    y = (xf - mu) * lax.rsqrt(var + eps)
    return (y * g.astype(jnp.float32) + b.astype(jnp.float32)).astype(x.dtype)


def modulate(h, shift, scale):
    return h * (1 + scale) + shift


def depthwise_conv(u, w):
    k = w.shape[0]
    pad = (k - 1) // 2
    return lax.conv_general_dilated(
        u, w[:, None, :].astype(u.dtype), window_strides=(1,), padding=[(pad, pad)],
        dimension_numbers=('NWC', 'WIO', 'NWC'), feature_group_count=u.shape[-1])


def conv_mixers(u, w_in, conv_a, conv_b, conv_b_bias, ln_g, ln_b, w_out):
    p = u @ w_in
    gate_out, gate_in, val, glu_a, glu_g = jnp.split(
        p, [A_WIDTH, 2 * A_WIDTH, 3 * A_WIDTH, 3 * A_WIDTH + B_WIDTH], axis=-1)
    y_a = gate_out * depthwise_conv(gate_in * val, conv_a)
    z = depthwise_conv(glu_a * jax.nn.sigmoid(glu_g), conv_b) + conv_b_bias
    y_b = jax.nn.silu(layer_norm(z, ln_g, ln_b))
    return jnp.concatenate([y_a, y_b], axis=-1) @ w_out


def axial_rope_angles(seq_len):
    rows = seq_len // GRID_W
    row = jnp.repeat(jnp.arange(rows), GRID_W).astype(jnp.float32)
    col = jnp.tile(jnp.arange(GRID_W), rows).astype(jnp.float32)
    n_freq = C_HEAD_DIM // 4
    inv = ROPE_THETA ** (-jnp.arange(n_freq, dtype=jnp.float32) / n_freq)
    return row[:, None] * inv[None], col[:, None] * inv[None]


def rope_half(x, ang):
    cos = jnp.cos(ang)[None, :, None, None, :].astype(x.dtype)
    sin = jnp.sin(ang)[None, :, None, None, :].astype(x.dtype)
    x1, x2 = jnp.split(x, 2, axis=-1)
    return jnp.concatenate([x1 * cos - x2 * sin, x1 * sin + x2 * cos], axis=-1)


def axial_rope(x, ang_row, ang_col):
    xr, xc = jnp.split(x, 2, axis=-1)
    return jnp.concatenate([rope_half(xr, ang_row), rope_half(xc, ang_col)], axis=-1)


def split_q(p_q, g_q):
    b, l, _ = p_q.shape
    return rms_norm(p_q.reshape(b, l, C_HEADS, 2, C_HEAD_DIM), g_q)


def split_kv(p_kv, g_k):
    b, l, _ = p_kv.shape
    k, v = jnp.split(p_kv, [C_QK], axis=-1)
    k = rms_norm(k.reshape(b, l, C_HEADS, 2, C_HEAD_DIM), g_k)
    return k, v.reshape(b, l, C_HEADS, C_V_DIM)


def diff_attention(q, k, v, lam):
    s = jnp.einsum('bqhjd,bkhjd->bhjqk', q, k, preferred_element_type=jnp.float32) * (C_HEAD_DIM ** -0.5)
    p = jax.nn.softmax(s, axis=-1)
    a = p[:, :, 0] - lam * p[:, :, 1]
    return jnp.einsum('bhqk,bkhe->bqhe', a.astype(v.dtype), v)


def diff_attention_blocked(q, k, v, lam):
    b, l, h, _, dh = q.shape
    nb = l // Q_BLOCK
    qb = jnp.moveaxis(q.reshape(b, nb, Q_BLOCK, h, 2, dh), 1, 0)
    o = lax.map(lambda qi: diff_attention(qi, k, v, lam), qb)
    return jnp.moveaxis(o, 0, 1).reshape(b, l, h, C_V_DIM)


def diff_head_out(o, g_sub, lam_init):
    b, l = o.shape[:2]
    return (rms_norm(o, g_sub, SUBLN_EPS) * (1.0 - lam_init)).reshape(b, l, C_V)


def hyena_filter(seq_len, w1, b1, freq, w2, b2, w3):
    f32 = jnp.float32
    t = jnp.arange(seq_len, dtype=f32)
    t_unit = jnp.linspace(0.0, 1.0, seq_len)[:, None]
    bands = jnp.linspace(1e-4, HY_BANDS - 1, HY_BANDS)
    ang = (2.0 * math.pi / seq_len) * t[:, None] * bands[None]
    z = jnp.concatenate([t_unit, jnp.cos(ang), -jnp.sin(ang)], axis=-1)
    fr = freq.astype(f32)
    hid = jnp.sin(fr * (z @ w1.astype(f32) + b1.astype(f32)))
    hid = jnp.sin(fr * (hid @ w2.astype(f32) + b2.astype(f32)))
    h = hid @ w3.astype(f32)
    centre = seq_len // 2
    dist = jnp.abs(t - centre) / max(centre, 1)
    decay = jnp.abs(jnp.linspace(HY_MIN_DECAY, HY_MAX_DECAY, h.shape[-1]))
    return h * jnp.exp(-dist[:, None] * decay[None])


def fft_long_conv(u, h, bias):
    l = u.shape[1]
    n = 2 * l
    c0 = l // 2
    uf = jnp.fft.rfft(u.astype(jnp.float32), n=n, axis=1)
    hf = jnp.fft.rfft(h, n=n, axis=0)
    y = jnp.fft.irfft(uf * hf[None], n=n, axis=1)[:, c0:c0 + l]
    return (y + u.astype(jnp.float32) * bias.astype(jnp.float32)).astype(u.dtype)


def hyena_mixer(p_hy, conv_d, filt, bias):
    z = depthwise_conv(p_hy, conv_d)
    gate_out, gate_in, val = jnp.split(z, 3, axis=-1)
    return gate_out * fft_long_conv(gate_in * val, filt, bias)


def expert_choice_ffn(h, w_router, w_gate, w_up, w_down):
    b, n, d = h.shape
    cap = max(1, (EC_CAPACITY * n) // N_EXPERTS)
    aff = jax.nn.softmax((h @ w_router).astype(jnp.float32), axis=-1)
    g, idx = lax.top_k(jnp.swapaxes(aff, 1, 2), cap)
    xe = jax.vmap(lambda hb, ib: hb[ib])(h, idx)
    a = jnp.einsum('becd,edf->becf', xe, w_gate)
    up = jnp.einsum('becd,edf->becf', xe, w_up)
    y = jnp.einsum('becf,efd->becd', jax.nn.silu(a) * up, w_down) * g[..., None].astype(h.dtype)
    return jax.vmap(lambda ib, yb: jnp.zeros((n, d), h.dtype).at[ib.reshape(-1)].add(yb.reshape(-1, d)))(idx, y)


def setup_inputs(seed: int = 0) -> dict:
    key = jax.random.key(seed)
    ks = iter(jax.random.split(key, 48))

    def nrm(shape, scale):
        return jax.random.normal(next(ks), shape, jnp.float32) * scale

    d = D_MODEL
    n_even = (DEPTH + 1) // 2
    n_odd = DEPTH // 2
    return {
        'x': nrm((BATCH, SEQ, d), 1.0),
        'c': nrm((BATCH, d), 1.0),
        'ctx': nrm((BATCH, CTX_LEN, d), 1.0),
        'c_ctx': nrm((d,), 1.0),
        'w_ada': nrm((DEPTH, d, 6 * d), 0.5 * d ** -0.5),
        'b_ada': nrm((DEPTH, 6 * d), 0.02),
        'g_mix': 1.0 + nrm((DEPTH, d), 0.1),
        'g_ffn': 1.0 + nrm((DEPTH, d), 0.1),
        'w_in_ab': nrm((n_even, d, AB_IN), d ** -0.5),
        'conv_a': nrm((n_even, SHORT_CONV_W, A_WIDTH), SHORT_CONV_W ** -0.5),
        'conv_b': nrm((n_even, CONFORMER_CONV_W, B_WIDTH), CONFORMER_CONV_W ** -0.5),
        'conv_b_bias': nrm((n_even, B_WIDTH), 0.02),
        'ln_b_g': 1.0 + nrm((n_even, B_WIDTH), 0.1),
        'ln_b_b': nrm((n_even, B_WIDTH), 0.02),
        'w_out_ab': nrm((n_even, MIX_WIDTH, d), MIX_WIDTH ** -0.5),
        'w_in_cd': nrm((n_odd, d, CD_IN), d ** -0.5),
        'g_q': 1.0 + nrm((n_odd, C_HEAD_DIM), 0.1),
        'g_k': 1.0 + nrm((n_odd, C_HEAD_DIM), 0.1),
        'lam_q1': nrm((n_odd, C_HEAD_DIM), 0.1),
        'lam_k1': nrm((n_odd, C_HEAD_DIM), 0.1),
        'lam_q2': nrm((n_odd, C_HEAD_DIM), 0.1),
        'lam_k2': nrm((n_odd, C_HEAD_DIM), 0.1),
        'g_subln': 1.0 + nrm((n_odd, C_V_DIM), 0.1),
        'conv_d': nrm((n_odd, SHORT_CONV_W, 3 * HY_WIDTH), SHORT_CONV_W ** -0.5),
        'hf_w1': nrm((n_odd, HY_EMB, HY_ORDER), HY_EMB ** -0.5),
        'hf_b1': nrm((n_odd, HY_ORDER), 0.1),
        'hf_freq': 1.0 + nrm((n_odd, HY_ORDER), 0.1),
        'hf_w2': nrm((n_odd, HY_ORDER, HY_ORDER), HY_ORDER ** -0.5),
        'hf_b2': nrm((n_odd, HY_ORDER), 0.1),
        'hf_w3': nrm((n_odd, HY_ORDER, HY_WIDTH), 0.02),
        'hf_bias': nrm((n_odd, HY_WIDTH), 0.1),
        'w_out_cd': nrm((n_odd, MIX_WIDTH, d), MIX_WIDTH ** -0.5),
        'w_router': nrm((DEPTH, d, N_EXPERTS), d ** -0.5),
        'w_gate': nrm((DEPTH, N_EXPERTS, d, EXPERT_FF), d ** -0.5),
        'w_up': nrm((DEPTH, N_EXPERTS, d, EXPERT_FF), d ** -0.5),
        'w_down': nrm((DEPTH, N_EXPERTS, EXPERT_FF, d), EXPERT_FF ** -0.5),
    }


def reference(x, c, ctx, c_ctx, w_ada, b_ada, g_mix, g_ffn, w_in_ab, conv_a, conv_b, conv_b_bias,
              ln_b_g, ln_b_b, w_out_ab, w_in_cd, g_q, g_k, lam_q1, lam_k1, lam_q2, lam_k2, g_subln,
              conv_d, hf_w1, hf_b1, hf_freq, hf_w2, hf_b2, hf_w3, hf_bias, w_out_cd,
              w_router, w_gate, w_up, w_down):
    f32 = jnp.float32
    seq_lat = x.shape[1]
    seq_ctx = ctx.shape[1]
    ang_row, ang_col = axial_rope_angles(seq_lat)
    cond_lat = jax.nn.silu(c)
    cond_ctx = jax.nn.silu(c_ctx)[None]
    h_lat, h_ctx = x, ctx
    kv_cols = slice(C_QK, 2 * C_QK + C_V)
    hy_cols = slice(2 * C_QK + C_V, CD_IN)
    for l in range(DEPTH):
        last = l == DEPTH - 1
        odd = l % 2 == 1
        i = l // 2
        mod_lat = jnp.split((cond_lat @ w_ada[l] + b_ada[l])[:, None, :], 6, axis=-1)
        u_lat = modulate(rms_norm(h_lat, g_mix[l]), mod_lat[0], mod_lat[1])
        if odd or not last:
            mod_ctx = jnp.split((cond_ctx @ w_ada[l] + b_ada[l])[:, None, :], 6, axis=-1)
            u_ctx = modulate(rms_norm(h_ctx, g_mix[l]), mod_ctx[0], mod_ctx[1])
        if not odd:
            ab = (w_in_ab[i], conv_a[i], conv_b[i], conv_b_bias[i], ln_b_g[i], ln_b_b[i], w_out_ab[i])
            y_lat = conv_mixers(u_lat, *ab)
            if not last:
                y_ctx = conv_mixers(u_ctx, *ab)
        else:
            lam_init = 0.8 - 0.6 * math.exp(-0.3 * l)
            lam = (jnp.exp(jnp.sum(lam_q1[i].astype(f32) * lam_k1[i].astype(f32)))
                   - jnp.exp(jnp.sum(lam_q2[i].astype(f32) * lam_k2[i].astype(f32))) + lam_init)
            w_in = w_in_cd[i]
            p_lat = u_lat @ w_in
            q_l = axial_rope(split_q(p_lat[..., :C_QK], g_q[i]), ang_row, ang_col)
            k_l, v_l = split_kv(p_lat[..., kv_cols], g_k[i])
            k_l = axial_rope(k_l, ang_row, ang_col)
            if last:
                k_c, v_c = split_kv(u_ctx @ w_in[:, kv_cols], g_k[i])
            else:
                p_ctx = u_ctx @ w_in
                q_c = split_q(p_ctx[..., :C_QK], g_q[i])
                k_c, v_c = split_kv(p_ctx[..., kv_cols], g_k[i])
            k_all = jnp.concatenate([k_c, k_l], axis=1)
            v_all = jnp.concatenate([v_c, v_l], axis=1)
            o_lat = diff_attention_blocked(q_l, k_all, v_all, lam)
            filt_lat = hyena_filter(seq_lat, hf_w1[i], hf_b1[i], hf_freq[i], hf_w2[i], hf_b2[i], hf_w3[i])
            y_hy_lat = hyena_mixer(p_lat[..., hy_cols], conv_d[i], filt_lat, hf_bias[i])
            y_lat = jnp.concatenate([diff_head_out(o_lat, g_subln[i], lam_init), y_hy_lat], axis=-1) @ w_out_cd[i]
            if not last:
                o_ctx = diff_attention(q_c, k_c, v_c, lam)
                filt_ctx = hyena_filter(seq_ctx, hf_w1[i], hf_b1[i], hf_freq[i], hf_w2[i], hf_b2[i], hf_w3[i])
                y_hy_ctx = hyena_mixer(p_ctx[..., hy_cols], conv_d[i], filt_ctx, hf_bias[i])
                y_ctx = jnp.concatenate([diff_head_out(o_ctx, g_subln[i], lam_init), y_hy_ctx], axis=-1) @ w_out_cd[i]
        h_lat = h_lat + mod_lat[2] * y_lat
        u2 = modulate(rms_norm(h_lat, g_ffn[l]), mod_lat[3], mod_lat[4])
        h_lat = h_lat + mod_lat[5] * expert_choice_ffn(u2, w_router[l], w_gate[l], w_up[l], w_down[l])
        if not last:
            h_ctx = h_ctx + mod_ctx[2] * y_ctx
            u2c = modulate(rms_norm(h_ctx, g_ffn[l]), mod_ctx[3], mod_ctx[4])
            h_ctx = h_ctx + mod_ctx[5] * expert_choice_ffn(u2c, w_router[l], w_gate[l], w_up[l], w_down[l])
    return h_lat
```

```python
import math
from contextlib import ExitStack

import numpy as np
import concourse.bass as bass
import concourse.mybir as mybir
from concourse.bass_utils import run_bass_kernel_spmd

F32 = mybir.dt.float32
BF16 = mybir.dt.bfloat16
I32 = mybir.dt.int32
U32 = mybir.dt.uint32
AF = mybir.ActivationFunctionType
ALU = mybir.AluOpType
AX = mybir.AxisListType

D = 1024
T = 2048
TC = 256
TT = T + TC
NTILE = TT // 128
DEPTH = 4
NE = 16
CAP_L = 256
CAP_C = 32
NSLOT = CAP_L + CAP_C
EPS = 1e-6

ENGS = ("pe", "dve", "act", "pool", "sp")
DMA_SLOTS = {"sp": 24, "act": 8, "pool": 24}


class Op:
    __slots__ = ("eng", "fn", "deps", "dma", "needs_inc", "sem", "val", "slot", "prev_slot_op")

    def __init__(self, eng, fn, dma):
        self.eng = eng
        self.fn = fn
        self.dma = dma
        self.deps = []
        self.needs_inc = False
        self.sem = None
        self.val = None
        self.slot = None
        self.prev_slot_op = None


class Prog:
    def __init__(self, nc):
        self.nc = nc
        self.ops = {e: [] for e in ENGS}
        self.last_w = {}
        self.readers = {}
        self.dma_rr = {q: 0 for q in DMA_SLOTS}
        self.slot_last = {}

    def _add(self, eng, fn, reads, writes, dma):
        op = Op(eng, fn, dma)
        deps = []
        for k in reads:
            w = self.last_w.get(k)
            if w is not None:
                deps.append(w)
        for k in writes:
            w = self.last_w.get(k)
            if w is not None:
                deps.append(w)
            deps.extend(self.readers.get(k, ()))
        seen = set()
        for d in deps:
            if id(d) in seen:
                continue
            seen.add(id(d))
            if (not dma) and (not d.dma) and d.eng == eng and eng == "pe":
                continue
            op.deps.append(d)
            d.needs_inc = True
        if dma:
            k = self.dma_rr[eng]
            self.dma_rr[eng] = (k + 1) % DMA_SLOTS[eng]
            op.slot = (eng, k)
            op.prev_slot_op = self.slot_last.get(op.slot)
            self.slot_last[op.slot] = op
        for k in reads:
            self.readers.setdefault(k, []).append(op)
        for k in writes:
            self.last_w[k] = op
            self.readers[k] = []
        self.ops[eng].append(op)
        return op

    def pe(self, fn, reads=(), writes=()):
        return self._add("pe", fn, reads, writes, False)

    def dve(self, fn, reads=(), writes=()):
        return self._add("dve", fn, reads, writes, False)

    def act(self, fn, reads=(), writes=()):
        return self._add("act", fn, reads, writes, False)

    def pool(self, fn, reads=(), writes=()):
        return self._add("pool", fn, reads, writes, False)

    def dma(self, fn, reads=(), writes=(), q="sp"):
        return self._add(q, fn, reads, writes, True)

    def barrier(self):
        lasts = []
        for e in ENGS:
            for op in reversed(self.ops[e]):
                if not op.dma:
                    lasts.append(op)
                    break
        lasts.extend(self.slot_last.values())
        for e in ENGS:
            op = Op(e, lambda eng: eng.nop(), False)
            for d in lasts:
                if d.eng == e and not d.dma and e == "pe":
                    continue
                op.deps.append(d)
                d.needs_inc = True
            self.ops[e].append(op)
        self.last_w = {}
        self.readers = {}

    def finalize(self, final_ops=()):
        nc = self.nc
        with ExitStack() as es:
            esem = {e: es.enter_context(nc.semaphore("s_" + e)) for e in ("pe", "dve", "act", "pool", "sp")}
            ssem = {}
            for q, n in DMA_SLOTS.items():
                for k in range(n):
                    ssem[(q, k)] = es.enter_context(nc.semaphore("d_%s%d" % (q, k)))
            for e in ENGS:
                cnt = 0
                for op in self.ops[e]:
                    if (not op.dma) and op.needs_inc:
                        cnt += 1
                        op.sem = esem[e]
                        op.val = cnt
            slot_cnt = {}
            for e in ENGS:
                for op in self.ops[e]:
                    if op.dma:
                        c = slot_cnt.get(op.slot, 0) + 16
                        slot_cnt[op.slot] = c
                        op.sem = ssem[op.slot]
                        op.val = c
            block = es.enter_context(nc.Block())
            finals = list(final_ops)

            def emit(e, engobj):
                waited = {}

                def w(s, v):
                    if waited.get(id(s), 0) >= v:
                        return
                    waited[id(s)] = v
                    engobj.wait_ge(s, v)

                for op in self.ops[e]:
                    for d in op.deps:
                        w(d.sem, d.val)
                    if op.dma and op.prev_slot_op is not None:
                        w(op.prev_slot_op.sem, op.prev_slot_op.val)
                    ins = op.fn(engobj)
                    if op.dma:
                        ins.then_inc(op.sem, 16)
                    elif op.needs_inc:
                        ins.then_inc(op.sem, 1)
                if e == "sp":
                    for f in finals:
                        w(f.sem, f.val)

            @block.tensor
            def _(eng):
                emit("pe", eng)

            @block.vector
            def _(eng):
                emit("dve", eng)

            @block.scalar
            def _(eng):
                emit("act", eng)

            @block.gpsimd
            def _(eng):
                emit("pool", eng)

            @block.sync
            def _(eng):
                emit("sp", eng)


def _tok_tiles():
    return [(0, TC)] + [(TC + i * 512, 512) for i in range(T // 512)]


SEQS = ((0, TC), (TC, T))


def _bf(a):
    import ml_dtypes
    return np.ascontiguousarray(np.asarray(a, dtype=np.float32).astype(ml_dtypes.bfloat16))


def _dft_tables(L):
    N = 3 * L // 2
    nf = N // 2
    npair = (nf + 127) // 128
    f = np.arange(npair * 128, dtype=np.float64)
    t = np.arange(L, dtype=np.float64)
    th = 2.0 * np.pi * np.outer(t, f + 0.5) / N
    valid = (f < nf)[None, :]
    fc = np.where(valid, np.cos(th), 0.0)
    fs = np.where(valid, -np.sin(th), 0.0)
    nt = L // 128
    fw = np.zeros((npair, 128, nt, 256))
    for i in range(npair):
        blkc = fc[:, i * 128:(i + 1) * 128].reshape(nt, 128, 128).transpose(1, 0, 2)
        blks = fs[:, i * 128:(i + 1) * 128].reshape(nt, 128, 128).transpose(1, 0, 2)
        fw[i, :, :, 0:128] = blkc
        fw[i, :, :, 128:256] = blks
    thi = 2.0 * np.pi * np.outer(f + 0.5, t + L // 2) / N
    validf = (f < nf)[:, None]
    ic = np.where(validf, (2.0 / N) * np.cos(thi), 0.0)
    isn = np.where(validf, -(2.0 / N) * np.sin(thi), 0.0)
    inv = np.stack([ic.reshape(npair, 128, L).transpose(1, 0, 2), isn.reshape(npair, 128, L).transpose(1, 0, 2)], 0)
    return fw, inv, npair


def _filter_tables(L):
    t = np.arange(L, dtype=np.float64)
    t_unit = np.linspace(0.0, 1.0, L)
    bands = np.linspace(1e-4, 16 - 1, 16)
    ang = (2.0 * np.pi / L) * t[:, None] * bands[None]
    z = np.concatenate([t_unit[:, None], np.cos(ang), -np.sin(ang)], axis=-1)
    centre = L // 2
    dist = np.abs(t - centre) / max(centre, 1)
    mn = math.log(1e-2) / 1.5
    mx = math.log(1e-2) / 0.3
    decay = np.abs(np.linspace(mn, mx, 512))
    win = np.exp(-dist[:, None] * decay[None])
    return z.T, win


def host_constants():
    c = {}
    c["ident"] = np.eye(128, dtype=np.float32)
    tt = np.arange(T)
    row = (tt // 64).astype(np.float64)
    col = (tt % 64).astype(np.float64)
    inv = 10000.0 ** (-np.arange(16, dtype=np.float64) / 16)
    cos = np.zeros((128, T))
    sin = np.zeros((128, T))
    for p in range(128):
        d = p % 64
        pos = row if d < 32 else col
        a = pos * inv[d % 16]
        cos[p] = np.cos(a)
        sin[p] = np.sin(a)
    c["ropec"] = _bf(cos)
    c["ropes"] = _bf(sin)
    rt = np.zeros((128, 128))
    for m in range(128):
        if m % 32 < 16:
            rt[m + 16, m] = -1.0
        else:
            rt[m - 16, m] = 1.0
    c["ropert"] = _bf(rt)
    bo = np.zeros((128, 128), dtype=np.float32)
    bo[0:64, 0:64] = 1.0
    bo[64:128, 64:128] = 1.0
    c["blockones"] = bo
    zl, wl = _filter_tables(T)
    zc, wc = _filter_tables(TC)
    c["zT"] = np.ascontiguousarray(np.concatenate([zc, zl], axis=1).astype(np.float32))
    c["win"] = np.ascontiguousarray(np.concatenate([wc, wl], axis=0).astype(np.float32))
    fwl, invl, _ = _dft_tables(T)
    fwc, invc, _ = _dft_tables(TC)
    c["fwl"] = _bf(fwl)
    c["fwc"] = _bf(fwc)
    il = invl.reshape(2, 128, 12, 4, 512).transpose(3, 0, 1, 2, 4)
    c["invl"] = _bf(il)
    c["invc"] = _bf(invc)
    return c


def build(n_layers=DEPTH, dbg=False):
    nc = bass.Bass("TRN2", target_bir_lowering=False)

    def din(name, shape, dt=F32):
        return nc.dram_tensor(name, list(shape), dt, kind="ExternalInput").ap()

    def dint(name, shape, dt=F32):
        return nc.dram_tensor(name, list(shape), dt, kind="Internal").ap()

    x_d = din("x", [T, D])
    ctx_d = din("ctx", [TC, D])
    cT_d = din("cT", [128, 8, 2])
    w_ada_d = din("w_ada", [DEPTH, D, 6 * D])
    b_ada_d = din("b_ada", [DEPTH, 6 * D])
    g_mix_d = din("g_mix", [DEPTH, D])
    g_ffn_d = din("g_ffn", [DEPTH, D])
    w_in_ab_d = din("w_in_ab", [2, D, 2560])
    ca_d = din("ca", [2, 128, 4, 3])
    cb_d = din("cb", [2, 128, 4, 31])
    lnp_d = din("lnp", [2, 128, 3, 4])
    w_out_ab_d = din("w_out_ab", [2, D, D])
    wr_d = din("wr", [DEPTH, 128, 8, NE])
    w_gate_d = din("w_gate", [DEPTH, NE, D, D])
    w_up_d = din("w_up", [DEPTH, NE, D, D])
    w_down_d = din("w_down", [DEPTH, NE, D, D])
    ident_d = din("ident", [128, 128])
    w_in_cd_d = din("w_in_cd", [2, D, 3072])
    w_out_cd_d = din("w_out_cd", [2, D, D])
    gqk_d = din("gqk", [2, 128, 2])
    lamv_d = din("lamv", [2, 4 * 64])
    gsub_d = din("gsub", [2, 128, 1])
    cd_d = din("cd", [2, 128, 12, 3])
    hfw1_d = din("hf_w1", [2, 33, 64])
    hfv_d = din("hfv", [2, 64, 3])
    hfw2_d = din("hf_w2", [2, 64, 64])
    hfw3_d = din("hf_w3", [2, 64, 512])
    hfb_d = din("hf_bias", [2, 512])
    ropec_d = din("ropec", [128, T], BF16)
    ropes_d = din("ropes", [128, T], BF16)
    ropert_d = din("ropert", [128, 128], BF16)
    blockones_d = din("blockones", [128, 128])
    zT_d = din("zT", [33, TT])
    win_d = din("win", [TT, 512])
    fwl_d = din("fwl", [12, 128, 16, 256], BF16)
    fwc_d = din("fwc", [2, 128, 2, 256], BF16)
    invl_d = din("invl", [4, 2, 128, 12, 512], BF16)
    invc_d = din("invc", [2, 128, 2, 256], BF16)
    out_d = nc.dram_tensor("out", [T, D], F32, kind="ExternalOutput").ap()

    H_d = dint("H", [TT, D])
    modv_d = dint("modv", [2, 6 * D])
    u2tok_d = dint("u2tok", [TT, D], BF16)
    M_d = dint("Macc", [TT, D])
    if dbg:
        hdbg_d = nc.dram_tensor("hdbg", [TT, D], F32, kind="ExternalOutput").ap()
        dbg_ix_d = nc.dram_tensor("dbg_ix", [NE, NSLOT], F32, kind="ExternalOutput").ap()
        dbg_gv_d = nc.dram_tensor("dbg_gv", [NE, NSLOT], F32, kind="ExternalOutput").ap()
        dbg_m_d = nc.dram_tensor("dbg_m", [TT, D], F32, kind="ExternalOutput").ap()
        dbg_xe_d = nc.dram_tensor("dbg_xe", [128, 3, D], BF16, kind="ExternalOutput").ap()
        dbg_yb_d = nc.dram_tensor("dbg_yb", [128, 3, D], F32, kind="ExternalOutput").ap()
        dbg_ht_d = nc.dram_tensor("dbg_ht", [128, 8, NSLOT], BF16, kind="ExternalOutput").ap()
        dbg_ip_d = nc.dram_tensor("dbg_ip", [128, 3, NE], I32, kind="ExternalOutput").ap()

    top = ExitStack()
    P = Prog(nc)

    uid = [0]

    def sb(es, name, shape, dt=F32):
        uid[0] += 1
        return es.enter_context(nc.sbuf_tensor("%s_%d" % (name, uid[0]), list(shape), dt))

    PS = [top.enter_context(nc.psum_tensor("ps%d" % i, [128, 512], F32)) for i in range(8)]
    PK = ["ps%d" % i for i in range(8)]

    ident_f = sb(top, "ident_f", [128, 128])
    ident_b = sb(top, "ident_b", [128, 128], BF16)
    ones_f = sb(top, "ones_f", [128, 128])
    ones_b = sb(top, "ones_b", [128, 128], BF16)
    zero_f = sb(top, "zero_f", [128, 1024])
    P.dma(lambda e: e.dma_start(out=ident_f[:], in_=ident_d), writes=["ident_f"])
    P.dve(lambda e: e.tensor_copy(ident_b[:], ident_f[:]), reads=["ident_f"], writes=["ident_b"])
    P.dve(lambda e: e.memset(ones_f[:], 1.0), writes=["ones_f"])
    P.dve(lambda e: e.memset(ones_b[:], 1.0), writes=["ones_b"])
    P.dve(lambda e: e.memset(zero_f[:], 0.0), writes=["zero_f"])

    def hsrc(l, tt):
        if l == 0:
            if tt < 2:
                return ctx_d[tt * 128:(tt + 1) * 128, :]
            return x_d[(tt - 2) * 128:(tt - 1) * 128, :]
        return H_d[tt * 128:(tt + 1) * 128, :]

    def hkey(tt):
        return ("H", tt)

    def phase_mod(l):
        with ExitStack() as es:
            cS = sb(es, "cS", [128, 8, 2])
            modr = sb(es, "modr", [2, 6 * D])
            brow = sb(es, "brow", [2, 6 * D])
            grow = sb(es, "grow", [2, 2, D])
            wbuf = [sb(es, "wada%d" % i, [128, 8, 512]) for i in range(2)]
            P.dma(lambda e: e.dma_start(out=cS[:], in_=cT_d), writes=["cS"])
            P.act(lambda e: e.activation(out=cS[:], in_=cS[:], func=AF.Silu), reads=["cS"], writes=["cS"])
            for r in range(2):
                P.dma(lambda e, r=r: e.dma_start(out=brow[r:r + 1, :], in_=b_ada_d[l:l + 1, :]), writes=["brow"])
                P.dma(lambda e, r=r: e.dma_start(out=grow[r:r + 1, 0, :], in_=g_mix_d[l:l + 1, :]), writes=["grow"])
                P.dma(lambda e, r=r: e.dma_start(out=grow[r:r + 1, 1, :], in_=g_ffn_d[l:l + 1, :]), writes=["grow"])
            wv = w_ada_d[l].rearrange("(k p) n -> p k n", p=128)
            for nt in range(12):
                wb = wbuf[nt % 2]
                wk = "wada%d" % (nt % 2)
                P.dma(lambda e, wb=wb, nt=nt: e.dma_start(out=wb[:], in_=wv[:, :, nt * 512:(nt + 1) * 512]),
                      writes=[wk])
                pk = nt % 2
                for k in range(8):
                    P.pe(lambda e, wb=wb, k=k, pk=pk: e.matmul(PS[pk][0:2, :], cS[:, k, :], wb[:, k, :],
                                                                  start=(k == 0), stop=(k == 7)),
                         reads=[wk, "cS"], writes=[PK[pk]])
                P.dve(lambda e, nt=nt, pk=pk: e.tensor_tensor(out=modr[:, nt * 512:(nt + 1) * 512], in0=PS[pk][0:2, :],
                                                               in1=brow[:, nt * 512:(nt + 1) * 512], op=ALU.add),
                      reads=[PK[pk], "brow"], writes=["modr"])
            for (seg, gi) in ((1, 0), (4, 1)):
                P.dve(lambda e, seg=seg, gi=gi: e.scalar_tensor_tensor(
                    out=modr[:, seg * D:(seg + 1) * D], in0=modr[:, seg * D:(seg + 1) * D], scalar=1.0,
                    in1=grow[:, gi, :], op0=ALU.add, op1=ALU.mult), reads=["modr", "grow"], writes=["modr"])
            P.dma(lambda e: e.dma_start(out=modv_d, in_=modr[:]), reads=["modr"], writes=["modv"])
        P.barrier()

    def load_mod(tile, key, r, seg):
        P.dma(lambda e: e.dma_start(out=tile[:], in_=modv_d[r:r + 1, seg * D:(seg + 1) * D].partition_broadcast(128)),
              reads=["modv"], writes=[key])

    def phase_norm(l, hl, which, U, AFF=None):
        seg_s, seg_a = (0, 1) if which == 1 else (3, 4)
        with ExitStack() as es:
            A = [sb(es, "nA%d" % r, [128, D]) for r in range(2)]
            S = [sb(es, "nS%d" % r, [128, D]) for r in range(2)]
            for r in range(2):
                load_mod(A[r], "nA%d" % r, r, seg_a)
                load_mod(S[r], "nS%d" % r, r, seg_s)
            xt = [sb(es, "nx%d" % i, [128, D]) for i in range(2)]
            junk = sb(es, "njunk", [128, D], BF16)
            ss = [sb(es, "nss%d" % i, [128, 2]) for i in range(2)]
            tmp = [sb(es, "ntmp%d" % i, [128, D]) for i in range(2)]
            if which == 1:
                ub = [sb(es, "nub%d" % i, [128, D], BF16) for i in range(2)]
            else:
                ub = [sb(es, "nub%d" % i, [128, D], BF16) for i in range(2)]
                uT = [sb(es, "nuT%d" % i, [128, 8, 128]) for i in range(2)]
                wr = sb(es, "nwr", [128, 8, NE])
                sm = [sb(es, "nsm%d" % i, [128, 4]) for i in range(2)]
                ex = [sb(es, "nex%d" % i, [128, NE]) for i in range(2)]
                P.dma(lambda e: e.dma_start(out=wr[:], in_=wr_d[l]), writes=["nwr"])
            for tt in range(NTILE):
                b = tt % 2
                r = 1 if tt < 2 else 0
                kx, kss, ktmp, kub = "nx%d" % b, "nss%d" % b, "ntmp%d" % b, "nub%d" % b
                P.dma(lambda e, b=b, tt=tt: e.dma_start(out=xt[b][:], in_=hsrc(hl, tt)), reads=[hkey(tt)], writes=[kx])
                P.act(lambda e, b=b: e.activation(out=junk[:], in_=xt[b][:], func=AF.Square, accum_out=ss[b][:, 0:1]),
                      reads=[kx], writes=["njunk", kss])
                P.act(lambda e, b=b: e.activation(out=ss[b][:, 1:2], in_=ss[b][:, 0:1], func=AF.Sqrt,
                                                  scale=1.0 / D, bias=EPS), reads=[kss], writes=[kss])
                P.dve(lambda e, b=b: e.reciprocal(ss[b][:, 1:2], ss[b][:, 1:2]), reads=[kss], writes=[kss])
                P.dve(lambda e, b=b, r=r: e.scalar_tensor_tensor(out=tmp[b][:], in0=xt[b][:], scalar=ss[b][:, 1:2],
                                                                   in1=A[r][:], op0=ALU.mult, op1=ALU.mult),
                      reads=[kx, kss, "nA%d" % r], writes=[ktmp])
                if which == 1:
                    P.pool(lambda e, b=b, r=r: e.tensor_tensor(out=ub[b][:], in0=tmp[b][:], in1=S[r][:], op=ALU.add),
                           reads=[ktmp, "nS%d" % r], writes=[kub])
                    psb = 2 + b
                    pv = PS[psb][:].bitcast(BF16)
                    for k in range(8):
                        P.pe(lambda e, b=b, k=k, pv=pv: e.transpose(pv[:, k * 128:(k + 1) * 128],
                                                                     ub[b][:, k * 128:(k + 1) * 128], ident_b[:]),
                             reads=[kub, "ident_b"], writes=[PK[psb]])
                    P.act(lambda e, tt=tt, pv=pv: e.activation(
                        out=U[:, :, tt * 128:(tt + 1) * 128], in_=pv.rearrange("p (k t) -> p k t", k=8), func=AF.Copy),
                        reads=[PK[psb]], writes=[("U", tt)])
                else:
                    P.pool(lambda e, b=b, r=r: e.tensor_tensor(out=tmp[b][:], in0=tmp[b][:], in1=S[r][:], op=ALU.add),
                           reads=[ktmp, "nS%d" % r], writes=[ktmp])
                    P.act(lambda e, b=b: e.activation(out=ub[b][:], in_=tmp[b][:], func=AF.Copy),
                          reads=[ktmp], writes=[kub])
                    P.dma(lambda e, b=b, tt=tt: e.dma_start(out=u2tok_d[tt * 128:(tt + 1) * 128, :], in_=ub[b][:]),
                          reads=[kub], writes=[("u2tok", tt)])
                    kuT = "nuT%d" % b
                    for hh in range(2):
                        psb = 2 + 2 * b + hh
                        for k4 in range(4):
                            k = hh * 4 + k4
                            P.pe(lambda e, b=b, k=k, k4=k4, psb=psb: e.transpose(
                                PS[psb][:, k4 * 128:(k4 + 1) * 128], tmp[b][:, k * 128:(k + 1) * 128], ident_f[:]),
                                reads=[ktmp, "ident_f"], writes=[PK[psb]])
                        P.dve(lambda e, b=b, hh=hh, psb=psb: e.tensor_copy(
                            uT[b][:, hh * 4:(hh + 1) * 4, :], PS[psb][:].rearrange("p (k t) -> p k t", k=4)),
                            reads=[PK[psb]], writes=[kuT])
                    pl = 6 + b
                    for k in range(8):
                        P.pe(lambda e, b=b, k=k, pl=pl: e.matmul(PS[pl][:, 0:NE], uT[b][:, k, :], wr[:, k, :],
                                                                   start=(k == 0), stop=(k == 7)),
                             reads=[kuT, "nwr"], writes=[PK[pl]])
                    ksm, kex = "nsm%d" % b, "nex%d" % b
                    P.dve(lambda e, b=b, pl=pl: e.reduce_max(out=sm[b][:, 0:1], in_=PS[pl][:, 0:NE], axis=AX.X),
                          reads=[PK[pl]], writes=[ksm])
                    P.dve(lambda e, b=b: e.tensor_scalar(out=sm[b][:, 1:2], in0=sm[b][:, 0:1], scalar1=-1.0,
                                                         scalar2=None, op0=ALU.mult), reads=[ksm], writes=[ksm])
                    P.act(lambda e, b=b, pl=pl: e.activation(out=ex[b][:], in_=PS[pl][:, 0:NE], func=AF.Exp,
                                                             bias=sm[b][:, 1:2], scale=1.0, accum_out=sm[b][:, 2:3]),
                          reads=[PK[pl], ksm], writes=[kex, ksm])
                    P.dve(lambda e, b=b: e.reciprocal(sm[b][:, 3:4], sm[b][:, 2:3]), reads=[ksm], writes=[ksm])
                    P.dve(lambda e, b=b, tt=tt: e.tensor_scalar(out=AFF[:, tt, :], in0=ex[b][:], scalar1=sm[b][:, 3:4],
                                                                scalar2=None, op0=ALU.mult),
                          reads=[kex, ksm], writes=[("AFF", tt)])
        P.barrier()

    def phase_outproj(l, Y, w_out_ap, last_layer_unused=False):
        with ExitStack() as es:
            Wo = sb(es, "Wo", [128, 8, D], BF16)
            G = [sb(es, "oG%d" % r, [128, D]) for r in range(2)]
            xt = [sb(es, "ox%d" % i, [128, D]) for i in range(2)]
            tm = [sb(es, "ot%d" % i, [128, D]) for i in range(2)]
            wv = w_out_ap.rearrange("(k p) n -> p k n", p=128)
            for k in range(8):
                P.dma(lambda e, k=k: e.dma_start(out=Wo[:, k, :], in_=wv[:, k, :]), writes=[("Wo", k)], q="pool")
            for r in range(2):
                load_mod(G[r], "oG%d" % r, r, 2)
            for tt in range(NTILE):
                b = tt % 2
                r = 1 if tt < 2 else 0
                kx, kt = "ox%d" % b, "ot%d" % b
                P.dma(lambda e, b=b, tt=tt: e.dma_start(out=xt[b][:], in_=hsrc(l, tt)), reads=[hkey(tt)], writes=[kx])
                for hf in range(2):
                    pb = 2 * b + hf
                    for k in range(8):
                        P.pe(lambda e, k=k, tt=tt, hf=hf, pb=pb: e.matmul(
                            PS[pb][:], Y[:, k, tt * 128:(tt + 1) * 128], Wo[:, k, hf * 512:(hf + 1) * 512],
                            start=(k == 0), stop=(k == 7)), reads=[("Y", tt), ("Wo", k)], writes=[PK[pb]])
                    P.dve(lambda e, b=b, hf=hf, pb=pb, r=r: e.tensor_tensor(
                        out=tm[b][:, hf * 512:(hf + 1) * 512], in0=PS[pb][:], in1=G[r][:, hf * 512:(hf + 1) * 512],
                        op=ALU.mult), reads=[PK[pb], "oG%d" % r], writes=[kt])
                P.pool(lambda e, b=b: e.tensor_tensor(out=xt[b][:], in0=xt[b][:], in1=tm[b][:], op=ALU.add),
                       reads=[kx, kt], writes=[kx])
                P.dma(lambda e, b=b, tt=tt: e.dma_start(out=H_d[tt * 128:(tt + 1) * 128, :], in_=xt[b][:]),
                      reads=[kx], writes=[hkey(tt)])
        P.barrier()

    def phase_conv(l, U, Y):
        i2 = l // 2
        tiles = _tok_tiles()
        with ExitStack() as es:
            Wg = [sb(es, "Wg%d" % g, [128, 8, 512], BF16) for g in range(5)]
            wv = w_in_ab_d[i2].rearrange("(k p) n -> p k n", p=128)
            for g in range(5):
                for k in range(8):
                    P.dma(lambda e, g=g, k=k: e.dma_start(out=Wg[g][:, k, :], in_=wv[:, k, g * 512:(g + 1) * 512]),
                          writes=[("Wg", g)], q="pool")
            CA = sb(es, "CA", [128, 4, 3])
            CB = sb(es, "CB", [128, 4, 31])
            LNP = sb(es, "LNP", [128, 3, 4])
            P.dma(lambda e: e.dma_start(out=CA[:], in_=ca_d[i2]), writes=["CA"])
            P.dma(lambda e: e.dma_start(out=CB[:], in_=cb_d[i2]), writes=["CB"])
            P.dma(lambda e: e.dma_start(out=LNP[:], in_=lnp_d[i2]), writes=["LNP"])
            B = [sb(es, "cB%d" % i, [128, TT]) for i in range(3)]
            ZC = sb(es, "ZC", [128, 4, TT])

            psrr = [0]

            def proj(chunk, dst, dkey):
                g, c = chunk // 4, chunk % 4
                for (t0, n) in tiles:
                    pb = psrr[0] % 4
                    psrr[0] += 1
                    for k in range(8):
                        P.pe(lambda e, g=g, c=c, k=k, t0=t0, n=n, pb=pb: e.matmul(
                            PS[pb][:, 0:n], Wg[g][:, k, c * 128:(c + 1) * 128], U[:, k, t0:t0 + n],
                            start=(k == 0), stop=(k == 7)),
                            reads=[("Wg", g)] + [("U", t) for t in range(t0 // 128, (t0 + n) // 128)],
                            writes=[PK[pb]])
                    P.act(lambda e, t0=t0, n=n, pb=pb: e.activation(out=dst[:, t0:t0 + n], in_=PS[pb][:, 0:n],
                                                                      func=AF.Copy), reads=[PK[pb]], writes=[dkey])

            for i in range(4):
                proj(i, B[0], "cB0")
                proj(4 + i, B[1], "cB1")
                proj(8 + i, B[2], "cB2")
                P.dve(lambda e: e.tensor_tensor(out=B[1][:], in0=B[1][:], in1=B[2][:], op=ALU.mult),
                      reads=["cB1", "cB2"], writes=["cB1"])
                P.dve(lambda e, i=i: e.tensor_scalar(out=B[2][:], in0=B[1][:], scalar1=CA[:, i, 1:2], scalar2=None,
                                                      op0=ALU.mult), reads=["cB1", "CA"], writes=["cB2"])
                for (s0, L) in SEQS:
                    P.dve(lambda e, i=i, s0=s0, L=L: e.scalar_tensor_tensor(
                        out=B[2][:, s0 + 1:s0 + L], in0=B[1][:, s0:s0 + L - 1], scalar=CA[:, i, 0:1],
                        in1=B[2][:, s0 + 1:s0 + L], op0=ALU.mult, op1=ALU.add), reads=["cB1", "cB2", "CA"],
                        writes=["cB2"])
                    P.dve(lambda e, i=i, s0=s0, L=L: e.scalar_tensor_tensor(
                        out=B[2][:, s0:s0 + L - 1], in0=B[1][:, s0 + 1:s0 + L], scalar=CA[:, i, 2:3],
                        in1=B[2][:, s0:s0 + L - 1], op0=ALU.mult, op1=ALU.add), reads=["cB1", "cB2", "CA"],
                        writes=["cB2"])
                P.dve(lambda e, i=i: e.tensor_tensor(out=Y[:, i, :], in0=B[0][:], in1=B[2][:], op=ALU.mult),
                      reads=["cB0", "cB2"], writes=[("Y", t) for t in range(NTILE)])
            for i in range(4):
                proj(12 + i, B[0], "cB0")
                proj(16 + i, B[1], "cB1")
                P.act(lambda e: e.activation(out=B[1][:], in_=B[1][:], func=AF.Sigmoid), reads=["cB1"], writes=["cB1"])
                P.dve(lambda e: e.tensor_tensor(out=B[0][:], in0=B[0][:], in1=B[1][:], op=ALU.mult),
                      reads=["cB0", "cB1"], writes=["cB0"])
                zk = ("ZC", i)
                P.dve(lambda e, i=i: e.tensor_scalar(out=ZC[:, i, :], in0=B[0][:], scalar1=CB[:, i, 15:16],
                                                      scalar2=LNP[:, 0, i:i + 1], op0=ALU.mult, op1=ALU.add),
                      reads=["cB0", "CB", "LNP"], writes=[zk])
                for kk in range(31):
                    o = kk - 15
                    if o == 0:
                        continue
                    for (s0, L) in SEQS:
                        if o > 0:
                            oa, ia, n = s0, s0 + o, L - o
                        else:
                            oa, ia, n = s0 - o, s0, L + o
                        P.dve(lambda e, i=i, kk=kk, oa=oa, ia=ia, n=n: e.scalar_tensor_tensor(
                            out=ZC[:, i, oa:oa + n], in0=B[0][:, ia:ia + n], scalar=CB[:, i, kk:kk + 1],
                            in1=ZC[:, i, oa:oa + n], op0=ALU.mult, op1=ALU.add), reads=["cB0", zk, "CB"], writes=[zk])
            MEAN = B[1]
            RSTD = B[2]
            SQ = [sb(es, "cSQ%d" % i, [128, 512]) for i in range(2)]
            sqi = 0
            for (t0, n) in tiles:
                for i in range(4):
                    sq = SQ[sqi % 2]
                    sqk = "cSQ%d" % (sqi % 2)
                    sqi += 1
                    P.act(lambda e, i=i, t0=t0, n=n, sq=sq: e.activation(out=sq[:, 0:n], in_=ZC[:, i, t0:t0 + n],
                                                                          func=AF.Square), reads=[("ZC", i)], writes=[sqk])
                    P.pe(lambda e, i=i, t0=t0, n=n: e.matmul(PS[4][:, 0:n], ones_f[:], ZC[:, i, t0:t0 + n],
                                                             start=(i == 0), stop=(i == 3)),
                         reads=[("ZC", i), "ones_f"], writes=[PK[4]])
                    P.pe(lambda e, i=i, n=n, sq=sq: e.matmul(PS[5][:, 0:n], ones_f[:], sq[:, 0:n],
                                                              start=(i == 0), stop=(i == 3)),
                         reads=[sqk, "ones_f"], writes=[PK[5]])
                P.dve(lambda e, t0=t0, n=n: e.tensor_scalar(out=MEAN[:, t0:t0 + n], in0=PS[4][:, 0:n], scalar1=1.0 / 512,
                                                             scalar2=None, op0=ALU.mult), reads=[PK[4]], writes=["cB1"])
                P.dve(lambda e, t0=t0, n=n: e.tensor_tensor(out=B[0][:, t0:t0 + n], in0=MEAN[:, t0:t0 + n],
                                                             in1=MEAN[:, t0:t0 + n], op=ALU.mult),
                      reads=["cB1"], writes=["cB0"])
                P.dve(lambda e, t0=t0, n=n: e.scalar_tensor_tensor(
                    out=RSTD[:, t0:t0 + n], in0=PS[5][:, 0:n], scalar=1.0 / 512, in1=B[0][:, t0:t0 + n],
                    op0=ALU.mult, op1=ALU.subtract), reads=[PK[5], "cB0"], writes=["cB2"])
            P.act(lambda e: e.activation(out=RSTD[:], in_=RSTD[:], func=AF.Sqrt, scale=1.0, bias=EPS),
                  reads=["cB2"], writes=["cB2"])
            P.dve(lambda e: e.reciprocal(RSTD[:], RSTD[:]), reads=["cB2"], writes=["cB2"])
            for i in range(4):
                zk = ("ZC", i)
                P.dve(lambda e, i=i: e.tensor_tensor(out=ZC[:, i, :], in0=ZC[:, i, :], in1=MEAN[:], op=ALU.subtract),
                      reads=[zk, "cB1"], writes=[zk])
                P.dve(lambda e, i=i: e.tensor_tensor(out=ZC[:, i, :], in0=ZC[:, i, :], in1=RSTD[:], op=ALU.mult),
                      reads=[zk, "cB2"], writes=[zk])
                P.act(lambda e, i=i: e.activation(out=Y[:, 4 + i, :], in_=ZC[:, i, :], func=AF.Silu,
                                                  scale=LNP[:, 1, i:i + 1], bias=LNP[:, 2, i:i + 1]),
                      reads=[zk, "LNP"], writes=[("Y", t) for t in range(NTILE)])
        P.barrier()


    def load_wgroup(Wt, key, src3, g):
        for k in range(8):
            P.dma(lambda e, k=k: e.dma_start(out=Wt[:, k, :], in_=src3[:, k, g * 512:(g + 1) * 512]),
                  writes=[(key, k)], q="pool")

    def phase_attn(l, U, Y):
        i2 = l // 2
        lam_init = 0.8 - 0.6 * math.exp(-0.3 * l)
        tiles = _tok_tiles()
        wv = w_in_cd_d[i2].rearrange("(k p) n -> p k n", p=128)
        with ExitStack() as es:
            qkT = [sb(es, "qT", [128, 4, TT], BF16), sb(es, "kT", [128, 4, TT], BF16)]
            V = sb(es, "V", [128, NTILE, 512], BF16)
            small = sb(es, "asmall", [128, 8])
            lamb = sb(es, "lamb", [128, 4 * 64])
            GQK = sb(es, "GQK", [128, 2])
            GS = sb(es, "GS", [128, 1])
            with ExitStack() as es2:
                Wt = [sb(es2, "Wt%d" % i, [128, 8, 512], BF16) for i in range(2)]
                COS = sb(es2, "COS", [128, T], BF16)
                SIN = sb(es2, "SIN", [128, T], BF16)
                RT = sb(es2, "RT", [128, 128], BF16)
                BO = sb(es2, "BO", [128, 128])
                QK = sb(es2, "QK", [128, TT])
                SQ = [sb(es2, "aSQ%d" % i, [128, 512]) for i in range(2)]
                RS = [sb(es2, "aRS%d" % i, [128, 512]) for i in range(2)]
                QN = [sb(es2, "aQN%d" % i, [128, 512], BF16) for i in range(2)]
                O1 = [sb(es2, "aO1%d" % i, [128, 512]) for i in range(2)]
                P.dma(lambda e: e.dma_start(out=COS[:], in_=ropec_d), writes=["COS"])
                P.dma(lambda e: e.dma_start(out=SIN[:], in_=ropes_d), writes=["SIN"])
                P.dma(lambda e: e.dma_start(out=RT[:], in_=ropert_d), writes=["RT"])
                P.dma(lambda e: e.dma_start(out=BO[:], in_=blockones_d), writes=["BO"])
                P.dma(lambda e: e.dma_start(out=GQK[:], in_=gqk_d[i2]), writes=["GQK"])
                P.dma(lambda e: e.dma_start(out=GS[:], in_=gsub_d[i2]), writes=["GS"])
                P.dma(lambda e: e.dma_start(out=lamb[:], in_=lamv_d[i2:i2 + 1, :].partition_broadcast(128)),
                      writes=["lamb"])
                for j in range(2):
                    P.dve(lambda e, j=j: e.tensor_tensor(out=lamb[:, j * 128:j * 128 + 64], in0=lamb[:, j * 128:j * 128 + 64],
                                                          in1=lamb[:, j * 128 + 64:j * 128 + 128], op=ALU.mult),
                          reads=["lamb"], writes=["lamb"])
                    P.dve(lambda e, j=j: e.reduce_sum(out=small[:, j:j + 1], in_=lamb[:, j * 128:j * 128 + 64], axis=AX.X),
                          reads=["lamb"], writes=["asmall"])
                P.act(lambda e: e.activation(out=small[:, 2:4], in_=small[:, 0:2], func=AF.Exp),
                      reads=["asmall"], writes=["asmall"])
                P.dve(lambda e: e.scalar_tensor_tensor(out=small[:, 4:5], in0=small[:, 3:4], scalar=-lam_init,
                                                        in1=small[:, 2:3], op0=ALU.add, op1=ALU.subtract),
                      reads=["asmall"], writes=["asmall"])
                P.dve(lambda e: e.tensor_scalar(out=GS[:], in0=GS[:], scalar1=1.0 - lam_init, scalar2=None, op0=ALU.mult),
                      reads=["GS"], writes=["GS"])
                cnt = 0
                for g in range(2):
                    wt = Wt[g % 2]
                    wkey = "Wt%d" % (g % 2)
                    load_wgroup(wt, wkey, wv, g)
                    for h in range(4):
                        for (t0, n) in tiles:
                            pb = cnt % 2
                            bb = cnt % 2
                            cnt += 1
                            for k in range(8):
                                P.pe(lambda e, wt=wt, h=h, k=k, t0=t0, n=n, pb=pb: e.matmul(
                                    PS[pb][:, 0:n], wt[:, k, h * 128:(h + 1) * 128], U[:, k, t0:t0 + n],
                                    start=(k == 0), stop=(k == 7)),
                                    reads=[(wkey, k)] + [("U", t) for t in range(t0 // 128, (t0 + n) // 128)],
                                    writes=[PK[pb]])
                            P.act(lambda e, t0=t0, n=n, pb=pb: e.activation(out=QK[:, t0:t0 + n], in_=PS[pb][:, 0:n],
                                                                              func=AF.Copy), reads=[PK[pb]], writes=["QK"])
                            P.act(lambda e, t0=t0, n=n, bb=bb: e.activation(out=SQ[bb][:, 0:n], in_=QK[:, t0:t0 + n],
                                                                              func=AF.Square), reads=["QK"],
                                  writes=["aSQ%d" % bb])
                            p2 = 2 + pb
                            P.pe(lambda e, n=n, bb=bb, p2=p2: e.matmul(PS[p2][:, 0:n], BO[:], SQ[bb][:, 0:n],
                                                                        start=True, stop=True),
                                 reads=["aSQ%d" % bb, "BO"], writes=[PK[p2]])
                            P.act(lambda e, n=n, bb=bb, p2=p2: e.activation(out=RS[bb][:, 0:n], in_=PS[p2][:, 0:n],
                                                                              func=AF.Sqrt, scale=1.0 / 64, bias=EPS),
                                  reads=[PK[p2]], writes=["aRS%d" % bb])
                            P.dve(lambda e, n=n, bb=bb: e.reciprocal(RS[bb][:, 0:n], RS[bb][:, 0:n]),
                                  reads=["aRS%d" % bb], writes=["aRS%d" % bb])
                            dstk = [("qk", g, h, t) for t in range(t0 // 128, (t0 + n) // 128)]
                            if t0 < TC:
                                P.dve(lambda e, g=g, h=h, t0=t0, n=n, bb=bb: e.scalar_tensor_tensor(
                                    out=qkT[g][:, h, t0:t0 + n], in0=QK[:, t0:t0 + n], scalar=GQK[:, g:g + 1],
                                    in1=RS[bb][:, 0:n], op0=ALU.mult, op1=ALU.mult),
                                    reads=["QK", "GQK", "aRS%d" % bb], writes=dstk)
                            else:
                                P.dve(lambda e, g=g, t0=t0, n=n, bb=bb: e.scalar_tensor_tensor(
                                    out=QN[bb][:, 0:n], in0=QK[:, t0:t0 + n], scalar=GQK[:, g:g + 1],
                                    in1=RS[bb][:, 0:n], op0=ALU.mult, op1=ALU.mult),
                                    reads=["QK", "GQK", "aRS%d" % bb], writes=["aQN%d" % bb])
                                p3 = 4 + pb
                                P.pe(lambda e, n=n, bb=bb, p3=p3: e.matmul(PS[p3][:, 0:n], RT[:], QN[bb][:, 0:n],
                                                                            start=True, stop=True),
                                     reads=["aQN%d" % bb, "RT"], writes=[PK[p3]])
                                c0 = t0 - TC
                                P.pool(lambda e, n=n, bb=bb, c0=c0: e.tensor_tensor(
                                    out=O1[bb][:, 0:n], in0=QN[bb][:, 0:n], in1=COS[:, c0:c0 + n], op=ALU.mult),
                                    reads=["aQN%d" % bb, "COS"], writes=["aO1%d" % bb])
                                P.dve(lambda e, n=n, bb=bb, c0=c0, p3=p3: e.tensor_tensor(
                                    out=RS[bb][:, 0:n], in0=PS[p3][:, 0:n], in1=SIN[:, c0:c0 + n], op=ALU.mult),
                                    reads=[PK[p3], "SIN"], writes=["aRS%d" % bb])
                                P.dve(lambda e, g=g, h=h, t0=t0, n=n, bb=bb: e.tensor_tensor(
                                    out=qkT[g][:, h, t0:t0 + n], in0=O1[bb][:, 0:n], in1=RS[bb][:, 0:n], op=ALU.add),
                                    reads=["aO1%d" % bb, "aRS%d" % bb], writes=dstk)
                wt = Wt[0]
                load_wgroup(wt, "Wt0", wv, 2)
                for tt in range(NTILE):
                    pb = 6 + tt % 2
                    for k in range(8):
                        P.pe(lambda e, tt=tt, k=k, pb=pb: e.matmul(PS[pb][:], U[:, k, tt * 128:(tt + 1) * 128], wt[:, k, :],
                                                                    start=(k == 0), stop=(k == 7)),
                             reads=[("U", tt), ("Wt0", k)], writes=[PK[pb]])
                    P.act(lambda e, tt=tt, pb=pb: e.activation(out=V[:, tt, :], in_=PS[pb][:], func=AF.Copy),
                          reads=[PK[pb]], writes=[("V", tt)])
            P.barrier()
            with ExitStack() as es2:
                Eb = [sb(es2, "Eb%d" % i, [128, 512], BF16) for i in range(3)]
                R0 = sb(es2, "aR0", [128, 512])
                R1 = sb(es2, "aR1", [128, 512])
                OO = sb(es2, "aOO", [128, 512])
                SQ2 = sb(es2, "aSQ2", [128, 512])
                ecnt = 0
                groups = [(TC + i * 512, 512, list(range(NTILE))) for i in range(T // 512)] + [(0, TC, [0, 1])]
                for h in range(4):
                    for (q0, n, ktl) in groups:
                        qkeys = [("qk", 0, h, t) for t in range(q0 // 128, (q0 + n) // 128)]
                        for j in range(2):
                            po, pz = 2 + 2 * j, 3 + 2 * j
                            for ki, kt in enumerate(ktl):
                                psb = ecnt % 2
                                eb = ecnt % 3
                                ecnt += 1
                                P.pe(lambda e, h=h, j=j, kt=kt, q0=q0, n=n, psb=psb: e.matmul(
                                    PS[psb][:, 0:n], qkT[1][j * 64:(j + 1) * 64, h, kt * 128:(kt + 1) * 128],
                                    qkT[0][j * 64:(j + 1) * 64, h, q0:q0 + n], start=True, stop=True),
                                    reads=qkeys + [("qk", 1, h, kt)], writes=[PK[psb]])
                                P.act(lambda e, n=n, psb=psb, eb=eb: e.activation(out=Eb[eb][:, 0:n], in_=PS[psb][:, 0:n],
                                                                                    func=AF.Exp, scale=0.125),
                                      reads=[PK[psb]], writes=["Eb%d" % eb])
                                P.pe(lambda e, h=h, kt=kt, n=n, eb=eb, po=po, ki=ki, nk=len(ktl): e.matmul(
                                    PS[po][:, 0:n], V[:, kt, h * 128:(h + 1) * 128], Eb[eb][:, 0:n],
                                    start=(ki == 0), stop=(ki == nk - 1)), reads=[("V", kt), "Eb%d" % eb], writes=[PK[po]])
                                P.pe(lambda e, n=n, eb=eb, pz=pz, ki=ki, nk=len(ktl): e.matmul(
                                    PS[pz][:, 0:n], ones_b[:], Eb[eb][:, 0:n], start=(ki == 0), stop=(ki == nk - 1)),
                                    reads=["ones_b", "Eb%d" % eb], writes=[PK[pz]])
                        P.dve(lambda e, n=n: e.reciprocal(R0[:, 0:n], PS[3][:, 0:n]), reads=[PK[3]], writes=["aR0"])
                        P.dve(lambda e, n=n: e.tensor_tensor(out=R0[:, 0:n], in0=PS[2][:, 0:n], in1=R0[:, 0:n], op=ALU.mult),
                              reads=[PK[2], "aR0"], writes=["aR0"])
                        P.dve(lambda e, n=n: e.reciprocal(R1[:, 0:n], PS[5][:, 0:n]), reads=[PK[5]], writes=["aR1"])
                        P.dve(lambda e, n=n: e.tensor_tensor(out=R1[:, 0:n], in0=PS[4][:, 0:n], in1=R1[:, 0:n], op=ALU.mult),
                              reads=[PK[4], "aR1"], writes=["aR1"])
                        P.dve(lambda e, n=n: e.scalar_tensor_tensor(out=OO[:, 0:n], in0=R1[:, 0:n], scalar=small[:, 4:5],
                                                                     in1=R0[:, 0:n], op0=ALU.mult, op1=ALU.add),
                              reads=["aR0", "aR1", "asmall"], writes=["aOO"])
                        P.act(lambda e, n=n: e.activation(out=SQ2[:, 0:n], in_=OO[:, 0:n], func=AF.Square),
                              reads=["aOO"], writes=["aSQ2"])
                        P.pe(lambda e, n=n: e.matmul(PS[6][:, 0:n], ones_f[:], SQ2[:, 0:n], start=True, stop=True),
                             reads=["aSQ2", "ones_f"], writes=[PK[6]])
                        P.act(lambda e, n=n: e.activation(out=SQ2[:, 0:n], in_=PS[6][:, 0:n], func=AF.Sqrt,
                                                          scale=1.0 / 128, bias=1e-5), reads=[PK[6]], writes=["aSQ2"])
                        P.dve(lambda e, n=n: e.reciprocal(SQ2[:, 0:n], SQ2[:, 0:n]), reads=["aSQ2"], writes=["aSQ2"])
                        P.dve(lambda e, h=h, q0=q0, n=n: e.scalar_tensor_tensor(
                            out=Y[:, h, q0:q0 + n], in0=OO[:, 0:n], scalar=GS[:, 0:1], in1=SQ2[:, 0:n],
                            op0=ALU.mult, op1=ALU.mult), reads=["aOO", "GS", "aSQ2"],
                            writes=[("Y", t) for t in range(q0 // 128, (q0 + n) // 128)])
        P.barrier()

    def phase_hyena(l, U, Y):
        i2 = l // 2
        tiles = _tok_tiles()
        wv = w_in_cd_d[i2].rearrange("(k p) n -> p k n", p=128)
        PI = math.pi
        with ExitStack() as es:
            h_tok = sb(es, "h_tok", [128, NTILE, 512], BF16)
            v_tok = sb(es, "v_tok", [128, NTILE, 512], BF16)
            GOc = sb(es, "GOc", [128, 4, TT], BF16)
            with ExitStack() as es2:
                zT = sb(es2, "zT", [33, TT])
                W1 = sb(es2, "hW1", [33, 64])
                W2 = sb(es2, "hW2", [64, 64])
                W3 = sb(es2, "hW3", [64, 512])
                HV = sb(es2, "hHV", [64, 3])
                HB = sb(es2, "hHB", [1, 512])
                HID = [sb(es2, "hHID%d" % i, [64, TT]) for i in range(2)]
                KI = sb(es2, "hKI", [64, 512], I32)
                KF = sb(es2, "hKF", [64, 512])
                AA = sb(es2, "hAA", [64, 512])
                WN = [sb(es2, "hWN%d" % i, [128, 512]) for i in range(2)]
                HT = [sb(es2, "hHT%d" % i, [128, 512]) for i in range(2)]
                P.dma(lambda e: e.dma_start(out=zT[:], in_=zT_d), writes=["zT"])
                P.dma(lambda e: e.dma_start(out=W1[:], in_=hfw1_d[i2]), writes=["hW1"])
                P.dma(lambda e: e.dma_start(out=W2[:], in_=hfw2_d[i2]), writes=["hW2"])
                P.dma(lambda e: e.dma_start(out=W3[:], in_=hfw3_d[i2]), writes=["hW3"])
                P.dma(lambda e: e.dma_start(out=HV[:], in_=hfv_d[i2]), writes=["hHV"])
                P.dma(lambda e: e.dma_start(out=HB[:], in_=hfb_d[i2:i2 + 1, :]), writes=["hHB"])
                for layer in range(2):
                    src = zT if layer == 0 else HID[0]
                    srck = "zT" if layer == 0 else "hHID0"
                    kdim = 33 if layer == 0 else 64
                    wm = W1 if layer == 0 else W2
                    wmk = "hW1" if layer == 0 else "hW2"
                    bcol = 0 if layer == 0 else 2
                    dst = HID[layer]
                    dk = "hHID%d" % layer
                    for ti, (t0, n) in enumerate(tiles):
                        pb = ti % 2
                        P.pe(lambda e, src=src, kdim=kdim, wm=wm, t0=t0, n=n, pb=pb: e.matmul(
                            PS[pb][0:64, 0:n], wm[0:kdim, :], src[0:kdim, t0:t0 + n], start=True, stop=True),
                            reads=[srck, wmk], writes=[PK[pb]])
                        P.dve(lambda e, n=n, pb=pb, bcol=bcol: e.tensor_scalar(
                            out=AA[:, 0:n], in0=PS[pb][0:64, 0:n], scalar1=HV[:, bcol:bcol + 1], scalar2=HV[:, 1:2],
                            op0=ALU.add, op1=ALU.mult), reads=[PK[pb], "hHV"], writes=["hAA"])
                        P.dve(lambda e, n=n: e.tensor_scalar(out=KI[:, 0:n], in0=AA[:, 0:n], scalar1=1.0 / (2 * PI),
                                                             scalar2=None, op0=ALU.mult), reads=["hAA"], writes=["hKI"])
                        P.dve(lambda e, n=n: e.tensor_copy(KF[:, 0:n], KI[:, 0:n]), reads=["hKI"], writes=["hKF"])
                        P.dve(lambda e, n=n: e.scalar_tensor_tensor(out=AA[:, 0:n], in0=KF[:, 0:n], scalar=-2 * PI,
                                                                     in1=AA[:, 0:n], op0=ALU.mult, op1=ALU.add),
                              reads=["hKF", "hAA"], writes=["hAA"])
                        P.dve(lambda e, n=n: e.tensor_scalar(out=KF[:, 0:n], in0=AA[:, 0:n], scalar1=PI, scalar2=-2 * PI,
                                                             op0=ALU.is_gt, op1=ALU.mult), reads=["hAA"], writes=["hKF"])
                        P.dve(lambda e, n=n: e.tensor_tensor(out=AA[:, 0:n], in0=AA[:, 0:n], in1=KF[:, 0:n], op=ALU.add),
                              reads=["hAA", "hKF"], writes=["hAA"])
                        P.dve(lambda e, n=n: e.tensor_scalar(out=KF[:, 0:n], in0=AA[:, 0:n], scalar1=-PI, scalar2=2 * PI,
                                                             op0=ALU.is_lt, op1=ALU.mult), reads=["hAA"], writes=["hKF"])
                        P.dve(lambda e, n=n: e.tensor_tensor(out=AA[:, 0:n], in0=AA[:, 0:n], in1=KF[:, 0:n], op=ALU.add),
                              reads=["hAA", "hKF"], writes=["hAA"])
                        P.dve(lambda e, n=n: e.tensor_scalar(out=AA[:, 0:n], in0=AA[:, 0:n], scalar1=-PI, scalar2=PI,
                                                             op0=ALU.max, op1=ALU.min), reads=["hAA"], writes=["hAA"])
                        P.act(lambda e, dst=dst, t0=t0, n=n: e.activation(out=dst[:, t0:t0 + n], in_=AA[:, 0:n], func=AF.Sin),
                              reads=["hAA"], writes=[dk])
                for tt in range(NTILE):
                    b = tt % 2
                    pb = 2 + b
                    P.dma(lambda e, b=b, tt=tt: e.dma_start(out=WN[b][:], in_=win_d[tt * 128:(tt + 1) * 128, :]),
                          writes=["hWN%d" % b])
                    P.pe(lambda e, tt=tt, pb=pb: e.matmul(PS[pb][:], HID[1][:, tt * 128:(tt + 1) * 128], W3[:],
                                                          start=True, stop=True), reads=["hHID1", "hW3"], writes=[PK[pb]])
                    centre = tt in (1, 2 + 8)
                    if centre:
                        P.dve(lambda e, b=b, pb=pb: e.tensor_tensor(out=HT[b][:], in0=PS[pb][:], in1=WN[b][:], op=ALU.mult),
                              reads=[PK[pb], "hWN%d" % b], writes=["hHT%d" % b])
                        P.dve(lambda e, b=b: e.tensor_tensor(out=HT[b][0:1, :], in0=HT[b][0:1, :], in1=HB[:], op=ALU.add),
                              reads=["hHT%d" % b, "hHB"], writes=["hHT%d" % b])
                        P.act(lambda e, b=b, tt=tt: e.activation(out=h_tok[:, tt, :], in_=HT[b][:], func=AF.Copy),
                              reads=["hHT%d" % b], writes=[("h_tok", tt)])
                    else:
                        P.dve(lambda e, b=b, pb=pb, tt=tt: e.tensor_tensor(out=h_tok[:, tt, :], in0=PS[pb][:], in1=WN[b][:],
                                                                            op=ALU.mult),
                              reads=[PK[pb], "hWN%d" % b], writes=[("h_tok", tt)])
            P.barrier()
            with ExitStack() as es2:
                Wt = [sb(es2, "hWt%d" % i, [128, 8, 512], BF16) for i in range(3)]
                CD = sb(es2, "CD", [128, 12, 3])
                Bf = [sb(es2, "hB%d" % i, [128, TT]) for i in range(3)]
                P.dma(lambda e: e.dma_start(out=CD[:], in_=cd_d[i2]), writes=["CD"])
                for g in range(3):
                    load_wgroup(Wt[g], "hWt%d" % g, wv, 3 + g)
                pcnt = [0]

                def projc(g, c, raw, rawk, dst, dstk):
                    for (t0, n) in tiles:
                        pb = pcnt[0] % 4
                        pcnt[0] += 1
                        for k in range(8):
                            P.pe(lambda e, g=g, c=c, k=k, t0=t0, n=n, pb=pb: e.matmul(
                                PS[pb][:, 0:n], Wt[g][:, k, c * 128:(c + 1) * 128], U[:, k, t0:t0 + n],
                                start=(k == 0), stop=(k == 7)),
                                reads=[("hWt%d" % g, k)] + [("U", t) for t in range(t0 // 128, (t0 + n) // 128)],
                                writes=[PK[pb]])
                        P.act(lambda e, t0=t0, n=n, pb=pb: e.activation(out=raw[:, t0:t0 + n], in_=PS[pb][:, 0:n],
                                                                          func=AF.Copy), reads=[PK[pb]], writes=[rawk])
                    ch = g * 4 + c
                    P.dve(lambda e: e.tensor_scalar(out=dst[:], in0=raw[:], scalar1=CD[:, ch, 1:2], scalar2=None,
                                                    op0=ALU.mult), reads=[rawk, "CD"], writes=[dstk])
                    for (s0, L) in SEQS:
                        P.dve(lambda e, s0=s0, L=L: e.scalar_tensor_tensor(
                            out=dst[:, s0 + 1:s0 + L], in0=raw[:, s0:s0 + L - 1], scalar=CD[:, ch, 0:1],
                            in1=dst[:, s0 + 1:s0 + L], op0=ALU.mult, op1=ALU.add), reads=[rawk, dstk, "CD"], writes=[dstk])
                        P.dve(lambda e, s0=s0, L=L: e.scalar_tensor_tensor(
                            out=dst[:, s0:s0 + L - 1], in0=raw[:, s0 + 1:s0 + L], scalar=CD[:, ch, 2:3],
                            in1=dst[:, s0:s0 + L - 1], op0=ALU.mult, op1=ALU.add), reads=[rawk, dstk, "CD"], writes=[dstk])

                for c in range(4):
                    projc(0, c, Bf[0], "hB0", Bf[1], "hB1")
                    P.act(lambda e, c=c: e.activation(out=GOc[:, c, :], in_=Bf[1][:], func=AF.Copy),
                          reads=["hB1"], writes=[("GOc", c)])
                    projc(1, c, Bf[0], "hB0", Bf[1], "hB1")
                    projc(2, c, Bf[0], "hB0", Bf[2], "hB2")
                    P.pool(lambda e: e.tensor_tensor(out=Bf[1][:], in0=Bf[1][:], in1=Bf[2][:], op=ALU.mult),
                           reads=["hB1", "hB2"], writes=["hB1"])
                    for t4 in range(0, NTILE, 4):
                        nt = min(4, NTILE - t4)
                        pb = 4 + (t4 // 4) % 2
                        for j in range(nt):
                            P.pe(lambda e, t4=t4, j=j, pb=pb: e.transpose(
                                PS[pb][:, j * 128:(j + 1) * 128], Bf[1][:, (t4 + j) * 128:(t4 + j + 1) * 128], ident_f[:]),
                                reads=["hB1", "ident_f"], writes=[PK[pb]])
                        P.act(lambda e, t4=t4, nt=nt, pb=pb, c=c: e.activation(
                            out=v_tok[:, t4:t4 + nt, c * 128:(c + 1) * 128],
                            in_=PS[pb][:, 0:nt * 128].rearrange("p (j t) -> p j t", j=nt), func=AF.Copy),
                            reads=[PK[pb]], writes=[("v_tok", c)])
            P.barrier()
            with ExitStack() as es2:
                YS = sb(es2, "YS", [128, 24, 512], BF16)
                YSc = sb(es2, "YSc", [128, 4, 512], BF16)
                es3 = ExitStack()
                FW = [sb(es3, "FW%d" % i, [128, 16, 256], BF16) for i in range(2)]
                HH = [sb(es3, "HH%d" % i, [128, 2, 512]) for i in range(2)]
                T1 = sb(es3, "hT1", [128, 512])
                T2 = sb(es3, "hT2", [128, 512])
                fcnt = 0
                for (npair, tt0, ntt, fwd, ys, ysk) in ((12, 2, 16, fwl_d, YS, "YS"), (2, 0, 2, fwc_d, YSc, "YSc")):
                    for i in range(npair):
                        fb = fcnt % 2
                        fcnt += 1
                        fw = FW[fb]
                        fwk = "FW%d" % fb
                        hh = HH[fb]
                        hhk = "HH%d" % fb
                        P.dma(lambda e, fw=fw, fwd=fwd, i=i, ntt=ntt: e.dma_start(out=fw[:, 0:ntt, :], in_=fwd[i]),
                              writes=[fwk])
                        for (srct, srck, pr, pi) in ((h_tok, "h_tok", 0, 1), (v_tok, "v_tok", 2, 3)):
                            for part, pb in ((0, pr), (1, pi)):
                                for tt in range(ntt):
                                    rk = [("h_tok", tt0 + tt)] if srct is h_tok else [("v_tok", c) for c in range(4)]
                                    P.pe(lambda e, fw=fw, tt=tt, part=part, pb=pb, srct=srct, tt0=tt0, ntt=ntt: e.matmul(
                                        PS[pb][:], fw[:, tt, part * 128:(part + 1) * 128], srct[:, tt0 + tt, :],
                                        start=(tt == 0), stop=(tt == ntt - 1)), reads=[fwk] + rk, writes=[PK[pb]])
                            if srct is h_tok:
                                P.act(lambda e, hh=hh: e.activation(out=hh[:, 0, :], in_=PS[0][:], func=AF.Copy),
                                      reads=[PK[0]], writes=[hhk])
                                P.act(lambda e, hh=hh: e.activation(out=hh[:, 1, :], in_=PS[1][:], func=AF.Copy),
                                      reads=[PK[1]], writes=[hhk])
                        nch = npair
                        P.dve(lambda e, hh=hh: e.tensor_tensor(out=T1[:], in0=PS[2][:], in1=hh[:, 0, :], op=ALU.mult),
                              reads=[PK[2], hhk], writes=["hT1"])
                        P.dve(lambda e, hh=hh: e.tensor_tensor(out=T2[:], in0=PS[3][:], in1=hh[:, 1, :], op=ALU.mult),
                              reads=[PK[3], hhk], writes=["hT2"])
                        P.pool(lambda e, ys=ys, i=i: e.tensor_tensor(out=ys[:, i, :], in0=T1[:], in1=T2[:], op=ALU.subtract),
                               reads=["hT1", "hT2"], writes=[(ysk, i)])
                        P.dve(lambda e, hh=hh: e.tensor_tensor(out=T1[:], in0=PS[2][:], in1=hh[:, 1, :], op=ALU.mult),
                              reads=[PK[2], hhk], writes=["hT1"])
                        P.dve(lambda e, hh=hh: e.tensor_tensor(out=T2[:], in0=PS[3][:], in1=hh[:, 0, :], op=ALU.mult),
                              reads=[PK[3], hhk], writes=["hT2"])
                        P.pool(lambda e, ys=ys, i=i, nch=nch: e.tensor_tensor(out=ys[:, nch + i, :], in0=T1[:], in1=T2[:],
                                                                               op=ALU.add),
                               reads=["hT1", "hT2"], writes=[(ysk, nch + i)])
                P.barrier()
                es3.close()
                IV = [sb(es2, "IV%d" % i, [128, 12, 512], BF16) for i in range(2)]
                icnt = 0
                for j in range(4):
                    for half in range(2):
                        ib = icnt % 2
                        icnt += 1
                        P.dma(lambda e, ib=ib, j=j, half=half: e.dma_start(out=IV[ib][:], in_=invl_d[j, half]),
                              writes=["IV%d" % ib])
                        for c in range(4):
                            for fc in range(12):
                                P.pe(lambda e, ib=ib, half=half, c=c, fc=fc: e.matmul(
                                    PS[c][:], YS[:, half * 12 + fc, c * 128:(c + 1) * 128], IV[ib][:, fc, :],
                                    start=(half == 0 and fc == 0), stop=(half == 1 and fc == 11)),
                                    reads=[("YS", half * 12 + fc), "IV%d" % ib], writes=[PK[c]])
                    t0 = TC + j * 512
                    for c in range(4):
                        P.dve(lambda e, c=c, t0=t0: e.tensor_tensor(out=Y[:, 4 + c, t0:t0 + 512], in0=PS[c][:],
                                                                    in1=GOc[:, c, t0:t0 + 512], op=ALU.mult),
                              reads=[PK[c], ("GOc", c)], writes=[("Y", t) for t in range(t0 // 128, t0 // 128 + 4)])
                IVc = [sb(es2, "IVc%d" % i, [128, 2, 256], BF16) for i in range(2)]
                for half in range(2):
                    P.dma(lambda e, half=half: e.dma_start(out=IVc[half][:], in_=invc_d[half]), writes=["IVc%d" % half])
                for c in range(4):
                    for half in range(2):
                        for fc in range(2):
                            P.pe(lambda e, half=half, c=c, fc=fc: e.matmul(
                                PS[4 + c][:, 0:TC], YSc[:, half * 2 + fc, c * 128:(c + 1) * 128], IVc[half][:, fc, :],
                                start=(half == 0 and fc == 0), stop=(half == 1 and fc == 1)),
                                reads=[("YSc", half * 2 + fc), "IVc%d" % half], writes=[PK[4 + c]])
                    P.dve(lambda e, c=c: e.tensor_tensor(out=Y[:, 4 + c, 0:TC], in0=PS[4 + c][:, 0:TC], in1=GOc[:, c, 0:TC],
                                                         op=ALU.mult),
                          reads=[PK[4 + c], ("GOc", c)], writes=[("Y", 0), ("Y", 1)])
        P.barrier()

    def phase_moe(l, AFF, last):
        with ExitStack() as es:
            affT = sb(es, "affT", [NE, TT])
            work = sb(es, "mwork", [NE, TT])
            GV = sb(es, "GV", [NE, NSLOT])
            IXu = sb(es, "IXu", [NE, NSLOT], U32)
            IXf = sb(es, "IXf", [NE, NSLOT])
            gP = sb(es, "gP", [128, 3, NE])
            iP = sb(es, "iP", [128, 3, NE], I32)
            for t4 in range(0, NTILE, 4):
                nt = min(4, NTILE - t4)
                pb = (t4 // 4) % 2
                for j in range(nt):
                    P.pe(lambda e, t4=t4, j=j, pb=pb: e.transpose(PS[pb][0:NE, j * 128:(j + 1) * 128],
                                                                   AFF[:, t4 + j, :], ident_f[:]),
                         reads=[("AFF", t4 + j), "ident_f"], writes=[PK[pb]])
                P.dve(lambda e, t4=t4, nt=nt, pb=pb: e.tensor_copy(affT[:, t4 * 128:(t4 + nt) * 128],
                                                                    PS[pb][0:NE, 0:nt * 128]),
                      reads=[PK[pb]], writes=["affT"])
            P.dve(lambda e: e.tensor_copy(work[:], affT[:]), reads=["affT"], writes=["mwork"])
            for (s0, L, cap, c0) in ((TC, T, CAP_L, 0), (0, TC, CAP_C, CAP_L)):
                for rd in range(cap // 8):
                    sl = slice(c0 + rd * 8, c0 + rd * 8 + 8)
                    P.dve(lambda e, s0=s0, L=L, sl=sl: e.max(out=GV[:, sl], in_=work[:, s0:s0 + L]),
                          reads=["mwork"], writes=["GV"])
                    P.dve(lambda e, s0=s0, L=L, sl=sl: e.max_index(out=IXu[:, sl], in_max=GV[:, sl],
                                                                   in_values=work[:, s0:s0 + L]),
                          reads=["mwork", "GV"], writes=["IXu"])
                    P.dve(lambda e, s0=s0, L=L, sl=sl: e.match_replace(out=work[:, s0:s0 + L], in_to_replace=GV[:, sl],
                                                                       in_values=work[:, s0:s0 + L], imm_value=-1.0),
                          reads=["mwork", "GV"], writes=["mwork"])
            P.dve(lambda e: e.tensor_copy(IXf[:], IXu[:]), reads=["IXu"], writes=["IXf"])
            P.dve(lambda e: e.tensor_scalar(out=IXf[:, 0:CAP_L], in0=IXf[:, 0:CAP_L], scalar1=float(TC), scalar2=None,
                                            op0=ALU.add), reads=["IXf"], writes=["IXf"])
            for (src, dst, dk, pb) in ((IXf, iP, "iP", 2), (GV, gP, "gP", 3)):
                sk = "IXf" if src is IXf else "GV"
                for s in range(3):
                    n = 128 if s < 2 else CAP_C
                    P.pe(lambda e, src=src, s=s, n=n, pb=pb: e.transpose(PS[pb][0:n, s * NE:(s + 1) * NE],
                                                                          src[:, s * 128:s * 128 + n], ident_f[0:NE, 0:NE]),
                         reads=[sk, "ident_f"], writes=[PK[pb]])
                P.dve(lambda e, dst=dst, pb=pb: e.tensor_copy(dst[:, 0:2, :],
                                                               PS[pb][:, 0:2 * NE].rearrange("p (s e) -> p s e", s=2)),
                      reads=[PK[pb]], writes=[dk])
                P.dve(lambda e, dst=dst, pb=pb: e.tensor_copy(dst[0:CAP_C, 2, :], PS[pb][0:CAP_C, 2 * NE:3 * NE]),
                      reads=[PK[pb]], writes=[dk])
            if dbg and last:
                P.dma(lambda e: e.dma_start(out=dbg_ix_d, in_=IXf[:]), reads=["IXf"], writes=["dbg_ix"])
                P.dma(lambda e: e.dma_start(out=dbg_gv_d, in_=GV[:]), reads=["GV"], writes=["dbg_gv"])
            for tt in range(NTILE):
                P.dma(lambda e, tt=tt: e.dma_start(out=M_d[tt * 128:(tt + 1) * 128, :], in_=zero_f[:]),
                      reads=["zero_f"], writes=["Macc"])
            WB = [sb(es, "WB%d" % i, [128, 8, D], BF16) for i in range(4)]
            xe = [sb(es, "xe%d" % i, [128, 3, D], BF16) for i in range(2)]
            xeT = [sb(es, "xeT%d" % i, [128, 8, NSLOT], BF16) for i in range(2)]
            hT = [sb(es, "hT%d" % i, [128, 8, NSLOT], BF16) for i in range(2)]
            sg = [sb(es, "sg%d" % i, [128, NSLOT]) for i in range(2)]
            yb = [sb(es, "yb%d" % i, [128, 3, D]) for i in range(2)]
            wcnt = [0]

            def load_w(src_ap):
                i = wcnt[0] % 4
                wcnt[0] += 1
                wv = src_ap.rearrange("(k p) n -> p k n", p=128)
                for k in range(8):
                    P.dma(lambda e, i=i, k=k: e.dma_start(out=WB[i][:, k, :], in_=wv[:, k, :]),
                          writes=[("WB", i, k)], q="pool")
                return i

            for ex_i in range(NE):
                b = ex_i % 2
                kxe, kxT, khT, ksg, kyb = "xe%d" % b, "xeT%d" % b, "hT%d" % b, "sg%d" % b, "yb%d" % b
                wg = load_w(w_gate_d[l, ex_i])
                wu = load_w(w_up_d[l, ex_i])
                wd = load_w(w_down_d[l, ex_i])
                for s in range(3):
                    n = 128 if s < 2 else CAP_C
                    P.dma(lambda e, b=b, s=s, n=n, ex_i=ex_i: e.indirect_dma_start(
                        out=xe[b][0:n, s, :], out_offset=None, in_=u2tok_d,
                        in_offset=bass.IndirectOffsetOnAxis(ap=iP[0:n, s, ex_i:ex_i + 1], axis=0)),
                        reads=["iP"] + [("u2tok", t) for t in range(NTILE)], writes=[(kxe, s)], q="pool")
                if dbg and last and ex_i == 0:
                    P.dma(lambda e: e.dma_start(out=dbg_xe_d, in_=xe[0][:]), reads=[("xe0", 0), ("xe0", 1), ("xe0", 2)], writes=["dbg_xe"])
                    P.dma(lambda e: e.dma_start(out=dbg_ip_d, in_=iP[:]), reads=["iP"], writes=["dbg_ip"])
                for s in range(3):
                    n = 128 if s < 2 else CAP_C
                    pb = 4 + (s % 2)
                    pv = PS[pb][:].bitcast(BF16)
                    for k in range(8):
                        P.pe(lambda e, b=b, s=s, n=n, k=k, pv=pv: e.transpose(
                            pv[:, k * 128:k * 128 + n], xe[b][0:n, s, k * 128:(k + 1) * 128], ident_b[0:n, 0:n]),
                            reads=[(kxe, s), "ident_b"], writes=[PK[pb]])
                    P.act(lambda e, b=b, s=s, n=n, pv=pv: e.activation(
                        out=xeT[b][:, :, s * 128:s * 128 + n],
                        in_=pv.rearrange("p (k t) -> p k t", k=8)[:, :, 0:n], func=AF.Copy),
                        reads=[PK[pb]], writes=[kxT])
                for f in range(8):
                    pa, pu = 0 + (f % 2) * 2, 1 + (f % 2) * 2
                    for k in range(8):
                        P.pe(lambda e, b=b, f=f, k=k, pa=pa, wg=wg: e.matmul(
                            PS[pa][:, 0:NSLOT], WB[wg][:, k, f * 128:(f + 1) * 128], xeT[b][:, k, :],
                            start=(k == 0), stop=(k == 7)), reads=[("WB", wg, k), kxT], writes=[PK[pa]])
                    for k in range(8):
                        P.pe(lambda e, b=b, f=f, k=k, pu=pu, wu=wu: e.matmul(
                            PS[pu][:, 0:NSLOT], WB[wu][:, k, f * 128:(f + 1) * 128], xeT[b][:, k, :],
                            start=(k == 0), stop=(k == 7)), reads=[("WB", wu, k), kxT], writes=[PK[pu]])
                    P.act(lambda e, b=b, pa=pa: e.activation(out=sg[b][:], in_=PS[pa][:, 0:NSLOT], func=AF.Silu),
                          reads=[PK[pa]], writes=[ksg])
                    P.dve(lambda e, b=b, f=f, pu=pu: e.tensor_tensor(out=hT[b][:, f, :], in0=sg[b][:],
                                                                      in1=PS[pu][:, 0:NSLOT], op=ALU.mult),
                          reads=[ksg, PK[pu]], writes=[khT])
                for s in range(3):
                    n = 128 if s < 2 else CAP_C
                    for hf in range(2):
                        pb = 6 + hf
                        for k in range(8):
                            P.pe(lambda e, b=b, s=s, n=n, hf=hf, k=k, pb=pb, wd=wd: e.matmul(
                                PS[pb][0:n, :], hT[b][:, k, s * 128:s * 128 + n], WB[wd][:, k, hf * 512:(hf + 1) * 512],
                                start=(k == 0), stop=(k == 7)), reads=[khT, ("WB", wd, k)], writes=[PK[pb]])
                        P.dve(lambda e, b=b, s=s, n=n, hf=hf, pb=pb, ex_i=ex_i: e.tensor_scalar(
                            out=yb[b][0:n, s, hf * 512:(hf + 1) * 512], in0=PS[pb][0:n, :],
                            scalar1=gP[0:n, s, ex_i:ex_i + 1], scalar2=None, op0=ALU.mult),
                            reads=[PK[pb], "gP"], writes=[(kyb, s)])
                    if dbg and last and ex_i == 0 and s == 2:
                        P.dma(lambda e: e.dma_start(out=dbg_yb_d, in_=yb[0][:]), reads=[("yb0", 0), ("yb0", 1), ("yb0", 2)], writes=["dbg_yb"])
                        P.dma(lambda e: e.dma_start(out=dbg_ht_d, in_=hT[0][:]), reads=["hT0"], writes=["dbg_ht"])
                    P.dma(lambda e, b=b, s=s, n=n, ex_i=ex_i: e.indirect_dma_start(
                        out=M_d, out_offset=bass.IndirectOffsetOnAxis(ap=iP[0:n, s, ex_i:ex_i + 1], axis=0),
                        in_=yb[b][0:n, s, :], in_offset=None, compute_op=ALU.add),
                        reads=[(kyb, s), "iP"], writes=["Macc"], q="pool")
            G = [sb(es, "mG%d" % r, [128, D]) for r in range(2)]
            for r in range(2):
                load_mod(G[r], "mG%d" % r, r, 5)
            xt = [sb(es, "mx%d" % i, [128, D]) for i in range(2)]
            mt = [sb(es, "mm%d" % i, [128, D]) for i in range(2)]
            outs = []
            for tt in range(NTILE):
                b = tt % 2
                r = 1 if tt < 2 else 0
                kx, km = "mx%d" % b, "mm%d" % b
                P.dma(lambda e, b=b, tt=tt: e.dma_start(out=xt[b][:], in_=H_d[tt * 128:(tt + 1) * 128, :]),
                      reads=[hkey(tt)], writes=[kx])
                P.dma(lambda e, b=b, tt=tt: e.dma_start(out=mt[b][:], in_=M_d[tt * 128:(tt + 1) * 128, :]),
                      reads=["Macc"], writes=[km])
                P.dve(lambda e, b=b, r=r: e.tensor_tensor(out=mt[b][:], in0=mt[b][:], in1=G[r][:], op=ALU.mult),
                      reads=[km, "mG%d" % r], writes=[km])
                P.pool(lambda e, b=b: e.tensor_tensor(out=xt[b][:], in0=xt[b][:], in1=mt[b][:], op=ALU.add),
                       reads=[kx, km], writes=[kx])
                if dbg and last:
                    P.dma(lambda e, b=b, tt=tt: e.dma_start(out=dbg_m_d[tt * 128:(tt + 1) * 128, :], in_=mt[b][:]),
                          reads=[km], writes=[("dbg_m", tt)])
                if last:
                    if tt >= 2:
                        outs.append(P.dma(lambda e, b=b, tt=tt: e.dma_start(
                            out=out_d[(tt - 2) * 128:(tt - 1) * 128, :], in_=xt[b][:]), reads=[kx], writes=[("out", tt)]))
                    if dbg:
                        outs.append(P.dma(lambda e, b=b, tt=tt: e.dma_start(
                            out=hdbg_d[tt * 128:(tt + 1) * 128, :], in_=xt[b][:]), reads=[kx], writes=[("hdbg", tt)]))
                else:
                    P.dma(lambda e, b=b, tt=tt: e.dma_start(out=H_d[tt * 128:(tt + 1) * 128, :], in_=xt[b][:]),
                          reads=[kx], writes=[hkey(tt)])
        P.barrier()
        return outs

    finals = []
    for l in range(n_layers):
        last = (l == n_layers - 1)
        phase_mod(l)
        with ExitStack() as es:
            U = sb(es, "U", [128, 8, TT], BF16)
            Y = sb(es, "Y", [128, 8, TT], BF16)
            phase_norm(l, l, 1, U)
            if l % 2 == 0:
                phase_conv(l, U, Y)
                phase_outproj(l, Y, w_out_ab_d[l // 2])
            else:
                phase_attn(l, U, Y)
                phase_hyena(l, U, Y)
                phase_outproj(l, Y, w_out_cd_d[l // 2])
        with ExitStack() as es:
            AFF = sb(es, "AFF", [128, NTILE, NE])
            phase_norm(l, 1, 2, None, AFF=AFF)
            finals = phase_moe(l, AFF, last)
    P.finalize(finals)
    top.close()
    return nc


def _host_inputs(inp):
    f = lambda a: np.ascontiguousarray(np.asarray(a, dtype=np.float32))
    sh = {}
    sh["w_ada"] = f(inp["w_ada"])
    sh["b_ada"] = f(inp["b_ada"])
    sh["g_mix"] = f(inp["g_mix"])
    sh["g_ffn"] = f(inp["g_ffn"])
    sh["w_in_ab"] = f(inp["w_in_ab"])
    sh["ca"] = f(np.asarray(inp["conv_a"]).reshape(2, 3, 4, 128).transpose(0, 3, 2, 1))
    sh["cb"] = f(np.asarray(inp["conv_b"]).reshape(2, 31, 4, 128).transpose(0, 3, 2, 1))
    lnp = np.stack([np.asarray(inp["conv_b_bias"]), np.asarray(inp["ln_b_g"]), np.asarray(inp["ln_b_b"])], axis=1)
    sh["lnp"] = f(lnp.reshape(2, 3, 4, 128).transpose(0, 3, 1, 2))
    sh["w_out_ab"] = f(inp["w_out_ab"])
    sh["w_in_cd"] = f(inp["w_in_cd"])
    sh["w_out_cd"] = f(inp["w_out_cd"])
    gq = np.asarray(inp["g_q"]); gk = np.asarray(inp["g_k"])
    sh["gqk"] = f(np.stack([np.tile(gq, (1, 2)), np.tile(gk, (1, 2))], axis=-1))
    sh["lamv"] = f(np.concatenate([np.asarray(inp[k]) for k in ("lam_q1", "lam_k1", "lam_q2", "lam_k2")], axis=1))
    sh["gsub"] = f(np.asarray(inp["g_subln"]).reshape(2, 128, 1))
    sh["cd"] = f(np.asarray(inp["conv_d"]).reshape(2, 3, 12, 128).transpose(0, 3, 2, 1))
    sh["hf_w1"] = f(inp["hf_w1"])
    sh["hfv"] = f(np.stack([np.asarray(inp["hf_b1"]), np.asarray(inp["hf_freq"]), np.asarray(inp["hf_b2"])], axis=-1))
    sh["hf_w2"] = f(inp["hf_w2"])
    sh["hf_w3"] = f(inp["hf_w3"])
    sh["hf_bias"] = f(inp["hf_bias"])
    sh["wr"] = f(np.asarray(inp["w_router"]).reshape(DEPTH, 8, 128, NE).transpose(0, 2, 1, 3))
    sh["w_gate"] = f(inp["w_gate"])
    sh["w_up"] = f(inp["w_up"])
    sh["w_down"] = f(inp["w_down"])
    sh.update(host_constants())
    return sh


def _core_inputs(inp, shared, b):
    m = dict(shared)
    m["x"] = np.ascontiguousarray(np.asarray(inp["x"][b], dtype=np.float32))
    m["ctx"] = np.ascontiguousarray(np.asarray(inp["ctx"][b], dtype=np.float32))
    cv = np.stack([np.asarray(inp["c"][b]), np.asarray(inp["c_ctx"])], axis=0)
    m["cT"] = np.ascontiguousarray(cv.reshape(2, 8, 128).transpose(2, 1, 0).astype(np.float32))
    return m


_NC_CACHE = {}


def kernel(**inputs):
    n = 8
    if "nc" not in _NC_CACHE:
        _NC_CACHE["nc"] = build(DEPTH)
    nc = _NC_CACHE["nc"]
    shared = _host_inputs(inputs)
    in_maps = [_core_inputs(inputs, shared, b) for b in range(n)]
    res = run_bass_kernel_spmd(nc, in_maps, core_ids=list(range(n)))
    return np.stack([np.asarray(r["out"]) for r in res.results], axis=0).astype(np.float32)
```

```python
import math
from contextlib import ExitStack

import numpy as np
import concourse.bass as bass
import concourse.mybir as mybir
from concourse.bass_utils import run_bass_kernel_spmd

F32 = mybir.dt.float32
BF16 = mybir.dt.bfloat16
I32 = mybir.dt.int32
U32 = mybir.dt.uint32
AF = mybir.ActivationFunctionType
ALU = mybir.AluOpType
AX = mybir.AxisListType

D = 1024
T = 2048
TC = 256
TT = T + TC
NTILE = TT // 128
DEPTH = 4
NE = 16
CAP_L = 256
CAP_C = 32
NSLOT = CAP_L + CAP_C
EPS = 1e-6

ENGS = ("pe", "dve", "act", "pool", "sp")
DMA_SLOTS = {"sp": 24, "act": 8, "pool": 24}


class Op:
    __slots__ = ("eng", "fn", "deps", "dma", "needs_inc", "sem", "val", "slot", "prev_slot_op")

    def __init__(self, eng, fn, dma):
        self.eng = eng
        self.fn = fn
        self.dma = dma
        self.deps = []
        self.needs_inc = False
        self.sem = None
        self.val = None
        self.slot = None
        self.prev_slot_op = None


class Prog:
    def __init__(self, nc):
        self.nc = nc
        self.ops = {e: [] for e in ENGS}
        self.last_w = {}
        self.readers = {}
        self.dma_rr = {q: 0 for q in DMA_SLOTS}
        self.slot_last = {}

    def _add(self, eng, fn, reads, writes, dma):
        op = Op(eng, fn, dma)
        deps = []
        for k in reads:
            w = self.last_w.get(k)
            if w is not None:
                deps.append(w)
        for k in writes:
            w = self.last_w.get(k)
            if w is not None:
                deps.append(w)
            deps.extend(self.readers.get(k, ()))
        seen = set()
        for d in deps:
            if id(d) in seen:
                continue
            seen.add(id(d))
            if (not dma) and (not d.dma) and d.eng == eng and eng == "pe":
                continue
            op.deps.append(d)
            d.needs_inc = True
        if dma:
            k = self.dma_rr[eng]
            self.dma_rr[eng] = (k + 1) % DMA_SLOTS[eng]
            op.slot = (eng, k)
            op.prev_slot_op = self.slot_last.get(op.slot)
            self.slot_last[op.slot] = op
        for k in reads:
            self.readers.setdefault(k, []).append(op)
        for k in writes:
            self.last_w[k] = op
            self.readers[k] = []
        self.ops[eng].append(op)
        return op

    def pe(self, fn, reads=(), writes=()):
        return self._add("pe", fn, reads, writes, False)

    def dve(self, fn, reads=(), writes=()):
        return self._add("dve", fn, reads, writes, False)

    def act(self, fn, reads=(), writes=()):
        return self._add("act", fn, reads, writes, False)

    def pool(self, fn, reads=(), writes=()):
        return self._add("pool", fn, reads, writes, False)

    def dma(self, fn, reads=(), writes=(), q="sp"):
        return self._add(q, fn, reads, writes, True)

    def barrier(self):
        lasts = []
        for e in ENGS:
            for op in reversed(self.ops[e]):
                if not op.dma:
                    lasts.append(op)
                    break
        lasts.extend(self.slot_last.values())
        for e in ENGS:
            op = Op(e, lambda eng: eng.nop(), False)
            for d in lasts:
                if d.eng == e and not d.dma and e == "pe":
                    continue
                op.deps.append(d)
                d.needs_inc = True
            self.ops[e].append(op)
        self.last_w = {}
        self.readers = {}

    def finalize(self, final_ops=()):
        nc = self.nc
        with ExitStack() as es:
            esem = {e: es.enter_context(nc.semaphore("s_" + e)) for e in ("pe", "dve", "act", "pool", "sp")}
            ssem = {}
            for q, n in DMA_SLOTS.items():
                for k in range(n):
                    ssem[(q, k)] = es.enter_context(nc.semaphore("d_%s%d" % (q, k)))
            for e in ENGS:
                cnt = 0
                for op in self.ops[e]:
                    if (not op.dma) and op.needs_inc:
                        cnt += 1
                        op.sem = esem[e]
                        op.val = cnt
            slot_cnt = {}
            for e in ENGS:
                for op in self.ops[e]:
                    if op.dma:
                        c = slot_cnt.get(op.slot, 0) + 16
                        slot_cnt[op.slot] = c
                        op.sem = ssem[op.slot]
                        op.val = c
            block = es.enter_context(nc.Block())
            finals = list(final_ops)

            def emit(e, engobj):
                waited = {}

                def w(s, v):
                    if waited.get(id(s), 0) >= v:
                        return
                    waited[id(s)] = v
                    engobj.wait_ge(s, v)

                for op in self.ops[e]:
                    for d in op.deps:
                        w(d.sem, d.val)
                    if op.dma and op.prev_slot_op is not None:
                        w(op.prev_slot_op.sem, op.prev_slot_op.val)
                    ins = op.fn(engobj)
                    if op.dma:
                        ins.then_inc(op.sem, 16)
                    elif op.needs_inc:
                        ins.then_inc(op.sem, 1)
                if e == "sp":
                    for f in finals:
                        w(f.sem, f.val)

            @block.tensor
            def _(eng):
                emit("pe", eng)

            @block.vector
            def _(eng):
                emit("dve", eng)

            @block.scalar
            def _(eng):
                emit("act", eng)

            @block.gpsimd
            def _(eng):
                emit("pool", eng)

            @block.sync
            def _(eng):
                emit("sp", eng)


def _tok_tiles():
    return [(0, TC)] + [(TC + i * 512, 512) for i in range(T // 512)]


SEQS = ((0, TC), (TC, T))


def _bf(a):
    import ml_dtypes
    return np.ascontiguousarray(np.asarray(a, dtype=np.float32).astype(ml_dtypes.bfloat16))


def _dft_tables(L):
    N = 3 * L // 2
    nf = N // 2
    npair = (nf + 127) // 128
    f = np.arange(npair * 128, dtype=np.float64)
    t = np.arange(L, dtype=np.float64)
    th = 2.0 * np.pi * np.outer(t, f + 0.5) / N
    valid = (f < nf)[None, :]
    fc = np.where(valid, np.cos(th), 0.0)
    fs = np.where(valid, -np.sin(th), 0.0)
    nt = L // 128
    fw = np.zeros((npair, 128, nt, 256))
    for i in range(npair):
        blkc = fc[:, i * 128:(i + 1) * 128].reshape(nt, 128, 128).transpose(1, 0, 2)
        blks = fs[:, i * 128:(i + 1) * 128].reshape(nt, 128, 128).transpose(1, 0, 2)
        fw[i, :, :, 0:128] = blkc
        fw[i, :, :, 128:256] = blks
    thi = 2.0 * np.pi * np.outer(f + 0.5, t + L // 2) / N
    validf = (f < nf)[:, None]
    ic = np.where(validf, (2.0 / N) * np.cos(thi), 0.0)
    isn = np.where(validf, -(2.0 / N) * np.sin(thi), 0.0)
    inv = np.stack([ic.reshape(npair, 128, L).transpose(1, 0, 2), isn.reshape(npair, 128, L).transpose(1, 0, 2)], 0)
    return fw, inv, npair


def _filter_tables(L):
    t = np.arange(L, dtype=np.float64)
    t_unit = np.linspace(0.0, 1.0, L)
    bands = np.linspace(1e-4, 16 - 1, 16)
    ang = (2.0 * np.pi / L) * t[:, None] * bands[None]
    z = np.concatenate([t_unit[:, None], np.cos(ang), -np.sin(ang)], axis=-1)
    centre = L // 2
    dist = np.abs(t - centre) / max(centre, 1)
    mn = math.log(1e-2) / 1.5
    mx = math.log(1e-2) / 0.3
    decay = np.abs(np.linspace(mn, mx, 512))
    win = np.exp(-dist[:, None] * decay[None])
    return z.T, win


def host_constants():
    c = {}
    c["ident"] = np.eye(128, dtype=np.float32)
    tt = np.arange(T)
    row = (tt // 64).astype(np.float64)
    col = (tt % 64).astype(np.float64)
    inv = 10000.0 ** (-np.arange(16, dtype=np.float64) / 16)
    cos = np.zeros((128, T))
    sin = np.zeros((128, T))
    for p in range(128):
        d = p % 64
        pos = row if d < 32 else col
        a = pos * inv[d % 16]
        cos[p] = np.cos(a)
        sin[p] = np.sin(a)
    c["ropec"] = _bf(cos)
    c["ropes"] = _bf(sin)
    rt = np.zeros((128, 128))
    for m in range(128):
        if m % 32 < 16:
            rt[m + 16, m] = -1.0
        else:
            rt[m - 16, m] = 1.0
    c["ropert"] = _bf(rt)
    bo = np.zeros((128, 128), dtype=np.float32)
    bo[0:64, 0:64] = 1.0
    bo[64:128, 64:128] = 1.0
    c["blockones"] = bo
    zl, wl = _filter_tables(T)
    zc, wc = _filter_tables(TC)
    c["zT"] = np.ascontiguousarray(np.concatenate([zc, zl], axis=1).astype(np.float32))
    c["win"] = np.ascontiguousarray(np.concatenate([wc, wl], axis=0).astype(np.float32))
    fwl, invl, _ = _dft_tables(T)
    fwc, invc, _ = _dft_tables(TC)
    c["fwl"] = _bf(fwl)
    c["fwc"] = _bf(fwc)
    il = invl.reshape(2, 128, 12, 4, 512).transpose(3, 0, 1, 2, 4)
    c["invl"] = _bf(il)
    c["invc"] = _bf(invc)
    return c


def build(n_layers=DEPTH, dbg=False):
    nc = bass.Bass("TRN2", target_bir_lowering=False)

    def din(name, shape, dt=F32):
        return nc.dram_tensor(name, list(shape), dt, kind="ExternalInput").ap()

    def dint(name, shape, dt=F32):
        return nc.dram_tensor(name, list(shape), dt, kind="Internal").ap()

    x_d = din("x", [T, D])
    ctx_d = din("ctx", [TC, D])
    cT_d = din("cT", [128, 8, 2])
    w_ada_d = din("w_ada", [DEPTH, D, 6 * D])
    b_ada_d = din("b_ada", [DEPTH, 6 * D])
    g_mix_d = din("g_mix", [DEPTH, D])
    g_ffn_d = din("g_ffn", [DEPTH, D])
    w_in_ab_d = din("w_in_ab", [2, D, 2560])
    ca_d = din("ca", [2, 128, 4, 3])
    cb_d = din("cb", [2, 128, 4, 31])
    lnp_d = din("lnp", [2, 128, 3, 4])
    w_out_ab_d = din("w_out_ab", [2, D, D])
    wr_d = din("wr", [DEPTH, 128, 8, NE])
    w_gate_d = din("w_gate", [DEPTH, NE, D, D])
    w_up_d = din("w_up", [DEPTH, NE, D, D])
    w_down_d = din("w_down", [DEPTH, NE, D, D])
    ident_d = din("ident", [128, 128])
    w_in_cd_d = din("w_in_cd", [2, D, 3072])
    w_out_cd_d = din("w_out_cd", [2, D, D])
    gqk_d = din("gqk", [2, 128, 2])
    lamv_d = din("lamv", [2, 4 * 64])
    gsub_d = din("gsub", [2, 128, 1])
    cd_d = din("cd", [2, 128, 12, 3])
    hfw1_d = din("hf_w1", [2, 33, 64])
    hfv_d = din("hfv", [2, 64, 3])
    hfw2_d = din("hf_w2", [2, 64, 64])
    hfw3_d = din("hf_w3", [2, 64, 512])
    hfb_d = din("hf_bias", [2, 512])
    ropec_d = din("ropec", [128, T], BF16)
    ropes_d = din("ropes", [128, T], BF16)
    ropert_d = din("ropert", [128, 128], BF16)
    blockones_d = din("blockones", [128, 128])
    zT_d = din("zT", [33, TT])
    win_d = din("win", [TT, 512])
    fwl_d = din("fwl", [12, 128, 16, 256], BF16)
    fwc_d = din("fwc", [2, 128, 2, 256], BF16)
    invl_d = din("invl", [4, 2, 128, 12, 512], BF16)
    invc_d = din("invc", [2, 128, 2, 256], BF16)
    out_d = nc.dram_tensor("out", [T, D], F32, kind="ExternalOutput").ap()

    H_d = dint("H", [TT, D])
    modv_d = dint("modv", [2, 6 * D])
    u2tok_d = dint("u2tok", [TT, D], BF16)
    M_d = dint("Macc", [TT, D])
    if dbg:
        hdbg_d = nc.dram_tensor("hdbg", [TT, D], F32, kind="ExternalOutput").ap()
        dbg_ix_d = nc.dram_tensor("dbg_ix", [NE, NSLOT], F32, kind="ExternalOutput").ap()
        dbg_gv_d = nc.dram_tensor("dbg_gv", [NE, NSLOT], F32, kind="ExternalOutput").ap()
        dbg_m_d = nc.dram_tensor("dbg_m", [TT, D], F32, kind="ExternalOutput").ap()
        dbg_xe_d = nc.dram_tensor("dbg_xe", [128, 3, D], BF16, kind="ExternalOutput").ap()
        dbg_yb_d = nc.dram_tensor("dbg_yb", [128, 3, D], F32, kind="ExternalOutput").ap()
        dbg_ht_d = nc.dram_tensor("dbg_ht", [128, 8, NSLOT], BF16, kind="ExternalOutput").ap()
        dbg_ip_d = nc.dram_tensor("dbg_ip", [128, 3, NE], I32, kind="ExternalOutput").ap()

    top = ExitStack()
    P = Prog(nc)

    uid = [0]

    def sb(es, name, shape, dt=F32):
        uid[0] += 1
        return es.enter_context(nc.sbuf_tensor("%s_%d" % (name, uid[0]), list(shape), dt))

    PS = [top.enter_context(nc.psum_tensor("ps%d" % i, [128, 512], F32)) for i in range(8)]
    PK = ["ps%d" % i for i in range(8)]

    ident_f = sb(top, "ident_f", [128, 128])
    ident_b = sb(top, "ident_b", [128, 128], BF16)
    ones_f = sb(top, "ones_f", [128, 128])
    ones_b = sb(top, "ones_b", [128, 128], BF16)
    zero_f = sb(top, "zero_f", [128, 1024])
    P.dma(lambda e: e.dma_start(out=ident_f[:], in_=ident_d), writes=["ident_f"])
    P.dve(lambda e: e.tensor_copy(ident_b[:], ident_f[:]), reads=["ident_f"], writes=["ident_b"])
    P.dve(lambda e: e.memset(ones_f[:], 1.0), writes=["ones_f"])
    P.dve(lambda e: e.memset(ones_b[:], 1.0), writes=["ones_b"])
    P.dve(lambda e: e.memset(zero_f[:], 0.0), writes=["zero_f"])

    def hsrc(l, tt):
        if l == 0:
            if tt < 2:
                return ctx_d[tt * 128:(tt + 1) * 128, :]
            return x_d[(tt - 2) * 128:(tt - 1) * 128, :]
        return H_d[tt * 128:(tt + 1) * 128, :]

    def hkey(tt):
        return ("H", tt)

    def phase_mod(l):
        with ExitStack() as es:
            cS = sb(es, "cS", [128, 8, 2])
            modr = sb(es, "modr", [2, 6 * D])
            brow = sb(es, "brow", [2, 6 * D])
            grow = sb(es, "grow", [2, 2, D])
            wbuf = [sb(es, "wada%d" % i, [128, 8, 512]) for i in range(2)]
            P.dma(lambda e: e.dma_start(out=cS[:], in_=cT_d), writes=["cS"])
            P.act(lambda e: e.activation(out=cS[:], in_=cS[:], func=AF.Silu), reads=["cS"], writes=["cS"])
            for r in range(2):
                P.dma(lambda e, r=r: e.dma_start(out=brow[r:r + 1, :], in_=b_ada_d[l:l + 1, :]), writes=["brow"])
                P.dma(lambda e, r=r: e.dma_start(out=grow[r:r + 1, 0, :], in_=g_mix_d[l:l + 1, :]), writes=["grow"])
                P.dma(lambda e, r=r: e.dma_start(out=grow[r:r + 1, 1, :], in_=g_ffn_d[l:l + 1, :]), writes=["grow"])
            wv = w_ada_d[l].rearrange("(k p) n -> p k n", p=128)
            for nt in range(12):
                wb = wbuf[nt % 2]
                wk = "wada%d" % (nt % 2)
                P.dma(lambda e, wb=wb, nt=nt: e.dma_start(out=wb[:], in_=wv[:, :, nt * 512:(nt + 1) * 512]),
                      writes=[wk])
                pk = nt % 2
                for k in range(8):
                    P.pe(lambda e, wb=wb, k=k, pk=pk: e.matmul(PS[pk][0:2, :], cS[:, k, :], wb[:, k, :],
                                                                  start=(k == 0), stop=(k == 7)),
                         reads=[wk, "cS"], writes=[PK[pk]])
                P.dve(lambda e, nt=nt, pk=pk: e.tensor_tensor(out=modr[:, nt * 512:(nt + 1) * 512], in0=PS[pk][0:2, :],
                                                               in1=brow[:, nt * 512:(nt + 1) * 512], op=ALU.add),
                      reads=[PK[pk], "brow"], writes=["modr"])
            for (seg, gi) in ((1, 0), (4, 1)):
                P.dve(lambda e, seg=seg, gi=gi: e.scalar_tensor_tensor(
                    out=modr[:, seg * D:(seg + 1) * D], in0=modr[:, seg * D:(seg + 1) * D], scalar=1.0,
                    in1=grow[:, gi, :], op0=ALU.add, op1=ALU.mult), reads=["modr", "grow"], writes=["modr"])
            P.dma(lambda e: e.dma_start(out=modv_d, in_=modr[:]), reads=["modr"], writes=["modv"])
        P.barrier()

    def load_mod(tile, key, r, seg):
        P.dma(lambda e: e.dma_start(out=tile[:], in_=modv_d[r:r + 1, seg * D:(seg + 1) * D].partition_broadcast(128)),
              reads=["modv"], writes=[key])

    def phase_norm(l, hl, which, U, AFF=None):
        seg_s, seg_a = (0, 1) if which == 1 else (3, 4)
        with ExitStack() as es:
            A = [sb(es, "nA%d" % r, [128, D]) for r in range(2)]
            S = [sb(es, "nS%d" % r, [128, D]) for r in range(2)]
            for r in range(2):
                load_mod(A[r], "nA%d" % r, r, seg_a)
                load_mod(S[r], "nS%d" % r, r, seg_s)
            xt = [sb(es, "nx%d" % i, [128, D]) for i in range(2)]
            junk = sb(es, "njunk", [128, D], BF16)
            ss = [sb(es, "nss%d" % i, [128, 2]) for i in range(2)]
            tmp = [sb(es, "ntmp%d" % i, [128, D]) for i in range(2)]
            if which == 1:
                ub = [sb(es, "nub%d" % i, [128, D], BF16) for i in range(2)]
            else:
                ub = [sb(es, "nub%d" % i, [128, D], BF16) for i in range(2)]
                uT = [sb(es, "nuT%d" % i, [128, 8, 128]) for i in range(2)]
                wr = sb(es, "nwr", [128, 8, NE])
                sm = [sb(es, "nsm%d" % i, [128, 4]) for i in range(2)]
                ex = [sb(es, "nex%d" % i, [128, NE]) for i in range(2)]
                P.dma(lambda e: e.dma_start(out=wr[:], in_=wr_d[l]), writes=["nwr"])
            for tt in range(NTILE):
                b = tt % 2
                r = 1 if tt < 2 else 0
                kx, kss, ktmp, kub = "nx%d" % b, "nss%d" % b, "ntmp%d" % b, "nub%d" % b
                P.dma(lambda e, b=b, tt=tt: e.dma_start(out=xt[b][:], in_=hsrc(hl, tt)), reads=[hkey(tt)], writes=[kx])
                P.act(lambda e, b=b: e.activation(out=junk[:], in_=xt[b][:], func=AF.Square, accum_out=ss[b][:, 0:1]),
                      reads=[kx], writes=["njunk", kss])
                P.act(lambda e, b=b: e.activation(out=ss[b][:, 1:2], in_=ss[b][:, 0:1], func=AF.Sqrt,
                                                  scale=1.0 / D, bias=EPS), reads=[kss], writes=[kss])
                P.dve(lambda e, b=b: e.reciprocal(ss[b][:, 1:2], ss[b][:, 1:2]), reads=[kss], writes=[kss])
                P.dve(lambda e, b=b, r=r: e.scalar_tensor_tensor(out=tmp[b][:], in0=xt[b][:], scalar=ss[b][:, 1:2],
                                                                   in1=A[r][:], op0=ALU.mult, op1=ALU.mult),
                      reads=[kx, kss, "nA%d" % r], writes=[ktmp])
                if which == 1:
                    P.pool(lambda e, b=b, r=r: e.tensor_tensor(out=ub[b][:], in0=tmp[b][:], in1=S[r][:], op=ALU.add),
                           reads=[ktmp, "nS%d" % r], writes=[kub])
                    psb = 2 + b
                    pv = PS[psb][:].bitcast(BF16)
                    for k in range(8):
                        P.pe(lambda e, b=b, k=k, pv=pv: e.transpose(pv[:, k * 128:(k + 1) * 128],
                                                                     ub[b][:, k * 128:(k + 1) * 128], ident_b[:]),
                             reads=[kub, "ident_b"], writes=[PK[psb]])
                    P.act(lambda e, tt=tt, pv=pv: e.activation(
                        out=U[:, :, tt * 128:(tt + 1) * 128], in_=pv.rearrange("p (k t) -> p k t", k=8), func=AF.Copy),
                        reads=[PK[psb]], writes=[("U", tt)])
                else:
                    P.pool(lambda e, b=b, r=r: e.tensor_tensor(out=tmp[b][:], in0=tmp[b][:], in1=S[r][:], op=ALU.add),
                           reads=[ktmp, "nS%d" % r], writes=[ktmp])
                    P.act(lambda e, b=b: e.activation(out=ub[b][:], in_=tmp[b][:], func=AF.Copy),
                          reads=[ktmp], writes=[kub])
                    P.dma(lambda e, b=b, tt=tt: e.dma_start(out=u2tok_d[tt * 128:(tt + 1) * 128, :], in_=ub[b][:]),
                          reads=[kub], writes=[("u2tok", tt)])
                    kuT = "nuT%d" % b
                    for hh in range(2):
                        psb = 2 + 2 * b + hh
                        for k4 in range(4):
                            k = hh * 4 + k4
                            P.pe(lambda e, b=b, k=k, k4=k4, psb=psb: e.transpose(
                                PS[psb][:, k4 * 128:(k4 + 1) * 128], tmp[b][:, k * 128:(k + 1) * 128], ident_f[:]),
                                reads=[ktmp, "ident_f"], writes=[PK[psb]])
                        P.dve(lambda e, b=b, hh=hh, psb=psb: e.tensor_copy(
                            uT[b][:, hh * 4:(hh + 1) * 4, :], PS[psb][:].rearrange("p (k t) -> p k t", k=4)),
                            reads=[PK[psb]], writes=[kuT])
                    pl = 6 + b
                    for k in range(8):
                        P.pe(lambda e, b=b, k=k, pl=pl: e.matmul(PS[pl][:, 0:NE], uT[b][:, k, :], wr[:, k, :],
                                                                   start=(k == 0), stop=(k == 7)),
                             reads=[kuT, "nwr"], writes=[PK[pl]])
                    ksm, kex = "nsm%d" % b, "nex%d" % b
                    P.dve(lambda e, b=b, pl=pl: e.reduce_max(out=sm[b][:, 0:1], in_=PS[pl][:, 0:NE], axis=AX.X),
                          reads=[PK[pl]], writes=[ksm])
                    P.dve(lambda e, b=b: e.tensor_scalar(out=sm[b][:, 1:2], in0=sm[b][:, 0:1], scalar1=-1.0,
                                                         scalar2=None, op0=ALU.mult), reads=[ksm], writes=[ksm])
                    P.act(lambda e, b=b, pl=pl: e.activation(out=ex[b][:], in_=PS[pl][:, 0:NE], func=AF.Exp,
                                                             bias=sm[b][:, 1:2], scale=1.0, accum_out=sm[b][:, 2:3]),
                          reads=[PK[pl], ksm], writes=[kex, ksm])
                    P.dve(lambda e, b=b: e.reciprocal(sm[b][:, 3:4], sm[b][:, 2:3]), reads=[ksm], writes=[ksm])
                    P.dve(lambda e, b=b, tt=tt: e.tensor_scalar(out=AFF[:, tt, :], in0=ex[b][:], scalar1=sm[b][:, 3:4],
                                                                scalar2=None, op0=ALU.mult),
                          reads=[kex, ksm], writes=[("AFF", tt)])
        P.barrier()

    def phase_outproj(l, Y, w_out_ap, last_layer_unused=False):
        with ExitStack() as es:
            Wo = sb(es, "Wo", [128, 8, D], BF16)
            G = [sb(es, "oG%d" % r, [128, D]) for r in range(2)]
            xt = [sb(es, "ox%d" % i, [128, D]) for i in range(2)]
            tm = [sb(es, "ot%d" % i, [128, D]) for i in range(2)]
            wv = w_out_ap.rearrange("(k p) n -> p k n", p=128)
            for k in range(8):
                P.dma(lambda e, k=k: e.dma_start(out=Wo[:, k, :], in_=wv[:, k, :]), writes=[("Wo", k)], q="pool")
            for r in range(2):
                load_mod(G[r], "oG%d" % r, r, 2)
            for tt in range(NTILE):
                b = tt % 2
                r = 1 if tt < 2 else 0
                kx, kt = "ox%d" % b, "ot%d" % b
                P.dma(lambda e, b=b, tt=tt: e.dma_start(out=xt[b][:], in_=hsrc(l, tt)), reads=[hkey(tt)], writes=[kx])
                for hf in range(2):
                    pb = 2 * b + hf
                    for k in range(8):
                        P.pe(lambda e, k=k, tt=tt, hf=hf, pb=pb: e.matmul(
                            PS[pb][:], Y[:, k, tt * 128:(tt + 1) * 128], Wo[:, k, hf * 512:(hf + 1) * 512],
                            start=(k == 0), stop=(k == 7)), reads=[("Y", tt), ("Wo", k)], writes=[PK[pb]])
                    P.dve(lambda e, b=b, hf=hf, pb=pb, r=r: e.tensor_tensor(
                        out=tm[b][:, hf * 512:(hf + 1) * 512], in0=PS[pb][:], in1=G[r][:, hf * 512:(hf + 1) * 512],
                        op=ALU.mult), reads=[PK[pb], "oG%d" % r], writes=[kt])
                P.pool(lambda e, b=b: e.tensor_tensor(out=xt[b][:], in0=xt[b][:], in1=tm[b][:], op=ALU.add),
                       reads=[kx, kt], writes=[kx])
                P.dma(lambda e, b=b, tt=tt: e.dma_start(out=H_d[tt * 128:(tt + 1) * 128, :], in_=xt[b][:]),
                      reads=[kx], writes=[hkey(tt)])
        P.barrier()

    def phase_conv(l, U, Y):
        i2 = l // 2
        tiles = _tok_tiles()
        with ExitStack() as es:
            Wg = [sb(es, "Wg%d" % g, [128, 8, 512], BF16) for g in range(5)]
            wv = w_in_ab_d[i2].rearrange("(k p) n -> p k n", p=128)
            for g in range(5):
                for k in range(8):
                    P.dma(lambda e, g=g, k=k: e.dma_start(out=Wg[g][:, k, :], in_=wv[:, k, g * 512:(g + 1) * 512]),
                          writes=[("Wg", g)], q="pool")
            CA = sb(es, "CA", [128, 4, 3])
            CB = sb(es, "CB", [128, 4, 31])
            LNP = sb(es, "LNP", [128, 3, 4])
            P.dma(lambda e: e.dma_start(out=CA[:], in_=ca_d[i2]), writes=["CA"])
            P.dma(lambda e: e.dma_start(out=CB[:], in_=cb_d[i2]), writes=["CB"])
            P.dma(lambda e: e.dma_start(out=LNP[:], in_=lnp_d[i2]), writes=["LNP"])
            B = [sb(es, "cB%d" % i, [128, TT]) for i in range(3)]
            ZC = sb(es, "ZC", [128, 4, TT])

            psrr = [0]

            def proj(chunk, dst, dkey):
                g, c = chunk // 4, chunk % 4
                for (t0, n) in tiles:
                    pb = psrr[0] % 4
                    psrr[0] += 1
                    for k in range(8):
                        P.pe(lambda e, g=g, c=c, k=k, t0=t0, n=n, pb=pb: e.matmul(
                            PS[pb][:, 0:n], Wg[g][:, k, c * 128:(c + 1) * 128], U[:, k, t0:t0 + n],
                            start=(k == 0), stop=(k == 7)),
                            reads=[("Wg", g)] + [("U", t) for t in range(t0 // 128, (t0 + n) // 128)],
                            writes=[PK[pb]])
                    P.act(lambda e, t0=t0, n=n, pb=pb: e.activation(out=dst[:, t0:t0 + n], in_=PS[pb][:, 0:n],
                                                                      func=AF.Copy), reads=[PK[pb]], writes=[dkey])

            for i in range(4):
                proj(i, B[0], "cB0")
                proj(4 + i, B[1], "cB1")
                proj(8 + i, B[2], "cB2")
                P.dve(lambda e: e.tensor_tensor(out=B[1][:], in0=B[1][:], in1=B[2][:], op=ALU.mult),
                      reads=["cB1", "cB2"], writes=["cB1"])
                P.dve(lambda e, i=i: e.tensor_scalar(out=B[2][:], in0=B[1][:], scalar1=CA[:, i, 1:2], scalar2=None,
                                                      op0=ALU.mult), reads=["cB1", "CA"], writes=["cB2"])
                for (s0, L) in SEQS:
                    P.dve(lambda e, i=i, s0=s0, L=L: e.scalar_tensor_tensor(
                        out=B[2][:, s0 + 1:s0 + L], in0=B[1][:, s0:s0 + L - 1], scalar=CA[:, i, 0:1],
                        in1=B[2][:, s0 + 1:s0 + L], op0=ALU.mult, op1=ALU.add), reads=["cB1", "cB2", "CA"],
                        writes=["cB2"])
                    P.dve(lambda e, i=i, s0=s0, L=L: e.scalar_tensor_tensor(
                        out=B[2][:, s0:s0 + L - 1], in0=B[1][:, s0 + 1:s0 + L], scalar=CA[:, i, 2:3],
                        in1=B[2][:, s0:s0 + L - 1], op0=ALU.mult, op1=ALU.add), reads=["cB1", "cB2", "CA"],
                        writes=["cB2"])
                P.dve(lambda e, i=i: e.tensor_tensor(out=Y[:, i, :], in0=B[0][:], in1=B[2][:], op=ALU.mult),
                      reads=["cB0", "cB2"], writes=[("Y", t) for t in range(NTILE)])
            for i in range(4):
                proj(12 + i, B[0], "cB0")
                proj(16 + i, B[1], "cB1")
                P.act(lambda e: e.activation(out=B[1][:], in_=B[1][:], func=AF.Sigmoid), reads=["cB1"], writes=["cB1"])
                P.dve(lambda e: e.tensor_tensor(out=B[0][:], in0=B[0][:], in1=B[1][:], op=ALU.mult),
                      reads=["cB0", "cB1"], writes=["cB0"])
                zk = ("ZC", i)
                P.dve(lambda e, i=i: e.tensor_scalar(out=ZC[:, i, :], in0=B[0][:], scalar1=CB[:, i, 15:16],
                                                      scalar2=LNP[:, 0, i:i + 1], op0=ALU.mult, op1=ALU.add),
                      reads=["cB0", "CB", "LNP"], writes=[zk])
                for kk in range(31):
                    o = kk - 15
                    if o == 0:
                        continue
                    for (s0, L) in SEQS:
                        if o > 0:
                            oa, ia, n = s0, s0 + o, L - o
                        else:
                            oa, ia, n = s0 - o, s0, L + o
                        P.dve(lambda e, i=i, kk=kk, oa=oa, ia=ia, n=n: e.scalar_tensor_tensor(
                            out=ZC[:, i, oa:oa + n], in0=B[0][:, ia:ia + n], scalar=CB[:, i, kk:kk + 1],
                            in1=ZC[:, i, oa:oa + n], op0=ALU.mult, op1=ALU.add), reads=["cB0", zk, "CB"], writes=[zk])
            MEAN = B[1]
            RSTD = B[2]
            SQ = [sb(es, "cSQ%d" % i, [128, 512]) for i in range(2)]
            sqi = 0
            for (t0, n) in tiles:
                for i in range(4):
                    sq = SQ[sqi % 2]
                    sqk = "cSQ%d" % (sqi % 2)
                    sqi += 1
                    P.act(lambda e, i=i, t0=t0, n=n, sq=sq: e.activation(out=sq[:, 0:n], in_=ZC[:, i, t0:t0 + n],
                                                                          func=AF.Square), reads=[("ZC", i)], writes=[sqk])
                    P.pe(lambda e, i=i, t0=t0, n=n: e.matmul(PS[4][:, 0:n], ones_f[:], ZC[:, i, t0:t0 + n],
                                                             start=(i == 0), stop=(i == 3)),
                         reads=[("ZC", i), "ones_f"], writes=[PK[4]])
                    P.pe(lambda e, i=i, n=n, sq=sq: e.matmul(PS[5][:, 0:n], ones_f[:], sq[:, 0:n],
                                                              start=(i == 0), stop=(i == 3)),
                         reads=[sqk, "ones_f"], writes=[PK[5]])
                P.dve(lambda e, t0=t0, n=n: e.tensor_scalar(out=MEAN[:, t0:t0 + n], in0=PS[4][:, 0:n], scalar1=1.0 / 512,
                                                             scalar2=None, op0=ALU.mult), reads=[PK[4]], writes=["cB1"])
                P.dve(lambda e, t0=t0, n=n: e.tensor_tensor(out=B[0][:, t0:t0 + n], in0=MEAN[:, t0:t0 + n],
                                                             in1=MEAN[:, t0:t0 + n], op=ALU.mult),
                      reads=["cB1"], writes=["cB0"])
                P.dve(lambda e, t0=t0, n=n: e.scalar_tensor_tensor(
                    out=RSTD[:, t0:t0 + n], in0=PS[5][:, 0:n], scalar=1.0 / 512, in1=B[0][:, t0:t0 + n],
                    op0=ALU.mult, op1=ALU.subtract), reads=[PK[5], "cB0"], writes=["cB2"])
            P.act(lambda e: e.activation(out=RSTD[:], in_=RSTD[:], func=AF.Sqrt, scale=1.0, bias=EPS),
                  reads=["cB2"], writes=["cB2"])
            P.dve(lambda e: e.reciprocal(RSTD[:], RSTD[:]), reads=["cB2"], writes=["cB2"])
            for i in range(4):
                zk = ("ZC", i)
                P.dve(lambda e, i=i: e.tensor_tensor(out=ZC[:, i, :], in0=ZC[:, i, :], in1=MEAN[:], op=ALU.subtract),
                      reads=[zk, "cB1"], writes=[zk])
                P.dve(lambda e, i=i: e.tensor_tensor(out=ZC[:, i, :], in0=ZC[:, i, :], in1=RSTD[:], op=ALU.mult),
                      reads=[zk, "cB2"], writes=[zk])
                P.act(lambda e, i=i: e.activation(out=Y[:, 4 + i, :], in_=ZC[:, i, :], func=AF.Silu,
                                                  scale=LNP[:, 1, i:i + 1], bias=LNP[:, 2, i:i + 1]),
                      reads=[zk, "LNP"], writes=[("Y", t) for t in range(NTILE)])
        P.barrier()


    def load_wgroup(Wt, key, src3, g):
        for k in range(8):
            P.dma(lambda e, k=k: e.dma_start(out=Wt[:, k, :], in_=src3[:, k, g * 512:(g + 1) * 512]),
                  writes=[(key, k)], q="pool")

    def phase_attn(l, U, Y):
        i2 = l // 2
        lam_init = 0.8 - 0.6 * math.exp(-0.3 * l)
        tiles = _tok_tiles()
        wv = w_in_cd_d[i2].rearrange("(k p) n -> p k n", p=128)
        with ExitStack() as es:
            qkT = [sb(es, "qT", [128, 4, TT], BF16), sb(es, "kT", [128, 4, TT], BF16)]
            V = sb(es, "V", [128, NTILE, 512], BF16)
            small = sb(es, "asmall", [128, 8])
            lamb = sb(es, "lamb", [128, 4 * 64])
            GQK = sb(es, "GQK", [128, 2])
            GS = sb(es, "GS", [128, 1])
            with ExitStack() as es2:
                Wt = [sb(es2, "Wt%d" % i, [128, 8, 512], BF16) for i in range(2)]
                COS = sb(es2, "COS", [128, T], BF16)
                SIN = sb(es2, "SIN", [128, T], BF16)
                RT = sb(es2, "RT", [128, 128], BF16)
                BO = sb(es2, "BO", [128, 128])
                QK = sb(es2, "QK", [128, TT])
                SQ = [sb(es2, "aSQ%d" % i, [128, 512]) for i in range(2)]
                RS = [sb(es2, "aRS%d" % i, [128, 512]) for i in range(2)]
                QN = [sb(es2, "aQN%d" % i, [128, 512], BF16) for i in range(2)]
                O1 = [sb(es2, "aO1%d" % i, [128, 512]) for i in range(2)]
                P.dma(lambda e: e.dma_start(out=COS[:], in_=ropec_d), writes=["COS"])
                P.dma(lambda e: e.dma_start(out=SIN[:], in_=ropes_d), writes=["SIN"])
                P.dma(lambda e: e.dma_start(out=RT[:], in_=ropert_d), writes=["RT"])
                P.dma(lambda e: e.dma_start(out=BO[:], in_=blockones_d), writes=["BO"])
                P.dma(lambda e: e.dma_start(out=GQK[:], in_=gqk_d[i2]), writes=["GQK"])
                P.dma(lambda e: e.dma_start(out=GS[:], in_=gsub_d[i2]), writes=["GS"])
                P.dma(lambda e: e.dma_start(out=lamb[:], in_=lamv_d[i2:i2 + 1, :].partition_broadcast(128)),
                      writes=["lamb"])
                for j in range(2):
                    P.dve(lambda e, j=j: e.tensor_tensor(out=lamb[:, j * 128:j * 128 + 64], in0=lamb[:, j * 128:j * 128 + 64],
                                                          in1=lamb[:, j * 128 + 64:j * 128 + 128], op=ALU.mult),
                          reads=["lamb"], writes=["lamb"])
                    P.dve(lambda e, j=j: e.reduce_sum(out=small[:, j:j + 1], in_=lamb[:, j * 128:j * 128 + 64], axis=AX.X),
                          reads=["lamb"], writes=["asmall"])
                P.act(lambda e: e.activation(out=small[:, 2:4], in_=small[:, 0:2], func=AF.Exp),
                      reads=["asmall"], writes=["asmall"])
                P.dve(lambda e: e.scalar_tensor_tensor(out=small[:, 4:5], in0=small[:, 3:4], scalar=-lam_init,
                                                        in1=small[:, 2:3], op0=ALU.add, op1=ALU.subtract),
                      reads=["asmall"], writes=["asmall"])
                P.dve(lambda e: e.tensor_scalar(out=GS[:], in0=GS[:], scalar1=1.0 - lam_init, scalar2=None, op0=ALU.mult),
                      reads=["GS"], writes=["GS"])
                cnt = 0
                for g in range(2):
                    wt = Wt[g % 2]
                    wkey = "Wt%d" % (g % 2)
                    load_wgroup(wt, wkey, wv, g)
                    for h in range(4):
                        for (t0, n) in tiles:
                            pb = cnt % 2
                            bb = cnt % 2
                            cnt += 1
                            for k in range(8):
                                P.pe(lambda e, wt=wt, h=h, k=k, t0=t0, n=n, pb=pb: e.matmul(
                                    PS[pb][:, 0:n], wt[:, k, h * 128:(h + 1) * 128], U[:, k, t0:t0 + n],
                                    start=(k == 0), stop=(k == 7)),
                                    reads=[(wkey, k)] + [("U", t) for t in range(t0 // 128, (t0 + n) // 128)],
                                    writes=[PK[pb]])
                            P.act(lambda e, t0=t0, n=n, pb=pb: e.activation(out=QK[:, t0:t0 + n], in_=PS[pb][:, 0:n],
                                                                              func=AF.Copy), reads=[PK[pb]], writes=["QK"])
                            P.act(lambda e, t0=t0, n=n, bb=bb: e.activation(out=SQ[bb][:, 0:n], in_=QK[:, t0:t0 + n],
                                                                              func=AF.Square), reads=["QK"],
                                  writes=["aSQ%d" % bb])
                            p2 = 2 + pb
                            P.pe(lambda e, n=n, bb=bb, p2=p2: e.matmul(PS[p2][:, 0:n], BO[:], SQ[bb][:, 0:n],
                                                                        start=True, stop=True),
                                 reads=["aSQ%d" % bb, "BO"], writes=[PK[p2]])
                            P.act(lambda e, n=n, bb=bb, p2=p2: e.activation(out=RS[bb][:, 0:n], in_=PS[p2][:, 0:n],
                                                                              func=AF.Sqrt, scale=1.0 / 64, bias=EPS),
                                  reads=[PK[p2]], writes=["aRS%d" % bb])
                            P.dve(lambda e, n=n, bb=bb: e.reciprocal(RS[bb][:, 0:n], RS[bb][:, 0:n]),
                                  reads=["aRS%d" % bb], writes=["aRS%d" % bb])
                            dstk = [("qk", g, h, t) for t in range(t0 // 128, (t0 + n) // 128)]
                            if t0 < TC:
                                P.dve(lambda e, g=g, h=h, t0=t0, n=n, bb=bb: e.scalar_tensor_tensor(
                                    out=qkT[g][:, h, t0:t0 + n], in0=QK[:, t0:t0 + n], scalar=GQK[:, g:g + 1],
                                    in1=RS[bb][:, 0:n], op0=ALU.mult, op1=ALU.mult),
                                    reads=["QK", "GQK", "aRS%d" % bb], writes=dstk)
                            else:
                                P.dve(lambda e, g=g, t0=t0, n=n, bb=bb: e.scalar_tensor_tensor(
                                    out=QN[bb][:, 0:n], in0=QK[:, t0:t0 + n], scalar=GQK[:, g:g + 1],
                                    in1=RS[bb][:, 0:n], op0=ALU.mult, op1=ALU.mult),
                                    reads=["QK", "GQK", "aRS%d" % bb], writes=["aQN%d" % bb])
                                p3 = 4 + pb
                                P.pe(lambda e, n=n, bb=bb, p3=p3: e.matmul(PS[p3][:, 0:n], RT[:], QN[bb][:, 0:n],
                                                                            start=True, stop=True),
                                     reads=["aQN%d" % bb, "RT"], writes=[PK[p3]])
                                c0 = t0 - TC
                                P.pool(lambda e, n=n, bb=bb, c0=c0: e.tensor_tensor(
                                    out=O1[bb][:, 0:n], in0=QN[bb][:, 0:n], in1=COS[:, c0:c0 + n], op=ALU.mult),
                                    reads=["aQN%d" % bb, "COS"], writes=["aO1%d" % bb])
                                P.dve(lambda e, n=n, bb=bb, c0=c0, p3=p3: e.tensor_tensor(
                                    out=RS[bb][:, 0:n], in0=PS[p3][:, 0:n], in1=SIN[:, c0:c0 + n], op=ALU.mult),
                                    reads=[PK[p3], "SIN"], writes=["aRS%d" % bb])
                                P.dve(lambda e, g=g, h=h, t0=t0, n=n, bb=bb: e.tensor_tensor(
                                    out=qkT[g][:, h, t0:t0 + n], in0=O1[bb][:, 0:n], in1=RS[bb][:, 0:n], op=ALU.add),
                                    reads=["aO1%d" % bb, "aRS%d" % bb], writes=dstk)
                wt = Wt[0]
                load_wgroup(wt, "Wt0", wv, 2)
                for tt in range(NTILE):
                    pb = 6 + tt % 2
                    for k in range(8):
                        P.pe(lambda e, tt=tt, k=k, pb=pb: e.matmul(PS[pb][:], U[:, k, tt * 128:(tt + 1) * 128], wt[:, k, :],
                                                                    start=(k == 0), stop=(k == 7)),
                             reads=[("U", tt), ("Wt0", k)], writes=[PK[pb]])
                    P.act(lambda e, tt=tt, pb=pb: e.activation(out=V[:, tt, :], in_=PS[pb][:], func=AF.Copy),
                          reads=[PK[pb]], writes=[("V", tt)])
            P.barrier()
            with ExitStack() as es2:
                Eb = [sb(es2, "Eb%d" % i, [128, 512], BF16) for i in range(3)]
                R0 = sb(es2, "aR0", [128, 512])
                R1 = sb(es2, "aR1", [128, 512])
                OO = sb(es2, "aOO", [128, 512])
                SQ2 = sb(es2, "aSQ2", [128, 512])
                ecnt = 0
                groups = [(TC + i * 512, 512, list(range(NTILE))) for i in range(T // 512)] + [(0, TC, [0, 1])]
                for h in range(4):
                    for (q0, n, ktl) in groups:
                        qkeys = [("qk", 0, h, t) for t in range(q0 // 128, (q0 + n) // 128)]
                        its = [(j, ki, kt) for j in range(2) for ki, kt in enumerate(ktl)]
                        nk = len(ktl)

                        def emit_s(idx, q0=q0, n=n, h=h, qkeys=qkeys):
                            j, ki, kt = its[idx]
                            psb = (ecnt + idx) % 2
                            P.pe(lambda e, h=h, j=j, kt=kt, q0=q0, n=n, psb=psb: e.matmul(
                                PS[psb][:, 0:n], qkT[1][j * 64:(j + 1) * 64, h, kt * 128:(kt + 1) * 128],
                                qkT[0][j * 64:(j + 1) * 64, h, q0:q0 + n], start=True, stop=True),
                                reads=qkeys + [("qk", 1, h, kt)], writes=[PK[psb]])

                        emit_s(0)
                        for idx, (j, ki, kt) in enumerate(its):
                            po, pz = 2 + 2 * j, 3 + 2 * j
                            psb = (ecnt + idx) % 2
                            eb = (ecnt + idx) % 3
                            P.act(lambda e, n=n, psb=psb, eb=eb: e.activation(out=Eb[eb][:, 0:n], in_=PS[psb][:, 0:n],
                                                                                func=AF.Exp, scale=0.125),
                                  reads=[PK[psb]], writes=["Eb%d" % eb])
                            if idx + 1 < len(its):
                                emit_s(idx + 1)
                            P.pe(lambda e, h=h, kt=kt, n=n, eb=eb, po=po, ki=ki, nk=nk: e.matmul(
                                PS[po][:, 0:n], V[:, kt, h * 128:(h + 1) * 128], Eb[eb][:, 0:n],
                                start=(ki == 0), stop=(ki == nk - 1)), reads=[("V", kt), "Eb%d" % eb], writes=[PK[po]])
                            P.pe(lambda e, n=n, eb=eb, pz=pz, ki=ki, nk=nk: e.matmul(
                                PS[pz][:, 0:n], ones_b[:], Eb[eb][:, 0:n], start=(ki == 0), stop=(ki == nk - 1)),
                                reads=["ones_b", "Eb%d" % eb], writes=[PK[pz]])
                        ecnt += len(its)
                        P.dve(lambda e, n=n: e.reciprocal(R0[:, 0:n], PS[3][:, 0:n]), reads=[PK[3]], writes=["aR0"])
                        P.dve(lambda e, n=n: e.tensor_tensor(out=R0[:, 0:n], in0=PS[2][:, 0:n], in1=R0[:, 0:n], op=ALU.mult),
                              reads=[PK[2], "aR0"], writes=["aR0"])
                        P.dve(lambda e, n=n: e.reciprocal(R1[:, 0:n], PS[5][:, 0:n]), reads=[PK[5]], writes=["aR1"])
                        P.dve(lambda e, n=n: e.tensor_tensor(out=R1[:, 0:n], in0=PS[4][:, 0:n], in1=R1[:, 0:n], op=ALU.mult),
                              reads=[PK[4], "aR1"], writes=["aR1"])
                        P.dve(lambda e, n=n: e.scalar_tensor_tensor(out=OO[:, 0:n], in0=R1[:, 0:n], scalar=small[:, 4:5],
                                                                     in1=R0[:, 0:n], op0=ALU.mult, op1=ALU.add),
                              reads=["aR0", "aR1", "asmall"], writes=["aOO"])
                        P.act(lambda e, n=n: e.activation(out=SQ2[:, 0:n], in_=OO[:, 0:n], func=AF.Square),
                              reads=["aOO"], writes=["aSQ2"])
                        P.pe(lambda e, n=n: e.matmul(PS[6][:, 0:n], ones_f[:], SQ2[:, 0:n], start=True, stop=True),
                             reads=["aSQ2", "ones_f"], writes=[PK[6]])
                        P.act(lambda e, n=n: e.activation(out=SQ2[:, 0:n], in_=PS[6][:, 0:n], func=AF.Sqrt,
                                                          scale=1.0 / 128, bias=1e-5), reads=[PK[6]], writes=["aSQ2"])
                        P.dve(lambda e, n=n: e.reciprocal(SQ2[:, 0:n], SQ2[:, 0:n]), reads=["aSQ2"], writes=["aSQ2"])
                        P.dve(lambda e, h=h, q0=q0, n=n: e.scalar_tensor_tensor(
                            out=Y[:, h, q0:q0 + n], in0=OO[:, 0:n], scalar=GS[:, 0:1], in1=SQ2[:, 0:n],
                            op0=ALU.mult, op1=ALU.mult), reads=["aOO", "GS", "aSQ2"],
                            writes=[("Y", t) for t in range(q0 // 128, (q0 + n) // 128)])
        P.barrier()

    def phase_hyena(l, U, Y):
        i2 = l // 2
        tiles = _tok_tiles()
        wv = w_in_cd_d[i2].rearrange("(k p) n -> p k n", p=128)
        PI = math.pi
        with ExitStack() as es:
            h_tok = sb(es, "h_tok", [128, NTILE, 512], BF16)
            v_tok = sb(es, "v_tok", [128, NTILE, 512], BF16)
            GOc = sb(es, "GOc", [128, 4, TT], BF16)
            with ExitStack() as es2:
                zT = sb(es2, "zT", [33, TT])
                W1 = sb(es2, "hW1", [33, 64])
                W2 = sb(es2, "hW2", [64, 64])
                W3 = sb(es2, "hW3", [64, 512])
                HV = sb(es2, "hHV", [64, 3])
                HB = sb(es2, "hHB", [1, 512])
                HID = [sb(es2, "hHID%d" % i, [64, TT]) for i in range(2)]
                KI = sb(es2, "hKI", [64, 512], I32)
                KF = sb(es2, "hKF", [64, 512])
                AA = sb(es2, "hAA", [64, 512])
                WN = [sb(es2, "hWN%d" % i, [128, 512]) for i in range(2)]
                HT = [sb(es2, "hHT%d" % i, [128, 512]) for i in range(2)]
                P.dma(lambda e: e.dma_start(out=zT[:], in_=zT_d), writes=["zT"])
                P.dma(lambda e: e.dma_start(out=W1[:], in_=hfw1_d[i2]), writes=["hW1"])
                P.dma(lambda e: e.dma_start(out=W2[:], in_=hfw2_d[i2]), writes=["hW2"])
                P.dma(lambda e: e.dma_start(out=W3[:], in_=hfw3_d[i2]), writes=["hW3"])
                P.dma(lambda e: e.dma_start(out=HV[:], in_=hfv_d[i2]), writes=["hHV"])
                P.dma(lambda e: e.dma_start(out=HB[:], in_=hfb_d[i2:i2 + 1, :]), writes=["hHB"])
                for layer in range(2):
                    src = zT if layer == 0 else HID[0]
                    srck = "zT" if layer == 0 else "hHID0"
                    kdim = 33 if layer == 0 else 64
                    wm = W1 if layer == 0 else W2
                    wmk = "hW1" if layer == 0 else "hW2"
                    bcol = 0 if layer == 0 else 2
                    dst = HID[layer]
                    dk = "hHID%d" % layer
                    for ti, (t0, n) in enumerate(tiles):
                        pb = ti % 2
                        P.pe(lambda e, src=src, kdim=kdim, wm=wm, t0=t0, n=n, pb=pb: e.matmul(
                            PS[pb][0:64, 0:n], wm[0:kdim, :], src[0:kdim, t0:t0 + n], start=True, stop=True),
                            reads=[srck, wmk], writes=[PK[pb]])
                        P.dve(lambda e, n=n, pb=pb, bcol=bcol: e.tensor_scalar(
                            out=AA[:, 0:n], in0=PS[pb][0:64, 0:n], scalar1=HV[:, bcol:bcol + 1], scalar2=HV[:, 1:2],
                            op0=ALU.add, op1=ALU.mult), reads=[PK[pb], "hHV"], writes=["hAA"])
                        P.dve(lambda e, n=n: e.tensor_scalar(out=KI[:, 0:n], in0=AA[:, 0:n], scalar1=1.0 / (2 * PI),
                                                             scalar2=None, op0=ALU.mult), reads=["hAA"], writes=["hKI"])
                        P.dve(lambda e, n=n: e.tensor_copy(KF[:, 0:n], KI[:, 0:n]), reads=["hKI"], writes=["hKF"])
                        P.dve(lambda e, n=n: e.scalar_tensor_tensor(out=AA[:, 0:n], in0=KF[:, 0:n], scalar=-2 * PI,
                                                                     in1=AA[:, 0:n], op0=ALU.mult, op1=ALU.add),
                              reads=["hKF", "hAA"], writes=["hAA"])
                        P.dve(lambda e, n=n: e.tensor_scalar(out=KF[:, 0:n], in0=AA[:, 0:n], scalar1=PI, scalar2=-2 * PI,
                                                             op0=ALU.is_gt, op1=ALU.mult), reads=["hAA"], writes=["hKF"])
                        P.dve(lambda e, n=n: e.tensor_tensor(out=AA[:, 0:n], in0=AA[:, 0:n], in1=KF[:, 0:n], op=ALU.add),
                              reads=["hAA", "hKF"], writes=["hAA"])
                        P.dve(lambda e, n=n: e.tensor_scalar(out=KF[:, 0:n], in0=AA[:, 0:n], scalar1=-PI, scalar2=2 * PI,
                                                             op0=ALU.is_lt, op1=ALU.mult), reads=["hAA"], writes=["hKF"])
                        P.dve(lambda e, n=n: e.tensor_tensor(out=AA[:, 0:n], in0=AA[:, 0:n], in1=KF[:, 0:n], op=ALU.add),
                              reads=["hAA", "hKF"], writes=["hAA"])
                        P.dve(lambda e, n=n: e.tensor_scalar(out=AA[:, 0:n], in0=AA[:, 0:n], scalar1=-PI, scalar2=PI,
                                                             op0=ALU.max, op1=ALU.min), reads=["hAA"], writes=["hAA"])
                        P.act(lambda e, dst=dst, t0=t0, n=n: e.activation(out=dst[:, t0:t0 + n], in_=AA[:, 0:n], func=AF.Sin),
                              reads=["hAA"], writes=[dk])
                for tt in range(NTILE):
                    b = tt % 2
                    pb = 2 + b
                    P.dma(lambda e, b=b, tt=tt: e.dma_start(out=WN[b][:], in_=win_d[tt * 128:(tt + 1) * 128, :]),
                          writes=["hWN%d" % b])
                    P.pe(lambda e, tt=tt, pb=pb: e.matmul(PS[pb][:], HID[1][:, tt * 128:(tt + 1) * 128], W3[:],
                                                          start=True, stop=True), reads=["hHID1", "hW3"], writes=[PK[pb]])
                    centre = tt in (1, 2 + 8)
                    if centre:
                        P.dve(lambda e, b=b, pb=pb: e.tensor_tensor(out=HT[b][:], in0=PS[pb][:], in1=WN[b][:], op=ALU.mult),
                              reads=[PK[pb], "hWN%d" % b], writes=["hHT%d" % b])
                        P.dve(lambda e, b=b: e.tensor_tensor(out=HT[b][0:1, :], in0=HT[b][0:1, :], in1=HB[:], op=ALU.add),
                              reads=["hHT%d" % b, "hHB"], writes=["hHT%d" % b])
                        P.act(lambda e, b=b, tt=tt: e.activation(out=h_tok[:, tt, :], in_=HT[b][:], func=AF.Copy),
                              reads=["hHT%d" % b], writes=[("h_tok", tt)])
                    else:
                        P.dve(lambda e, b=b, pb=pb, tt=tt: e.tensor_tensor(out=h_tok[:, tt, :], in0=PS[pb][:], in1=WN[b][:],
                                                                            op=ALU.mult),
                              reads=[PK[pb], "hWN%d" % b], writes=[("h_tok", tt)])
            P.barrier()
            with ExitStack() as es2:
                Wt = [sb(es2, "hWt%d" % i, [128, 8, 512], BF16) for i in range(3)]
                CD = sb(es2, "CD", [128, 12, 3])
                Bf = [sb(es2, "hB%d" % i, [128, TT]) for i in range(3)]
                P.dma(lambda e: e.dma_start(out=CD[:], in_=cd_d[i2]), writes=["CD"])
                for g in range(3):
                    load_wgroup(Wt[g], "hWt%d" % g, wv, 3 + g)
                pcnt = [0]

                def projc(g, c, raw, rawk, dst, dstk):
                    for (t0, n) in tiles:
                        pb = pcnt[0] % 4
                        pcnt[0] += 1
                        for k in range(8):
                            P.pe(lambda e, g=g, c=c, k=k, t0=t0, n=n, pb=pb: e.matmul(
                                PS[pb][:, 0:n], Wt[g][:, k, c * 128:(c + 1) * 128], U[:, k, t0:t0 + n],
                                start=(k == 0), stop=(k == 7)),
                                reads=[("hWt%d" % g, k)] + [("U", t) for t in range(t0 // 128, (t0 + n) // 128)],
                                writes=[PK[pb]])
                        P.act(lambda e, t0=t0, n=n, pb=pb: e.activation(out=raw[:, t0:t0 + n], in_=PS[pb][:, 0:n],
                                                                          func=AF.Copy), reads=[PK[pb]], writes=[rawk])
                    ch = g * 4 + c
                    P.dve(lambda e: e.tensor_scalar(out=dst[:], in0=raw[:], scalar1=CD[:, ch, 1:2], scalar2=None,
                                                    op0=ALU.mult), reads=[rawk, "CD"], writes=[dstk])
                    for (s0, L) in SEQS:
                        P.dve(lambda e, s0=s0, L=L: e.scalar_tensor_tensor(
                            out=dst[:, s0 + 1:s0 + L], in0=raw[:, s0:s0 + L - 1], scalar=CD[:, ch, 0:1],
                            in1=dst[:, s0 + 1:s0 + L], op0=ALU.mult, op1=ALU.add), reads=[rawk, dstk, "CD"], writes=[dstk])
                        P.dve(lambda e, s0=s0, L=L: e.scalar_tensor_tensor(
                            out=dst[:, s0:s0 + L - 1], in0=raw[:, s0 + 1:s0 + L], scalar=CD[:, ch, 2:3],
                            in1=dst[:, s0:s0 + L - 1], op0=ALU.mult, op1=ALU.add), reads=[rawk, dstk, "CD"], writes=[dstk])

                for c in range(4):
                    projc(0, c, Bf[0], "hB0", Bf[1], "hB1")
                    P.act(lambda e, c=c: e.activation(out=GOc[:, c, :], in_=Bf[1][:], func=AF.Copy),
                          reads=["hB1"], writes=[("GOc", c)])
                    projc(1, c, Bf[0], "hB0", Bf[1], "hB1")
                    projc(2, c, Bf[0], "hB0", Bf[2], "hB2")
                    P.pool(lambda e: e.tensor_tensor(out=Bf[1][:], in0=Bf[1][:], in1=Bf[2][:], op=ALU.mult),
                           reads=["hB1", "hB2"], writes=["hB1"])
                    for t4 in range(0, NTILE, 4):
                        nt = min(4, NTILE - t4)
                        pb = 4 + (t4 // 4) % 2
                        for j in range(nt):
                            P.pe(lambda e, t4=t4, j=j, pb=pb: e.transpose(
                                PS[pb][:, j * 128:(j + 1) * 128], Bf[1][:, (t4 + j) * 128:(t4 + j + 1) * 128], ident_f[:]),
                                reads=["hB1", "ident_f"], writes=[PK[pb]])
                        P.act(lambda e, t4=t4, nt=nt, pb=pb, c=c: e.activation(
                            out=v_tok[:, t4:t4 + nt, c * 128:(c + 1) * 128],
                            in_=PS[pb][:, 0:nt * 128].rearrange("p (j t) -> p j t", j=nt), func=AF.Copy),
                            reads=[PK[pb]], writes=[("v_tok", c)])
            P.barrier()
            with ExitStack() as es2:
                YS = sb(es2, "YS", [128, 24, 512], BF16)
                YSc = sb(es2, "YSc", [128, 4, 512], BF16)
                es3 = ExitStack()
                FW = [sb(es3, "FW%d" % i, [128, 16, 256], BF16) for i in range(2)]
                HH = [sb(es3, "HH%d" % i, [128, 2, 512]) for i in range(2)]
                T1 = sb(es3, "hT1", [128, 512])
                T2 = sb(es3, "hT2", [128, 512])
                T3 = sb(es3, "hT3", [128, 512])
                T4 = sb(es3, "hT4", [128, 512])
                fcnt = 0
                for (npair, tt0, ntt, fwd, ys, ysk) in ((12, 2, 16, fwl_d, YS, "YS"), (2, 0, 2, fwc_d, YSc, "YSc")):
                    for i in range(npair):
                        fb = fcnt % 2
                        fcnt += 1
                        fw = FW[fb]
                        fwk = "FW%d" % fb
                        hh = HH[fb]
                        hhk = "HH%d" % fb
                        P.dma(lambda e, fw=fw, fwd=fwd, i=i, ntt=ntt: e.dma_start(out=fw[:, 0:ntt, :], in_=fwd[i]),
                              writes=[fwk])
                        pbase = 4 * fb
                        for (srct, srck, pr, pi) in ((h_tok, "h_tok", pbase, pbase + 1), (v_tok, "v_tok", pbase + 2, pbase + 3)):
                            for part, pb in ((0, pr), (1, pi)):
                                for tt in range(ntt):
                                    rk = [("h_tok", tt0 + tt)] if srct is h_tok else [("v_tok", c) for c in range(4)]
                                    P.pe(lambda e, fw=fw, tt=tt, part=part, pb=pb, srct=srct, tt0=tt0, ntt=ntt: e.matmul(
                                        PS[pb][:], fw[:, tt, part * 128:(part + 1) * 128], srct[:, tt0 + tt, :],
                                        start=(tt == 0), stop=(tt == ntt - 1)), reads=[fwk] + rk, writes=[PK[pb]])
                            if srct is h_tok:
                                P.act(lambda e, hh=hh, pbase=pbase: e.activation(out=hh[:, 0, :], in_=PS[pbase][:], func=AF.Copy),
                                      reads=[PK[pbase]], writes=[hhk])
                                P.act(lambda e, hh=hh, pbase=pbase: e.activation(out=hh[:, 1, :], in_=PS[pbase + 1][:], func=AF.Copy),
                                      reads=[PK[pbase + 1]], writes=[hhk])
                        nch = npair
                        pu, pv = pbase + 2, pbase + 3
                        P.dve(lambda e, hh=hh, pu=pu: e.tensor_tensor(out=T1[:], in0=PS[pu][:], in1=hh[:, 0, :], op=ALU.mult),
                              reads=[PK[pu], hhk], writes=["hT1"])
                        P.dve(lambda e, hh=hh, pv=pv: e.tensor_tensor(out=T2[:], in0=PS[pv][:], in1=hh[:, 1, :], op=ALU.mult),
                              reads=[PK[pv], hhk], writes=["hT2"])
                        P.pool(lambda e, ys=ys, i=i: e.tensor_tensor(out=ys[:, i, :], in0=T1[:], in1=T2[:], op=ALU.subtract),
                               reads=["hT1", "hT2"], writes=[(ysk, i)])
                        P.dve(lambda e, hh=hh, pu=pu: e.tensor_tensor(out=T3[:], in0=PS[pu][:], in1=hh[:, 1, :], op=ALU.mult),
                              reads=[PK[pu], hhk], writes=["hT3"])
                        P.dve(lambda e, hh=hh, pv=pv: e.tensor_tensor(out=T4[:], in0=PS[pv][:], in1=hh[:, 0, :], op=ALU.mult),
                              reads=[PK[pv], hhk], writes=["hT4"])
                        P.pool(lambda e, ys=ys, i=i, nch=nch: e.tensor_tensor(out=ys[:, nch + i, :], in0=T3[:], in1=T4[:],
                                                                               op=ALU.add),
                               reads=["hT3", "hT4"], writes=[(ysk, nch + i)])
                P.barrier()
                es3.close()
                IV = [sb(es2, "IV%d" % i, [128, 12, 512], BF16) for i in range(2)]
                icnt = 0
                for j in range(4):
                    for half in range(2):
                        ib = icnt % 2
                        icnt += 1
                        P.dma(lambda e, ib=ib, j=j, half=half: e.dma_start(out=IV[ib][:], in_=invl_d[j, half]),
                              writes=["IV%d" % ib])
                        for c in range(4):
                            pc = 4 * (j % 2) + c
                            for fc in range(12):
                                P.pe(lambda e, ib=ib, half=half, c=c, fc=fc, pc=pc: e.matmul(
                                    PS[pc][:], YS[:, half * 12 + fc, c * 128:(c + 1) * 128], IV[ib][:, fc, :],
                                    start=(half == 0 and fc == 0), stop=(half == 1 and fc == 11)),
                                    reads=[("YS", half * 12 + fc), "IV%d" % ib], writes=[PK[pc]])
                    t0 = TC + j * 512
                    for c in range(4):
                        pc = 4 * (j % 2) + c
                        P.dve(lambda e, c=c, t0=t0, pc=pc: e.tensor_tensor(out=Y[:, 4 + c, t0:t0 + 512], in0=PS[pc][:],
                                                                           in1=GOc[:, c, t0:t0 + 512], op=ALU.mult),
                              reads=[PK[pc], ("GOc", c)], writes=[("Y", t) for t in range(t0 // 128, t0 // 128 + 4)])
                IVc = [sb(es2, "IVc%d" % i, [128, 2, 256], BF16) for i in range(2)]
                for half in range(2):
                    P.dma(lambda e, half=half: e.dma_start(out=IVc[half][:], in_=invc_d[half]), writes=["IVc%d" % half])
                for c in range(4):
                    for half in range(2):
                        for fc in range(2):
                            P.pe(lambda e, half=half, c=c, fc=fc: e.matmul(
                                PS[4 + c][:, 0:TC], YSc[:, half * 2 + fc, c * 128:(c + 1) * 128], IVc[half][:, fc, :],
                                start=(half == 0 and fc == 0), stop=(half == 1 and fc == 1)),
                                reads=[("YSc", half * 2 + fc), "IVc%d" % half], writes=[PK[4 + c]])
                    P.dve(lambda e, c=c: e.tensor_tensor(out=Y[:, 4 + c, 0:TC], in0=PS[4 + c][:, 0:TC], in1=GOc[:, c, 0:TC],
                                                         op=ALU.mult),
                          reads=[PK[4 + c], ("GOc", c)], writes=[("Y", 0), ("Y", 1)])
        P.barrier()

    def phase_moe(l, AFF, last):
        with ExitStack() as es:
            affT = sb(es, "affT", [NE, TT])
            work = sb(es, "mwork", [NE, TT])
            GV = sb(es, "GV", [NE, NSLOT])
            IXu = sb(es, "IXu", [NE, NSLOT], U32)
            IXf = sb(es, "IXf", [NE, NSLOT])
            gP = sb(es, "gP", [128, 3, NE])
            iP = sb(es, "iP", [128, 3, NE], I32)
            for t4 in range(0, NTILE, 4):
                nt = min(4, NTILE - t4)
                pb = (t4 // 4) % 2
                for j in range(nt):
                    P.pe(lambda e, t4=t4, j=j, pb=pb: e.transpose(PS[pb][0:NE, j * 128:(j + 1) * 128],
                                                                   AFF[:, t4 + j, :], ident_f[:]),
                         reads=[("AFF", t4 + j), "ident_f"], writes=[PK[pb]])
                P.dve(lambda e, t4=t4, nt=nt, pb=pb: e.tensor_copy(affT[:, t4 * 128:(t4 + nt) * 128],
                                                                    PS[pb][0:NE, 0:nt * 128]),
                      reads=[PK[pb]], writes=["affT"])
            P.dve(lambda e: e.tensor_copy(work[:], affT[:]), reads=["affT"], writes=["mwork"])
            for (s0, L, cap, c0) in ((TC, T, CAP_L, 0), (0, TC, CAP_C, CAP_L)):
                for rd in range(cap // 8):
                    sl = slice(c0 + rd * 8, c0 + rd * 8 + 8)
                    P.dve(lambda e, s0=s0, L=L, sl=sl: e.max(out=GV[:, sl], in_=work[:, s0:s0 + L]),
                          reads=["mwork"], writes=["GV"])
                    P.dve(lambda e, s0=s0, L=L, sl=sl: e.max_index(out=IXu[:, sl], in_max=GV[:, sl],
                                                                   in_values=work[:, s0:s0 + L]),
                          reads=["mwork", "GV"], writes=["IXu"])
                    P.dve(lambda e, s0=s0, L=L, sl=sl: e.match_replace(out=work[:, s0:s0 + L], in_to_replace=GV[:, sl],
                                                                       in_values=work[:, s0:s0 + L], imm_value=-1.0),
                          reads=["mwork", "GV"], writes=["mwork"])
            P.dve(lambda e: e.tensor_copy(IXf[:], IXu[:]), reads=["IXu"], writes=["IXf"])
            P.dve(lambda e: e.tensor_scalar(out=IXf[:, 0:CAP_L], in0=IXf[:, 0:CAP_L], scalar1=float(TC), scalar2=None,
                                            op0=ALU.add), reads=["IXf"], writes=["IXf"])
            for (src, dst, dk, pb) in ((IXf, iP, "iP", 2), (GV, gP, "gP", 3)):
                sk = "IXf" if src is IXf else "GV"
                for s in range(3):
                    n = 128 if s < 2 else CAP_C
                    P.pe(lambda e, src=src, s=s, n=n, pb=pb: e.transpose(PS[pb][0:n, s * NE:(s + 1) * NE],
                                                                          src[:, s * 128:s * 128 + n], ident_f[0:NE, 0:NE]),
                         reads=[sk, "ident_f"], writes=[PK[pb]])
                P.dve(lambda e, dst=dst, pb=pb: e.tensor_copy(dst[:, 0:2, :],
                                                               PS[pb][:, 0:2 * NE].rearrange("p (s e) -> p s e", s=2)),
                      reads=[PK[pb]], writes=[dk])
                P.dve(lambda e, dst=dst, pb=pb: e.tensor_copy(dst[0:CAP_C, 2, :], PS[pb][0:CAP_C, 2 * NE:3 * NE]),
                      reads=[PK[pb]], writes=[dk])
            if dbg and last:
                P.dma(lambda e: e.dma_start(out=dbg_ix_d, in_=IXf[:]), reads=["IXf"], writes=["dbg_ix"])
                P.dma(lambda e: e.dma_start(out=dbg_gv_d, in_=GV[:]), reads=["GV"], writes=["dbg_gv"])
            for tt in range(NTILE):
                P.dma(lambda e, tt=tt: e.dma_start(out=M_d[tt * 128:(tt + 1) * 128, :], in_=zero_f[:]),
                      reads=["zero_f"], writes=["Macc"])
            WB = [sb(es, "WB%d" % i, [128, 8, D], BF16) for i in range(6)]
            xe = [sb(es, "xe%d" % i, [128, 3, D], BF16) for i in range(2)]
            xeT = [sb(es, "xeT%d" % i, [128, 8, NSLOT], BF16) for i in range(2)]
            hT = [sb(es, "hT%d" % i, [128, 8, NSLOT], BF16) for i in range(2)]
            sg = [sb(es, "sg%d" % i, [128, NSLOT]) for i in range(2)]
            yb = [sb(es, "yb%d" % i, [128, 3, D]) for i in range(2)]
            wcnt = [0]

            def load_w(src_ap):
                i = wcnt[0] % 6
                wcnt[0] += 1
                wv = src_ap.rearrange("(k p) n -> p k n", p=128)
                for k in range(8):
                    P.dma(lambda e, i=i, k=k: e.dma_start(out=WB[i][:, k, :], in_=wv[:, k, :]),
                          writes=[("WB", i, k)], q="pool")
                return i

            for ex_i in range(NE):
                b = ex_i % 2
                kxe, kxT, khT, ksg, kyb = "xe%d" % b, "xeT%d" % b, "hT%d" % b, "sg%d" % b, "yb%d" % b
                if ex_i == 0:
                    wnext = (load_w(w_gate_d[l, 0]), load_w(w_up_d[l, 0]), load_w(w_down_d[l, 0]))
                wg, wu, wd = wnext
                for s in range(3):
                    n = 128 if s < 2 else CAP_C
                    P.dma(lambda e, b=b, s=s, n=n, ex_i=ex_i: e.indirect_dma_start(
                        out=xe[b][0:n, s, :], out_offset=None, in_=u2tok_d,
                        in_offset=bass.IndirectOffsetOnAxis(ap=iP[0:n, s, ex_i:ex_i + 1], axis=0)),
                        reads=["iP"] + [("u2tok", t) for t in range(NTILE)], writes=[(kxe, s)], q="pool")
                if ex_i + 1 < NE:
                    wnext = (load_w(w_gate_d[l, ex_i + 1]), load_w(w_up_d[l, ex_i + 1]), load_w(w_down_d[l, ex_i + 1]))
                if dbg and last and ex_i == 0:
                    P.dma(lambda e: e.dma_start(out=dbg_xe_d, in_=xe[0][:]), reads=[("xe0", 0), ("xe0", 1), ("xe0", 2)], writes=["dbg_xe"])
                    P.dma(lambda e: e.dma_start(out=dbg_ip_d, in_=iP[:]), reads=["iP"], writes=["dbg_ip"])
                for s in range(3):
                    n = 128 if s < 2 else CAP_C
                    pb = 4 + (s % 2)
                    pv = PS[pb][:].bitcast(BF16)
                    for k in range(8):
                        P.pe(lambda e, b=b, s=s, n=n, k=k, pv=pv: e.transpose(
                            pv[:, k * 128:k * 128 + n], xe[b][0:n, s, k * 128:(k + 1) * 128], ident_b[0:n, 0:n]),
                            reads=[(kxe, s), "ident_b"], writes=[PK[pb]])
                    P.act(lambda e, b=b, s=s, n=n, pv=pv: e.activation(
                        out=xeT[b][:, :, s * 128:s * 128 + n],
                        in_=pv.rearrange("p (k t) -> p k t", k=8)[:, :, 0:n], func=AF.Copy),
                        reads=[PK[pb]], writes=[kxT])
                for f in range(8):
                    pa, pu = 0 + (f % 2) * 2, 1 + (f % 2) * 2
                    for k in range(8):
                        P.pe(lambda e, b=b, f=f, k=k, pa=pa, wg=wg: e.matmul(
                            PS[pa][:, 0:NSLOT], WB[wg][:, k, f * 128:(f + 1) * 128], xeT[b][:, k, :],
                            start=(k == 0), stop=(k == 7)), reads=[("WB", wg, k), kxT], writes=[PK[pa]])
                    for k in range(8):
                        P.pe(lambda e, b=b, f=f, k=k, pu=pu, wu=wu: e.matmul(
                            PS[pu][:, 0:NSLOT], WB[wu][:, k, f * 128:(f + 1) * 128], xeT[b][:, k, :],
                            start=(k == 0), stop=(k == 7)), reads=[("WB", wu, k), kxT], writes=[PK[pu]])
                    P.act(lambda e, b=b, pa=pa: e.activation(out=sg[b][:], in_=PS[pa][:, 0:NSLOT], func=AF.Silu),
                          reads=[PK[pa]], writes=[ksg])
                    P.dve(lambda e, b=b, f=f, pu=pu: e.tensor_tensor(out=hT[b][:, f, :], in0=sg[b][:],
                                                                      in1=PS[pu][:, 0:NSLOT], op=ALU.mult),
                          reads=[ksg, PK[pu]], writes=[khT])
                for s in range(3):
                    n = 128 if s < 2 else CAP_C
                    for hf in range(2):
                        pb = 6 + hf
                        for k in range(8):
                            P.pe(lambda e, b=b, s=s, n=n, hf=hf, k=k, pb=pb, wd=wd: e.matmul(
                                PS[pb][0:n, :], hT[b][:, k, s * 128:s * 128 + n], WB[wd][:, k, hf * 512:(hf + 1) * 512],
                                start=(k == 0), stop=(k == 7)), reads=[khT, ("WB", wd, k)], writes=[PK[pb]])
                        P.dve(lambda e, b=b, s=s, n=n, hf=hf, pb=pb, ex_i=ex_i: e.tensor_scalar(
                            out=yb[b][0:n, s, hf * 512:(hf + 1) * 512], in0=PS[pb][0:n, :],
                            scalar1=gP[0:n, s, ex_i:ex_i + 1], scalar2=None, op0=ALU.mult),
                            reads=[PK[pb], "gP"], writes=[(kyb, s)])
                    if dbg and last and ex_i == 0 and s == 2:
                        P.dma(lambda e: e.dma_start(out=dbg_yb_d, in_=yb[0][:]), reads=[("yb0", 0), ("yb0", 1), ("yb0", 2)], writes=["dbg_yb"])
                        P.dma(lambda e: e.dma_start(out=dbg_ht_d, in_=hT[0][:]), reads=["hT0"], writes=["dbg_ht"])
                    P.dma(lambda e, b=b, s=s, n=n, ex_i=ex_i: e.indirect_dma_start(
                        out=M_d, out_offset=bass.IndirectOffsetOnAxis(ap=iP[0:n, s, ex_i:ex_i + 1], axis=0),
                        in_=yb[b][0:n, s, :], in_offset=None, compute_op=ALU.add),
                        reads=[(kyb, s), "iP"], writes=["Macc"], q="pool")
            G = [sb(es, "mG%d" % r, [128, D]) for r in range(2)]
            for r in range(2):
                load_mod(G[r], "mG%d" % r, r, 5)
            xt = [sb(es, "mx%d" % i, [128, D]) for i in range(2)]
            mt = [sb(es, "mm%d" % i, [128, D]) for i in range(2)]
            outs = []
            for tt in range(NTILE):
                b = tt % 2
                r = 1 if tt < 2 else 0
                kx, km = "mx%d" % b, "mm%d" % b
                P.dma(lambda e, b=b, tt=tt: e.dma_start(out=xt[b][:], in_=H_d[tt * 128:(tt + 1) * 128, :]),
                      reads=[hkey(tt)], writes=[kx])
                P.dma(lambda e, b=b, tt=tt: e.dma_start(out=mt[b][:], in_=M_d[tt * 128:(tt + 1) * 128, :]),
                      reads=["Macc"], writes=[km])
                P.dve(lambda e, b=b, r=r: e.tensor_tensor(out=mt[b][:], in0=mt[b][:], in1=G[r][:], op=ALU.mult),
                      reads=[km, "mG%d" % r], writes=[km])
                P.pool(lambda e, b=b: e.tensor_tensor(out=xt[b][:], in0=xt[b][:], in1=mt[b][:], op=ALU.add),
                       reads=[kx, km], writes=[kx])
                if dbg and last:
                    P.dma(lambda e, b=b, tt=tt: e.dma_start(out=dbg_m_d[tt * 128:(tt + 1) * 128, :], in_=mt[b][:]),
                          reads=[km], writes=[("dbg_m", tt)])
                if last:
                    if tt >= 2:
                        outs.append(P.dma(lambda e, b=b, tt=tt: e.dma_start(
                            out=out_d[(tt - 2) * 128:(tt - 1) * 128, :], in_=xt[b][:]), reads=[kx], writes=[("out", tt)]))
                    if dbg:
                        outs.append(P.dma(lambda e, b=b, tt=tt: e.dma_start(
                            out=hdbg_d[tt * 128:(tt + 1) * 128, :], in_=xt[b][:]), reads=[kx], writes=[("hdbg", tt)]))
                else:
                    P.dma(lambda e, b=b, tt=tt: e.dma_start(out=H_d[tt * 128:(tt + 1) * 128, :], in_=xt[b][:]),
                          reads=[kx], writes=[hkey(tt)])
        P.barrier()
        return outs

    finals = []
    for l in range(n_layers):
        last = (l == n_layers - 1)
        phase_mod(l)
        with ExitStack() as es:
            U = sb(es, "U", [128, 8, TT], BF16)
            Y = sb(es, "Y", [128, 8, TT], BF16)
            phase_norm(l, l, 1, U)
            if l % 2 == 0:
                phase_conv(l, U, Y)
                phase_outproj(l, Y, w_out_ab_d[l // 2])
            else:
                phase_attn(l, U, Y)
                phase_hyena(l, U, Y)
                phase_outproj(l, Y, w_out_cd_d[l // 2])
        with ExitStack() as es:
            AFF = sb(es, "AFF", [128, NTILE, NE])
            phase_norm(l, 1, 2, None, AFF=AFF)
            finals = phase_moe(l, AFF, last)
    P.finalize(finals)
    top.close()
    return nc


def _host_inputs(inp):
    f = lambda a: np.ascontiguousarray(np.asarray(a, dtype=np.float32))
    sh = {}
    sh["w_ada"] = f(inp["w_ada"])
    sh["b_ada"] = f(inp["b_ada"])
    sh["g_mix"] = f(inp["g_mix"])
    sh["g_ffn"] = f(inp["g_ffn"])
    sh["w_in_ab"] = f(inp["w_in_ab"])
    sh["ca"] = f(np.asarray(inp["conv_a"]).reshape(2, 3, 4, 128).transpose(0, 3, 2, 1))
    sh["cb"] = f(np.asarray(inp["conv_b"]).reshape(2, 31, 4, 128).transpose(0, 3, 2, 1))
    lnp = np.stack([np.asarray(inp["conv_b_bias"]), np.asarray(inp["ln_b_g"]), np.asarray(inp["ln_b_b"])], axis=1)
    sh["lnp"] = f(lnp.reshape(2, 3, 4, 128).transpose(0, 3, 1, 2))
    sh["w_out_ab"] = f(inp["w_out_ab"])
    sh["w_in_cd"] = f(inp["w_in_cd"])
    sh["w_out_cd"] = f(inp["w_out_cd"])
    gq = np.asarray(inp["g_q"]); gk = np.asarray(inp["g_k"])
    sh["gqk"] = f(np.stack([np.tile(gq, (1, 2)), np.tile(gk, (1, 2))], axis=-1))
    sh["lamv"] = f(np.concatenate([np.asarray(inp[k]) for k in ("lam_q1", "lam_k1", "lam_q2", "lam_k2")], axis=1))
    sh["gsub"] = f(np.asarray(inp["g_subln"]).reshape(2, 128, 1))
    sh["cd"] = f(np.asarray(inp["conv_d"]).reshape(2, 3, 12, 128).transpose(0, 3, 2, 1))
    sh["hf_w1"] = f(inp["hf_w1"])
    sh["hfv"] = f(np.stack([np.asarray(inp["hf_b1"]), np.asarray(inp["hf_freq"]), np.asarray(inp["hf_b2"])], axis=-1))
    sh["hf_w2"] = f(inp["hf_w2"])
    sh["hf_w3"] = f(inp["hf_w3"])
    sh["hf_bias"] = f(inp["hf_bias"])
    sh["wr"] = f(np.asarray(inp["w_router"]).reshape(DEPTH, 8, 128, NE).transpose(0, 2, 1, 3))
    sh["w_gate"] = f(inp["w_gate"])
    sh["w_up"] = f(inp["w_up"])
    sh["w_down"] = f(inp["w_down"])
    sh.update(host_constants())
    return sh


def _core_inputs(inp, shared, b):
    m = dict(shared)
    m["x"] = np.ascontiguousarray(np.asarray(inp["x"][b], dtype=np.float32))
    m["ctx"] = np.ascontiguousarray(np.asarray(inp["ctx"][b], dtype=np.float32))
    cv = np.stack([np.asarray(inp["c"][b]), np.asarray(inp["c_ctx"])], axis=0)
    m["cT"] = np.ascontiguousarray(cv.reshape(2, 8, 128).transpose(2, 1, 0).astype(np.float32))
    return m


_NC_CACHE = {}


def kernel(**inputs):
    n = 8
    if "nc" not in _NC_CACHE:
        _NC_CACHE["nc"] = build(DEPTH)
    nc = _NC_CACHE["nc"]
    shared = _host_inputs(inputs)
    in_maps = [_core_inputs(inputs, shared, b) for b in range(n)]
    res = run_bass_kernel_spmd(nc, in_maps, core_ids=list(range(n)))
    return np.stack([np.asarray(r["out"]) for r in res.results], axis=0).astype(np.float32)
```

```python
import math
from contextlib import ExitStack

import numpy as np
import concourse.bass as bass
import concourse.mybir as mybir
from concourse.bass_utils import run_bass_kernel_spmd

F32 = mybir.dt.float32
BF16 = mybir.dt.bfloat16
I32 = mybir.dt.int32
U32 = mybir.dt.uint32
AF = mybir.ActivationFunctionType
ALU = mybir.AluOpType
AX = mybir.AxisListType

D = 1024
T = 2048
TC = 256
TT = T + TC
NTILE = TT // 128
DEPTH = 4
NE = 16
CAP_L = 256
CAP_C = 32
NSLOT = CAP_L + CAP_C
EPS = 1e-6

ENGS = ("pe", "dve", "act", "pool", "sp")
DMA_SLOTS = {"sp": 24, "act": 8, "pool": 24}


class Op:
    __slots__ = ("eng", "fn", "deps", "dma", "needs_inc", "sem", "val", "slot", "prev_slot_op")

    def __init__(self, eng, fn, dma):
        self.eng = eng
        self.fn = fn
        self.dma = dma
        self.deps = []
        self.needs_inc = False
        self.sem = None
        self.val = None
        self.slot = None
        self.prev_slot_op = None


class Prog:
    def __init__(self, nc):
        self.nc = nc
        self.ops = {e: [] for e in ENGS}
        self.last_w = {}
        self.readers = {}
        self.dma_rr = {q: 0 for q in DMA_SLOTS}
        self.slot_last = {}

    def _add(self, eng, fn, reads, writes, dma):
        op = Op(eng, fn, dma)
        deps = []
        for k in reads:
            w = self.last_w.get(k)
            if w is not None:
                deps.append(w)
        for k in writes:
            w = self.last_w.get(k)
            if w is not None:
                deps.append(w)
            deps.extend(self.readers.get(k, ()))
        seen = set()
        for d in deps:
            if id(d) in seen:
                continue
            seen.add(id(d))
            if (not dma) and (not d.dma) and d.eng == eng and eng == "pe":
                continue
            op.deps.append(d)
            d.needs_inc = True
        if dma:
            k = self.dma_rr[eng]
            self.dma_rr[eng] = (k + 1) % DMA_SLOTS[eng]
            op.slot = (eng, k)
            op.prev_slot_op = self.slot_last.get(op.slot)
            self.slot_last[op.slot] = op
        for k in reads:
            self.readers.setdefault(k, []).append(op)
        for k in writes:
            self.last_w[k] = op
            self.readers[k] = []
        self.ops[eng].append(op)
        return op

    def pe(self, fn, reads=(), writes=()):
        return self._add("pe", fn, reads, writes, False)

    def dve(self, fn, reads=(), writes=()):
        return self._add("dve", fn, reads, writes, False)

    def act(self, fn, reads=(), writes=()):
        return self._add("act", fn, reads, writes, False)

    def pool(self, fn, reads=(), writes=()):
        return self._add("pool", fn, reads, writes, False)

    def dma(self, fn, reads=(), writes=(), q="sp"):
        return self._add(q, fn, reads, writes, True)

    def barrier(self):
        lasts = []
        for e in ENGS:
            for op in reversed(self.ops[e]):
                if not op.dma:
                    lasts.append(op)
                    break
        lasts.extend(self.slot_last.values())
        for e in ENGS:
            op = Op(e, lambda eng: eng.nop(), False)
            for d in lasts:
                if d.eng == e and not d.dma and e == "pe":
                    continue
                op.deps.append(d)
                d.needs_inc = True
            self.ops[e].append(op)
        self.last_w = {}
        self.readers = {}

    def finalize(self, final_ops=()):
        nc = self.nc
        with ExitStack() as es:
            esem = {e: es.enter_context(nc.semaphore("s_" + e)) for e in ("pe", "dve", "act", "pool", "sp")}
            ssem = {}
            for q, n in DMA_SLOTS.items():
                for k in range(n):
                    ssem[(q, k)] = es.enter_context(nc.semaphore("d_%s%d" % (q, k)))
            for e in ENGS:
                cnt = 0
                for op in self.ops[e]:
                    if (not op.dma) and op.needs_inc:
                        cnt += 1
                        op.sem = esem[e]
                        op.val = cnt
            slot_cnt = {}
            for e in ENGS:
                for op in self.ops[e]:
                    if op.dma:
                        c = slot_cnt.get(op.slot, 0) + 16
                        slot_cnt[op.slot] = c
                        op.sem = ssem[op.slot]
                        op.val = c
            block = es.enter_context(nc.Block())
            finals = list(final_ops)

            def emit(e, engobj):
                waited = {}

                def w(s, v):
                    if waited.get(id(s), 0) >= v:
                        return
                    waited[id(s)] = v
                    engobj.wait_ge(s, v)

                for op in self.ops[e]:
                    for d in op.deps:
                        w(d.sem, d.val)
                    if op.dma and op.prev_slot_op is not None:
                        w(op.prev_slot_op.sem, op.prev_slot_op.val)
                    ins = op.fn(engobj)
                    if op.dma:
                        ins.then_inc(op.sem, 16)
                    elif op.needs_inc:
                        ins.then_inc(op.sem, 1)
                if e == "sp":
                    for f in finals:
                        w(f.sem, f.val)

            @block.tensor
            def _(eng):
                emit("pe", eng)

            @block.vector
            def _(eng):
                emit("dve", eng)

            @block.scalar
            def _(eng):
                emit("act", eng)

            @block.gpsimd
            def _(eng):
                emit("pool", eng)

            @block.sync
            def _(eng):
                emit("sp", eng)


def _tok_tiles():
    return [(0, TC)] + [(TC + i * 512, 512) for i in range(T // 512)]


SEQS = ((0, TC), (TC, T))


def _bf(a):
    import ml_dtypes
    return np.ascontiguousarray(np.asarray(a, dtype=np.float32).astype(ml_dtypes.bfloat16))


def _dft_tables(L):
    N = 3 * L // 2
    nf = N // 2
    npair = (nf + 127) // 128
    f = np.arange(npair * 128, dtype=np.float64)
    t = np.arange(L, dtype=np.float64)
    th = 2.0 * np.pi * np.outer(t, f + 0.5) / N
    valid = (f < nf)[None, :]
    fc = np.where(valid, np.cos(th), 0.0)
    fs = np.where(valid, -np.sin(th), 0.0)
    nt = L // 128
    fw = np.zeros((npair, 128, nt, 256))
    for i in range(npair):
        blkc = fc[:, i * 128:(i + 1) * 128].reshape(nt, 128, 128).transpose(1, 0, 2)
        blks = fs[:, i * 128:(i + 1) * 128].reshape(nt, 128, 128).transpose(1, 0, 2)
        fw[i, :, :, 0:128] = blkc
        fw[i, :, :, 128:256] = blks
    thi = 2.0 * np.pi * np.outer(f + 0.5, t + L // 2) / N
    validf = (f < nf)[:, None]
    ic = np.where(validf, (2.0 / N) * np.cos(thi), 0.0)
    isn = np.where(validf, -(2.0 / N) * np.sin(thi), 0.0)
    inv = np.stack([ic.reshape(npair, 128, L).transpose(1, 0, 2), isn.reshape(npair, 128, L).transpose(1, 0, 2)], 0)
    return fw, inv, npair


def _filter_tables(L):
    t = np.arange(L, dtype=np.float64)
    t_unit = np.linspace(0.0, 1.0, L)
    bands = np.linspace(1e-4, 16 - 1, 16)
    ang = (2.0 * np.pi / L) * t[:, None] * bands[None]
    z = np.concatenate([t_unit[:, None], np.cos(ang), -np.sin(ang)], axis=-1)
    centre = L // 2
    dist = np.abs(t - centre) / max(centre, 1)
    mn = math.log(1e-2) / 1.5
    mx = math.log(1e-2) / 0.3
    decay = np.abs(np.linspace(mn, mx, 512))
    win = np.exp(-dist[:, None] * decay[None])
    return z.T, win


def host_constants():
    c = {}
    c["ident"] = np.eye(128, dtype=np.float32)
    tt = np.arange(T)
    row = (tt // 64).astype(np.float64)
    col = (tt % 64).astype(np.float64)
    inv = 10000.0 ** (-np.arange(16, dtype=np.float64) / 16)
    cos = np.zeros((128, T))
    sin = np.zeros((128, T))
    for p in range(128):
        d = p % 64
        pos = row if d < 32 else col
        a = pos * inv[d % 16]
        cos[p] = np.cos(a)
        sin[p] = np.sin(a)
    c["ropec"] = _bf(cos)
    c["ropes"] = _bf(sin)
    rt = np.zeros((128, 128))
    for m in range(128):
        if m % 32 < 16:
            rt[m + 16, m] = -1.0
        else:
            rt[m - 16, m] = 1.0
    c["ropert"] = _bf(rt)
    bo = np.zeros((128, 128), dtype=np.float32)
    bo[0:64, 0:64] = 1.0
    bo[64:128, 64:128] = 1.0
    c["blockones"] = bo
    zl, wl = _filter_tables(T)
    zc, wc = _filter_tables(TC)
    c["zT"] = np.ascontiguousarray(np.concatenate([zc, zl], axis=1).astype(np.float32))
    c["win"] = np.ascontiguousarray(np.concatenate([wc, wl], axis=0).astype(np.float32))
    fwl, invl, _ = _dft_tables(T)
    fwc, invc, _ = _dft_tables(TC)
    c["fwl"] = _bf(fwl)
    c["fwc"] = _bf(fwc)
    il = invl.reshape(2, 128, 12, 4, 512).transpose(3, 0, 1, 2, 4)
    c["invl"] = _bf(il)
    c["invc"] = _bf(invc)
    return c


def build(n_layers=DEPTH, dbg=False):
    nc = bass.Bass("TRN2", target_bir_lowering=False)

    def din(name, shape, dt=F32):
        return nc.dram_tensor(name, list(shape), dt, kind="ExternalInput").ap()

    def dint(name, shape, dt=F32):
        return nc.dram_tensor(name, list(shape), dt, kind="Internal").ap()

    x_d = din("x", [T, D])
    ctx_d = din("ctx", [TC, D])
    cT_d = din("cT", [128, 8, 2])
    w_ada_d = din("w_ada", [DEPTH, D, 6 * D])
    b_ada_d = din("b_ada", [DEPTH, 6 * D])
    g_mix_d = din("g_mix", [DEPTH, D])
    g_ffn_d = din("g_ffn", [DEPTH, D])
    w_in_ab_d = din("w_in_ab", [2, D, 2560])
    ca_d = din("ca", [2, 128, 4, 3])
    cb_d = din("cb", [2, 128, 4, 31])
    lnp_d = din("lnp", [2, 128, 3, 4])
    w_out_ab_d = din("w_out_ab", [2, D, D])
    wr_d = din("wr", [DEPTH, 128, 8, NE])
    w_gate_d = din("w_gate", [DEPTH, NE, D, D])
    w_up_d = din("w_up", [DEPTH, NE, D, D])
    w_down_d = din("w_down", [DEPTH, NE, D, D])
    ident_d = din("ident", [128, 128])
    w_in_cd_d = din("w_in_cd", [2, D, 3072])
    w_out_cd_d = din("w_out_cd", [2, D, D])
    gqk_d = din("gqk", [2, 128, 2])
    lamv_d = din("lamv", [2, 4 * 64])
    gsub_d = din("gsub", [2, 128, 1])
    cd_d = din("cd", [2, 128, 12, 3])
    hfw1_d = din("hf_w1", [2, 33, 64])
    hfv_d = din("hfv", [2, 64, 3])
    hfw2_d = din("hf_w2", [2, 64, 64])
    hfw3_d = din("hf_w3", [2, 64, 512])
    hfb_d = din("hf_bias", [2, 512])
    ropec_d = din("ropec", [128, T], BF16)
    ropes_d = din("ropes", [128, T], BF16)
    ropert_d = din("ropert", [128, 128], BF16)
    blockones_d = din("blockones", [128, 128])
    zT_d = din("zT", [33, TT])
    win_d = din("win", [TT, 512])
    fwl_d = din("fwl", [12, 128, 16, 256], BF16)
    fwc_d = din("fwc", [2, 128, 2, 256], BF16)
    invl_d = din("invl", [4, 2, 128, 12, 512], BF16)
    invc_d = din("invc", [2, 128, 2, 256], BF16)
    out_d = nc.dram_tensor("out", [T, D], F32, kind="ExternalOutput").ap()

    H_d = dint("H", [TT, D])
    modv_d = dint("modv", [2, 6 * D])
    u2tok_d = dint("u2tok", [TT, D], BF16)
    M_d = dint("Macc", [TT, D])
    if dbg:
        hdbg_d = nc.dram_tensor("hdbg", [TT, D], F32, kind="ExternalOutput").ap()
        dbg_ix_d = nc.dram_tensor("dbg_ix", [NE, NSLOT], F32, kind="ExternalOutput").ap()
        dbg_gv_d = nc.dram_tensor("dbg_gv", [NE, NSLOT], F32, kind="ExternalOutput").ap()
        dbg_m_d = nc.dram_tensor("dbg_m", [TT, D], F32, kind="ExternalOutput").ap()
        dbg_xe_d = nc.dram_tensor("dbg_xe", [128, 3, D], BF16, kind="ExternalOutput").ap()
        dbg_yb_d = nc.dram_tensor("dbg_yb", [128, 3, D], F32, kind="ExternalOutput").ap()
        dbg_ht_d = nc.dram_tensor("dbg_ht", [128, 8, NSLOT], BF16, kind="ExternalOutput").ap()
        dbg_ip_d = nc.dram_tensor("dbg_ip", [128, 3, NE], I32, kind="ExternalOutput").ap()

    top = ExitStack()
    P = Prog(nc)

    uid = [0]

    def sb(es, name, shape, dt=F32):
        uid[0] += 1
        return es.enter_context(nc.sbuf_tensor("%s_%d" % (name, uid[0]), list(shape), dt))

    PS = [top.enter_context(nc.psum_tensor("ps%d" % i, [128, 512], F32)) for i in range(8)]
    PK = ["ps%d" % i for i in range(8)]

    ident_f = sb(top, "ident_f", [128, 128])
    ident_b = sb(top, "ident_b", [128, 128], BF16)
    ones_f = sb(top, "ones_f", [128, 128])
    ones_b = sb(top, "ones_b", [128, 128], BF16)
    zero_f = sb(top, "zero_f", [128, 1024])
    P.dma(lambda e: e.dma_start(out=ident_f[:], in_=ident_d), writes=["ident_f"])
    P.dve(lambda e: e.tensor_copy(ident_b[:], ident_f[:]), reads=["ident_f"], writes=["ident_b"])
    P.dve(lambda e: e.memset(ones_f[:], 1.0), writes=["ones_f"])
    P.dve(lambda e: e.memset(ones_b[:], 1.0), writes=["ones_b"])
    P.dve(lambda e: e.memset(zero_f[:], 0.0), writes=["zero_f"])

    def hsrc(l, tt):
        if l == 0:
            if tt < 2:
                return ctx_d[tt * 128:(tt + 1) * 128, :]
            return x_d[(tt - 2) * 128:(tt - 1) * 128, :]
        return H_d[tt * 128:(tt + 1) * 128, :]

    def hkey(tt):
        return ("H", tt)

    def phase_mod(l):
        with ExitStack() as es:
            cS = sb(es, "cS", [128, 8, 2])
            modr = sb(es, "modr", [2, 6 * D])
            brow = sb(es, "brow", [2, 6 * D])
            grow = sb(es, "grow", [2, 2, D])
            wbuf = [sb(es, "wada%d" % i, [128, 8, 512]) for i in range(2)]
            P.dma(lambda e: e.dma_start(out=cS[:], in_=cT_d), writes=["cS"])
            P.act(lambda e: e.activation(out=cS[:], in_=cS[:], func=AF.Silu), reads=["cS"], writes=["cS"])
            for r in range(2):
                P.dma(lambda e, r=r: e.dma_start(out=brow[r:r + 1, :], in_=b_ada_d[l:l + 1, :]), writes=["brow"])
                P.dma(lambda e, r=r: e.dma_start(out=grow[r:r + 1, 0, :], in_=g_mix_d[l:l + 1, :]), writes=["grow"])
                P.dma(lambda e, r=r: e.dma_start(out=grow[r:r + 1, 1, :], in_=g_ffn_d[l:l + 1, :]), writes=["grow"])
            wv = w_ada_d[l].rearrange("(k p) n -> p k n", p=128)
            for nt in range(12):
                wb = wbuf[nt % 2]
                wk = "wada%d" % (nt % 2)
                P.dma(lambda e, wb=wb, nt=nt: e.dma_start(out=wb[:], in_=wv[:, :, nt * 512:(nt + 1) * 512]),
                      writes=[wk])
                pk = nt % 2
                for k in range(8):
                    P.pe(lambda e, wb=wb, k=k, pk=pk: e.matmul(PS[pk][0:2, :], cS[:, k, :], wb[:, k, :],
                                                                  start=(k == 0), stop=(k == 7)),
                         reads=[wk, "cS"], writes=[PK[pk]])
                P.dve(lambda e, nt=nt, pk=pk: e.tensor_tensor(out=modr[:, nt * 512:(nt + 1) * 512], in0=PS[pk][0:2, :],
                                                               in1=brow[:, nt * 512:(nt + 1) * 512], op=ALU.add),
                      reads=[PK[pk], "brow"], writes=["modr"])
            for (seg, gi) in ((1, 0), (4, 1)):
                P.dve(lambda e, seg=seg, gi=gi: e.scalar_tensor_tensor(
                    out=modr[:, seg * D:(seg + 1) * D], in0=modr[:, seg * D:(seg + 1) * D], scalar=1.0,
                    in1=grow[:, gi, :], op0=ALU.add, op1=ALU.mult), reads=["modr", "grow"], writes=["modr"])
            P.dma(lambda e: e.dma_start(out=modv_d, in_=modr[:]), reads=["modr"], writes=["modv"])
        P.barrier()

    def load_mod(tile, key, r, seg):
        P.dma(lambda e: e.dma_start(out=tile[:], in_=modv_d[r:r + 1, seg * D:(seg + 1) * D].partition_broadcast(128)),
              reads=["modv"], writes=[key])

    def phase_norm(l, hl, which, U, AFF=None):
        seg_s, seg_a = (0, 1) if which == 1 else (3, 4)
        with ExitStack() as es:
            A = [sb(es, "nA%d" % r, [128, D]) for r in range(2)]
            S = [sb(es, "nS%d" % r, [128, D]) for r in range(2)]
            for r in range(2):
                load_mod(A[r], "nA%d" % r, r, seg_a)
                load_mod(S[r], "nS%d" % r, r, seg_s)
            xt = [sb(es, "nx%d" % i, [128, D]) for i in range(2)]
            junk = sb(es, "njunk", [128, D], BF16)
            ss = [sb(es, "nss%d" % i, [128, 2]) for i in range(2)]
            tmp = [sb(es, "ntmp%d" % i, [128, D]) for i in range(2)]
            if which == 1:
                ub = [sb(es, "nub%d" % i, [128, D], BF16) for i in range(2)]
            else:
                ub = [sb(es, "nub%d" % i, [128, D], BF16) for i in range(2)]
                uT = [sb(es, "nuT%d" % i, [128, 8, 128]) for i in range(2)]
                wr = sb(es, "nwr", [128, 8, NE])
                sm = [sb(es, "nsm%d" % i, [128, 4]) for i in range(2)]
                ex = [sb(es, "nex%d" % i, [128, NE]) for i in range(2)]
                P.dma(lambda e: e.dma_start(out=wr[:], in_=wr_d[l]), writes=["nwr"])
            for tt in range(NTILE):
                b = tt % 2
                r = 1 if tt < 2 else 0
                kx, kss, ktmp, kub = "nx%d" % b, "nss%d" % b, "ntmp%d" % b, "nub%d" % b
                P.dma(lambda e, b=b, tt=tt: e.dma_start(out=xt[b][:], in_=hsrc(hl, tt)), reads=[hkey(tt)], writes=[kx])
                P.act(lambda e, b=b: e.activation(out=junk[:], in_=xt[b][:], func=AF.Square, accum_out=ss[b][:, 0:1]),
                      reads=[kx], writes=["njunk", kss])
                P.act(lambda e, b=b: e.activation(out=ss[b][:, 1:2], in_=ss[b][:, 0:1], func=AF.Sqrt,
                                                  scale=1.0 / D, bias=EPS), reads=[kss], writes=[kss])
                P.dve(lambda e, b=b: e.reciprocal(ss[b][:, 1:2], ss[b][:, 1:2]), reads=[kss], writes=[kss])
                P.dve(lambda e, b=b, r=r: e.scalar_tensor_tensor(out=tmp[b][:], in0=xt[b][:], scalar=ss[b][:, 1:2],
                                                                   in1=A[r][:], op0=ALU.mult, op1=ALU.mult),
                      reads=[kx, kss, "nA%d" % r], writes=[ktmp])
                if which == 1:
                    P.pool(lambda e, b=b, r=r: e.tensor_tensor(out=ub[b][:], in0=tmp[b][:], in1=S[r][:], op=ALU.add),
                           reads=[ktmp, "nS%d" % r], writes=[kub])
                    psb = 2 + b
                    pv = PS[psb][:].bitcast(BF16)
                    for k in range(8):
                        P.pe(lambda e, b=b, k=k, pv=pv: e.transpose(pv[:, k * 128:(k + 1) * 128],
                                                                     ub[b][:, k * 128:(k + 1) * 128], ident_b[:]),
                             reads=[kub, "ident_b"], writes=[PK[psb]])
                    P.act(lambda e, tt=tt, pv=pv: e.activation(
                        out=U[:, :, tt * 128:(tt + 1) * 128], in_=pv.rearrange("p (k t) -> p k t", k=8), func=AF.Copy),
                        reads=[PK[psb]], writes=[("U", tt)])
                else:
                    P.pool(lambda e, b=b, r=r: e.tensor_tensor(out=tmp[b][:], in0=tmp[b][:], in1=S[r][:], op=ALU.add),
                           reads=[ktmp, "nS%d" % r], writes=[ktmp])
                    P.act(lambda e, b=b: e.activation(out=ub[b][:], in_=tmp[b][:], func=AF.Copy),
                          reads=[ktmp], writes=[kub])
                    P.dma(lambda e, b=b, tt=tt: e.dma_start(out=u2tok_d[tt * 128:(tt + 1) * 128, :], in_=ub[b][:]),
                          reads=[kub], writes=[("u2tok", tt)])
                    kuT = "nuT%d" % b
                    for hh in range(2):
                        psb = 2 + 2 * b + hh
                        for k4 in range(4):
                            k = hh * 4 + k4
                            P.pe(lambda e, b=b, k=k, k4=k4, psb=psb: e.transpose(
                                PS[psb][:, k4 * 128:(k4 + 1) * 128], tmp[b][:, k * 128:(k + 1) * 128], ident_f[:]),
                                reads=[ktmp, "ident_f"], writes=[PK[psb]])
                        P.dve(lambda e, b=b, hh=hh, psb=psb: e.tensor_copy(
                            uT[b][:, hh * 4:(hh + 1) * 4, :], PS[psb][:].rearrange("p (k t) -> p k t", k=4)),
                            reads=[PK[psb]], writes=[kuT])
                    pl = 6 + b
                    for k in range(8):
                        P.pe(lambda e, b=b, k=k, pl=pl: e.matmul(PS[pl][:, 0:NE], uT[b][:, k, :], wr[:, k, :],
                                                                   start=(k == 0), stop=(k == 7)),
                             reads=[kuT, "nwr"], writes=[PK[pl]])
                    ksm, kex = "nsm%d" % b, "nex%d" % b
                    P.dve(lambda e, b=b, pl=pl: e.reduce_max(out=sm[b][:, 0:1], in_=PS[pl][:, 0:NE], axis=AX.X),
                          reads=[PK[pl]], writes=[ksm])
                    P.dve(lambda e, b=b: e.tensor_scalar(out=sm[b][:, 1:2], in0=sm[b][:, 0:1], scalar1=-1.0,
                                                         scalar2=None, op0=ALU.mult), reads=[ksm], writes=[ksm])
                    P.act(lambda e, b=b, pl=pl: e.activation(out=ex[b][:], in_=PS[pl][:, 0:NE], func=AF.Exp,
                                                             bias=sm[b][:, 1:2], scale=1.0, accum_out=sm[b][:, 2:3]),
                          reads=[PK[pl], ksm], writes=[kex, ksm])
                    P.dve(lambda e, b=b: e.reciprocal(sm[b][:, 3:4], sm[b][:, 2:3]), reads=[ksm], writes=[ksm])
                    P.dve(lambda e, b=b, tt=tt: e.tensor_scalar(out=AFF[:, tt, :], in0=ex[b][:], scalar1=sm[b][:, 3:4],
                                                                scalar2=None, op0=ALU.mult),
                          reads=[kex, ksm], writes=[("AFF", tt)])
        P.barrier()

    def phase_outproj(l, Y, w_out_ap, last_layer_unused=False):
        with ExitStack() as es:
            Wo = sb(es, "Wo", [128, 8, D], BF16)
            G = [sb(es, "oG%d" % r, [128, D]) for r in range(2)]
            xt = [sb(es, "ox%d" % i, [128, D]) for i in range(2)]
            tm = [sb(es, "ot%d" % i, [128, D]) for i in range(2)]
            wv = w_out_ap.rearrange("(k p) n -> p k n", p=128)
            for k in range(8):
                P.dma(lambda e, k=k: e.dma_start(out=Wo[:, k, :], in_=wv[:, k, :]), writes=[("Wo", k)], q="pool")
            for r in range(2):
                load_mod(G[r], "oG%d" % r, r, 2)
            for tt in range(NTILE):
                b = tt % 2
                r = 1 if tt < 2 else 0
                kx, kt = "ox%d" % b, "ot%d" % b
                P.dma(lambda e, b=b, tt=tt: e.dma_start(out=xt[b][:], in_=hsrc(l, tt)), reads=[hkey(tt)], writes=[kx])
                for hf in range(2):
                    pb = 2 * b + hf
                    for k in range(8):
                        P.pe(lambda e, k=k, tt=tt, hf=hf, pb=pb: e.matmul(
                            PS[pb][:], Y[:, k, tt * 128:(tt + 1) * 128], Wo[:, k, hf * 512:(hf + 1) * 512],
                            start=(k == 0), stop=(k == 7)), reads=[("Y", tt), ("Wo", k)], writes=[PK[pb]])
                    P.dve(lambda e, b=b, hf=hf, pb=pb, r=r: e.tensor_tensor(
                        out=tm[b][:, hf * 512:(hf + 1) * 512], in0=PS[pb][:], in1=G[r][:, hf * 512:(hf + 1) * 512],
                        op=ALU.mult), reads=[PK[pb], "oG%d" % r], writes=[kt])
                P.pool(lambda e, b=b: e.tensor_tensor(out=xt[b][:], in0=xt[b][:], in1=tm[b][:], op=ALU.add),
                       reads=[kx, kt], writes=[kx])
                P.dma(lambda e, b=b, tt=tt: e.dma_start(out=H_d[tt * 128:(tt + 1) * 128, :], in_=xt[b][:]),
                      reads=[kx], writes=[hkey(tt)])
        P.barrier()

    def phase_conv(l, U, Y):
        i2 = l // 2
        tiles = _tok_tiles()
        with ExitStack() as es:
            Wg = [sb(es, "Wg%d" % g, [128, 8, 512], BF16) for g in range(5)]
            wv = w_in_ab_d[i2].rearrange("(k p) n -> p k n", p=128)
            for g in range(5):
                for k in range(8):
                    P.dma(lambda e, g=g, k=k: e.dma_start(out=Wg[g][:, k, :], in_=wv[:, k, g * 512:(g + 1) * 512]),
                          writes=[("Wg", g)], q="pool")
            CA = sb(es, "CA", [128, 4, 3])
            CB = sb(es, "CB", [128, 4, 31])
            LNP = sb(es, "LNP", [128, 3, 4])
            P.dma(lambda e: e.dma_start(out=CA[:], in_=ca_d[i2]), writes=["CA"])
            P.dma(lambda e: e.dma_start(out=CB[:], in_=cb_d[i2]), writes=["CB"])
            P.dma(lambda e: e.dma_start(out=LNP[:], in_=lnp_d[i2]), writes=["LNP"])
            B = [sb(es, "cB%d" % i, [128, TT]) for i in range(3)]
            ZC = sb(es, "ZC", [128, 4, TT])
            Dg = [sb(es, "Dg%d" % i, [128, 31, 128], BF16) for i in range(2)]
            Zb = sb(es, "Zb", [128, TT], BF16)

            psrr = [0]

            def proj(chunk, dst, dkey):
                g, c = chunk // 4, chunk % 4
                for (t0, n) in tiles:
                    pb = psrr[0] % 4
                    psrr[0] += 1
                    for k in range(8):
                        P.pe(lambda e, g=g, c=c, k=k, t0=t0, n=n, pb=pb: e.matmul(
                            PS[pb][:, 0:n], Wg[g][:, k, c * 128:(c + 1) * 128], U[:, k, t0:t0 + n],
                            start=(k == 0), stop=(k == 7)),
                            reads=[("Wg", g)] + [("U", t) for t in range(t0 // 128, (t0 + n) // 128)],
                            writes=[PK[pb]])
                    P.act(lambda e, t0=t0, n=n, pb=pb: e.activation(out=dst[:, t0:t0 + n], in_=PS[pb][:, 0:n],
                                                                      func=AF.Copy), reads=[PK[pb]], writes=[dkey])

            for i in range(4):
                proj(i, B[0], "cB0")
                proj(4 + i, B[1], "cB1")
                proj(8 + i, B[2], "cB2")
                P.dve(lambda e: e.tensor_tensor(out=B[1][:], in0=B[1][:], in1=B[2][:], op=ALU.mult),
                      reads=["cB1", "cB2"], writes=["cB1"])
                P.dve(lambda e, i=i: e.tensor_scalar(out=B[2][:], in0=B[1][:], scalar1=CA[:, i, 1:2], scalar2=None,
                                                      op0=ALU.mult), reads=["cB1", "CA"], writes=["cB2"])
                for (s0, L) in SEQS:
                    P.dve(lambda e, i=i, s0=s0, L=L: e.scalar_tensor_tensor(
                        out=B[2][:, s0 + 1:s0 + L], in0=B[1][:, s0:s0 + L - 1], scalar=CA[:, i, 0:1],
                        in1=B[2][:, s0 + 1:s0 + L], op0=ALU.mult, op1=ALU.add), reads=["cB1", "cB2", "CA"],
                        writes=["cB2"])
                    P.dve(lambda e, i=i, s0=s0, L=L: e.scalar_tensor_tensor(
                        out=B[2][:, s0:s0 + L - 1], in0=B[1][:, s0 + 1:s0 + L], scalar=CA[:, i, 2:3],
                        in1=B[2][:, s0:s0 + L - 1], op0=ALU.mult, op1=ALU.add), reads=["cB1", "cB2", "CA"],
                        writes=["cB2"])
                P.dve(lambda e, i=i: e.tensor_tensor(out=Y[:, i, :], in0=B[0][:], in1=B[2][:], op=ALU.mult),
                      reads=["cB0", "cB2"], writes=[("Y", t) for t in range(NTILE)])
            for i in range(4):
                proj(12 + i, B[0], "cB0")
                proj(16 + i, B[1], "cB1")
                P.act(lambda e: e.activation(out=B[1][:], in_=B[1][:], func=AF.Sigmoid), reads=["cB1"], writes=["cB1"])
                P.dve(lambda e: e.tensor_tensor(out=Zb[:], in0=B[0][:], in1=B[1][:], op=ALU.mult),
                      reads=["cB0", "cB1"], writes=["Zb"])
                zk = ("ZC", i)
                dgi = i % 2
                dg = Dg[dgi]
                dgk = "Dg%d" % dgi
                for kk in range(31):
                    P.pool(lambda e, i=i, kk=kk, dg=dg: e.tensor_scalar(out=dg[:, kk, :], in0=ident_b[:],
                                                                        scalar1=CB[:, i, kk:kk + 1], scalar2=None,
                                                                        op0=ALU.mult), reads=["ident_b", "CB"], writes=[dgk])
                for ti, (t0, n) in enumerate(tiles):
                    s0, L = SEQS[0] if t0 < TC else SEQS[1]
                    pb = 6 + (ti % 2)
                    taps = [15] + [kk for kk in range(31) if kk != 15]
                    for q, kk in enumerate(taps):
                        o = kk - 15
                        a = max(0, s0 - o - t0)
                        bnd = min(n, s0 + L - o - t0)
                        P.pe(lambda e, dg=dg, kk=kk, o=o, a=a, bnd=bnd, t0=t0, pb=pb, q=q: e.matmul(
                            PS[pb][:, a:bnd], dg[:, kk, :], Zb[:, t0 + a + o:t0 + bnd + o],
                            start=(q == 0), stop=(q == 30)), reads=[dgk, "Zb"], writes=[PK[pb]])
                    P.act(lambda e, i=i, t0=t0, n=n, pb=pb: e.activation(out=ZC[:, i, t0:t0 + n], in_=PS[pb][:, 0:n],
                                                                          func=AF.Identity, bias=LNP[:, 0, i:i + 1], scale=1.0),
                          reads=[PK[pb], "LNP"], writes=[zk])
            MEAN = B[1]
            RSTD = B[2]
            SQ = [sb(es, "cSQ%d" % i, [128, 512]) for i in range(2)]
            sqi = 0
            for (t0, n) in tiles:
                for i in range(4):
                    sq = SQ[sqi % 2]
                    sqk = "cSQ%d" % (sqi % 2)
                    sqi += 1
                    P.act(lambda e, i=i, t0=t0, n=n, sq=sq: e.activation(out=sq[:, 0:n], in_=ZC[:, i, t0:t0 + n],
                                                                          func=AF.Square), reads=[("ZC", i)], writes=[sqk])
                    P.pe(lambda e, i=i, t0=t0, n=n: e.matmul(PS[4][:, 0:n], ones_f[:], ZC[:, i, t0:t0 + n],
                                                             start=(i == 0), stop=(i == 3)),
                         reads=[("ZC", i), "ones_f"], writes=[PK[4]])
                    P.pe(lambda e, i=i, n=n, sq=sq: e.matmul(PS[5][:, 0:n], ones_f[:], sq[:, 0:n],
                                                              start=(i == 0), stop=(i == 3)),
                         reads=[sqk, "ones_f"], writes=[PK[5]])
                P.dve(lambda e, t0=t0, n=n: e.tensor_scalar(out=MEAN[:, t0:t0 + n], in0=PS[4][:, 0:n], scalar1=1.0 / 512,
                                                             scalar2=None, op0=ALU.mult), reads=[PK[4]], writes=["cB1"])
                P.dve(lambda e, t0=t0, n=n: e.tensor_tensor(out=B[0][:, t0:t0 + n], in0=MEAN[:, t0:t0 + n],
                                                             in1=MEAN[:, t0:t0 + n], op=ALU.mult),
                      reads=["cB1"], writes=["cB0"])
                P.dve(lambda e, t0=t0, n=n: e.scalar_tensor_tensor(
                    out=RSTD[:, t0:t0 + n], in0=PS[5][:, 0:n], scalar=1.0 / 512, in1=B[0][:, t0:t0 + n],
                    op0=ALU.mult, op1=ALU.subtract), reads=[PK[5], "cB0"], writes=["cB2"])
            P.act(lambda e: e.activation(out=RSTD[:], in_=RSTD[:], func=AF.Sqrt, scale=1.0, bias=EPS),
                  reads=["cB2"], writes=["cB2"])
            P.dve(lambda e: e.reciprocal(RSTD[:], RSTD[:]), reads=["cB2"], writes=["cB2"])
            for i in range(4):
                zk = ("ZC", i)
                P.dve(lambda e, i=i: e.tensor_tensor(out=ZC[:, i, :], in0=ZC[:, i, :], in1=MEAN[:], op=ALU.subtract),
                      reads=[zk, "cB1"], writes=[zk])
                P.dve(lambda e, i=i: e.tensor_tensor(out=ZC[:, i, :], in0=ZC[:, i, :], in1=RSTD[:], op=ALU.mult),
                      reads=[zk, "cB2"], writes=[zk])
                P.act(lambda e, i=i: e.activation(out=Y[:, 4 + i, :], in_=ZC[:, i, :], func=AF.Silu,
                                                  scale=LNP[:, 1, i:i + 1], bias=LNP[:, 2, i:i + 1]),
                      reads=[zk, "LNP"], writes=[("Y", t) for t in range(NTILE)])
        P.barrier()


    def load_wgroup(Wt, key, src3, g):
        for k in range(8):
            P.dma(lambda e, k=k: e.dma_start(out=Wt[:, k, :], in_=src3[:, k, g * 512:(g + 1) * 512]),
                  writes=[(key, k)], q="pool")

    def phase_attn(l, U, Y):
        i2 = l // 2
        lam_init = 0.8 - 0.6 * math.exp(-0.3 * l)
        tiles = _tok_tiles()
        wv = w_in_cd_d[i2].rearrange("(k p) n -> p k n", p=128)
        with ExitStack() as es:
            qkT = [sb(es, "qT", [128, 4, TT], BF16), sb(es, "kT", [128, 4, TT], BF16)]
            V = sb(es, "V", [128, NTILE, 512], BF16)
            small = sb(es, "asmall", [128, 8])
            lamb = sb(es, "lamb", [128, 4 * 64])
            GQK = sb(es, "GQK", [128, 2])
            GS = sb(es, "GS", [128, 1])
            with ExitStack() as es2:
                Wt = [sb(es2, "Wt%d" % i, [128, 8, 512], BF16) for i in range(2)]
                COS = sb(es2, "COS", [128, T], BF16)
                SIN = sb(es2, "SIN", [128, T], BF16)
                RT = sb(es2, "RT", [128, 128], BF16)
                BO = sb(es2, "BO", [128, 128])
                QK = sb(es2, "QK", [128, TT])
                SQ = [sb(es2, "aSQ%d" % i, [128, 512]) for i in range(2)]
                RS = [sb(es2, "aRS%d" % i, [128, 512]) for i in range(2)]
                QN = [sb(es2, "aQN%d" % i, [128, 512], BF16) for i in range(2)]
                O1 = [sb(es2, "aO1%d" % i, [128, 512]) for i in range(2)]
                P.dma(lambda e: e.dma_start(out=COS[:], in_=ropec_d), writes=["COS"])
                P.dma(lambda e: e.dma_start(out=SIN[:], in_=ropes_d), writes=["SIN"])
                P.dma(lambda e: e.dma_start(out=RT[:], in_=ropert_d), writes=["RT"])
                P.dma(lambda e: e.dma_start(out=BO[:], in_=blockones_d), writes=["BO"])
                P.dma(lambda e: e.dma_start(out=GQK[:], in_=gqk_d[i2]), writes=["GQK"])
                P.dma(lambda e: e.dma_start(out=GS[:], in_=gsub_d[i2]), writes=["GS"])
                P.dma(lambda e: e.dma_start(out=lamb[:], in_=lamv_d[i2:i2 + 1, :].partition_broadcast(128)),
                      writes=["lamb"])
                for j in range(2):
                    P.dve(lambda e, j=j: e.tensor_tensor(out=lamb[:, j * 128:j * 128 + 64], in0=lamb[:, j * 128:j * 128 + 64],
                                                          in1=lamb[:, j * 128 + 64:j * 128 + 128], op=ALU.mult),
                          reads=["lamb"], writes=["lamb"])
                    P.dve(lambda e, j=j: e.reduce_sum(out=small[:, j:j + 1], in_=lamb[:, j * 128:j * 128 + 64], axis=AX.X),
                          reads=["lamb"], writes=["asmall"])
                P.act(lambda e: e.activation(out=small[:, 2:4], in_=small[:, 0:2], func=AF.Exp),
                      reads=["asmall"], writes=["asmall"])
                P.dve(lambda e: e.scalar_tensor_tensor(out=small[:, 4:5], in0=small[:, 3:4], scalar=-lam_init,
                                                        in1=small[:, 2:3], op0=ALU.add, op1=ALU.subtract),
                      reads=["asmall"], writes=["asmall"])
                P.dve(lambda e: e.tensor_scalar(out=GS[:], in0=GS[:], scalar1=1.0 - lam_init, scalar2=None, op0=ALU.mult),
                      reads=["GS"], writes=["GS"])
                cnt = 0
                for g in range(2):
                    wt = Wt[g % 2]
                    wkey = "Wt%d" % (g % 2)
                    load_wgroup(wt, wkey, wv, g)
                    for h in range(4):
                        for (t0, n) in tiles:
                            pb = cnt % 2
                            bb = cnt % 2
                            cnt += 1
                            for k in range(8):
                                P.pe(lambda e, wt=wt, h=h, k=k, t0=t0, n=n, pb=pb: e.matmul(
                                    PS[pb][:, 0:n], wt[:, k, h * 128:(h + 1) * 128], U[:, k, t0:t0 + n],
                                    start=(k == 0), stop=(k == 7)),
                                    reads=[(wkey, k)] + [("U", t) for t in range(t0 // 128, (t0 + n) // 128)],
                                    writes=[PK[pb]])
                            P.act(lambda e, t0=t0, n=n, pb=pb: e.activation(out=QK[:, t0:t0 + n], in_=PS[pb][:, 0:n],
                                                                              func=AF.Copy), reads=[PK[pb]], writes=["QK"])
                            P.act(lambda e, t0=t0, n=n, bb=bb: e.activation(out=SQ[bb][:, 0:n], in_=QK[:, t0:t0 + n],
                                                                              func=AF.Square), reads=["QK"],
                                  writes=["aSQ%d" % bb])
                            p2 = 2 + pb
                            P.pe(lambda e, n=n, bb=bb, p2=p2: e.matmul(PS[p2][:, 0:n], BO[:], SQ[bb][:, 0:n],
                                                                        start=True, stop=True),
                                 reads=["aSQ%d" % bb, "BO"], writes=[PK[p2]])
                            P.act(lambda e, n=n, bb=bb, p2=p2: e.activation(out=RS[bb][:, 0:n], in_=PS[p2][:, 0:n],
                                                                              func=AF.Sqrt, scale=1.0 / 64, bias=EPS),
                                  reads=[PK[p2]], writes=["aRS%d" % bb])
                            P.dve(lambda e, n=n, bb=bb: e.reciprocal(RS[bb][:, 0:n], RS[bb][:, 0:n]),
                                  reads=["aRS%d" % bb], writes=["aRS%d" % bb])
                            dstk = [("qk", g, h, t) for t in range(t0 // 128, (t0 + n) // 128)]
                            if t0 < TC:
                                P.dve(lambda e, g=g, h=h, t0=t0, n=n, bb=bb: e.scalar_tensor_tensor(
                                    out=qkT[g][:, h, t0:t0 + n], in0=QK[:, t0:t0 + n], scalar=GQK[:, g:g + 1],
                                    in1=RS[bb][:, 0:n], op0=ALU.mult, op1=ALU.mult),
                                    reads=["QK", "GQK", "aRS%d" % bb], writes=dstk)
                            else:
                                P.dve(lambda e, g=g, t0=t0, n=n, bb=bb: e.scalar_tensor_tensor(
                                    out=QN[bb][:, 0:n], in0=QK[:, t0:t0 + n], scalar=GQK[:, g:g + 1],
                                    in1=RS[bb][:, 0:n], op0=ALU.mult, op1=ALU.mult),
                                    reads=["QK", "GQK", "aRS%d" % bb], writes=["aQN%d" % bb])
                                p3 = 4 + pb
                                P.pe(lambda e, n=n, bb=bb, p3=p3: e.matmul(PS[p3][:, 0:n], RT[:], QN[bb][:, 0:n],
                                                                            start=True, stop=True),
                                     reads=["aQN%d" % bb, "RT"], writes=[PK[p3]])
                                c0 = t0 - TC
                                P.pool(lambda e, n=n, bb=bb, c0=c0: e.tensor_tensor(
                                    out=O1[bb][:, 0:n], in0=QN[bb][:, 0:n], in1=COS[:, c0:c0 + n], op=ALU.mult),
                                    reads=["aQN%d" % bb, "COS"], writes=["aO1%d" % bb])
                                P.dve(lambda e, n=n, bb=bb, c0=c0, p3=p3: e.tensor_tensor(
                                    out=RS[bb][:, 0:n], in0=PS[p3][:, 0:n], in1=SIN[:, c0:c0 + n], op=ALU.mult),
                                    reads=[PK[p3], "SIN"], writes=["aRS%d" % bb])
                                P.dve(lambda e, g=g, h=h, t0=t0, n=n, bb=bb: e.tensor_tensor(
                                    out=qkT[g][:, h, t0:t0 + n], in0=O1[bb][:, 0:n], in1=RS[bb][:, 0:n], op=ALU.add),
                                    reads=["aO1%d" % bb, "aRS%d" % bb], writes=dstk)
                wt = Wt[0]
                load_wgroup(wt, "Wt0", wv, 2)
                for tt in range(NTILE):
                    pb = 6 + tt % 2
                    for k in range(8):
                        P.pe(lambda e, tt=tt, k=k, pb=pb: e.matmul(PS[pb][:], U[:, k, tt * 128:(tt + 1) * 128], wt[:, k, :],
                                                                    start=(k == 0), stop=(k == 7)),
                             reads=[("U", tt), ("Wt0", k)], writes=[PK[pb]])
                    P.act(lambda e, tt=tt, pb=pb: e.activation(out=V[:, tt, :], in_=PS[pb][:], func=AF.Copy),
                          reads=[PK[pb]], writes=[("V", tt)])
            P.barrier()
            with ExitStack() as es2:
                Eb = [sb(es2, "Eb%d" % i, [128, 512], BF16) for i in range(4)]
                SBK = (0, 1, 7)
                R0 = sb(es2, "aR0", [128, 512])
                R1 = sb(es2, "aR1", [128, 512])
                OO = sb(es2, "aOO", [128, 512])
                SQ2 = sb(es2, "aSQ2", [128, 512])
                ecnt = 0
                groups = [(TC + i * 512, 512, list(range(NTILE))) for i in range(T // 512)] + [(0, TC, [0, 1])]
                for h in range(4):
                    for (q0, n, ktl) in groups:
                        qkeys = [("qk", 0, h, t) for t in range(q0 // 128, (q0 + n) // 128)]
                        its = [(j, ki, kt) for j in range(2) for ki, kt in enumerate(ktl)]
                        nk = len(ktl)

                        def emit_s(idx, q0=q0, n=n, h=h, qkeys=qkeys):
                            j, ki, kt = its[idx]
                            psb = SBK[(ecnt + idx) % 3]
                            P.pe(lambda e, h=h, j=j, kt=kt, q0=q0, n=n, psb=psb: e.matmul(
                                PS[psb][:, 0:n], qkT[1][j * 64:(j + 1) * 64, h, kt * 128:(kt + 1) * 128],
                                qkT[0][j * 64:(j + 1) * 64, h, q0:q0 + n], start=True, stop=True),
                                reads=qkeys + [("qk", 1, h, kt)], writes=[PK[psb]])

                        emit_s(0)
                        emit_s(1)
                        for idx, (j, ki, kt) in enumerate(its):
                            po, pz = 2 + 2 * j, 3 + 2 * j
                            psb = SBK[(ecnt + idx) % 3]
                            eb = (ecnt + idx) % 4
                            P.act(lambda e, n=n, psb=psb, eb=eb: e.activation(out=Eb[eb][:, 0:n], in_=PS[psb][:, 0:n],
                                                                                func=AF.Exp, scale=0.125),
                                  reads=[PK[psb]], writes=["Eb%d" % eb])
                            if idx + 2 < len(its):
                                emit_s(idx + 2)
                            P.pe(lambda e, h=h, kt=kt, n=n, eb=eb, po=po, ki=ki, nk=nk: e.matmul(
                                PS[po][:, 0:n], V[:, kt, h * 128:(h + 1) * 128], Eb[eb][:, 0:n],
                                start=(ki == 0), stop=(ki == nk - 1)), reads=[("V", kt), "Eb%d" % eb], writes=[PK[po]])
                            P.pe(lambda e, n=n, eb=eb, pz=pz, ki=ki, nk=nk: e.matmul(
                                PS[pz][:, 0:n], ones_b[:], Eb[eb][:, 0:n], start=(ki == 0), stop=(ki == nk - 1)),
                                reads=["ones_b", "Eb%d" % eb], writes=[PK[pz]])
                        ecnt += len(its)
                        P.dve(lambda e, n=n: e.reciprocal(R0[:, 0:n], PS[3][:, 0:n]), reads=[PK[3]], writes=["aR0"])
                        P.dve(lambda e, n=n: e.tensor_tensor(out=R0[:, 0:n], in0=PS[2][:, 0:n], in1=R0[:, 0:n], op=ALU.mult),
                              reads=[PK[2], "aR0"], writes=["aR0"])
                        P.dve(lambda e, n=n: e.reciprocal(R1[:, 0:n], PS[5][:, 0:n]), reads=[PK[5]], writes=["aR1"])
                        P.dve(lambda e, n=n: e.tensor_tensor(out=R1[:, 0:n], in0=PS[4][:, 0:n], in1=R1[:, 0:n], op=ALU.mult),
                              reads=[PK[4], "aR1"], writes=["aR1"])
                        P.dve(lambda e, n=n: e.scalar_tensor_tensor(out=OO[:, 0:n], in0=R1[:, 0:n], scalar=small[:, 4:5],
                                                                     in1=R0[:, 0:n], op0=ALU.mult, op1=ALU.add),
                              reads=["aR0", "aR1", "asmall"], writes=["aOO"])
                        P.act(lambda e, n=n: e.activation(out=SQ2[:, 0:n], in_=OO[:, 0:n], func=AF.Square),
                              reads=["aOO"], writes=["aSQ2"])
                        P.pe(lambda e, n=n: e.matmul(PS[6][:, 0:n], ones_f[:], SQ2[:, 0:n], start=True, stop=True),
                             reads=["aSQ2", "ones_f"], writes=[PK[6]])
                        P.act(lambda e, n=n: e.activation(out=SQ2[:, 0:n], in_=PS[6][:, 0:n], func=AF.Sqrt,
                                                          scale=1.0 / 128, bias=1e-5), reads=[PK[6]], writes=["aSQ2"])
                        P.dve(lambda e, n=n: e.reciprocal(SQ2[:, 0:n], SQ2[:, 0:n]), reads=["aSQ2"], writes=["aSQ2"])
                        P.dve(lambda e, h=h, q0=q0, n=n: e.scalar_tensor_tensor(
                            out=Y[:, h, q0:q0 + n], in0=OO[:, 0:n], scalar=GS[:, 0:1], in1=SQ2[:, 0:n],
                            op0=ALU.mult, op1=ALU.mult), reads=["aOO", "GS", "aSQ2"],
                            writes=[("Y", t) for t in range(q0 // 128, (q0 + n) // 128)])
        P.barrier()

    def phase_hyena(l, U, Y):
        i2 = l // 2
        tiles = _tok_tiles()
        wv = w_in_cd_d[i2].rearrange("(k p) n -> p k n", p=128)
        PI = math.pi
        with ExitStack() as es:
            h_tok = sb(es, "h_tok", [128, NTILE, 512], BF16)
            v_tok = sb(es, "v_tok", [128, NTILE, 512], BF16)
            GOc = sb(es, "GOc", [128, 4, TT], BF16)
            with ExitStack() as es2:
                zT = sb(es2, "zT", [33, TT])
                W1 = sb(es2, "hW1", [33, 64])
                W2 = sb(es2, "hW2", [64, 64])
                W3 = sb(es2, "hW3", [64, 512])
                HV = sb(es2, "hHV", [64, 3])
                HB = sb(es2, "hHB", [1, 512])
                HID = [sb(es2, "hHID%d" % i, [64, TT]) for i in range(2)]
                KI = sb(es2, "hKI", [64, 512], I32)
                KF = sb(es2, "hKF", [64, 512])
                AA = sb(es2, "hAA", [64, 512])
                WN = [sb(es2, "hWN%d" % i, [128, 512]) for i in range(2)]
                HT = [sb(es2, "hHT%d" % i, [128, 512]) for i in range(2)]
                P.dma(lambda e: e.dma_start(out=zT[:], in_=zT_d), writes=["zT"])
                P.dma(lambda e: e.dma_start(out=W1[:], in_=hfw1_d[i2]), writes=["hW1"])
                P.dma(lambda e: e.dma_start(out=W2[:], in_=hfw2_d[i2]), writes=["hW2"])
                P.dma(lambda e: e.dma_start(out=W3[:], in_=hfw3_d[i2]), writes=["hW3"])
                P.dma(lambda e: e.dma_start(out=HV[:], in_=hfv_d[i2]), writes=["hHV"])
                P.dma(lambda e: e.dma_start(out=HB[:], in_=hfb_d[i2:i2 + 1, :]), writes=["hHB"])
                for layer in range(2):
                    src = zT if layer == 0 else HID[0]
                    srck = "zT" if layer == 0 else "hHID0"
                    kdim = 33 if layer == 0 else 64
                    wm = W1 if layer == 0 else W2
                    wmk = "hW1" if layer == 0 else "hW2"
                    bcol = 0 if layer == 0 else 2
                    dst = HID[layer]
                    dk = "hHID%d" % layer
                    for ti, (t0, n) in enumerate(tiles):
                        pb = ti % 2
                        P.pe(lambda e, src=src, kdim=kdim, wm=wm, t0=t0, n=n, pb=pb: e.matmul(
                            PS[pb][0:64, 0:n], wm[0:kdim, :], src[0:kdim, t0:t0 + n], start=True, stop=True),
                            reads=[srck, wmk], writes=[PK[pb]])
                        P.dve(lambda e, n=n, pb=pb, bcol=bcol: e.tensor_scalar(
                            out=AA[:, 0:n], in0=PS[pb][0:64, 0:n], scalar1=HV[:, bcol:bcol + 1], scalar2=HV[:, 1:2],
                            op0=ALU.add, op1=ALU.mult), reads=[PK[pb], "hHV"], writes=["hAA"])
                        P.dve(lambda e, n=n: e.tensor_scalar(out=KI[:, 0:n], in0=AA[:, 0:n], scalar1=1.0 / (2 * PI),
                                                             scalar2=None, op0=ALU.mult), reads=["hAA"], writes=["hKI"])
                        P.dve(lambda e, n=n: e.tensor_copy(KF[:, 0:n], KI[:, 0:n]), reads=["hKI"], writes=["hKF"])
                        P.dve(lambda e, n=n: e.scalar_tensor_tensor(out=AA[:, 0:n], in0=KF[:, 0:n], scalar=-2 * PI,
                                                                     in1=AA[:, 0:n], op0=ALU.mult, op1=ALU.add),
                              reads=["hKF", "hAA"], writes=["hAA"])
                        P.dve(lambda e, n=n: e.tensor_scalar(out=KF[:, 0:n], in0=AA[:, 0:n], scalar1=PI, scalar2=-2 * PI,
                                                             op0=ALU.is_gt, op1=ALU.mult), reads=["hAA"], writes=["hKF"])
                        P.dve(lambda e, n=n: e.tensor_tensor(out=AA[:, 0:n], in0=AA[:, 0:n], in1=KF[:, 0:n], op=ALU.add),
                              reads=["hAA", "hKF"], writes=["hAA"])
                        P.dve(lambda e, n=n: e.tensor_scalar(out=KF[:, 0:n], in0=AA[:, 0:n], scalar1=-PI, scalar2=2 * PI,
                                                             op0=ALU.is_lt, op1=ALU.mult), reads=["hAA"], writes=["hKF"])
                        P.dve(lambda e, n=n: e.tensor_tensor(out=AA[:, 0:n], in0=AA[:, 0:n], in1=KF[:, 0:n], op=ALU.add),
                              reads=["hAA", "hKF"], writes=["hAA"])
                        P.dve(lambda e, n=n: e.tensor_scalar(out=AA[:, 0:n], in0=AA[:, 0:n], scalar1=-PI, scalar2=PI,
                                                             op0=ALU.max, op1=ALU.min), reads=["hAA"], writes=["hAA"])
                        P.act(lambda e, dst=dst, t0=t0, n=n: e.activation(out=dst[:, t0:t0 + n], in_=AA[:, 0:n], func=AF.Sin),
                              reads=["hAA"], writes=[dk])
                for tt in range(NTILE):
                    b = tt % 2
                    pb = 2 + b
                    P.dma(lambda e, b=b, tt=tt: e.dma_start(out=WN[b][:], in_=win_d[tt * 128:(tt + 1) * 128, :]),
                          writes=["hWN%d" % b])
                    P.pe(lambda e, tt=tt, pb=pb: e.matmul(PS[pb][:], HID[1][:, tt * 128:(tt + 1) * 128], W3[:],
                                                          start=True, stop=True), reads=["hHID1", "hW3"], writes=[PK[pb]])
                    centre = tt in (1, 2 + 8)
                    if centre:
                        P.dve(lambda e, b=b, pb=pb: e.tensor_tensor(out=HT[b][:], in0=PS[pb][:], in1=WN[b][:], op=ALU.mult),
                              reads=[PK[pb], "hWN%d" % b], writes=["hHT%d" % b])
                        P.dve(lambda e, b=b: e.tensor_tensor(out=HT[b][0:1, :], in0=HT[b][0:1, :], in1=HB[:], op=ALU.add),
                              reads=["hHT%d" % b, "hHB"], writes=["hHT%d" % b])
                        P.act(lambda e, b=b, tt=tt: e.activation(out=h_tok[:, tt, :], in_=HT[b][:], func=AF.Copy),
                              reads=["hHT%d" % b], writes=[("h_tok", tt)])
                    else:
                        P.dve(lambda e, b=b, pb=pb, tt=tt: e.tensor_tensor(out=h_tok[:, tt, :], in0=PS[pb][:], in1=WN[b][:],
                                                                            op=ALU.mult),
                              reads=[PK[pb], "hWN%d" % b], writes=[("h_tok", tt)])
            P.barrier()
            with ExitStack() as es2:
                Wt = [sb(es2, "hWt%d" % i, [128, 8, 512], BF16) for i in range(3)]
                CD = sb(es2, "CD", [128, 12, 3])
                Bf = [sb(es2, "hB%d" % i, [128, TT]) for i in range(3)]
                P.dma(lambda e: e.dma_start(out=CD[:], in_=cd_d[i2]), writes=["CD"])
                for g in range(3):
                    load_wgroup(Wt[g], "hWt%d" % g, wv, 3 + g)
                pcnt = [0]

                def projc(g, c, raw, rawk, dst, dstk):
                    for (t0, n) in tiles:
                        pb = pcnt[0] % 4
                        pcnt[0] += 1
                        for k in range(8):
                            P.pe(lambda e, g=g, c=c, k=k, t0=t0, n=n, pb=pb: e.matmul(
                                PS[pb][:, 0:n], Wt[g][:, k, c * 128:(c + 1) * 128], U[:, k, t0:t0 + n],
                                start=(k == 0), stop=(k == 7)),
                                reads=[("hWt%d" % g, k)] + [("U", t) for t in range(t0 // 128, (t0 + n) // 128)],
                                writes=[PK[pb]])
                        P.act(lambda e, t0=t0, n=n, pb=pb: e.activation(out=raw[:, t0:t0 + n], in_=PS[pb][:, 0:n],
                                                                          func=AF.Copy), reads=[PK[pb]], writes=[rawk])
                    ch = g * 4 + c
                    P.dve(lambda e: e.tensor_scalar(out=dst[:], in0=raw[:], scalar1=CD[:, ch, 1:2], scalar2=None,
                                                    op0=ALU.mult), reads=[rawk, "CD"], writes=[dstk])
                    for (s0, L) in SEQS:
                        P.dve(lambda e, s0=s0, L=L: e.scalar_tensor_tensor(
                            out=dst[:, s0 + 1:s0 + L], in0=raw[:, s0:s0 + L - 1], scalar=CD[:, ch, 0:1],
                            in1=dst[:, s0 + 1:s0 + L], op0=ALU.mult, op1=ALU.add), reads=[rawk, dstk, "CD"], writes=[dstk])
                        P.dve(lambda e, s0=s0, L=L: e.scalar_tensor_tensor(
                            out=dst[:, s0:s0 + L - 1], in0=raw[:, s0 + 1:s0 + L], scalar=CD[:, ch, 2:3],
                            in1=dst[:, s0:s0 + L - 1], op0=ALU.mult, op1=ALU.add), reads=[rawk, dstk, "CD"], writes=[dstk])

                for c in range(4):
                    projc(0, c, Bf[0], "hB0", Bf[1], "hB1")
                    P.act(lambda e, c=c: e.activation(out=GOc[:, c, :], in_=Bf[1][:], func=AF.Copy),
                          reads=["hB1"], writes=[("GOc", c)])
                    projc(1, c, Bf[0], "hB0", Bf[1], "hB1")
                    projc(2, c, Bf[0], "hB0", Bf[2], "hB2")
                    P.pool(lambda e: e.tensor_tensor(out=Bf[1][:], in0=Bf[1][:], in1=Bf[2][:], op=ALU.mult),
                           reads=["hB1", "hB2"], writes=["hB1"])
                    for t4 in range(0, NTILE, 4):
                        nt = min(4, NTILE - t4)
                        pb = 4 + (t4 // 4) % 2
                        for j in range(nt):
                            P.pe(lambda e, t4=t4, j=j, pb=pb: e.transpose(
                                PS[pb][:, j * 128:(j + 1) * 128], Bf[1][:, (t4 + j) * 128:(t4 + j + 1) * 128], ident_f[:]),
                                reads=["hB1", "ident_f"], writes=[PK[pb]])
                        P.act(lambda e, t4=t4, nt=nt, pb=pb, c=c: e.activation(
                            out=v_tok[:, t4:t4 + nt, c * 128:(c + 1) * 128],
                            in_=PS[pb][:, 0:nt * 128].rearrange("p (j t) -> p j t", j=nt), func=AF.Copy),
                            reads=[PK[pb]], writes=[("v_tok", c)])
            P.barrier()
            with ExitStack() as es2:
                YS = sb(es2, "YS", [128, 24, 512], BF16)
                YSc = sb(es2, "YSc", [128, 4, 512], BF16)
                es3 = ExitStack()
                FW = [sb(es3, "FW%d" % i, [128, 16, 256], BF16) for i in range(2)]
                HH = [sb(es3, "HH%d" % i, [128, 2, 512]) for i in range(2)]
                T1 = sb(es3, "hT1", [128, 512])
                T2 = sb(es3, "hT2", [128, 512])
                T3 = sb(es3, "hT3", [128, 512])
                T4 = sb(es3, "hT4", [128, 512])
                fcnt = 0
                for (npair, tt0, ntt, fwd, ys, ysk) in ((12, 2, 16, fwl_d, YS, "YS"), (2, 0, 2, fwc_d, YSc, "YSc")):
                    for i in range(npair):
                        fb = fcnt % 2
                        fcnt += 1
                        fw = FW[fb]
                        fwk = "FW%d" % fb
                        hh = HH[fb]
                        hhk = "HH%d" % fb
                        P.dma(lambda e, fw=fw, fwd=fwd, i=i, ntt=ntt: e.dma_start(out=fw[:, 0:ntt, :], in_=fwd[i]),
                              writes=[fwk])
                        pbase = 4 * fb
                        for (srct, srck, pr, pi) in ((h_tok, "h_tok", pbase, pbase + 1), (v_tok, "v_tok", pbase + 2, pbase + 3)):
                            for part, pb in ((0, pr), (1, pi)):
                                for tt in range(ntt):
                                    rk = [("h_tok", tt0 + tt)] if srct is h_tok else [("v_tok", c) for c in range(4)]
                                    P.pe(lambda e, fw=fw, tt=tt, part=part, pb=pb, srct=srct, tt0=tt0, ntt=ntt: e.matmul(
                                        PS[pb][:], fw[:, tt, part * 128:(part + 1) * 128], srct[:, tt0 + tt, :],
                                        start=(tt == 0), stop=(tt == ntt - 1)), reads=[fwk] + rk, writes=[PK[pb]])
                            if srct is h_tok:
                                P.act(lambda e, hh=hh, pbase=pbase: e.activation(out=hh[:, 0, :], in_=PS[pbase][:], func=AF.Copy),
                                      reads=[PK[pbase]], writes=[hhk])
                                P.act(lambda e, hh=hh, pbase=pbase: e.activation(out=hh[:, 1, :], in_=PS[pbase + 1][:], func=AF.Copy),
                                      reads=[PK[pbase + 1]], writes=[hhk])
                        nch = npair
                        pu, pv = pbase + 2, pbase + 3
                        P.dve(lambda e, hh=hh, pu=pu: e.tensor_tensor(out=T1[:], in0=PS[pu][:], in1=hh[:, 0, :], op=ALU.mult),
                              reads=[PK[pu], hhk], writes=["hT1"])
                        P.dve(lambda e, hh=hh, pv=pv: e.tensor_tensor(out=T2[:], in0=PS[pv][:], in1=hh[:, 1, :], op=ALU.mult),
                              reads=[PK[pv], hhk], writes=["hT2"])
                        P.pool(lambda e, ys=ys, i=i: e.tensor_tensor(out=ys[:, i, :], in0=T1[:], in1=T2[:], op=ALU.subtract),
                               reads=["hT1", "hT2"], writes=[(ysk, i)])
                        P.dve(lambda e, hh=hh, pu=pu: e.tensor_tensor(out=T3[:], in0=PS[pu][:], in1=hh[:, 1, :], op=ALU.mult),
                              reads=[PK[pu], hhk], writes=["hT3"])
                        P.dve(lambda e, hh=hh, pv=pv: e.tensor_tensor(out=T4[:], in0=PS[pv][:], in1=hh[:, 0, :], op=ALU.mult),
                              reads=[PK[pv], hhk], writes=["hT4"])
                        P.pool(lambda e, ys=ys, i=i, nch=nch: e.tensor_tensor(out=ys[:, nch + i, :], in0=T3[:], in1=T4[:],
                                                                               op=ALU.add),
                               reads=["hT3", "hT4"], writes=[(ysk, nch + i)])
                P.barrier()
                es3.close()
                IV = [sb(es2, "IV%d" % i, [128, 12, 512], BF16) for i in range(2)]
                icnt = 0
                for j in range(4):
                    for half in range(2):
                        ib = icnt % 2
                        icnt += 1
                        P.dma(lambda e, ib=ib, j=j, half=half: e.dma_start(out=IV[ib][:], in_=invl_d[j, half]),
                              writes=["IV%d" % ib])
                        for c in range(4):
                            pc = 4 * (j % 2) + c
                            for fc in range(12):
                                P.pe(lambda e, ib=ib, half=half, c=c, fc=fc, pc=pc: e.matmul(
                                    PS[pc][:], YS[:, half * 12 + fc, c * 128:(c + 1) * 128], IV[ib][:, fc, :],
                                    start=(half == 0 and fc == 0), stop=(half == 1 and fc == 11)),
                                    reads=[("YS", half * 12 + fc), "IV%d" % ib], writes=[PK[pc]])
                    t0 = TC + j * 512
                    for c in range(4):
                        pc = 4 * (j % 2) + c
                        P.dve(lambda e, c=c, t0=t0, pc=pc: e.tensor_tensor(out=Y[:, 4 + c, t0:t0 + 512], in0=PS[pc][:],
                                                                           in1=GOc[:, c, t0:t0 + 512], op=ALU.mult),
                              reads=[PK[pc], ("GOc", c)], writes=[("Y", t) for t in range(t0 // 128, t0 // 128 + 4)])
                IVc = [sb(es2, "IVc%d" % i, [128, 2, 256], BF16) for i in range(2)]
                for half in range(2):
                    P.dma(lambda e, half=half: e.dma_start(out=IVc[half][:], in_=invc_d[half]), writes=["IVc%d" % half])
                for c in range(4):
                    for half in range(2):
                        for fc in range(2):
                            P.pe(lambda e, half=half, c=c, fc=fc: e.matmul(
                                PS[4 + c][:, 0:TC], YSc[:, half * 2 + fc, c * 128:(c + 1) * 128], IVc[half][:, fc, :],
                                start=(half == 0 and fc == 0), stop=(half == 1 and fc == 1)),
                                reads=[("YSc", half * 2 + fc), "IVc%d" % half], writes=[PK[4 + c]])
                    P.dve(lambda e, c=c: e.tensor_tensor(out=Y[:, 4 + c, 0:TC], in0=PS[4 + c][:, 0:TC], in1=GOc[:, c, 0:TC],
                                                         op=ALU.mult),
                          reads=[PK[4 + c], ("GOc", c)], writes=[("Y", 0), ("Y", 1)])
        P.barrier()

    def phase_moe(l, AFF, last):
        with ExitStack() as es:
            affT = sb(es, "affT", [NE, TT])
            work = sb(es, "mwork", [NE, TT])
            GV = sb(es, "GV", [NE, NSLOT])
            IXu = sb(es, "IXu", [NE, NSLOT], U32)
            IXf = sb(es, "IXf", [NE, NSLOT])
            gP = sb(es, "gP", [128, 3, NE])
            iP = sb(es, "iP", [128, 3, NE], I32)
            for t4 in range(0, NTILE, 4):
                nt = min(4, NTILE - t4)
                pb = (t4 // 4) % 2
                for j in range(nt):
                    P.pe(lambda e, t4=t4, j=j, pb=pb: e.transpose(PS[pb][0:NE, j * 128:(j + 1) * 128],
                                                                   AFF[:, t4 + j, :], ident_f[:]),
                         reads=[("AFF", t4 + j), "ident_f"], writes=[PK[pb]])
                P.dve(lambda e, t4=t4, nt=nt, pb=pb: e.tensor_copy(affT[:, t4 * 128:(t4 + nt) * 128],
                                                                    PS[pb][0:NE, 0:nt * 128]),
                      reads=[PK[pb]], writes=["affT"])
            P.dve(lambda e: e.tensor_copy(work[:], affT[:]), reads=["affT"], writes=["mwork"])
            for (s0, L, cap, c0) in ((TC, T, CAP_L, 0), (0, TC, CAP_C, CAP_L)):
                for rd in range(cap // 8):
                    sl = slice(c0 + rd * 8, c0 + rd * 8 + 8)
                    P.dve(lambda e, s0=s0, L=L, sl=sl: e.max(out=GV[:, sl], in_=work[:, s0:s0 + L]),
                          reads=["mwork"], writes=["GV"])
                    P.dve(lambda e, s0=s0, L=L, sl=sl: e.max_index(out=IXu[:, sl], in_max=GV[:, sl],
                                                                   in_values=work[:, s0:s0 + L]),
                          reads=["mwork", "GV"], writes=["IXu"])
                    P.dve(lambda e, s0=s0, L=L, sl=sl: e.match_replace(out=work[:, s0:s0 + L], in_to_replace=GV[:, sl],
                                                                       in_values=work[:, s0:s0 + L], imm_value=-1.0),
                          reads=["mwork", "GV"], writes=["mwork"])
            P.dve(lambda e: e.tensor_copy(IXf[:], IXu[:]), reads=["IXu"], writes=["IXf"])
            P.dve(lambda e: e.tensor_scalar(out=IXf[:, 0:CAP_L], in0=IXf[:, 0:CAP_L], scalar1=float(TC), scalar2=None,
                                            op0=ALU.add), reads=["IXf"], writes=["IXf"])
            for (src, dst, dk, pb) in ((IXf, iP, "iP", 2), (GV, gP, "gP", 3)):
                sk = "IXf" if src is IXf else "GV"
                for s in range(3):
                    n = 128 if s < 2 else CAP_C
                    P.pe(lambda e, src=src, s=s, n=n, pb=pb: e.transpose(PS[pb][0:n, s * NE:(s + 1) * NE],
                                                                          src[:, s * 128:s * 128 + n], ident_f[0:NE, 0:NE]),
                         reads=[sk, "ident_f"], writes=[PK[pb]])
                P.dve(lambda e, dst=dst, pb=pb: e.tensor_copy(dst[:, 0:2, :],
                                                               PS[pb][:, 0:2 * NE].rearrange("p (s e) -> p s e", s=2)),
                      reads=[PK[pb]], writes=[dk])
                P.dve(lambda e, dst=dst, pb=pb: e.tensor_copy(dst[0:CAP_C, 2, :], PS[pb][0:CAP_C, 2 * NE:3 * NE]),
                      reads=[PK[pb]], writes=[dk])
            if dbg and last:
                P.dma(lambda e: e.dma_start(out=dbg_ix_d, in_=IXf[:]), reads=["IXf"], writes=["dbg_ix"])
                P.dma(lambda e: e.dma_start(out=dbg_gv_d, in_=GV[:]), reads=["GV"], writes=["dbg_gv"])
            for tt in range(NTILE):
                P.dma(lambda e, tt=tt: e.dma_start(out=M_d[tt * 128:(tt + 1) * 128, :], in_=zero_f[:]),
                      reads=["zero_f"], writes=["Macc"])
            WB = [sb(es, "WB%d" % i, [128, 8, D], BF16) for i in range(6)]
            xe = [sb(es, "xe%d" % i, [128, 3, D], BF16) for i in range(2)]
            xeT = [sb(es, "xeT%d" % i, [128, 8, NSLOT], BF16) for i in range(2)]
            hT = [sb(es, "hT%d" % i, [128, 8, NSLOT], BF16) for i in range(2)]
            sg = [sb(es, "sg%d" % i, [128, NSLOT]) for i in range(2)]
            yb = [sb(es, "yb%d" % i, [128, 3, D]) for i in range(2)]
            wcnt = [0]

            def load_w(src_ap):
                i = wcnt[0] % 6
                wcnt[0] += 1
                wv = src_ap.rearrange("(k p) n -> p k n", p=128)
                for k in range(8):
                    P.dma(lambda e, i=i, k=k: e.dma_start(out=WB[i][:, k, :], in_=wv[:, k, :]),
                          writes=[("WB", i, k)], q="pool")
                return i

            for ex_i in range(NE):
                b = ex_i % 2
                kxe, kxT, khT, ksg, kyb = "xe%d" % b, "xeT%d" % b, "hT%d" % b, "sg%d" % b, "yb%d" % b
                if ex_i == 0:
                    wnext = (load_w(w_gate_d[l, 0]), load_w(w_up_d[l, 0]), load_w(w_down_d[l, 0]))
                wg, wu, wd = wnext
                for s in range(3):
                    n = 128 if s < 2 else CAP_C
                    P.dma(lambda e, b=b, s=s, n=n, ex_i=ex_i: e.indirect_dma_start(
                        out=xe[b][0:n, s, :], out_offset=None, in_=u2tok_d,
                        in_offset=bass.IndirectOffsetOnAxis(ap=iP[0:n, s, ex_i:ex_i + 1], axis=0)),
                        reads=["iP"] + [("u2tok", t) for t in range(NTILE)], writes=[(kxe, s)], q="pool")
                if ex_i + 1 < NE:
                    wnext = (load_w(w_gate_d[l, ex_i + 1]), load_w(w_up_d[l, ex_i + 1]), load_w(w_down_d[l, ex_i + 1]))
                if dbg and last and ex_i == 0:
                    P.dma(lambda e: e.dma_start(out=dbg_xe_d, in_=xe[0][:]), reads=[("xe0", 0), ("xe0", 1), ("xe0", 2)], writes=["dbg_xe"])
                    P.dma(lambda e: e.dma_start(out=dbg_ip_d, in_=iP[:]), reads=["iP"], writes=["dbg_ip"])
                for s in range(3):
                    n = 128 if s < 2 else CAP_C
                    pb = 4 + (s % 2)
                    pv = PS[pb][:].bitcast(BF16)
                    for k in range(8):
                        P.pe(lambda e, b=b, s=s, n=n, k=k, pv=pv: e.transpose(
                            pv[:, k * 128:k * 128 + n], xe[b][0:n, s, k * 128:(k + 1) * 128], ident_b[0:n, 0:n]),
                            reads=[(kxe, s), "ident_b"], writes=[PK[pb]])
                    P.act(lambda e, b=b, s=s, n=n, pv=pv: e.activation(
                        out=xeT[b][:, :, s * 128:s * 128 + n],
                        in_=pv.rearrange("p (k t) -> p k t", k=8)[:, :, 0:n], func=AF.Copy),
                        reads=[PK[pb]], writes=[kxT])
                for f in range(8):
                    pa, pu = 0 + (f % 2) * 2, 1 + (f % 2) * 2
                    for k in range(8):
                        P.pe(lambda e, b=b, f=f, k=k, pa=pa, wg=wg: e.matmul(
                            PS[pa][:, 0:NSLOT], WB[wg][:, k, f * 128:(f + 1) * 128], xeT[b][:, k, :],
                            start=(k == 0), stop=(k == 7)), reads=[("WB", wg, k), kxT], writes=[PK[pa]])
                    for k in range(8):
                        P.pe(lambda e, b=b, f=f, k=k, pu=pu, wu=wu: e.matmul(
                            PS[pu][:, 0:NSLOT], WB[wu][:, k, f * 128:(f + 1) * 128], xeT[b][:, k, :],
                            start=(k == 0), stop=(k == 7)), reads=[("WB", wu, k), kxT], writes=[PK[pu]])
                    P.act(lambda e, b=b, pa=pa: e.activation(out=sg[b][:], in_=PS[pa][:, 0:NSLOT], func=AF.Silu),
                          reads=[PK[pa]], writes=[ksg])
                    P.dve(lambda e, b=b, f=f, pu=pu: e.tensor_tensor(out=hT[b][:, f, :], in0=sg[b][:],
                                                                      in1=PS[pu][:, 0:NSLOT], op=ALU.mult),
                          reads=[ksg, PK[pu]], writes=[khT])
                for s in range(3):
                    n = 128 if s < 2 else CAP_C
                    for hf in range(2):
                        pb = 6 + hf
                        for k in range(8):
                            P.pe(lambda e, b=b, s=s, n=n, hf=hf, k=k, pb=pb, wd=wd: e.matmul(
                                PS[pb][0:n, :], hT[b][:, k, s * 128:s * 128 + n], WB[wd][:, k, hf * 512:(hf + 1) * 512],
                                start=(k == 0), stop=(k == 7)), reads=[khT, ("WB", wd, k)], writes=[PK[pb]])
                        P.dve(lambda e, b=b, s=s, n=n, hf=hf, pb=pb, ex_i=ex_i: e.tensor_scalar(
                            out=yb[b][0:n, s, hf * 512:(hf + 1) * 512], in0=PS[pb][0:n, :],
                            scalar1=gP[0:n, s, ex_i:ex_i + 1], scalar2=None, op0=ALU.mult),
                            reads=[PK[pb], "gP"], writes=[(kyb, s)])
                    if dbg and last and ex_i == 0 and s == 2:
                        P.dma(lambda e: e.dma_start(out=dbg_yb_d, in_=yb[0][:]), reads=[("yb0", 0), ("yb0", 1), ("yb0", 2)], writes=["dbg_yb"])
                        P.dma(lambda e: e.dma_start(out=dbg_ht_d, in_=hT[0][:]), reads=["hT0"], writes=["dbg_ht"])
                    P.dma(lambda e, b=b, s=s, n=n, ex_i=ex_i: e.indirect_dma_start(
                        out=M_d, out_offset=bass.IndirectOffsetOnAxis(ap=iP[0:n, s, ex_i:ex_i + 1], axis=0),
                        in_=yb[b][0:n, s, :], in_offset=None, compute_op=ALU.add),
                        reads=[(kyb, s), "iP"], writes=["Macc"], q="pool")
            G = [sb(es, "mG%d" % r, [128, D]) for r in range(2)]
            for r in range(2):
                load_mod(G[r], "mG%d" % r, r, 5)
            xt = [sb(es, "mx%d" % i, [128, D]) for i in range(2)]
            mt = [sb(es, "mm%d" % i, [128, D]) for i in range(2)]
            outs = []
            for tt in range(NTILE):
                b = tt % 2
                r = 1 if tt < 2 else 0
                kx, km = "mx%d" % b, "mm%d" % b
                P.dma(lambda e, b=b, tt=tt: e.dma_start(out=xt[b][:], in_=H_d[tt * 128:(tt + 1) * 128, :]),
                      reads=[hkey(tt)], writes=[kx])
                P.dma(lambda e, b=b, tt=tt: e.dma_start(out=mt[b][:], in_=M_d[tt * 128:(tt + 1) * 128, :]),
                      reads=["Macc"], writes=[km])
                P.dve(lambda e, b=b, r=r: e.tensor_tensor(out=mt[b][:], in0=mt[b][:], in1=G[r][:], op=ALU.mult),
                      reads=[km, "mG%d" % r], writes=[km])
                P.pool(lambda e, b=b: e.tensor_tensor(out=xt[b][:], in0=xt[b][:], in1=mt[b][:], op=ALU.add),
                       reads=[kx, km], writes=[kx])
                if dbg and last:
                    P.dma(lambda e, b=b, tt=tt: e.dma_start(out=dbg_m_d[tt * 128:(tt + 1) * 128, :], in_=mt[b][:]),
                          reads=[km], writes=[("dbg_m", tt)])
                if last:
                    if tt >= 2:
                        outs.append(P.dma(lambda e, b=b, tt=tt: e.dma_start(
                            out=out_d[(tt - 2) * 128:(tt - 1) * 128, :], in_=xt[b][:]), reads=[kx], writes=[("out", tt)]))
                    if dbg:
                        outs.append(P.dma(lambda e, b=b, tt=tt: e.dma_start(
                            out=hdbg_d[tt * 128:(tt + 1) * 128, :], in_=xt[b][:]), reads=[kx], writes=[("hdbg", tt)]))
                else:
                    P.dma(lambda e, b=b, tt=tt: e.dma_start(out=H_d[tt * 128:(tt + 1) * 128, :], in_=xt[b][:]),
                          reads=[kx], writes=[hkey(tt)])
        P.barrier()
        return outs

    finals = []
    for l in range(n_layers):
        last = (l == n_layers - 1)
        phase_mod(l)
        with ExitStack() as es:
            U = sb(es, "U", [128, 8, TT], BF16)
            Y = sb(es, "Y", [128, 8, TT], BF16)
            phase_norm(l, l, 1, U)
            if l % 2 == 0:
                phase_conv(l, U, Y)
                phase_outproj(l, Y, w_out_ab_d[l // 2])
            else:
                phase_attn(l, U, Y)
                phase_hyena(l, U, Y)
                phase_outproj(l, Y, w_out_cd_d[l // 2])
        with ExitStack() as es:
            AFF = sb(es, "AFF", [128, NTILE, NE])
            phase_norm(l, 1, 2, None, AFF=AFF)
            finals = phase_moe(l, AFF, last)
    P.finalize(finals)
    top.close()
    return nc


def _host_inputs(inp):
    f = lambda a: np.ascontiguousarray(np.asarray(a, dtype=np.float32))
    sh = {}
    sh["w_ada"] = f(inp["w_ada"])
    sh["b_ada"] = f(inp["b_ada"])
    sh["g_mix"] = f(inp["g_mix"])
    sh["g_ffn"] = f(inp["g_ffn"])
    sh["w_in_ab"] = f(inp["w_in_ab"])
    sh["ca"] = f(np.asarray(inp["conv_a"]).reshape(2, 3, 4, 128).transpose(0, 3, 2, 1))
    sh["cb"] = f(np.asarray(inp["conv_b"]).reshape(2, 31, 4, 128).transpose(0, 3, 2, 1))
    lnp = np.stack([np.asarray(inp["conv_b_bias"]), np.asarray(inp["ln_b_g"]), np.asarray(inp["ln_b_b"])], axis=1)
    sh["lnp"] = f(lnp.reshape(2, 3, 4, 128).transpose(0, 3, 1, 2))
    sh["w_out_ab"] = f(inp["w_out_ab"])
    sh["w_in_cd"] = f(inp["w_in_cd"])
    sh["w_out_cd"] = f(inp["w_out_cd"])
    gq = np.asarray(inp["g_q"]); gk = np.asarray(inp["g_k"])
    sh["gqk"] = f(np.stack([np.tile(gq, (1, 2)), np.tile(gk, (1, 2))], axis=-1))
    sh["lamv"] = f(np.concatenate([np.asarray(inp[k]) for k in ("lam_q1", "lam_k1", "lam_q2", "lam_k2")], axis=1))
    sh["gsub"] = f(np.asarray(inp["g_subln"]).reshape(2, 128, 1))
    sh["cd"] = f(np.asarray(inp["conv_d"]).reshape(2, 3, 12, 128).transpose(0, 3, 2, 1))
    sh["hf_w1"] = f(inp["hf_w1"])
    sh["hfv"] = f(np.stack([np.asarray(inp["hf_b1"]), np.asarray(inp["hf_freq"]), np.asarray(inp["hf_b2"])], axis=-1))
    sh["hf_w2"] = f(inp["hf_w2"])
    sh["hf_w3"] = f(inp["hf_w3"])
    sh["hf_bias"] = f(inp["hf_bias"])
    sh["wr"] = f(np.asarray(inp["w_router"]).reshape(DEPTH, 8, 128, NE).transpose(0, 2, 1, 3))
    sh["w_gate"] = f(inp["w_gate"])
    sh["w_up"] = f(inp["w_up"])
    sh["w_down"] = f(inp["w_down"])
    sh.update(host_constants())
    return sh


def _core_inputs(inp, shared, b):
    m = dict(shared)
    m["x"] = np.ascontiguousarray(np.asarray(inp["x"][b], dtype=np.float32))
    m["ctx"] = np.ascontiguousarray(np.asarray(inp["ctx"][b], dtype=np.float32))
    cv = np.stack([np.asarray(inp["c"][b]), np.asarray(inp["c_ctx"])], axis=0)
    m["cT"] = np.ascontiguousarray(cv.reshape(2, 8, 128).transpose(2, 1, 0).astype(np.float32))
    return m


_NC_CACHE = {}


def kernel(**inputs):
    n = 8
    if "nc" not in _NC_CACHE:
        _NC_CACHE["nc"] = build(DEPTH)
    nc = _NC_CACHE["nc"]
    shared = _host_inputs(inputs)
    in_maps = [_core_inputs(inputs, shared, b) for b in range(n)]
    res = run_bass_kernel_spmd(nc, in_maps, core_ids=list(range(n)))
    return np.stack([np.asarray(r["out"]) for r in res.results], axis=0).astype(np.float32)
```

```python
import math
from contextlib import ExitStack

import numpy as np
import concourse.bass as bass
import concourse.mybir as mybir
from concourse.bass_utils import run_bass_kernel_spmd

F32 = mybir.dt.float32
BF16 = mybir.dt.bfloat16
I32 = mybir.dt.int32
U32 = mybir.dt.uint32
AF = mybir.ActivationFunctionType
ALU = mybir.AluOpType
AX = mybir.AxisListType

D = 1024
T = 2048
TC = 256
TT = T + TC
NTILE = TT // 128
DEPTH = 4
NE = 16
CAP_L = 256
CAP_C = 32
NSLOT = CAP_L + CAP_C
EPS = 1e-6

ENGS = ("pe", "dve", "act", "pool", "sp")
DMA_SLOTS = {"sp": 24, "act": 8, "pool": 24}


class Op:
    __slots__ = ("eng", "fn", "deps", "dma", "needs_inc", "sem", "val", "slot", "prev_slot_op", "seq")

    def __init__(self, eng, fn, dma):
        self.eng = eng
        self.fn = fn
        self.dma = dma
        self.deps = []
        self.needs_inc = False
        self.sem = None
        self.val = None
        self.slot = None
        self.prev_slot_op = None


class Prog:
    def __init__(self, nc):
        self.nc = nc
        self.ops = {e: [] for e in ENGS}
        self.last_w = {}
        self.readers = {}
        self.dma_rr = {q: 0 for q in DMA_SLOTS}
        self.slot_last = {}

    def _add(self, eng, fn, reads, writes, dma):
        op = Op(eng, fn, dma)
        deps = []
        for k in reads:
            w = self.last_w.get(k)
            if w is not None:
                deps.append(w)
        for k in writes:
            w = self.last_w.get(k)
            if w is not None:
                deps.append(w)
            deps.extend(self.readers.get(k, ()))
        seen = set()
        latest = {}
        for d in deps:
            if id(d) in seen:
                continue
            seen.add(id(d))
            if (not dma) and (not d.dma) and d.eng == eng and eng == "pe":
                continue
            if d.dma:
                op.deps.append(d)
                d.needs_inc = True
            else:
                cur = latest.get(d.eng)
                if cur is None or d.seq > cur.seq:
                    latest[d.eng] = d
        for d in latest.values():
            op.deps.append(d)
            d.needs_inc = True
        if dma:
            k = self.dma_rr[eng]
            self.dma_rr[eng] = (k + 1) % DMA_SLOTS[eng]
            op.slot = (eng, k)
            op.prev_slot_op = self.slot_last.get(op.slot)
            self.slot_last[op.slot] = op
        for k in reads:
            self.readers.setdefault(k, []).append(op)
        for k in writes:
            self.last_w[k] = op
            self.readers[k] = []
        op.seq = len(self.ops[eng])
        self.ops[eng].append(op)
        return op

    def pe(self, fn, reads=(), writes=()):
        return self._add("pe", fn, reads, writes, False)

    def dve(self, fn, reads=(), writes=()):
        return self._add("dve", fn, reads, writes, False)

    def act(self, fn, reads=(), writes=()):
        return self._add("act", fn, reads, writes, False)

    def pool(self, fn, reads=(), writes=()):
        return self._add("pool", fn, reads, writes, False)

    def dma(self, fn, reads=(), writes=(), q="sp"):
        return self._add(q, fn, reads, writes, True)

    def barrier(self):
        lasts = []
        for e in ENGS:
            for op in reversed(self.ops[e]):
                if not op.dma:
                    lasts.append(op)
                    break
        lasts.extend(self.slot_last.values())
        for e in ENGS:
            op = Op(e, lambda eng: eng.nop(), False)
            for d in lasts:
                if d.eng == e and not d.dma and e == "pe":
                    continue
                op.deps.append(d)
                d.needs_inc = True
            op.seq = len(self.ops[e])
            self.ops[e].append(op)
        self.last_w = {}
        self.readers = {}

    def finalize(self, final_ops=()):
        nc = self.nc
        with ExitStack() as es:
            esem = {e: es.enter_context(nc.semaphore("s_" + e)) for e in ("pe", "dve", "act", "pool", "sp")}
            ssem = {}
            for q, n in DMA_SLOTS.items():
                for k in range(n):
                    ssem[(q, k)] = es.enter_context(nc.semaphore("d_%s%d" % (q, k)))
            for e in ENGS:
                cnt = 0
                for op in self.ops[e]:
                    if (not op.dma) and op.needs_inc:
                        cnt += 1
                        op.sem = esem[e]
                        op.val = cnt
            slot_cnt = {}
            for e in ENGS:
                for op in self.ops[e]:
                    if op.dma:
                        c = slot_cnt.get(op.slot, 0) + 16
                        slot_cnt[op.slot] = c
                        op.sem = ssem[op.slot]
                        op.val = c
            block = es.enter_context(nc.Block())
            finals = list(final_ops)

            def emit(e, engobj):
                waited = {}

                def w(s, v):
                    if waited.get(id(s), 0) >= v:
                        return
                    waited[id(s)] = v
                    engobj.wait_ge(s, v)

                for op in self.ops[e]:
                    for d in op.deps:
                        w(d.sem, d.val)
                    if op.dma and op.prev_slot_op is not None:
                        w(op.prev_slot_op.sem, op.prev_slot_op.val)
                    ins = op.fn(engobj)
                    if op.dma:
                        ins.then_inc(op.sem, 16)
                    elif op.needs_inc:
                        ins.then_inc(op.sem, 1)
                if e == "sp":
                    for f in finals:
                        w(f.sem, f.val)

            @block.tensor
            def _(eng):
                emit("pe", eng)

            @block.vector
            def _(eng):
                emit("dve", eng)

            @block.scalar
            def _(eng):
                emit("act", eng)

            @block.gpsimd
            def _(eng):
                emit("pool", eng)

            @block.sync
            def _(eng):
                emit("sp", eng)


def _tok_tiles():
    return [(0, TC)] + [(TC + i * 512, 512) for i in range(T // 512)]


SEQS = ((0, TC), (TC, T))


def _bf(a):
    import ml_dtypes
    return np.ascontiguousarray(np.asarray(a, dtype=np.float32).astype(ml_dtypes.bfloat16))


def _dft_tables(L):
    N = 3 * L // 2
    nf = N // 2
    npair = (nf + 127) // 128
    f = np.arange(npair * 128, dtype=np.float64)
    t = np.arange(L, dtype=np.float64)
    th = 2.0 * np.pi * np.outer(t, f + 0.5) / N
    valid = (f < nf)[None, :]
    fc = np.where(valid, np.cos(th), 0.0)
    fs = np.where(valid, -np.sin(th), 0.0)
    nt = L // 128
    fw = np.zeros((npair, 128, nt, 256))
    for i in range(npair):
        blkc = fc[:, i * 128:(i + 1) * 128].reshape(nt, 128, 128).transpose(1, 0, 2)
        blks = fs[:, i * 128:(i + 1) * 128].reshape(nt, 128, 128).transpose(1, 0, 2)
        fw[i, :, :, 0:128] = blkc
        fw[i, :, :, 128:256] = blks
    thi = 2.0 * np.pi * np.outer(f + 0.5, t + L // 2) / N
    validf = (f < nf)[:, None]
    ic = np.where(validf, (2.0 / N) * np.cos(thi), 0.0)
    isn = np.where(validf, -(2.0 / N) * np.sin(thi), 0.0)
    inv = np.stack([ic.reshape(npair, 128, L).transpose(1, 0, 2), isn.reshape(npair, 128, L).transpose(1, 0, 2)], 0)
    return fw, inv, npair


def _filter_tables(L):
    t = np.arange(L, dtype=np.float64)
    t_unit = np.linspace(0.0, 1.0, L)
    bands = np.linspace(1e-4, 16 - 1, 16)
    ang = (2.0 * np.pi / L) * t[:, None] * bands[None]
    z = np.concatenate([t_unit[:, None], np.cos(ang), -np.sin(ang)], axis=-1)
    centre = L // 2
    dist = np.abs(t - centre) / max(centre, 1)
    mn = math.log(1e-2) / 1.5
    mx = math.log(1e-2) / 0.3
    decay = np.abs(np.linspace(mn, mx, 512))
    win = np.exp(-dist[:, None] * decay[None])
    return z.T, win


def host_constants():
    c = {}
    c["ident"] = np.eye(128, dtype=np.float32)
    tt = np.arange(T)
    row = (tt // 64).astype(np.float64)
    col = (tt % 64).astype(np.float64)
    inv = 10000.0 ** (-np.arange(16, dtype=np.float64) / 16)
    cos = np.zeros((128, T))
    sin = np.zeros((128, T))
    for p in range(128):
        d = p % 64
        pos = row if d < 32 else col
        a = pos * inv[d % 16]
        cos[p] = np.cos(a)
        sin[p] = np.sin(a)
    c["ropec"] = _bf(cos)
    c["ropes"] = _bf(sin)
    rt = np.zeros((128, 128))
    for m in range(128):
        if m % 32 < 16:
            rt[m + 16, m] = -1.0
        else:
            rt[m - 16, m] = 1.0
    c["ropert"] = _bf(rt)
    bo = np.zeros((128, 128), dtype=np.float32)
    bo[0:64, 0:64] = 1.0
    bo[64:128, 64:128] = 1.0
    c["blockones"] = bo
    zl, wl = _filter_tables(T)
    zc, wc = _filter_tables(TC)
    c["zT"] = np.ascontiguousarray(np.concatenate([zc, zl], axis=1).astype(np.float32))
    c["win"] = np.ascontiguousarray(np.concatenate([wc, wl], axis=0).astype(np.float32))
    fwl, invl, _ = _dft_tables(T)
    fwc, invc, _ = _dft_tables(TC)
    c["fwl"] = _bf(fwl)
    c["fwc"] = _bf(fwc)
    il = invl.reshape(2, 128, 12, 4, 512).transpose(3, 0, 1, 2, 4)
    c["invl"] = _bf(il)
    c["invc"] = _bf(invc)
    return c


def build(n_layers=DEPTH, dbg=False):
    nc = bass.Bass("TRN2", target_bir_lowering=False)

    def din(name, shape, dt=F32):
        return nc.dram_tensor(name, list(shape), dt, kind="ExternalInput").ap()

    def dint(name, shape, dt=F32):
        return nc.dram_tensor(name, list(shape), dt, kind="Internal").ap()

    x_d = din("x", [T, D])
    ctx_d = din("ctx", [TC, D])
    cT_d = din("cT", [128, 8, 2])
    w_ada_d = din("w_ada", [DEPTH, D, 6 * D])
    b_ada_d = din("b_ada", [DEPTH, 6 * D])
    g_mix_d = din("g_mix", [DEPTH, D])
    g_ffn_d = din("g_ffn", [DEPTH, D])
    w_in_ab_d = din("w_in_ab", [2, D, 2560])
    ca_d = din("ca", [2, 128, 4, 3])
    cb_d = din("cb", [2, 128, 4, 31])
    lnp_d = din("lnp", [2, 128, 3, 4])
    w_out_ab_d = din("w_out_ab", [2, D, D])
    wr_d = din("wr", [DEPTH, 128, 8, NE])
    w_gate_d = din("w_gate", [DEPTH, NE, D, D])
    w_up_d = din("w_up", [DEPTH, NE, D, D])
    w_down_d = din("w_down", [DEPTH, NE, D, D])
    ident_d = din("ident", [128, 128])
    w_in_cd_d = din("w_in_cd", [2, D, 3072])
    w_out_cd_d = din("w_out_cd", [2, D, D])
    gqk_d = din("gqk", [2, 128, 2])
    lamv_d = din("lamv", [2, 4 * 64])
    gsub_d = din("gsub", [2, 128, 1])
    cd_d = din("cd", [2, 128, 12, 3])
    hfw1_d = din("hf_w1", [2, 33, 64])
    hfv_d = din("hfv", [2, 64, 3])
    hfw2_d = din("hf_w2", [2, 64, 64])
    hfw3_d = din("hf_w3", [2, 64, 512])
    hfb_d = din("hf_bias", [2, 512])
    ropec_d = din("ropec", [128, T], BF16)
    ropes_d = din("ropes", [128, T], BF16)
    ropert_d = din("ropert", [128, 128], BF16)
    blockones_d = din("blockones", [128, 128])
    zT_d = din("zT", [33, TT])
    win_d = din("win", [TT, 512])
    fwl_d = din("fwl", [12, 128, 16, 256], BF16)
    fwc_d = din("fwc", [2, 128, 2, 256], BF16)
    invl_d = din("invl", [4, 2, 128, 12, 512], BF16)
    invc_d = din("invc", [2, 128, 2, 256], BF16)
    out_d = nc.dram_tensor("out", [T, D], F32, kind="ExternalOutput").ap()

    H_d = dint("H", [TT, D])
    modv_d = dint("modv", [2, 6 * D])
    u2tok_d = dint("u2tok", [TT, D], BF16)
    M_d = dint("Macc", [TT, D])
    if dbg:
        hdbg_d = nc.dram_tensor("hdbg", [TT, D], F32, kind="ExternalOutput").ap()
        dbg_ix_d = nc.dram_tensor("dbg_ix", [NE, NSLOT], F32, kind="ExternalOutput").ap()
        dbg_gv_d = nc.dram_tensor("dbg_gv", [NE, NSLOT], F32, kind="ExternalOutput").ap()
        dbg_m_d = nc.dram_tensor("dbg_m", [TT, D], F32, kind="ExternalOutput").ap()
        dbg_xe_d = nc.dram_tensor("dbg_xe", [128, 3, D], BF16, kind="ExternalOutput").ap()
        dbg_yb_d = nc.dram_tensor("dbg_yb", [128, 3, D], F32, kind="ExternalOutput").ap()
        dbg_ht_d = nc.dram_tensor("dbg_ht", [128, 8, NSLOT], BF16, kind="ExternalOutput").ap()
        dbg_ip_d = nc.dram_tensor("dbg_ip", [128, 3, NE], I32, kind="ExternalOutput").ap()

    top = ExitStack()
    P = Prog(nc)

    uid = [0]

    def sb(es, name, shape, dt=F32):
        uid[0] += 1
        return es.enter_context(nc.sbuf_tensor("%s_%d" % (name, uid[0]), list(shape), dt))

    PSD = [top.enter_context(nc.psum_tensor("psd%d" % i, [128, 1024], F32)) for i in range(4)]
    PS = [PSD[i // 2][:, (i % 2) * 512:(i % 2 + 1) * 512] for i in range(8)]
    PK = ["ps%d" % i for i in range(8)]

    ident_f = sb(top, "ident_f", [128, 128])
    ident_b = sb(top, "ident_b", [128, 128], BF16)
    ones_f = sb(top, "ones_f", [128, 128])
    ones_b = sb(top, "ones_b", [128, 128], BF16)
    zero_f = sb(top, "zero_f", [128, 1024])
    P.dma(lambda e: e.dma_start(out=ident_f[:], in_=ident_d), writes=["ident_f"])
    P.dve(lambda e: e.tensor_copy(ident_b[:], ident_f[:]), reads=["ident_f"], writes=["ident_b"])
    P.dve(lambda e: e.memset(ones_f[:], 1.0), writes=["ones_f"])
    P.dve(lambda e: e.memset(ones_b[:], 1.0), writes=["ones_b"])
    P.dve(lambda e: e.memset(zero_f[:], 0.0), writes=["zero_f"])

    def hsrc(l, tt):
        if l == 0:
            if tt < 2:
                return ctx_d[tt * 128:(tt + 1) * 128, :]
            return x_d[(tt - 2) * 128:(tt - 1) * 128, :]
        return H_d[tt * 128:(tt + 1) * 128, :]

    def hkey(tt):
        return ("H", tt)

    def phase_mod(l):
        with ExitStack() as es:
            cS = sb(es, "cS", [128, 8, 2])
            modr = sb(es, "modr", [2, 6 * D])
            brow = sb(es, "brow", [2, 6 * D])
            grow = sb(es, "grow", [2, 2, D])
            wbuf = [sb(es, "wada%d" % i, [128, 8, 512]) for i in range(2)]
            P.dma(lambda e: e.dma_start(out=cS[:], in_=cT_d), writes=["cS"])
            P.act(lambda e: e.activation(out=cS[:], in_=cS[:], func=AF.Silu), reads=["cS"], writes=["cS"])
            for r in range(2):
                P.dma(lambda e, r=r: e.dma_start(out=brow[r:r + 1, :], in_=b_ada_d[l:l + 1, :]), writes=["brow"])
                P.dma(lambda e, r=r: e.dma_start(out=grow[r:r + 1, 0, :], in_=g_mix_d[l:l + 1, :]), writes=["grow"])
                P.dma(lambda e, r=r: e.dma_start(out=grow[r:r + 1, 1, :], in_=g_ffn_d[l:l + 1, :]), writes=["grow"])
            wv = w_ada_d[l].rearrange("(k p) n -> p k n", p=128)
            for nt in range(12):
                wb = wbuf[nt % 2]
                wk = "wada%d" % (nt % 2)
                P.dma(lambda e, wb=wb, nt=nt: e.dma_start(out=wb[:], in_=wv[:, :, nt * 512:(nt + 1) * 512]),
                      writes=[wk])
                pk = nt % 2
                for k in range(8):
                    P.pe(lambda e, wb=wb, k=k, pk=pk: e.matmul(PS[pk][0:2, :], cS[:, k, :], wb[:, k, :],
                                                                  start=(k == 0), stop=(k == 7)),
                         reads=[wk, "cS"], writes=[PK[pk]])
                P.dve(lambda e, nt=nt, pk=pk: e.tensor_tensor(out=modr[:, nt * 512:(nt + 1) * 512], in0=PS[pk][0:2, :],
                                                               in1=brow[:, nt * 512:(nt + 1) * 512], op=ALU.add),
                      reads=[PK[pk], "brow"], writes=["modr"])
            for (seg, gi) in ((1, 0), (4, 1)):
                P.dve(lambda e, seg=seg, gi=gi: e.scalar_tensor_tensor(
                    out=modr[:, seg * D:(seg + 1) * D], in0=modr[:, seg * D:(seg + 1) * D], scalar=1.0,
                    in1=grow[:, gi, :], op0=ALU.add, op1=ALU.mult), reads=["modr", "grow"], writes=["modr"])
            P.dma(lambda e: e.dma_start(out=modv_d, in_=modr[:]), reads=["modr"], writes=["modv"])
        P.barrier()

    def load_mod(tile, key, r, seg):
        P.dma(lambda e: e.dma_start(out=tile[:], in_=modv_d[r:r + 1, seg * D:(seg + 1) * D].partition_broadcast(128)),
              reads=["modv"], writes=[key])

    def phase_norm(l, hl, which, U, AFF=None):
        seg_s, seg_a = (0, 1) if which == 1 else (3, 4)
        with ExitStack() as es:
            A = [sb(es, "nA%d" % r, [128, D]) for r in range(2)]
            S = [sb(es, "nS%d" % r, [128, D]) for r in range(2)]
            for r in range(2):
                load_mod(A[r], "nA%d" % r, r, seg_a)
                load_mod(S[r], "nS%d" % r, r, seg_s)
            xt = [sb(es, "nx%d" % i, [128, D]) for i in range(2)]
            junk = sb(es, "njunk", [128, D], BF16)
            ss = [sb(es, "nss%d" % i, [128, 2]) for i in range(2)]
            tmp = [sb(es, "ntmp%d" % i, [128, D]) for i in range(2)]
            if which == 1:
                ub = [sb(es, "nub%d" % i, [128, D], BF16) for i in range(2)]
            else:
                ub = [sb(es, "nub%d" % i, [128, D], BF16) for i in range(2)]
                uT = [sb(es, "nuT%d" % i, [128, 8, 128]) for i in range(2)]
                wr = sb(es, "nwr", [128, 8, NE])
                sm = [sb(es, "nsm%d" % i, [128, 4]) for i in range(2)]
                ex = [sb(es, "nex%d" % i, [128, NE]) for i in range(2)]
                P.dma(lambda e: e.dma_start(out=wr[:], in_=wr_d[l]), writes=["nwr"])
            for tt in range(NTILE):
                b = tt % 2
                r = 1 if tt < 2 else 0
                kx, kss, ktmp, kub = "nx%d" % b, "nss%d" % b, "ntmp%d" % b, "nub%d" % b
                P.dma(lambda e, b=b, tt=tt: e.dma_start(out=xt[b][:], in_=hsrc(hl, tt)), reads=[hkey(tt)], writes=[kx])
                P.act(lambda e, b=b: e.activation(out=junk[:], in_=xt[b][:], func=AF.Square, accum_out=ss[b][:, 0:1]),
                      reads=[kx], writes=["njunk", kss])
                P.act(lambda e, b=b: e.activation(out=ss[b][:, 1:2], in_=ss[b][:, 0:1], func=AF.Sqrt,
                                                  scale=1.0 / D, bias=EPS), reads=[kss], writes=[kss])
                P.dve(lambda e, b=b: e.reciprocal(ss[b][:, 1:2], ss[b][:, 1:2]), reads=[kss], writes=[kss])
                P.dve(lambda e, b=b, r=r: e.scalar_tensor_tensor(out=tmp[b][:], in0=xt[b][:], scalar=ss[b][:, 1:2],
                                                                   in1=A[r][:], op0=ALU.mult, op1=ALU.mult),
                      reads=[kx, kss, "nA%d" % r], writes=[ktmp])
                if which == 1:
                    P.pool(lambda e, b=b, r=r: e.tensor_tensor(out=ub[b][:], in0=tmp[b][:], in1=S[r][:], op=ALU.add),
                           reads=[ktmp, "nS%d" % r], writes=[kub])
                    psb = 2 + b
                    pv = PS[psb][:].bitcast(BF16)
                    for k in range(8):
                        P.pe(lambda e, b=b, k=k, pv=pv: e.transpose(pv[:, k * 128:(k + 1) * 128],
                                                                     ub[b][:, k * 128:(k + 1) * 128], ident_b[:]),
                             reads=[kub, "ident_b"], writes=[PK[psb]])
                    P.act(lambda e, tt=tt, pv=pv: e.activation(
                        out=U[:, :, tt * 128:(tt + 1) * 128], in_=pv.rearrange("p (k t) -> p k t", k=8), func=AF.Copy),
                        reads=[PK[psb]], writes=[("U", tt)])
                else:
                    P.pool(lambda e, b=b, r=r: e.tensor_tensor(out=tmp[b][:], in0=tmp[b][:], in1=S[r][:], op=ALU.add),
                           reads=[ktmp, "nS%d" % r], writes=[ktmp])
                    P.act(lambda e, b=b: e.activation(out=ub[b][:], in_=tmp[b][:], func=AF.Copy),
                          reads=[ktmp], writes=[kub])
                    P.dma(lambda e, b=b, tt=tt: e.dma_start(out=u2tok_d[tt * 128:(tt + 1) * 128, :], in_=ub[b][:]),
                          reads=[kub], writes=[("u2tok", tt)])
                    kuT = "nuT%d" % b
                    for hh in range(2):
                        psb = 2 + 2 * b + hh
                        for k4 in range(4):
                            k = hh * 4 + k4
                            P.pe(lambda e, b=b, k=k, k4=k4, psb=psb: e.transpose(
                                PS[psb][:, k4 * 128:(k4 + 1) * 128], tmp[b][:, k * 128:(k + 1) * 128], ident_f[:]),
                                reads=[ktmp, "ident_f"], writes=[PK[psb]])
                        P.dve(lambda e, b=b, hh=hh, psb=psb: e.tensor_copy(
                            uT[b][:, hh * 4:(hh + 1) * 4, :], PS[psb][:].rearrange("p (k t) -> p k t", k=4)),
                            reads=[PK[psb]], writes=[kuT])
                    pl = 6 + b
                    for k in range(8):
                        P.pe(lambda e, b=b, k=k, pl=pl: e.matmul(PS[pl][:, 0:NE], uT[b][:, k, :], wr[:, k, :],
                                                                   start=(k == 0), stop=(k == 7)),
                             reads=[kuT, "nwr"], writes=[PK[pl]])
                    ksm, kex = "nsm%d" % b, "nex%d" % b
                    P.dve(lambda e, b=b, pl=pl: e.reduce_max(out=sm[b][:, 0:1], in_=PS[pl][:, 0:NE], axis=AX.X),
                          reads=[PK[pl]], writes=[ksm])
                    P.dve(lambda e, b=b: e.tensor_scalar(out=sm[b][:, 1:2], in0=sm[b][:, 0:1], scalar1=-1.0,
                                                         scalar2=None, op0=ALU.mult), reads=[ksm], writes=[ksm])
                    P.act(lambda e, b=b, pl=pl: e.activation(out=ex[b][:], in_=PS[pl][:, 0:NE], func=AF.Exp,
                                                             bias=sm[b][:, 1:2], scale=1.0, accum_out=sm[b][:, 2:3]),
                          reads=[PK[pl], ksm], writes=[kex, ksm])
                    P.dve(lambda e, b=b: e.reciprocal(sm[b][:, 3:4], sm[b][:, 2:3]), reads=[ksm], writes=[ksm])
                    P.dve(lambda e, b=b, tt=tt: e.tensor_scalar(out=AFF[:, tt, :], in0=ex[b][:], scalar1=sm[b][:, 3:4],
                                                                scalar2=None, op0=ALU.mult),
                          reads=[kex, ksm], writes=[("AFF", tt)])
        P.barrier()

    def phase_outproj(l, Y, w_out_ap, last_layer_unused=False):
        with ExitStack() as es:
            Wo = sb(es, "Wo", [128, 8, D], BF16)
            G = [sb(es, "oG%d" % r, [128, D]) for r in range(2)]
            xt = [sb(es, "ox%d" % i, [128, D]) for i in range(2)]
            tm = [sb(es, "ot%d" % i, [128, D]) for i in range(2)]
            wv = w_out_ap.rearrange("(k p) n -> p k n", p=128)
            for k in range(8):
                P.dma(lambda e, k=k: e.dma_start(out=Wo[:, k, :], in_=wv[:, k, :]), writes=[("Wo", k)], q="pool")
            for r in range(2):
                load_mod(G[r], "oG%d" % r, r, 2)
            for tt in range(NTILE):
                b = tt % 2
                r = 1 if tt < 2 else 0
                kx, kt = "ox%d" % b, "ot%d" % b
                P.dma(lambda e, b=b, tt=tt: e.dma_start(out=xt[b][:], in_=hsrc(l, tt)), reads=[hkey(tt)], writes=[kx])
                for hf in range(2):
                    pb = 2 * b + hf
                    for k in range(8):
                        P.pe(lambda e, k=k, tt=tt, hf=hf, pb=pb: e.matmul(
                            PS[pb][:], Y[:, k, tt * 128:(tt + 1) * 128], Wo[:, k, hf * 512:(hf + 1) * 512],
                            start=(k == 0), stop=(k == 7)), reads=[("Y", tt), ("Wo", k)], writes=[PK[pb]])
                    P.dve(lambda e, b=b, hf=hf, pb=pb, r=r: e.tensor_tensor(
                        out=tm[b][:, hf * 512:(hf + 1) * 512], in0=PS[pb][:], in1=G[r][:, hf * 512:(hf + 1) * 512],
                        op=ALU.mult), reads=[PK[pb], "oG%d" % r], writes=[kt])
                P.pool(lambda e, b=b: e.tensor_tensor(out=xt[b][:], in0=xt[b][:], in1=tm[b][:], op=ALU.add),
                       reads=[kx, kt], writes=[kx])
                P.dma(lambda e, b=b, tt=tt: e.dma_start(out=H_d[tt * 128:(tt + 1) * 128, :], in_=xt[b][:]),
                      reads=[kx], writes=[hkey(tt)])
        P.barrier()

    def phase_conv(l, U, Y):
        i2 = l // 2
        tiles = _tok_tiles()
        with ExitStack() as es:
            Wg = [sb(es, "Wg%d" % g, [128, 8, 512], BF16) for g in range(5)]
            wv = w_in_ab_d[i2].rearrange("(k p) n -> p k n", p=128)
            for g in range(5):
                for k in range(8):
                    P.dma(lambda e, g=g, k=k: e.dma_start(out=Wg[g][:, k, :], in_=wv[:, k, g * 512:(g + 1) * 512]),
                          writes=[("Wg", g)], q="pool")
            CA = sb(es, "CA", [128, 4, 3])
            CB = sb(es, "CB", [128, 4, 31])
            LNP = sb(es, "LNP", [128, 3, 4])
            P.dma(lambda e: e.dma_start(out=CA[:], in_=ca_d[i2]), writes=["CA"])
            P.dma(lambda e: e.dma_start(out=CB[:], in_=cb_d[i2]), writes=["CB"])
            P.dma(lambda e: e.dma_start(out=LNP[:], in_=lnp_d[i2]), writes=["LNP"])
            B = [sb(es, "cB%d" % i, [128, TT]) for i in range(3)]
            ZC = sb(es, "ZC", [128, 4, TT])
            Dg = [sb(es, "Dg%d" % i, [128, 31, 128], BF16) for i in range(2)]
            Zb = sb(es, "Zb", [128, TT], BF16)

            psrr = [0]

            def proj(chunk, dst, dkey):
                g, c = chunk // 4, chunk % 4
                for (t0, n) in tiles:
                    pb = psrr[0] % 4
                    psrr[0] += 1
                    for k in range(8):
                        P.pe(lambda e, g=g, c=c, k=k, t0=t0, n=n, pb=pb: e.matmul(
                            PS[pb][:, 0:n], Wg[g][:, k, c * 128:(c + 1) * 128], U[:, k, t0:t0 + n],
                            start=(k == 0), stop=(k == 7)),
                            reads=[("Wg", g)] + [("U", t) for t in range(t0 // 128, (t0 + n) // 128)],
                            writes=[PK[pb]])
                    P.act(lambda e, t0=t0, n=n, pb=pb: e.activation(out=dst[:, t0:t0 + n], in_=PS[pb][:, 0:n],
                                                                      func=AF.Copy), reads=[PK[pb]], writes=[dkey])

            for i in range(4):
                proj(i, B[0], "cB0")
                proj(4 + i, B[1], "cB1")
                proj(8 + i, B[2], "cB2")
                P.dve(lambda e: e.tensor_tensor(out=B[1][:], in0=B[1][:], in1=B[2][:], op=ALU.mult),
                      reads=["cB1", "cB2"], writes=["cB1"])
                P.dve(lambda e, i=i: e.tensor_scalar(out=B[2][:], in0=B[1][:], scalar1=CA[:, i, 1:2], scalar2=None,
                                                      op0=ALU.mult), reads=["cB1", "CA"], writes=["cB2"])
                for (s0, L) in SEQS:
                    P.dve(lambda e, i=i, s0=s0, L=L: e.scalar_tensor_tensor(
                        out=B[2][:, s0 + 1:s0 + L], in0=B[1][:, s0:s0 + L - 1], scalar=CA[:, i, 0:1],
                        in1=B[2][:, s0 + 1:s0 + L], op0=ALU.mult, op1=ALU.add), reads=["cB1", "cB2", "CA"],
                        writes=["cB2"])
                    P.dve(lambda e, i=i, s0=s0, L=L: e.scalar_tensor_tensor(
                        out=B[2][:, s0:s0 + L - 1], in0=B[1][:, s0 + 1:s0 + L], scalar=CA[:, i, 2:3],
                        in1=B[2][:, s0:s0 + L - 1], op0=ALU.mult, op1=ALU.add), reads=["cB1", "cB2", "CA"],
                        writes=["cB2"])
                P.dve(lambda e, i=i: e.tensor_tensor(out=Y[:, i, :], in0=B[0][:], in1=B[2][:], op=ALU.mult),
                      reads=["cB0", "cB2"], writes=[("Y", t) for t in range(NTILE)])
            for i in range(4):
                proj(12 + i, B[0], "cB0")
                proj(16 + i, B[1], "cB1")
                P.act(lambda e: e.activation(out=B[1][:], in_=B[1][:], func=AF.Sigmoid), reads=["cB1"], writes=["cB1"])
                P.dve(lambda e: e.tensor_tensor(out=Zb[:], in0=B[0][:], in1=B[1][:], op=ALU.mult),
                      reads=["cB0", "cB1"], writes=["Zb"])
                zk = ("ZC", i)
                dgi = i % 2
                dg = Dg[dgi]
                dgk = "Dg%d" % dgi
                for kk in range(31):
                    P.pool(lambda e, i=i, kk=kk, dg=dg: e.tensor_scalar(out=dg[:, kk, :], in0=ident_b[:],
                                                                        scalar1=CB[:, i, kk:kk + 1], scalar2=None,
                                                                        op0=ALU.mult), reads=["ident_b", "CB"], writes=[dgk])
                for ti, (t0, n) in enumerate(tiles):
                    s0, L = SEQS[0] if t0 < TC else SEQS[1]
                    pb = 6 + (ti % 2)
                    taps = [15] + [kk for kk in range(31) if kk != 15]
                    for q, kk in enumerate(taps):
                        o = kk - 15
                        a = max(0, s0 - o - t0)
                        bnd = min(n, s0 + L - o - t0)
                        P.pe(lambda e, dg=dg, kk=kk, o=o, a=a, bnd=bnd, t0=t0, pb=pb, q=q: e.matmul(
                            PS[pb][:, a:bnd], dg[:, kk, :], Zb[:, t0 + a + o:t0 + bnd + o],
                            start=(q == 0), stop=(q == 30)), reads=[dgk, "Zb"], writes=[PK[pb]])
                    P.act(lambda e, i=i, t0=t0, n=n, pb=pb: e.activation(out=ZC[:, i, t0:t0 + n], in_=PS[pb][:, 0:n],
                                                                          func=AF.Identity, bias=LNP[:, 0, i:i + 1], scale=1.0),
                          reads=[PK[pb], "LNP"], writes=[zk])
            MEAN = B[1]
            RSTD = B[2]
            SQ = [sb(es, "cSQ%d" % i, [128, 512]) for i in range(2)]
            sqi = 0
            for (t0, n) in tiles:
                for i in range(4):
                    sq = SQ[sqi % 2]
                    sqk = "cSQ%d" % (sqi % 2)
                    sqi += 1
                    P.act(lambda e, i=i, t0=t0, n=n, sq=sq: e.activation(out=sq[:, 0:n], in_=ZC[:, i, t0:t0 + n],
                                                                          func=AF.Square), reads=[("ZC", i)], writes=[sqk])
                    P.pe(lambda e, i=i, t0=t0, n=n: e.matmul(PS[4][:, 0:n], ones_f[:], ZC[:, i, t0:t0 + n],
                                                             start=(i == 0), stop=(i == 3)),
                         reads=[("ZC", i), "ones_f"], writes=[PK[4]])
                    P.pe(lambda e, i=i, n=n, sq=sq: e.matmul(PS[5][:, 0:n], ones_f[:], sq[:, 0:n],
                                                              start=(i == 0), stop=(i == 3)),
                         reads=[sqk, "ones_f"], writes=[PK[5]])
                P.dve(lambda e, t0=t0, n=n: e.tensor_scalar(out=MEAN[:, t0:t0 + n], in0=PS[4][:, 0:n], scalar1=1.0 / 512,
                                                             scalar2=None, op0=ALU.mult), reads=[PK[4]], writes=["cB1"])
                P.dve(lambda e, t0=t0, n=n: e.tensor_tensor(out=B[0][:, t0:t0 + n], in0=MEAN[:, t0:t0 + n],
                                                             in1=MEAN[:, t0:t0 + n], op=ALU.mult),
                      reads=["cB1"], writes=["cB0"])
                P.dve(lambda e, t0=t0, n=n: e.scalar_tensor_tensor(
                    out=RSTD[:, t0:t0 + n], in0=PS[5][:, 0:n], scalar=1.0 / 512, in1=B[0][:, t0:t0 + n],
                    op0=ALU.mult, op1=ALU.subtract), reads=[PK[5], "cB0"], writes=["cB2"])
            P.act(lambda e: e.activation(out=RSTD[:], in_=RSTD[:], func=AF.Sqrt, scale=1.0, bias=EPS),
                  reads=["cB2"], writes=["cB2"])
            P.dve(lambda e: e.reciprocal(RSTD[:], RSTD[:]), reads=["cB2"], writes=["cB2"])
            for i in range(4):
                zk = ("ZC", i)
                P.dve(lambda e, i=i: e.tensor_tensor(out=ZC[:, i, :], in0=ZC[:, i, :], in1=MEAN[:], op=ALU.subtract),
                      reads=[zk, "cB1"], writes=[zk])
                P.dve(lambda e, i=i: e.tensor_tensor(out=ZC[:, i, :], in0=ZC[:, i, :], in1=RSTD[:], op=ALU.mult),
                      reads=[zk, "cB2"], writes=[zk])
                P.act(lambda e, i=i: e.activation(out=Y[:, 4 + i, :], in_=ZC[:, i, :], func=AF.Silu,
                                                  scale=LNP[:, 1, i:i + 1], bias=LNP[:, 2, i:i + 1]),
                      reads=[zk, "LNP"], writes=[("Y", t) for t in range(NTILE)])
        P.barrier()


    def load_wgroup(Wt, key, src3, g):
        for k in range(8):
            P.dma(lambda e, k=k: e.dma_start(out=Wt[:, k, :], in_=src3[:, k, g * 512:(g + 1) * 512]),
                  writes=[(key, k)], q="pool")

    def phase_attn(l, U, Y):
        i2 = l // 2
        lam_init = 0.8 - 0.6 * math.exp(-0.3 * l)
        tiles = _tok_tiles()
        wv = w_in_cd_d[i2].rearrange("(k p) n -> p k n", p=128)
        with ExitStack() as es:
            qkT = [sb(es, "qT", [128, 4, TT], BF16), sb(es, "kT", [128, 4, TT], BF16)]
            V = sb(es, "V", [128, NTILE, 512], BF16)
            small = sb(es, "asmall", [128, 8])
            lamb = sb(es, "lamb", [128, 4 * 64])
            GQK = sb(es, "GQK", [128, 2])
            GS = sb(es, "GS", [128, 1])
            with ExitStack() as es2:
                Wt = [sb(es2, "Wt%d" % i, [128, 8, 512], BF16) for i in range(2)]
                COS = sb(es2, "COS", [128, T], BF16)
                SIN = sb(es2, "SIN", [128, T], BF16)
                RT = sb(es2, "RT", [128, 128], BF16)
                BO = sb(es2, "BO", [128, 128])
                QK = sb(es2, "QK", [128, TT])
                SQ = [sb(es2, "aSQ%d" % i, [128, 512]) for i in range(2)]
                RS = [sb(es2, "aRS%d" % i, [128, 512]) for i in range(2)]
                QN = [sb(es2, "aQN%d" % i, [128, 512], BF16) for i in range(2)]
                O1 = [sb(es2, "aO1%d" % i, [128, 512]) for i in range(2)]
                P.dma(lambda e: e.dma_start(out=COS[:], in_=ropec_d), writes=["COS"])
                P.dma(lambda e: e.dma_start(out=SIN[:], in_=ropes_d), writes=["SIN"])
                P.dma(lambda e: e.dma_start(out=RT[:], in_=ropert_d), writes=["RT"])
                P.dma(lambda e: e.dma_start(out=BO[:], in_=blockones_d), writes=["BO"])
                P.dma(lambda e: e.dma_start(out=GQK[:], in_=gqk_d[i2]), writes=["GQK"])
                P.dma(lambda e: e.dma_start(out=GS[:], in_=gsub_d[i2]), writes=["GS"])
                P.dma(lambda e: e.dma_start(out=lamb[:], in_=lamv_d[i2:i2 + 1, :].partition_broadcast(128)),
                      writes=["lamb"])
                for j in range(2):
                    P.dve(lambda e, j=j: e.tensor_tensor(out=lamb[:, j * 128:j * 128 + 64], in0=lamb[:, j * 128:j * 128 + 64],
                                                          in1=lamb[:, j * 128 + 64:j * 128 + 128], op=ALU.mult),
                          reads=["lamb"], writes=["lamb"])
                    P.dve(lambda e, j=j: e.reduce_sum(out=small[:, j:j + 1], in_=lamb[:, j * 128:j * 128 + 64], axis=AX.X),
                          reads=["lamb"], writes=["asmall"])
                P.act(lambda e: e.activation(out=small[:, 2:4], in_=small[:, 0:2], func=AF.Exp),
                      reads=["asmall"], writes=["asmall"])
                P.dve(lambda e: e.scalar_tensor_tensor(out=small[:, 4:5], in0=small[:, 3:4], scalar=-lam_init,
                                                        in1=small[:, 2:3], op0=ALU.add, op1=ALU.subtract),
                      reads=["asmall"], writes=["asmall"])
                P.dve(lambda e: e.tensor_scalar(out=GS[:], in0=GS[:], scalar1=1.0 - lam_init, scalar2=None, op0=ALU.mult),
                      reads=["GS"], writes=["GS"])
                cnt = 0
                for g in range(2):
                    wt = Wt[g % 2]
                    wkey = "Wt%d" % (g % 2)
                    load_wgroup(wt, wkey, wv, g)
                    for h in range(4):
                        for (t0, n) in tiles:
                            pb = cnt % 2
                            bb = cnt % 2
                            cnt += 1
                            for k in range(8):
                                P.pe(lambda e, wt=wt, h=h, k=k, t0=t0, n=n, pb=pb: e.matmul(
                                    PS[pb][:, 0:n], wt[:, k, h * 128:(h + 1) * 128], U[:, k, t0:t0 + n],
                                    start=(k == 0), stop=(k == 7)),
                                    reads=[(wkey, k)] + [("U", t) for t in range(t0 // 128, (t0 + n) // 128)],
                                    writes=[PK[pb]])
                            P.act(lambda e, t0=t0, n=n, pb=pb: e.activation(out=QK[:, t0:t0 + n], in_=PS[pb][:, 0:n],
                                                                              func=AF.Copy), reads=[PK[pb]], writes=["QK"])
                            P.act(lambda e, t0=t0, n=n, bb=bb: e.activation(out=SQ[bb][:, 0:n], in_=QK[:, t0:t0 + n],
                                                                              func=AF.Square), reads=["QK"],
                                  writes=["aSQ%d" % bb])
                            p2 = 2 + pb
                            P.pe(lambda e, n=n, bb=bb, p2=p2: e.matmul(PS[p2][:, 0:n], BO[:], SQ[bb][:, 0:n],
                                                                        start=True, stop=True),
                                 reads=["aSQ%d" % bb, "BO"], writes=[PK[p2]])
                            P.act(lambda e, n=n, bb=bb, p2=p2: e.activation(out=RS[bb][:, 0:n], in_=PS[p2][:, 0:n],
                                                                              func=AF.Sqrt, scale=1.0 / 64, bias=EPS),
                                  reads=[PK[p2]], writes=["aRS%d" % bb])
                            P.dve(lambda e, n=n, bb=bb: e.reciprocal(RS[bb][:, 0:n], RS[bb][:, 0:n]),
                                  reads=["aRS%d" % bb], writes=["aRS%d" % bb])
                            dstk = [("qk", g, h, t) for t in range(t0 // 128, (t0 + n) // 128)]
                            if t0 < TC:
                                P.dve(lambda e, g=g, h=h, t0=t0, n=n, bb=bb: e.scalar_tensor_tensor(
                                    out=qkT[g][:, h, t0:t0 + n], in0=QK[:, t0:t0 + n], scalar=GQK[:, g:g + 1],
                                    in1=RS[bb][:, 0:n], op0=ALU.mult, op1=ALU.mult),
                                    reads=["QK", "GQK", "aRS%d" % bb], writes=dstk)
                            else:
                                P.dve(lambda e, g=g, t0=t0, n=n, bb=bb: e.scalar_tensor_tensor(
                                    out=QN[bb][:, 0:n], in0=QK[:, t0:t0 + n], scalar=GQK[:, g:g + 1],
                                    in1=RS[bb][:, 0:n], op0=ALU.mult, op1=ALU.mult),
                                    reads=["QK", "GQK", "aRS%d" % bb], writes=["aQN%d" % bb])
                                p3 = 4 + pb
                                P.pe(lambda e, n=n, bb=bb, p3=p3: e.matmul(PS[p3][:, 0:n], RT[:], QN[bb][:, 0:n],
                                                                            start=True, stop=True),
                                     reads=["aQN%d" % bb, "RT"], writes=[PK[p3]])
                                c0 = t0 - TC
                                P.pool(lambda e, n=n, bb=bb, c0=c0: e.tensor_tensor(
                                    out=O1[bb][:, 0:n], in0=QN[bb][:, 0:n], in1=COS[:, c0:c0 + n], op=ALU.mult),
                                    reads=["aQN%d" % bb, "COS"], writes=["aO1%d" % bb])
                                P.dve(lambda e, n=n, bb=bb, c0=c0, p3=p3: e.tensor_tensor(
                                    out=RS[bb][:, 0:n], in0=PS[p3][:, 0:n], in1=SIN[:, c0:c0 + n], op=ALU.mult),
                                    reads=[PK[p3], "SIN"], writes=["aRS%d" % bb])
                                P.dve(lambda e, g=g, h=h, t0=t0, n=n, bb=bb: e.tensor_tensor(
                                    out=qkT[g][:, h, t0:t0 + n], in0=O1[bb][:, 0:n], in1=RS[bb][:, 0:n], op=ALU.add),
                                    reads=["aO1%d" % bb, "aRS%d" % bb], writes=dstk)
                wt = Wt[0]
                load_wgroup(wt, "Wt0", wv, 2)
                for tt in range(NTILE):
                    pb = 6 + tt % 2
                    for k in range(8):
                        P.pe(lambda e, tt=tt, k=k, pb=pb: e.matmul(PS[pb][:], U[:, k, tt * 128:(tt + 1) * 128], wt[:, k, :],
                                                                    start=(k == 0), stop=(k == 7)),
                             reads=[("U", tt), ("Wt0", k)], writes=[PK[pb]])
                    P.act(lambda e, tt=tt, pb=pb: e.activation(out=V[:, tt, :], in_=PS[pb][:], func=AF.Copy),
                          reads=[PK[pb]], writes=[("V", tt)])
            P.barrier()
            with ExitStack() as es2:
                Eb = [sb(es2, "Eb%d" % i, [128, 2, 512], BF16) for i in range(3)]
                R0 = sb(es2, "aR0", [128, 512])
                R1 = sb(es2, "aR1", [128, 512])
                OO = sb(es2, "aOO", [128, 512])
                SQ2 = sb(es2, "aSQ2", [128, 512])
                ecnt = 0
                groups = [(TC + i * 512, 512, list(range(NTILE))) for i in range(T // 512)] + [(0, TC, [0, 1])]
                SPD = (0, 3)
                for h in range(4):
                    for (q0, n, ktl) in groups:
                        qkeys = [("qk", 0, h, t) for t in range(q0 // 128, (q0 + n) // 128)]
                        npair = len(ktl) // 2
                        its = [(j, pi) for j in range(2) for pi in range(npair)]

                        def emit_s(idx, q0=q0, n=n, h=h, qkeys=qkeys, ktl=ktl):
                            j, pi = its[idx]
                            sd = SPD[(ecnt + idx) % 2]
                            for half in range(2):
                                kt = ktl[2 * pi + half]
                                P.pe(lambda e, h=h, j=j, kt=kt, q0=q0, n=n, sd=sd, half=half: e.matmul(
                                    PSD[sd][:, half * 512:half * 512 + n],
                                    qkT[1][j * 64:(j + 1) * 64, h, kt * 128:(kt + 1) * 128],
                                    qkT[0][j * 64:(j + 1) * 64, h, q0:q0 + n], start=True, stop=True),
                                    reads=qkeys + [("qk", 1, h, kt)], writes=[PK[2 * sd], PK[2 * sd + 1]])

                        emit_s(0)
                        for idx, (j, pi) in enumerate(its):
                            po, pz = 2 + 2 * j, 3 + 2 * j
                            sd = SPD[(ecnt + idx) % 2]
                            eb = (ecnt + idx) % 3
                            P.act(lambda e, n=n, sd=sd, eb=eb: e.activation(
                                out=Eb[eb][:, :, 0:n], in_=PSD[sd][:].rearrange("p (a b) -> p a b", a=2)[:, :, 0:n],
                                func=AF.Exp, scale=0.125), reads=[PK[2 * sd], PK[2 * sd + 1]], writes=["Eb%d" % eb])
                            if idx + 1 < len(its):
                                emit_s(idx + 1)
                            for half in range(2):
                                kt = ktl[2 * pi + half]
                                first = (pi == 0 and half == 0)
                                lastk = (pi == npair - 1 and half == 1)
                                P.pe(lambda e, h=h, kt=kt, n=n, eb=eb, po=po, half=half, first=first, lastk=lastk: e.matmul(
                                    PS[po][:, 0:n], V[:, kt, h * 128:(h + 1) * 128], Eb[eb][:, half, 0:n],
                                    start=first, stop=lastk), reads=[("V", kt), "Eb%d" % eb], writes=[PK[po]])
                                P.pe(lambda e, n=n, eb=eb, pz=pz, half=half, first=first, lastk=lastk: e.matmul(
                                    PS[pz][:, 0:n], ones_b[:], Eb[eb][:, half, 0:n], start=first, stop=lastk),
                                    reads=["ones_b", "Eb%d" % eb], writes=[PK[pz]])
                        ecnt += len(its)
                        P.dve(lambda e, n=n: e.reciprocal(R0[:, 0:n], PS[3][:, 0:n]), reads=[PK[3]], writes=["aR0"])
                        P.dve(lambda e, n=n: e.tensor_tensor(out=R0[:, 0:n], in0=PS[2][:, 0:n], in1=R0[:, 0:n], op=ALU.mult),
                              reads=[PK[2], "aR0"], writes=["aR0"])
                        P.dve(lambda e, n=n: e.reciprocal(R1[:, 0:n], PS[5][:, 0:n]), reads=[PK[5]], writes=["aR1"])
                        P.dve(lambda e, n=n: e.tensor_tensor(out=R1[:, 0:n], in0=PS[4][:, 0:n], in1=R1[:, 0:n], op=ALU.mult),
                              reads=[PK[4], "aR1"], writes=["aR1"])
                        P.dve(lambda e, n=n: e.scalar_tensor_tensor(out=OO[:, 0:n], in0=R1[:, 0:n], scalar=small[:, 4:5],
                                                                     in1=R0[:, 0:n], op0=ALU.mult, op1=ALU.add),
                              reads=["aR0", "aR1", "asmall"], writes=["aOO"])
                        P.act(lambda e, n=n: e.activation(out=SQ2[:, 0:n], in_=OO[:, 0:n], func=AF.Square),
                              reads=["aOO"], writes=["aSQ2"])
                        P.pe(lambda e, n=n: e.matmul(PS[0][:, 0:n], ones_f[:], SQ2[:, 0:n], start=True, stop=True),
                             reads=["aSQ2", "ones_f"], writes=[PK[0]])
                        P.act(lambda e, n=n: e.activation(out=SQ2[:, 0:n], in_=PS[0][:, 0:n], func=AF.Sqrt,
                                                          scale=1.0 / 128, bias=1e-5), reads=[PK[0]], writes=["aSQ2"])
                        P.dve(lambda e, n=n: e.reciprocal(SQ2[:, 0:n], SQ2[:, 0:n]), reads=["aSQ2"], writes=["aSQ2"])
                        P.dve(lambda e, h=h, q0=q0, n=n: e.scalar_tensor_tensor(
                            out=Y[:, h, q0:q0 + n], in0=OO[:, 0:n], scalar=GS[:, 0:1], in1=SQ2[:, 0:n],
                            op0=ALU.mult, op1=ALU.mult), reads=["aOO", "GS", "aSQ2"],
                            writes=[("Y", t) for t in range(q0 // 128, (q0 + n) // 128)])
        P.barrier()

    def phase_hyena(l, U, Y):
        i2 = l // 2
        tiles = _tok_tiles()
        wv = w_in_cd_d[i2].rearrange("(k p) n -> p k n", p=128)
        PI = math.pi
        with ExitStack() as es:
            h_tok = sb(es, "h_tok", [128, NTILE, 512], BF16)
            v_tok = sb(es, "v_tok", [128, NTILE, 512], BF16)
            GOc = sb(es, "GOc", [128, 4, TT], BF16)
            with ExitStack() as es2:
                zT = sb(es2, "zT", [33, TT])
                W1 = sb(es2, "hW1", [33, 64])
                W2 = sb(es2, "hW2", [64, 64])
                W3 = sb(es2, "hW3", [64, 512])
                HV = sb(es2, "hHV", [64, 3])
                HB = sb(es2, "hHB", [1, 512])
                HID = [sb(es2, "hHID%d" % i, [64, TT]) for i in range(2)]
                KI = sb(es2, "hKI", [64, 512], I32)
                KF = sb(es2, "hKF", [64, 512])
                AA = sb(es2, "hAA", [64, 512])
                WN = [sb(es2, "hWN%d" % i, [128, 512]) for i in range(2)]
                HT = [sb(es2, "hHT%d" % i, [128, 512]) for i in range(2)]
                P.dma(lambda e: e.dma_start(out=zT[:], in_=zT_d), writes=["zT"])
                P.dma(lambda e: e.dma_start(out=W1[:], in_=hfw1_d[i2]), writes=["hW1"])
                P.dma(lambda e: e.dma_start(out=W2[:], in_=hfw2_d[i2]), writes=["hW2"])
                P.dma(lambda e: e.dma_start(out=W3[:], in_=hfw3_d[i2]), writes=["hW3"])
                P.dma(lambda e: e.dma_start(out=HV[:], in_=hfv_d[i2]), writes=["hHV"])
                P.dma(lambda e: e.dma_start(out=HB[:], in_=hfb_d[i2:i2 + 1, :]), writes=["hHB"])
                for layer in range(2):
                    src = zT if layer == 0 else HID[0]
                    srck = "zT" if layer == 0 else "hHID0"
                    kdim = 33 if layer == 0 else 64
                    wm = W1 if layer == 0 else W2
                    wmk = "hW1" if layer == 0 else "hW2"
                    bcol = 0 if layer == 0 else 2
                    dst = HID[layer]
                    dk = "hHID%d" % layer
                    for ti, (t0, n) in enumerate(tiles):
                        pb = ti % 2
                        P.pe(lambda e, src=src, kdim=kdim, wm=wm, t0=t0, n=n, pb=pb: e.matmul(
                            PS[pb][0:64, 0:n], wm[0:kdim, :], src[0:kdim, t0:t0 + n], start=True, stop=True),
                            reads=[srck, wmk], writes=[PK[pb]])
                        P.dve(lambda e, n=n, pb=pb, bcol=bcol: e.tensor_scalar(
                            out=AA[:, 0:n], in0=PS[pb][0:64, 0:n], scalar1=HV[:, bcol:bcol + 1], scalar2=HV[:, 1:2],
                            op0=ALU.add, op1=ALU.mult), reads=[PK[pb], "hHV"], writes=["hAA"])
                        P.dve(lambda e, n=n: e.tensor_scalar(out=KI[:, 0:n], in0=AA[:, 0:n], scalar1=1.0 / (2 * PI),
                                                             scalar2=None, op0=ALU.mult), reads=["hAA"], writes=["hKI"])
                        P.dve(lambda e, n=n: e.tensor_copy(KF[:, 0:n], KI[:, 0:n]), reads=["hKI"], writes=["hKF"])
                        P.dve(lambda e, n=n: e.scalar_tensor_tensor(out=AA[:, 0:n], in0=KF[:, 0:n], scalar=-2 * PI,
                                                                     in1=AA[:, 0:n], op0=ALU.mult, op1=ALU.add),
                              reads=["hKF", "hAA"], writes=["hAA"])
                        P.dve(lambda e, n=n: e.tensor_scalar(out=KF[:, 0:n], in0=AA[:, 0:n], scalar1=PI, scalar2=-2 * PI,
                                                             op0=ALU.is_gt, op1=ALU.mult), reads=["hAA"], writes=["hKF"])
                        P.dve(lambda e, n=n: e.tensor_tensor(out=AA[:, 0:n], in0=AA[:, 0:n], in1=KF[:, 0:n], op=ALU.add),
                              reads=["hAA", "hKF"], writes=["hAA"])
                        P.dve(lambda e, n=n: e.tensor_scalar(out=KF[:, 0:n], in0=AA[:, 0:n], scalar1=-PI, scalar2=2 * PI,
                                                             op0=ALU.is_lt, op1=ALU.mult), reads=["hAA"], writes=["hKF"])
                        P.dve(lambda e, n=n: e.tensor_tensor(out=AA[:, 0:n], in0=AA[:, 0:n], in1=KF[:, 0:n], op=ALU.add),
                              reads=["hAA", "hKF"], writes=["hAA"])
                        P.dve(lambda e, n=n: e.tensor_scalar(out=AA[:, 0:n], in0=AA[:, 0:n], scalar1=-PI, scalar2=PI,
                                                             op0=ALU.max, op1=ALU.min), reads=["hAA"], writes=["hAA"])
                        P.act(lambda e, dst=dst, t0=t0, n=n: e.activation(out=dst[:, t0:t0 + n], in_=AA[:, 0:n], func=AF.Sin),
                              reads=["hAA"], writes=[dk])
                for tt in range(NTILE):
                    b = tt % 2
                    pb = 2 + b
                    P.dma(lambda e, b=b, tt=tt: e.dma_start(out=WN[b][:], in_=win_d[tt * 128:(tt + 1) * 128, :]),
                          writes=["hWN%d" % b])
                    P.pe(lambda e, tt=tt, pb=pb: e.matmul(PS[pb][:], HID[1][:, tt * 128:(tt + 1) * 128], W3[:],
                                                          start=True, stop=True), reads=["hHID1", "hW3"], writes=[PK[pb]])
                    centre = tt in (1, 2 + 8)
                    if centre:
                        P.dve(lambda e, b=b, pb=pb: e.tensor_tensor(out=HT[b][:], in0=PS[pb][:], in1=WN[b][:], op=ALU.mult),
                              reads=[PK[pb], "hWN%d" % b], writes=["hHT%d" % b])
                        P.dve(lambda e, b=b: e.tensor_tensor(out=HT[b][0:1, :], in0=HT[b][0:1, :], in1=HB[:], op=ALU.add),
                              reads=["hHT%d" % b, "hHB"], writes=["hHT%d" % b])
                        P.act(lambda e, b=b, tt=tt: e.activation(out=h_tok[:, tt, :], in_=HT[b][:], func=AF.Copy),
                              reads=["hHT%d" % b], writes=[("h_tok", tt)])
                    else:
                        P.dve(lambda e, b=b, pb=pb, tt=tt: e.tensor_tensor(out=h_tok[:, tt, :], in0=PS[pb][:], in1=WN[b][:],
                                                                            op=ALU.mult),
                              reads=[PK[pb], "hWN%d" % b], writes=[("h_tok", tt)])
            P.barrier()
            with ExitStack() as es2:
                Wt = [sb(es2, "hWt%d" % i, [128, 8, 512], BF16) for i in range(3)]
                CD = sb(es2, "CD", [128, 12, 3])
                Bf = [sb(es2, "hB%d" % i, [128, TT]) for i in range(3)]
                P.dma(lambda e: e.dma_start(out=CD[:], in_=cd_d[i2]), writes=["CD"])
                for g in range(3):
                    load_wgroup(Wt[g], "hWt%d" % g, wv, 3 + g)
                pcnt = [0]

                def projc(g, c, raw, rawk, dst, dstk):
                    for (t0, n) in tiles:
                        pb = pcnt[0] % 4
                        pcnt[0] += 1
                        for k in range(8):
                            P.pe(lambda e, g=g, c=c, k=k, t0=t0, n=n, pb=pb: e.matmul(
                                PS[pb][:, 0:n], Wt[g][:, k, c * 128:(c + 1) * 128], U[:, k, t0:t0 + n],
                                start=(k == 0), stop=(k == 7)),
                                reads=[("hWt%d" % g, k)] + [("U", t) for t in range(t0 // 128, (t0 + n) // 128)],
                                writes=[PK[pb]])
                        P.act(lambda e, t0=t0, n=n, pb=pb: e.activation(out=raw[:, t0:t0 + n], in_=PS[pb][:, 0:n],
                                                                          func=AF.Copy), reads=[PK[pb]], writes=[rawk])
                    ch = g * 4 + c
                    P.dve(lambda e: e.tensor_scalar(out=dst[:], in0=raw[:], scalar1=CD[:, ch, 1:2], scalar2=None,
                                                    op0=ALU.mult), reads=[rawk, "CD"], writes=[dstk])
                    for (s0, L) in SEQS:
                        P.dve(lambda e, s0=s0, L=L: e.scalar_tensor_tensor(
                            out=dst[:, s0 + 1:s0 + L], in0=raw[:, s0:s0 + L - 1], scalar=CD[:, ch, 0:1],
                            in1=dst[:, s0 + 1:s0 + L], op0=ALU.mult, op1=ALU.add), reads=[rawk, dstk, "CD"], writes=[dstk])
                        P.dve(lambda e, s0=s0, L=L: e.scalar_tensor_tensor(
                            out=dst[:, s0:s0 + L - 1], in0=raw[:, s0 + 1:s0 + L], scalar=CD[:, ch, 2:3],
                            in1=dst[:, s0:s0 + L - 1], op0=ALU.mult, op1=ALU.add), reads=[rawk, dstk, "CD"], writes=[dstk])

                for c in range(4):
                    projc(0, c, Bf[0], "hB0", Bf[1], "hB1")
                    P.act(lambda e, c=c: e.activation(out=GOc[:, c, :], in_=Bf[1][:], func=AF.Copy),
                          reads=["hB1"], writes=[("GOc", c)])
                    projc(1, c, Bf[0], "hB0", Bf[1], "hB1")
                    projc(2, c, Bf[0], "hB0", Bf[2], "hB2")
                    P.pool(lambda e: e.tensor_tensor(out=Bf[1][:], in0=Bf[1][:], in1=Bf[2][:], op=ALU.mult),
                           reads=["hB1", "hB2"], writes=["hB1"])
                    for t4 in range(0, NTILE, 4):
                        nt = min(4, NTILE - t4)
                        pb = 4 + (t4 // 4) % 2
                        for j in range(nt):
                            P.pe(lambda e, t4=t4, j=j, pb=pb: e.transpose(
                                PS[pb][:, j * 128:(j + 1) * 128], Bf[1][:, (t4 + j) * 128:(t4 + j + 1) * 128], ident_f[:]),
                                reads=["hB1", "ident_f"], writes=[PK[pb]])
                        P.act(lambda e, t4=t4, nt=nt, pb=pb, c=c: e.activation(
                            out=v_tok[:, t4:t4 + nt, c * 128:(c + 1) * 128],
                            in_=PS[pb][:, 0:nt * 128].rearrange("p (j t) -> p j t", j=nt), func=AF.Copy),
                            reads=[PK[pb]], writes=[("v_tok", c)])
            P.barrier()
            with ExitStack() as es2:
                YS = sb(es2, "YS", [128, 24, 512], BF16)
                YSc = sb(es2, "YSc", [128, 4, 512], BF16)
                es3 = ExitStack()
                FW = [sb(es3, "FW%d" % i, [128, 16, 256], BF16) for i in range(2)]
                HH = [sb(es3, "HH%d" % i, [128, 2, 512]) for i in range(2)]
                T1 = sb(es3, "hT1", [128, 512])
                T2 = sb(es3, "hT2", [128, 512])
                T3 = sb(es3, "hT3", [128, 512])
                T4 = sb(es3, "hT4", [128, 512])
                fcnt = 0
                for (npair, tt0, ntt, fwd, ys, ysk) in ((12, 2, 16, fwl_d, YS, "YS"), (2, 0, 2, fwc_d, YSc, "YSc")):
                    for i in range(npair):
                        fb = fcnt % 2
                        fcnt += 1
                        fw = FW[fb]
                        fwk = "FW%d" % fb
                        hh = HH[fb]
                        hhk = "HH%d" % fb
                        P.dma(lambda e, fw=fw, fwd=fwd, i=i, ntt=ntt: e.dma_start(out=fw[:, 0:ntt, :], in_=fwd[i]),
                              writes=[fwk])
                        pbase = 4 * fb
                        for (srct, srck, pr, pi) in ((h_tok, "h_tok", pbase, pbase + 1), (v_tok, "v_tok", pbase + 2, pbase + 3)):
                            for part, pb in ((0, pr), (1, pi)):
                                for tt in range(ntt):
                                    rk = [("h_tok", tt0 + tt)] if srct is h_tok else [("v_tok", c) for c in range(4)]
                                    P.pe(lambda e, fw=fw, tt=tt, part=part, pb=pb, srct=srct, tt0=tt0, ntt=ntt: e.matmul(
                                        PS[pb][:], fw[:, tt, part * 128:(part + 1) * 128], srct[:, tt0 + tt, :],
                                        start=(tt == 0), stop=(tt == ntt - 1)), reads=[fwk] + rk, writes=[PK[pb]])
                            if srct is h_tok:
                                P.act(lambda e, hh=hh, pbase=pbase: e.activation(out=hh[:, 0, :], in_=PS[pbase][:], func=AF.Copy),
                                      reads=[PK[pbase]], writes=[hhk])
                                P.act(lambda e, hh=hh, pbase=pbase: e.activation(out=hh[:, 1, :], in_=PS[pbase + 1][:], func=AF.Copy),
                                      reads=[PK[pbase + 1]], writes=[hhk])
                        nch = npair
                        pu, pv = pbase + 2, pbase + 3
                        P.dve(lambda e, hh=hh, pu=pu: e.tensor_tensor(out=T1[:], in0=PS[pu][:], in1=hh[:, 0, :], op=ALU.mult),
                              reads=[PK[pu], hhk], writes=["hT1"])
                        P.dve(lambda e, hh=hh, pv=pv: e.tensor_tensor(out=T2[:], in0=PS[pv][:], in1=hh[:, 1, :], op=ALU.mult),
                              reads=[PK[pv], hhk], writes=["hT2"])
                        P.pool(lambda e, ys=ys, i=i: e.tensor_tensor(out=ys[:, i, :], in0=T1[:], in1=T2[:], op=ALU.subtract),
                               reads=["hT1", "hT2"], writes=[(ysk, i)])
                        P.dve(lambda e, hh=hh, pu=pu: e.tensor_tensor(out=T3[:], in0=PS[pu][:], in1=hh[:, 1, :], op=ALU.mult),
                              reads=[PK[pu], hhk], writes=["hT3"])
                        P.dve(lambda e, hh=hh, pv=pv: e.tensor_tensor(out=T4[:], in0=PS[pv][:], in1=hh[:, 0, :], op=ALU.mult),
                              reads=[PK[pv], hhk], writes=["hT4"])
                        P.pool(lambda e, ys=ys, i=i, nch=nch: e.tensor_tensor(out=ys[:, nch + i, :], in0=T3[:], in1=T4[:],
                                                                               op=ALU.add),
                               reads=["hT3", "hT4"], writes=[(ysk, nch + i)])
                P.barrier()
                es3.close()
                IV = [sb(es2, "IV%d" % i, [128, 12, 512], BF16) for i in range(2)]
                icnt = 0
                for j in range(4):
                    for half in range(2):
                        ib = icnt % 2
                        icnt += 1
                        P.dma(lambda e, ib=ib, j=j, half=half: e.dma_start(out=IV[ib][:], in_=invl_d[j, half]),
                              writes=["IV%d" % ib])
                        for c in range(4):
                            pc = 4 * (j % 2) + c
                            for fc in range(12):
                                P.pe(lambda e, ib=ib, half=half, c=c, fc=fc, pc=pc: e.matmul(
                                    PS[pc][:], YS[:, half * 12 + fc, c * 128:(c + 1) * 128], IV[ib][:, fc, :],
                                    start=(half == 0 and fc == 0), stop=(half == 1 and fc == 11)),
                                    reads=[("YS", half * 12 + fc), "IV%d" % ib], writes=[PK[pc]])
                    t0 = TC + j * 512
                    for c in range(4):
                        pc = 4 * (j % 2) + c
                        P.dve(lambda e, c=c, t0=t0, pc=pc: e.tensor_tensor(out=Y[:, 4 + c, t0:t0 + 512], in0=PS[pc][:],
                                                                           in1=GOc[:, c, t0:t0 + 512], op=ALU.mult),
                              reads=[PK[pc], ("GOc", c)], writes=[("Y", t) for t in range(t0 // 128, t0 // 128 + 4)])
                IVc = [sb(es2, "IVc%d" % i, [128, 2, 256], BF16) for i in range(2)]
                for half in range(2):
                    P.dma(lambda e, half=half: e.dma_start(out=IVc[half][:], in_=invc_d[half]), writes=["IVc%d" % half])
                for c in range(4):
                    for half in range(2):
                        for fc in range(2):
                            P.pe(lambda e, half=half, c=c, fc=fc: e.matmul(
                                PS[4 + c][:, 0:TC], YSc[:, half * 2 + fc, c * 128:(c + 1) * 128], IVc[half][:, fc, :],
                                start=(half == 0 and fc == 0), stop=(half == 1 and fc == 1)),
                                reads=[("YSc", half * 2 + fc), "IVc%d" % half], writes=[PK[4 + c]])
                    P.dve(lambda e, c=c: e.tensor_tensor(out=Y[:, 4 + c, 0:TC], in0=PS[4 + c][:, 0:TC], in1=GOc[:, c, 0:TC],
                                                         op=ALU.mult),
                          reads=[PK[4 + c], ("GOc", c)], writes=[("Y", 0), ("Y", 1)])
        P.barrier()

    def phase_moe(l, AFF, last):
        with ExitStack() as es:
            affT = sb(es, "affT", [NE, TT])
            work = sb(es, "mwork", [NE, TT])
            GV = sb(es, "GV", [NE, NSLOT])
            IXu = sb(es, "IXu", [NE, NSLOT], U32)
            IXf = sb(es, "IXf", [NE, NSLOT])
            gP = sb(es, "gP", [128, 3, NE])
            iP = sb(es, "iP", [128, 3, NE], I32)
            for t4 in range(0, NTILE, 4):
                nt = min(4, NTILE - t4)
                pb = (t4 // 4) % 2
                for j in range(nt):
                    P.pe(lambda e, t4=t4, j=j, pb=pb: e.transpose(PS[pb][0:NE, j * 128:(j + 1) * 128],
                                                                   AFF[:, t4 + j, :], ident_f[:]),
                         reads=[("AFF", t4 + j), "ident_f"], writes=[PK[pb]])
                P.dve(lambda e, t4=t4, nt=nt, pb=pb: e.tensor_copy(affT[:, t4 * 128:(t4 + nt) * 128],
                                                                    PS[pb][0:NE, 0:nt * 128]),
                      reads=[PK[pb]], writes=["affT"])
            P.dve(lambda e: e.tensor_copy(work[:], affT[:]), reads=["affT"], writes=["mwork"])
            for (s0, L, cap, c0) in ((TC, T, CAP_L, 0), (0, TC, CAP_C, CAP_L)):
                for rd in range(cap // 8):
                    sl = slice(c0 + rd * 8, c0 + rd * 8 + 8)
                    P.dve(lambda e, s0=s0, L=L, sl=sl: e.max(out=GV[:, sl], in_=work[:, s0:s0 + L]),
                          reads=["mwork"], writes=["GV"])
                    P.dve(lambda e, s0=s0, L=L, sl=sl: e.max_index(out=IXu[:, sl], in_max=GV[:, sl],
                                                                   in_values=work[:, s0:s0 + L]),
                          reads=["mwork", "GV"], writes=["IXu"])
                    P.dve(lambda e, s0=s0, L=L, sl=sl: e.match_replace(out=work[:, s0:s0 + L], in_to_replace=GV[:, sl],
                                                                       in_values=work[:, s0:s0 + L], imm_value=-1.0),
                          reads=["mwork", "GV"], writes=["mwork"])
            P.dve(lambda e: e.tensor_copy(IXf[:], IXu[:]), reads=["IXu"], writes=["IXf"])
            P.dve(lambda e: e.tensor_scalar(out=IXf[:, 0:CAP_L], in0=IXf[:, 0:CAP_L], scalar1=float(TC), scalar2=None,
                                            op0=ALU.add), reads=["IXf"], writes=["IXf"])
            for (src, dst, dk, pb) in ((IXf, iP, "iP", 2), (GV, gP, "gP", 3)):
                sk = "IXf" if src is IXf else "GV"
                for s in range(3):
                    n = 128 if s < 2 else CAP_C
                    P.pe(lambda e, src=src, s=s, n=n, pb=pb: e.transpose(PS[pb][0:n, s * NE:(s + 1) * NE],
                                                                          src[:, s * 128:s * 128 + n], ident_f[0:NE, 0:NE]),
                         reads=[sk, "ident_f"], writes=[PK[pb]])
                P.dve(lambda e, dst=dst, pb=pb: e.tensor_copy(dst[:, 0:2, :],
                                                               PS[pb][:, 0:2 * NE].rearrange("p (s e) -> p s e", s=2)),
                      reads=[PK[pb]], writes=[dk])
                P.dve(lambda e, dst=dst, pb=pb: e.tensor_copy(dst[0:CAP_C, 2, :], PS[pb][0:CAP_C, 2 * NE:3 * NE]),
                      reads=[PK[pb]], writes=[dk])
            if dbg and last:
                P.dma(lambda e: e.dma_start(out=dbg_ix_d, in_=IXf[:]), reads=["IXf"], writes=["dbg_ix"])
                P.dma(lambda e: e.dma_start(out=dbg_gv_d, in_=GV[:]), reads=["GV"], writes=["dbg_gv"])
            for tt in range(NTILE):
                P.dma(lambda e, tt=tt: e.dma_start(out=M_d[tt * 128:(tt + 1) * 128, :], in_=zero_f[:]),
                      reads=["zero_f"], writes=["Macc"])
            WB = [sb(es, "WB%d" % i, [128, 8, D], BF16) for i in range(6)]
            xe = [sb(es, "xe%d" % i, [128, 3, D], BF16) for i in range(2)]
            xeT = [sb(es, "xeT%d" % i, [128, 8, NSLOT], BF16) for i in range(2)]
            hT = [sb(es, "hT%d" % i, [128, 8, NSLOT], BF16) for i in range(2)]
            sg = [sb(es, "sg%d" % i, [128, NSLOT]) for i in range(2)]
            yb = [sb(es, "yb%d" % i, [128, 3, D]) for i in range(2)]
            wcnt = [0]

            def load_w(src_ap):
                i = wcnt[0] % 6
                wcnt[0] += 1
                wv = src_ap.rearrange("(k p) n -> p k n", p=128)
                for k in range(8):
                    P.dma(lambda e, i=i, k=k: e.dma_start(out=WB[i][:, k, :], in_=wv[:, k, :]),
                          writes=[("WB", i, k)], q="pool")
                return i

            for ex_i in range(NE):
                b = ex_i % 2
                kxe, kxT, khT, ksg, kyb = "xe%d" % b, "xeT%d" % b, "hT%d" % b, "sg%d" % b, "yb%d" % b
                if ex_i == 0:
                    wnext = (load_w(w_gate_d[l, 0]), load_w(w_up_d[l, 0]), load_w(w_down_d[l, 0]))
                wg, wu, wd = wnext
                for s in range(3):
                    n = 128 if s < 2 else CAP_C
                    P.dma(lambda e, b=b, s=s, n=n, ex_i=ex_i: e.indirect_dma_start(
                        out=xe[b][0:n, s, :], out_offset=None, in_=u2tok_d,
                        in_offset=bass.IndirectOffsetOnAxis(ap=iP[0:n, s, ex_i:ex_i + 1], axis=0)),
                        reads=["iP"] + [("u2tok", t) for t in range(NTILE)], writes=[(kxe, s)], q="pool")
                if ex_i + 1 < NE:
                    wnext = (load_w(w_gate_d[l, ex_i + 1]), load_w(w_up_d[l, ex_i + 1]), load_w(w_down_d[l, ex_i + 1]))
                if dbg and last and ex_i == 0:
                    P.dma(lambda e: e.dma_start(out=dbg_xe_d, in_=xe[0][:]), reads=[("xe0", 0), ("xe0", 1), ("xe0", 2)], writes=["dbg_xe"])
                    P.dma(lambda e: e.dma_start(out=dbg_ip_d, in_=iP[:]), reads=["iP"], writes=["dbg_ip"])
                for s in range(3):
                    n = 128 if s < 2 else CAP_C
                    pb = 4 + (s % 2)
                    pv = PS[pb][:].bitcast(BF16)
                    for k in range(8):
                        P.pe(lambda e, b=b, s=s, n=n, k=k, pv=pv: e.transpose(
                            pv[:, k * 128:k * 128 + n], xe[b][0:n, s, k * 128:(k + 1) * 128], ident_b[0:n, 0:n]),
                            reads=[(kxe, s), "ident_b"], writes=[PK[pb]])
                    P.act(lambda e, b=b, s=s, n=n, pv=pv: e.activation(
                        out=xeT[b][:, :, s * 128:s * 128 + n],
                        in_=pv.rearrange("p (k t) -> p k t", k=8)[:, :, 0:n], func=AF.Copy),
                        reads=[PK[pb]], writes=[kxT])
                for f in range(8):
                    pa, pu = 0 + (f % 2) * 2, 1 + (f % 2) * 2
                    for k in range(8):
                        P.pe(lambda e, b=b, f=f, k=k, pa=pa, wg=wg: e.matmul(
                            PS[pa][:, 0:NSLOT], WB[wg][:, k, f * 128:(f + 1) * 128], xeT[b][:, k, :],
                            start=(k == 0), stop=(k == 7)), reads=[("WB", wg, k), kxT], writes=[PK[pa]])
                    for k in range(8):
                        P.pe(lambda e, b=b, f=f, k=k, pu=pu, wu=wu: e.matmul(
                            PS[pu][:, 0:NSLOT], WB[wu][:, k, f * 128:(f + 1) * 128], xeT[b][:, k, :],
                            start=(k == 0), stop=(k == 7)), reads=[("WB", wu, k), kxT], writes=[PK[pu]])
                    P.act(lambda e, b=b, pa=pa: e.activation(out=sg[b][:], in_=PS[pa][:, 0:NSLOT], func=AF.Silu),
                          reads=[PK[pa]], writes=[ksg])
                    P.dve(lambda e, b=b, f=f, pu=pu: e.tensor_tensor(out=hT[b][:, f, :], in0=sg[b][:],
                                                                      in1=PS[pu][:, 0:NSLOT], op=ALU.mult),
                          reads=[ksg, PK[pu]], writes=[khT])
                for s in range(3):
                    n = 128 if s < 2 else CAP_C
                    for hf in range(2):
                        pb = 6 + hf
                        for k in range(8):
                            P.pe(lambda e, b=b, s=s, n=n, hf=hf, k=k, pb=pb, wd=wd: e.matmul(
                                PS[pb][0:n, :], hT[b][:, k, s * 128:s * 128 + n], WB[wd][:, k, hf * 512:(hf + 1) * 512],
                                start=(k == 0), stop=(k == 7)), reads=[khT, ("WB", wd, k)], writes=[PK[pb]])
                        P.dve(lambda e, b=b, s=s, n=n, hf=hf, pb=pb, ex_i=ex_i: e.tensor_scalar(
                            out=yb[b][0:n, s, hf * 512:(hf + 1) * 512], in0=PS[pb][0:n, :],
                            scalar1=gP[0:n, s, ex_i:ex_i + 1], scalar2=None, op0=ALU.mult),
                            reads=[PK[pb], "gP"], writes=[(kyb, s)])
                    if dbg and last and ex_i == 0 and s == 2:
                        P.dma(lambda e: e.dma_start(out=dbg_yb_d, in_=yb[0][:]), reads=[("yb0", 0), ("yb0", 1), ("yb0", 2)], writes=["dbg_yb"])
                        P.dma(lambda e: e.dma_start(out=dbg_ht_d, in_=hT[0][:]), reads=["hT0"], writes=["dbg_ht"])
                    P.dma(lambda e, b=b, s=s, n=n, ex_i=ex_i: e.indirect_dma_start(
                        out=M_d, out_offset=bass.IndirectOffsetOnAxis(ap=iP[0:n, s, ex_i:ex_i + 1], axis=0),
                        in_=yb[b][0:n, s, :], in_offset=None, compute_op=ALU.add),
                        reads=[(kyb, s), "iP"], writes=["Macc"], q="pool")
            G = [sb(es, "mG%d" % r, [128, D]) for r in range(2)]
            for r in range(2):
                load_mod(G[r], "mG%d" % r, r, 5)
            xt = [sb(es, "mx%d" % i, [128, D]) for i in range(2)]
            mt = [sb(es, "mm%d" % i, [128, D]) for i in range(2)]
            outs = []
            for tt in range(NTILE):
                b = tt % 2
                r = 1 if tt < 2 else 0
                kx, km = "mx%d" % b, "mm%d" % b
                P.dma(lambda e, b=b, tt=tt: e.dma_start(out=xt[b][:], in_=H_d[tt * 128:(tt + 1) * 128, :]),
                      reads=[hkey(tt)], writes=[kx])
                P.dma(lambda e, b=b, tt=tt: e.dma_start(out=mt[b][:], in_=M_d[tt * 128:(tt + 1) * 128, :]),
                      reads=["Macc"], writes=[km])
                P.dve(lambda e, b=b, r=r: e.tensor_tensor(out=mt[b][:], in0=mt[b][:], in1=G[r][:], op=ALU.mult),
                      reads=[km, "mG%d" % r], writes=[km])
                P.pool(lambda e, b=b: e.tensor_tensor(out=xt[b][:], in0=xt[b][:], in1=mt[b][:], op=ALU.add),
                       reads=[kx, km], writes=[kx])
                if dbg and last:
                    P.dma(lambda e, b=b, tt=tt: e.dma_start(out=dbg_m_d[tt * 128:(tt + 1) * 128, :], in_=mt[b][:]),
                          reads=[km], writes=[("dbg_m", tt)])
                if last:
                    if tt >= 2:
                        outs.append(P.dma(lambda e, b=b, tt=tt: e.dma_start(
                            out=out_d[(tt - 2) * 128:(tt - 1) * 128, :], in_=xt[b][:]), reads=[kx], writes=[("out", tt)]))
                    if dbg:
                        outs.append(P.dma(lambda e, b=b, tt=tt: e.dma_start(
                            out=hdbg_d[tt * 128:(tt + 1) * 128, :], in_=xt[b][:]), reads=[kx], writes=[("hdbg", tt)]))
                else:
                    P.dma(lambda e, b=b, tt=tt: e.dma_start(out=H_d[tt * 128:(tt + 1) * 128, :], in_=xt[b][:]),
                          reads=[kx], writes=[hkey(tt)])
        P.barrier()
        return outs

    finals = []
    for l in range(n_layers):
        last = (l == n_layers - 1)
        phase_mod(l)
        with ExitStack() as es:
            U = sb(es, "U", [128, 8, TT], BF16)
            Y = sb(es, "Y", [128, 8, TT], BF16)
            phase_norm(l, l, 1, U)
            if l % 2 == 0:
                phase_conv(l, U, Y)
                phase_outproj(l, Y, w_out_ab_d[l // 2])
            else:
                phase_attn(l, U, Y)
                phase_hyena(l, U, Y)
                phase_outproj(l, Y, w_out_cd_d[l // 2])
        with ExitStack() as es:
            AFF = sb(es, "AFF", [128, NTILE, NE])
            phase_norm(l, 1, 2, None, AFF=AFF)
            finals = phase_moe(l, AFF, last)
    P.finalize(finals)
    top.close()
    return nc


def _host_inputs(inp):
    f = lambda a: np.ascontiguousarray(np.asarray(a, dtype=np.float32))
    sh = {}
    sh["w_ada"] = f(inp["w_ada"])
    sh["b_ada"] = f(inp["b_ada"])
    sh["g_mix"] = f(inp["g_mix"])
    sh["g_ffn"] = f(inp["g_ffn"])
    sh["w_in_ab"] = f(inp["w_in_ab"])
    sh["ca"] = f(np.asarray(inp["conv_a"]).reshape(2, 3, 4, 128).transpose(0, 3, 2, 1))
    sh["cb"] = f(np.asarray(inp["conv_b"]).reshape(2, 31, 4, 128).transpose(0, 3, 2, 1))
    lnp = np.stack([np.asarray(inp["conv_b_bias"]), np.asarray(inp["ln_b_g"]), np.asarray(inp["ln_b_b"])], axis=1)
    sh["lnp"] = f(lnp.reshape(2, 3, 4, 128).transpose(0, 3, 1, 2))
    sh["w_out_ab"] = f(inp["w_out_ab"])
    sh["w_in_cd"] = f(inp["w_in_cd"])
    sh["w_out_cd"] = f(inp["w_out_cd"])
    gq = np.asarray(inp["g_q"]); gk = np.asarray(inp["g_k"])
    sh["gqk"] = f(np.stack([np.tile(gq, (1, 2)), np.tile(gk, (1, 2))], axis=-1))
    sh["lamv"] = f(np.concatenate([np.asarray(inp[k]) for k in ("lam_q1", "lam_k1", "lam_q2", "lam_k2")], axis=1))
    sh["gsub"] = f(np.asarray(inp["g_subln"]).reshape(2, 128, 1))
    sh["cd"] = f(np.asarray(inp["conv_d"]).reshape(2, 3, 12, 128).transpose(0, 3, 2, 1))
    sh["hf_w1"] = f(inp["hf_w1"])
    sh["hfv"] = f(np.stack([np.asarray(inp["hf_b1"]), np.asarray(inp["hf_freq"]), np.asarray(inp["hf_b2"])], axis=-1))
    sh["hf_w2"] = f(inp["hf_w2"])
    sh["hf_w3"] = f(inp["hf_w3"])
    sh["hf_bias"] = f(inp["hf_bias"])
    sh["wr"] = f(np.asarray(inp["w_router"]).reshape(DEPTH, 8, 128, NE).transpose(0, 2, 1, 3))
    sh["w_gate"] = f(inp["w_gate"])
    sh["w_up"] = f(inp["w_up"])
    sh["w_down"] = f(inp["w_down"])
    sh.update(host_constants())
    return sh


def _core_inputs(inp, shared, b):
    m = dict(shared)
    m["x"] = np.ascontiguousarray(np.asarray(inp["x"][b], dtype=np.float32))
    m["ctx"] = np.ascontiguousarray(np.asarray(inp["ctx"][b], dtype=np.float32))
    cv = np.stack([np.asarray(inp["c"][b]), np.asarray(inp["c_ctx"])], axis=0)
    m["cT"] = np.ascontiguousarray(cv.reshape(2, 8, 128).transpose(2, 1, 0).astype(np.float32))
    return m


_NC_CACHE = {}


def kernel(**inputs):
    n = 8
    if "nc" not in _NC_CACHE:
        _NC_CACHE["nc"] = build(DEPTH)
    nc = _NC_CACHE["nc"]
    shared = _host_inputs(inputs)
    in_maps = [_core_inputs(inputs, shared, b) for b in range(n)]
    res = run_bass_kernel_spmd(nc, in_maps, core_ids=list(range(n)))
    return np.stack([np.asarray(r["out"]) for r in res.results], axis=0).astype(np.float32)
```

```python
import math
from contextlib import ExitStack

import numpy as np
import concourse.bass as bass
import concourse.mybir as mybir
from concourse.bass_utils import run_bass_kernel_spmd

F32 = mybir.dt.float32
BF16 = mybir.dt.bfloat16
I32 = mybir.dt.int32
U32 = mybir.dt.uint32
AF = mybir.ActivationFunctionType
ALU = mybir.AluOpType
AX = mybir.AxisListType

D = 1024
T = 2048
TC = 256
TT = T + TC
NTILE = TT // 128
DEPTH = 4
NE = 16
CAP_L = 256
CAP_C = 32
NSLOT = CAP_L + CAP_C
EPS = 1e-6

ENGS = ("pe", "dve", "act", "pool", "sp")
DMA_SLOTS = {"sp": 24, "act": 8, "pool": 24}


class Op:
    __slots__ = ("eng", "fn", "deps", "dma", "needs_inc", "sem", "val", "slot", "prev_slot_op", "seq")

    def __init__(self, eng, fn, dma):
        self.eng = eng
        self.fn = fn
        self.dma = dma
        self.deps = []
        self.needs_inc = False
        self.sem = None
        self.val = None
        self.slot = None
        self.prev_slot_op = None


class Prog:
    def __init__(self, nc):
        self.nc = nc
        self.ops = {e: [] for e in ENGS}
        self.last_w = {}
        self.readers = {}
        self.dma_rr = {q: 0 for q in DMA_SLOTS}
        self.slot_last = {}

    def _add(self, eng, fn, reads, writes, dma):
        op = Op(eng, fn, dma)
        deps = []
        for k in reads:
            w = self.last_w.get(k)
            if w is not None:
                deps.append(w)
        for k in writes:
            w = self.last_w.get(k)
            if w is not None:
                deps.append(w)
            deps.extend(self.readers.get(k, ()))
        seen = set()
        latest = {}
        for d in deps:
            if id(d) in seen:
                continue
            seen.add(id(d))
            if (not dma) and (not d.dma) and d.eng == eng and eng == "pe":
                continue
            if d.dma:
                op.deps.append(d)
                d.needs_inc = True
            else:
                cur = latest.get(d.eng)
                if cur is None or d.seq > cur.seq:
                    latest[d.eng] = d
        for d in latest.values():
            op.deps.append(d)
            d.needs_inc = True
        if dma:
            k = self.dma_rr[eng]
            self.dma_rr[eng] = (k + 1) % DMA_SLOTS[eng]
            op.slot = (eng, k)
            op.prev_slot_op = self.slot_last.get(op.slot)
            self.slot_last[op.slot] = op
        for k in reads:
            self.readers.setdefault(k, []).append(op)
        for k in writes:
            self.last_w[k] = op
            self.readers[k] = []
        op.seq = len(self.ops[eng])
        self.ops[eng].append(op)
        return op

    def pe(self, fn, reads=(), writes=()):
        return self._add("pe", fn, reads, writes, False)

    def dve(self, fn, reads=(), writes=()):
        return self._add("dve", fn, reads, writes, False)

    def act(self, fn, reads=(), writes=()):
        return self._add("act", fn, reads, writes, False)

    def pool(self, fn, reads=(), writes=()):
        return self._add("pool", fn, reads, writes, False)

    def dma(self, fn, reads=(), writes=(), q="sp"):
        return self._add(q, fn, reads, writes, True)

    def barrier(self):
        lasts = []
        for e in ENGS:
            for op in reversed(self.ops[e]):
                if not op.dma:
                    lasts.append(op)
                    break
        lasts.extend(self.slot_last.values())
        for e in ENGS:
            op = Op(e, lambda eng: eng.nop(), False)
            for d in lasts:
                if d.eng == e and not d.dma and e == "pe":
                    continue
                op.deps.append(d)
                d.needs_inc = True
            op.seq = len(self.ops[e])
            self.ops[e].append(op)
        self.last_w = {}
        self.readers = {}

    def finalize(self, final_ops=()):
        nc = self.nc
        with ExitStack() as es:
            esem = {e: es.enter_context(nc.semaphore("s_" + e)) for e in ("pe", "dve", "act", "pool", "sp")}
            ssem = {}
            for q, n in DMA_SLOTS.items():
                for k in range(n):
                    ssem[(q, k)] = es.enter_context(nc.semaphore("d_%s%d" % (q, k)))
            for e in ENGS:
                cnt = 0
                for op in self.ops[e]:
                    if (not op.dma) and op.needs_inc:
                        cnt += 1
                        op.sem = esem[e]
                        op.val = cnt
            slot_cnt = {}
            for e in ENGS:
                for op in self.ops[e]:
                    if op.dma:
                        c = slot_cnt.get(op.slot, 0) + 16
                        slot_cnt[op.slot] = c
                        op.sem = ssem[op.slot]
                        op.val = c
            block = es.enter_context(nc.Block())
            finals = list(final_ops)

            def emit(e, engobj):
                waited = {}

                def w(s, v):
                    if waited.get(id(s), 0) >= v:
                        return
                    waited[id(s)] = v
                    engobj.wait_ge(s, v)

                for op in self.ops[e]:
                    for d in op.deps:
                        w(d.sem, d.val)
                    if op.dma and op.prev_slot_op is not None:
                        w(op.prev_slot_op.sem, op.prev_slot_op.val)
                    ins = op.fn(engobj)
                    if op.dma:
                        ins.then_inc(op.sem, 16)
                    elif op.needs_inc:
                        ins.then_inc(op.sem, 1)
                if e == "sp":
                    for f in finals:
                        w(f.sem, f.val)

            @block.tensor
            def _(eng):
                emit("pe", eng)

            @block.vector
            def _(eng):
                emit("dve", eng)

            @block.scalar
            def _(eng):
                emit("act", eng)

            @block.gpsimd
            def _(eng):
                emit("pool", eng)

            @block.sync
            def _(eng):
                emit("sp", eng)


def _tok_tiles():
    return [(0, TC)] + [(TC + i * 512, 512) for i in range(T // 512)]


SEQS = ((0, TC), (TC, T))


def _bf(a):
    import ml_dtypes
    return np.ascontiguousarray(np.asarray(a, dtype=np.float32).astype(ml_dtypes.bfloat16))


def _dft_tables(L):
    N = 3 * L // 2
    nf = N // 2
    npair = (nf + 127) // 128
    f = np.arange(npair * 128, dtype=np.float64)
    t = np.arange(L, dtype=np.float64)
    th = 2.0 * np.pi * np.outer(t, f + 0.5) / N
    valid = (f < nf)[None, :]
    fc = np.where(valid, np.cos(th), 0.0)
    fs = np.where(valid, -np.sin(th), 0.0)
    nt = L // 128
    fw = np.zeros((npair, 128, nt, 256))
    for i in range(npair):
        blkc = fc[:, i * 128:(i + 1) * 128].reshape(nt, 128, 128).transpose(1, 0, 2)
        blks = fs[:, i * 128:(i + 1) * 128].reshape(nt, 128, 128).transpose(1, 0, 2)
        fw[i, :, :, 0:128] = blkc
        fw[i, :, :, 128:256] = blks
    thi = 2.0 * np.pi * np.outer(f + 0.5, t + L // 2) / N
    validf = (f < nf)[:, None]
    ic = np.where(validf, (2.0 / N) * np.cos(thi), 0.0)
    isn = np.where(validf, -(2.0 / N) * np.sin(thi), 0.0)
    inv = np.stack([ic.reshape(npair, 128, L).transpose(1, 0, 2), isn.reshape(npair, 128, L).transpose(1, 0, 2)], 0)
    return fw, inv, npair


def _filter_tables(L):
    t = np.arange(L, dtype=np.float64)
    t_unit = np.linspace(0.0, 1.0, L)
    bands = np.linspace(1e-4, 16 - 1, 16)
    ang = (2.0 * np.pi / L) * t[:, None] * bands[None]
    z = np.concatenate([t_unit[:, None], np.cos(ang), -np.sin(ang)], axis=-1)
    centre = L // 2
    dist = np.abs(t - centre) / max(centre, 1)
    mn = math.log(1e-2) / 1.5
    mx = math.log(1e-2) / 0.3
    decay = np.abs(np.linspace(mn, mx, 512))
    win = np.exp(-dist[:, None] * decay[None])
    return z.T, win


def host_constants():
    c = {}
    c["ident"] = np.eye(128, dtype=np.float32)
    tt = np.arange(T)
    row = (tt // 64).astype(np.float64)
    col = (tt % 64).astype(np.float64)
    inv = 10000.0 ** (-np.arange(16, dtype=np.float64) / 16)
    cos = np.zeros((128, T))
    sin = np.zeros((128, T))
    for p in range(128):
        d = p % 64
        pos = row if d < 32 else col
        a = pos * inv[d % 16]
        cos[p] = np.cos(a)
        sin[p] = np.sin(a)
    c["ropec"] = _bf(cos)
    c["ropes"] = _bf(sin)
    rt = np.zeros((128, 128))
    for m in range(128):
        if m % 32 < 16:
            rt[m + 16, m] = -1.0
        else:
            rt[m - 16, m] = 1.0
    c["ropert"] = _bf(rt)
    bo = np.zeros((128, 128), dtype=np.float32)
    bo[0:64, 0:64] = 1.0
    bo[64:128, 64:128] = 1.0
    c["blockones"] = bo
    zl, wl = _filter_tables(T)
    zc, wc = _filter_tables(TC)
    c["zT"] = np.ascontiguousarray(np.concatenate([zc, zl], axis=1).astype(np.float32))
    c["win"] = np.ascontiguousarray(np.concatenate([wc, wl], axis=0).astype(np.float32))
    fwl, invl, _ = _dft_tables(T)
    fwc, invc, _ = _dft_tables(TC)
    c["fwl"] = _bf(fwl)
    c["fwc"] = _bf(fwc)
    il = invl.reshape(2, 128, 12, 4, 512).transpose(3, 0, 1, 2, 4)
    c["invl"] = _bf(il)
    c["invc"] = _bf(invc)
    return c


def build(n_layers=DEPTH, dbg=False):
    nc = bass.Bass("TRN2", target_bir_lowering=False)

    def din(name, shape, dt=F32):
        return nc.dram_tensor(name, list(shape), dt, kind="ExternalInput").ap()

    def dint(name, shape, dt=F32):
        return nc.dram_tensor(name, list(shape), dt, kind="Internal").ap()

    x_d = din("x", [T, D])
    ctx_d = din("ctx", [TC, D])
    cT_d = din("cT", [128, 8, 2])
    w_ada_d = din("w_ada", [DEPTH, D, 6 * D])
    b_ada_d = din("b_ada", [DEPTH, 6 * D])
    g_mix_d = din("g_mix", [DEPTH, D])
    g_ffn_d = din("g_ffn", [DEPTH, D])
    w_in_ab_d = din("w_in_ab", [2, D, 2560])
    ca_d = din("ca", [2, 128, 4, 3])
    cb_d = din("cb", [2, 128, 4, 31])
    lnp_d = din("lnp", [2, 128, 3, 4])
    w_out_ab_d = din("w_out_ab", [2, D, D])
    wr_d = din("wr", [DEPTH, 128, 8, NE])
    w_gate_d = din("w_gate", [DEPTH, NE, D, D])
    w_up_d = din("w_up", [DEPTH, NE, D, D])
    w_down_d = din("w_down", [DEPTH, NE, D, D])
    ident_d = din("ident", [128, 128])
    w_in_cd_d = din("w_in_cd", [2, D, 3072])
    w_out_cd_d = din("w_out_cd", [2, D, D])
    gqk_d = din("gqk", [2, 128, 2])
    lamv_d = din("lamv", [2, 4 * 64])
    gsub_d = din("gsub", [2, 128, 1])
    cd_d = din("cd", [2, 128, 12, 3])
    hfw1_d = din("hf_w1", [2, 33, 64])
    hfv_d = din("hfv", [2, 64, 3])
    hfw2_d = din("hf_w2", [2, 64, 64])
    hfw3_d = din("hf_w3", [2, 64, 512])
    hfb_d = din("hf_bias", [2, 512])
    ropec_d = din("ropec", [128, T], BF16)
    ropes_d = din("ropes", [128, T], BF16)
    ropert_d = din("ropert", [128, 128], BF16)
    blockones_d = din("blockones", [128, 128])
    zT_d = din("zT", [33, TT])
    win_d = din("win", [TT, 512])
    fwl_d = din("fwl", [12, 128, 16, 256], BF16)
    fwc_d = din("fwc", [2, 128, 2, 256], BF16)
    invl_d = din("invl", [4, 2, 128, 12, 512], BF16)
    invc_d = din("invc", [2, 128, 2, 256], BF16)
    out_d = nc.dram_tensor("out", [T, D], F32, kind="ExternalOutput").ap()

    H_d = dint("H", [TT, D])
    modv_d = dint("modv", [2, 6 * D])
    u2tok_d = dint("u2tok", [TT, D], BF16)
    M_d = dint("Macc", [TT, D])
    if dbg:
        hdbg_d = nc.dram_tensor("hdbg", [TT, D], F32, kind="ExternalOutput").ap()
        dbg_ix_d = nc.dram_tensor("dbg_ix", [NE, NSLOT], F32, kind="ExternalOutput").ap()
        dbg_gv_d = nc.dram_tensor("dbg_gv", [NE, NSLOT], F32, kind="ExternalOutput").ap()
        dbg_m_d = nc.dram_tensor("dbg_m", [TT, D], F32, kind="ExternalOutput").ap()
        dbg_xe_d = nc.dram_tensor("dbg_xe", [128, 3, D], BF16, kind="ExternalOutput").ap()
        dbg_yb_d = nc.dram_tensor("dbg_yb", [128, 3, D], F32, kind="ExternalOutput").ap()
        dbg_ht_d = nc.dram_tensor("dbg_ht", [128, 8, NSLOT], BF16, kind="ExternalOutput").ap()
        dbg_ip_d = nc.dram_tensor("dbg_ip", [128, 3, NE], I32, kind="ExternalOutput").ap()

    top = ExitStack()
    P = Prog(nc)

    uid = [0]

    def sb(es, name, shape, dt=F32):
        uid[0] += 1
        return es.enter_context(nc.sbuf_tensor("%s_%d" % (name, uid[0]), list(shape), dt))

    PSD = [top.enter_context(nc.psum_tensor("psd%d" % i, [128, 1024], F32)) for i in range(4)]
    PS = [PSD[i // 2][:, (i % 2) * 512:(i % 2 + 1) * 512] for i in range(8)]
    PK = ["ps%d" % i for i in range(8)]

    ident_f = sb(top, "ident_f", [128, 128])
    ident_b = sb(top, "ident_b", [128, 128], BF16)
    ones_f = sb(top, "ones_f", [128, 128])
    ones_b = sb(top, "ones_b", [128, 128], BF16)
    zero_f = sb(top, "zero_f", [128, 1024])
    P.dma(lambda e: e.dma_start(out=ident_f[:], in_=ident_d), writes=["ident_f"])
    P.dve(lambda e: e.tensor_copy(ident_b[:], ident_f[:]), reads=["ident_f"], writes=["ident_b"])
    P.dve(lambda e: e.memset(ones_f[:], 1.0), writes=["ones_f"])
    P.dve(lambda e: e.memset(ones_b[:], 1.0), writes=["ones_b"])
    P.dve(lambda e: e.memset(zero_f[:], 0.0), writes=["zero_f"])

    def hsrc(l, tt):
        if l == 0:
            if tt < 2:
                return ctx_d[tt * 128:(tt + 1) * 128, :]
            return x_d[(tt - 2) * 128:(tt - 1) * 128, :]
        return H_d[tt * 128:(tt + 1) * 128, :]

    def hkey(tt):
        return ("H", tt)

    def phase_mod(l):
        with ExitStack() as es:
            cS = sb(es, "cS", [128, 8, 2])
            modr = sb(es, "modr", [2, 6 * D])
            brow = sb(es, "brow", [2, 6 * D])
            grow = sb(es, "grow", [2, 2, D])
            wbuf = [sb(es, "wada%d" % i, [128, 8, 512]) for i in range(2)]
            P.dma(lambda e: e.dma_start(out=cS[:], in_=cT_d), writes=["cS"])
            P.act(lambda e: e.activation(out=cS[:], in_=cS[:], func=AF.Silu), reads=["cS"], writes=["cS"])
            for r in range(2):
                P.dma(lambda e, r=r: e.dma_start(out=brow[r:r + 1, :], in_=b_ada_d[l:l + 1, :]), writes=["brow"])
                P.dma(lambda e, r=r: e.dma_start(out=grow[r:r + 1, 0, :], in_=g_mix_d[l:l + 1, :]), writes=["grow"])
                P.dma(lambda e, r=r: e.dma_start(out=grow[r:r + 1, 1, :], in_=g_ffn_d[l:l + 1, :]), writes=["grow"])
            wv = w_ada_d[l].rearrange("(k p) n -> p k n", p=128)
            for nt in range(12):
                wb = wbuf[nt % 2]
                wk = "wada%d" % (nt % 2)
                P.dma(lambda e, wb=wb, nt=nt: e.dma_start(out=wb[:], in_=wv[:, :, nt * 512:(nt + 1) * 512]),
                      writes=[wk])
                pk = nt % 2
                for k in range(8):
                    P.pe(lambda e, wb=wb, k=k, pk=pk: e.matmul(PS[pk][0:2, :], cS[:, k, :], wb[:, k, :],
                                                                  start=(k == 0), stop=(k == 7)),
                         reads=[wk, "cS"], writes=[PK[pk]])
                P.dve(lambda e, nt=nt, pk=pk: e.tensor_tensor(out=modr[:, nt * 512:(nt + 1) * 512], in0=PS[pk][0:2, :],
                                                               in1=brow[:, nt * 512:(nt + 1) * 512], op=ALU.add),
                      reads=[PK[pk], "brow"], writes=["modr"])
            for (seg, gi) in ((1, 0), (4, 1)):
                P.dve(lambda e, seg=seg, gi=gi: e.scalar_tensor_tensor(
                    out=modr[:, seg * D:(seg + 1) * D], in0=modr[:, seg * D:(seg + 1) * D], scalar=1.0,
                    in1=grow[:, gi, :], op0=ALU.add, op1=ALU.mult), reads=["modr", "grow"], writes=["modr"])
            P.dma(lambda e: e.dma_start(out=modv_d, in_=modr[:]), reads=["modr"], writes=["modv"])
        P.barrier()

    def load_mod(tile, key, r, seg):
        P.dma(lambda e: e.dma_start(out=tile[:], in_=modv_d[r:r + 1, seg * D:(seg + 1) * D].partition_broadcast(128)),
              reads=["modv"], writes=[key])

    def phase_norm(l, hl, which, U, AFF=None):
        seg_s, seg_a = (0, 1) if which == 1 else (3, 4)
        with ExitStack() as es:
            A = [sb(es, "nA%d" % r, [128, D]) for r in range(2)]
            S = [sb(es, "nS%d" % r, [128, D]) for r in range(2)]
            for r in range(2):
                load_mod(A[r], "nA%d" % r, r, seg_a)
                load_mod(S[r], "nS%d" % r, r, seg_s)
            xt = [sb(es, "nx%d" % i, [128, D]) for i in range(2)]
            junk = sb(es, "njunk", [128, D], BF16)
            ss = [sb(es, "nss%d" % i, [128, 2]) for i in range(2)]
            tmp = [sb(es, "ntmp%d" % i, [128, D]) for i in range(2)]
            if which == 1:
                ub = [sb(es, "nub%d" % i, [128, D], BF16) for i in range(2)]
            else:
                ub = [sb(es, "nub%d" % i, [128, D], BF16) for i in range(2)]
                uT = [sb(es, "nuT%d" % i, [128, 8, 128]) for i in range(2)]
                wr = sb(es, "nwr", [128, 8, NE])
                sm = [sb(es, "nsm%d" % i, [128, 4]) for i in range(2)]
                ex = [sb(es, "nex%d" % i, [128, NE]) for i in range(2)]
                P.dma(lambda e: e.dma_start(out=wr[:], in_=wr_d[l]), writes=["nwr"])
            for tt in range(NTILE):
                b = tt % 2
                r = 1 if tt < 2 else 0
                kx, kss, ktmp, kub = "nx%d" % b, "nss%d" % b, "ntmp%d" % b, "nub%d" % b
                P.dma(lambda e, b=b, tt=tt: e.dma_start(out=xt[b][:], in_=hsrc(hl, tt)), reads=[hkey(tt)], writes=[kx])
                P.act(lambda e, b=b: e.activation(out=junk[:], in_=xt[b][:], func=AF.Square, accum_out=ss[b][:, 0:1]),
                      reads=[kx], writes=["njunk", kss])
                P.act(lambda e, b=b: e.activation(out=ss[b][:, 1:2], in_=ss[b][:, 0:1], func=AF.Sqrt,
                                                  scale=1.0 / D, bias=EPS), reads=[kss], writes=[kss])
                P.dve(lambda e, b=b: e.reciprocal(ss[b][:, 1:2], ss[b][:, 1:2]), reads=[kss], writes=[kss])
                P.dve(lambda e, b=b, r=r: e.scalar_tensor_tensor(out=tmp[b][:], in0=xt[b][:], scalar=ss[b][:, 1:2],
                                                                   in1=A[r][:], op0=ALU.mult, op1=ALU.mult),
                      reads=[kx, kss, "nA%d" % r], writes=[ktmp])
                if which == 1:
                    P.pool(lambda e, b=b, r=r: e.tensor_tensor(out=ub[b][:], in0=tmp[b][:], in1=S[r][:], op=ALU.add),
                           reads=[ktmp, "nS%d" % r], writes=[kub])
                    psb = 2 + b
                    pv = PS[psb][:].bitcast(BF16)
                    for k in range(8):
                        P.pe(lambda e, b=b, k=k, pv=pv: e.transpose(pv[:, k * 128:(k + 1) * 128],
                                                                     ub[b][:, k * 128:(k + 1) * 128], ident_b[:]),
                             reads=[kub, "ident_b"], writes=[PK[psb]])
                    P.act(lambda e, tt=tt, pv=pv: e.activation(
                        out=U[:, :, tt * 128:(tt + 1) * 128], in_=pv.rearrange("p (k t) -> p k t", k=8), func=AF.Copy),
                        reads=[PK[psb]], writes=[("U", tt)])
                else:
                    P.pool(lambda e, b=b, r=r: e.tensor_tensor(out=tmp[b][:], in0=tmp[b][:], in1=S[r][:], op=ALU.add),
                           reads=[ktmp, "nS%d" % r], writes=[ktmp])
                    P.act(lambda e, b=b: e.activation(out=ub[b][:], in_=tmp[b][:], func=AF.Copy),
                          reads=[ktmp], writes=[kub])
                    P.dma(lambda e, b=b, tt=tt: e.dma_start(out=u2tok_d[tt * 128:(tt + 1) * 128, :], in_=ub[b][:]),
                          reads=[kub], writes=[("u2tok", tt)])
                    kuT = "nuT%d" % b
                    for hh in range(2):
                        psb = 2 + 2 * b + hh
                        for k4 in range(4):
                            k = hh * 4 + k4
                            P.pe(lambda e, b=b, k=k, k4=k4, psb=psb: e.transpose(
                                PS[psb][:, k4 * 128:(k4 + 1) * 128], tmp[b][:, k * 128:(k + 1) * 128], ident_f[:]),
                                reads=[ktmp, "ident_f"], writes=[PK[psb]])
                        P.dve(lambda e, b=b, hh=hh, psb=psb: e.tensor_copy(
                            uT[b][:, hh * 4:(hh + 1) * 4, :], PS[psb][:].rearrange("p (k t) -> p k t", k=4)),
                            reads=[PK[psb]], writes=[kuT])
                    pl = 6 + b
                    for k in range(8):
                        P.pe(lambda e, b=b, k=k, pl=pl: e.matmul(PS[pl][:, 0:NE], uT[b][:, k, :], wr[:, k, :],
                                                                   start=(k == 0), stop=(k == 7)),
                             reads=[kuT, "nwr"], writes=[PK[pl]])
                    ksm, kex = "nsm%d" % b, "nex%d" % b
                    P.dve(lambda e, b=b, pl=pl: e.reduce_max(out=sm[b][:, 0:1], in_=PS[pl][:, 0:NE], axis=AX.X),
                          reads=[PK[pl]], writes=[ksm])
                    P.dve(lambda e, b=b: e.tensor_scalar(out=sm[b][:, 1:2], in0=sm[b][:, 0:1], scalar1=-1.0,
                                                         scalar2=None, op0=ALU.mult), reads=[ksm], writes=[ksm])
                    P.act(lambda e, b=b, pl=pl: e.activation(out=ex[b][:], in_=PS[pl][:, 0:NE], func=AF.Exp,
                                                             bias=sm[b][:, 1:2], scale=1.0, accum_out=sm[b][:, 2:3]),
                          reads=[PK[pl], ksm], writes=[kex, ksm])
                    P.dve(lambda e, b=b: e.reciprocal(sm[b][:, 3:4], sm[b][:, 2:3]), reads=[ksm], writes=[ksm])
                    P.dve(lambda e, b=b, tt=tt: e.tensor_scalar(out=AFF[:, tt, :], in0=ex[b][:], scalar1=sm[b][:, 3:4],
                                                                scalar2=None, op0=ALU.mult),
                          reads=[kex, ksm], writes=[("AFF", tt)])
        P.barrier()

    def phase_outproj(l, Y, w_out_ap, last_layer_unused=False):
        with ExitStack() as es:
            Wo = sb(es, "Wo", [128, 8, D], BF16)
            G = [sb(es, "oG%d" % r, [128, D]) for r in range(2)]
            xt = [sb(es, "ox%d" % i, [128, D]) for i in range(2)]
            tm = [sb(es, "ot%d" % i, [128, D]) for i in range(2)]
            wv = w_out_ap.rearrange("(k p) n -> p k n", p=128)
            for k in range(8):
                P.dma(lambda e, k=k: e.dma_start(out=Wo[:, k, :], in_=wv[:, k, :]), writes=[("Wo", k)], q="pool")
            for r in range(2):
                load_mod(G[r], "oG%d" % r, r, 2)
            for tt in range(NTILE):
                b = tt % 2
                r = 1 if tt < 2 else 0
                kx, kt = "ox%d" % b, "ot%d" % b
                P.dma(lambda e, b=b, tt=tt: e.dma_start(out=xt[b][:], in_=hsrc(l, tt)), reads=[hkey(tt)], writes=[kx])
                for hf in range(2):
                    pb = 2 * b + hf
                    for k in range(8):
                        P.pe(lambda e, k=k, tt=tt, hf=hf, pb=pb: e.matmul(
                            PS[pb][:], Y[:, k, tt * 128:(tt + 1) * 128], Wo[:, k, hf * 512:(hf + 1) * 512],
                            start=(k == 0), stop=(k == 7)), reads=[("Y", tt), ("Wo", k)], writes=[PK[pb]])
                    P.dve(lambda e, b=b, hf=hf, pb=pb, r=r: e.tensor_tensor(
                        out=tm[b][:, hf * 512:(hf + 1) * 512], in0=PS[pb][:], in1=G[r][:, hf * 512:(hf + 1) * 512],
                        op=ALU.mult), reads=[PK[pb], "oG%d" % r], writes=[kt])
                P.pool(lambda e, b=b: e.tensor_tensor(out=xt[b][:], in0=xt[b][:], in1=tm[b][:], op=ALU.add),
                       reads=[kx, kt], writes=[kx])
                P.dma(lambda e, b=b, tt=tt: e.dma_start(out=H_d[tt * 128:(tt + 1) * 128, :], in_=xt[b][:]),
                      reads=[kx], writes=[hkey(tt)])
        P.barrier()

    def phase_conv(l, U, Y):
        i2 = l // 2
        tiles = _tok_tiles()
        with ExitStack() as es:
            Wg = [sb(es, "Wg%d" % g, [128, 8, 512], BF16) for g in range(5)]
            wv = w_in_ab_d[i2].rearrange("(k p) n -> p k n", p=128)
            for g in range(5):
                for k in range(8):
                    P.dma(lambda e, g=g, k=k: e.dma_start(out=Wg[g][:, k, :], in_=wv[:, k, g * 512:(g + 1) * 512]),
                          writes=[("Wg", g)], q="pool")
            CA = sb(es, "CA", [128, 4, 3])
            CB = sb(es, "CB", [128, 4, 31])
            LNP = sb(es, "LNP", [128, 3, 4])
            P.dma(lambda e: e.dma_start(out=CA[:], in_=ca_d[i2]), writes=["CA"])
            P.dma(lambda e: e.dma_start(out=CB[:], in_=cb_d[i2]), writes=["CB"])
            P.dma(lambda e: e.dma_start(out=LNP[:], in_=lnp_d[i2]), writes=["LNP"])
            B = [sb(es, "cB%d" % i, [128, TT]) for i in range(3)]
            ZC = sb(es, "ZC", [128, 4, TT])
            Dg = [sb(es, "Dg%d" % i, [128, 31, 128], BF16) for i in range(2)]
            Zb = sb(es, "Zb", [128, TT], BF16)

            psrr = [0]

            def proj(chunk, dst, dkey):
                g, c = chunk // 4, chunk % 4
                for (t0, n) in tiles:
                    pb = psrr[0] % 4
                    psrr[0] += 1
                    for k in range(8):
                        P.pe(lambda e, g=g, c=c, k=k, t0=t0, n=n, pb=pb: e.matmul(
                            PS[pb][:, 0:n], Wg[g][:, k, c * 128:(c + 1) * 128], U[:, k, t0:t0 + n],
                            start=(k == 0), stop=(k == 7)),
                            reads=[("Wg", g)] + [("U", t) for t in range(t0 // 128, (t0 + n) // 128)],
                            writes=[PK[pb]])
                    P.act(lambda e, t0=t0, n=n, pb=pb: e.activation(out=dst[:, t0:t0 + n], in_=PS[pb][:, 0:n],
                                                                      func=AF.Copy), reads=[PK[pb]], writes=[dkey])

            for i in range(4):
                proj(i, B[0], "cB0")
                proj(4 + i, B[1], "cB1")
                proj(8 + i, B[2], "cB2")
                P.dve(lambda e: e.tensor_tensor(out=B[1][:], in0=B[1][:], in1=B[2][:], op=ALU.mult),
                      reads=["cB1", "cB2"], writes=["cB1"])
                P.dve(lambda e, i=i: e.tensor_scalar(out=B[2][:], in0=B[1][:], scalar1=CA[:, i, 1:2], scalar2=None,
                                                      op0=ALU.mult), reads=["cB1", "CA"], writes=["cB2"])
                for (s0, L) in SEQS:
                    P.dve(lambda e, i=i, s0=s0, L=L: e.scalar_tensor_tensor(
                        out=B[2][:, s0 + 1:s0 + L], in0=B[1][:, s0:s0 + L - 1], scalar=CA[:, i, 0:1],
                        in1=B[2][:, s0 + 1:s0 + L], op0=ALU.mult, op1=ALU.add), reads=["cB1", "cB2", "CA"],
                        writes=["cB2"])
                    P.dve(lambda e, i=i, s0=s0, L=L: e.scalar_tensor_tensor(
                        out=B[2][:, s0:s0 + L - 1], in0=B[1][:, s0 + 1:s0 + L], scalar=CA[:, i, 2:3],
                        in1=B[2][:, s0:s0 + L - 1], op0=ALU.mult, op1=ALU.add), reads=["cB1", "cB2", "CA"],
                        writes=["cB2"])
                P.dve(lambda e, i=i: e.tensor_tensor(out=Y[:, i, :], in0=B[0][:], in1=B[2][:], op=ALU.mult),
                      reads=["cB0", "cB2"], writes=[("Y", t) for t in range(NTILE)])
            for i in range(4):
                proj(12 + i, B[0], "cB0")
                proj(16 + i, B[1], "cB1")
                P.act(lambda e: e.activation(out=B[1][:], in_=B[1][:], func=AF.Sigmoid), reads=["cB1"], writes=["cB1"])
                P.dve(lambda e: e.tensor_tensor(out=Zb[:], in0=B[0][:], in1=B[1][:], op=ALU.mult),
                      reads=["cB0", "cB1"], writes=["Zb"])
                zk = ("ZC", i)
                dgi = i % 2
                dg = Dg[dgi]
                dgk = "Dg%d" % dgi
                for kk in range(31):
                    P.pool(lambda e, i=i, kk=kk, dg=dg: e.tensor_scalar(out=dg[:, kk, :], in0=ident_b[:],
                                                                        scalar1=CB[:, i, kk:kk + 1], scalar2=None,
                                                                        op0=ALU.mult), reads=["ident_b", "CB"], writes=[dgk])
                for ti, (t0, n) in enumerate(tiles):
                    s0, L = SEQS[0] if t0 < TC else SEQS[1]
                    pb = 6 + (ti % 2)
                    taps = [15] + [kk for kk in range(31) if kk != 15]
                    for q, kk in enumerate(taps):
                        o = kk - 15
                        a = max(0, s0 - o - t0)
                        bnd = min(n, s0 + L - o - t0)
                        P.pe(lambda e, dg=dg, kk=kk, o=o, a=a, bnd=bnd, t0=t0, pb=pb, q=q: e.matmul(
                            PS[pb][:, a:bnd], dg[:, kk, :], Zb[:, t0 + a + o:t0 + bnd + o],
                            start=(q == 0), stop=(q == 30)), reads=[dgk, "Zb"], writes=[PK[pb]])
                    P.act(lambda e, i=i, t0=t0, n=n, pb=pb: e.activation(out=ZC[:, i, t0:t0 + n], in_=PS[pb][:, 0:n],
                                                                          func=AF.Identity, bias=LNP[:, 0, i:i + 1], scale=1.0),
                          reads=[PK[pb], "LNP"], writes=[zk])
            MEAN = B[1]
            RSTD = B[2]
            SQ = [sb(es, "cSQ%d" % i, [128, 512]) for i in range(2)]
            sqi = 0
            for (t0, n) in tiles:
                for i in range(4):
                    sq = SQ[sqi % 2]
                    sqk = "cSQ%d" % (sqi % 2)
                    sqi += 1
                    P.act(lambda e, i=i, t0=t0, n=n, sq=sq: e.activation(out=sq[:, 0:n], in_=ZC[:, i, t0:t0 + n],
                                                                          func=AF.Square), reads=[("ZC", i)], writes=[sqk])
                    P.pe(lambda e, i=i, t0=t0, n=n: e.matmul(PS[4][:, 0:n], ones_f[:], ZC[:, i, t0:t0 + n],
                                                             start=(i == 0), stop=(i == 3)),
                         reads=[("ZC", i), "ones_f"], writes=[PK[4]])
                    P.pe(lambda e, i=i, n=n, sq=sq: e.matmul(PS[5][:, 0:n], ones_f[:], sq[:, 0:n],
                                                              start=(i == 0), stop=(i == 3)),
                         reads=[sqk, "ones_f"], writes=[PK[5]])
                P.dve(lambda e, t0=t0, n=n: e.tensor_scalar(out=MEAN[:, t0:t0 + n], in0=PS[4][:, 0:n], scalar1=1.0 / 512,
                                                             scalar2=None, op0=ALU.mult), reads=[PK[4]], writes=["cB1"])
                P.dve(lambda e, t0=t0, n=n: e.tensor_tensor(out=B[0][:, t0:t0 + n], in0=MEAN[:, t0:t0 + n],
                                                             in1=MEAN[:, t0:t0 + n], op=ALU.mult),
                      reads=["cB1"], writes=["cB0"])
                P.dve(lambda e, t0=t0, n=n: e.scalar_tensor_tensor(
                    out=RSTD[:, t0:t0 + n], in0=PS[5][:, 0:n], scalar=1.0 / 512, in1=B[0][:, t0:t0 + n],
                    op0=ALU.mult, op1=ALU.subtract), reads=[PK[5], "cB0"], writes=["cB2"])
            P.act(lambda e: e.activation(out=RSTD[:], in_=RSTD[:], func=AF.Sqrt, scale=1.0, bias=EPS),
                  reads=["cB2"], writes=["cB2"])
            P.dve(lambda e: e.reciprocal(RSTD[:], RSTD[:]), reads=["cB2"], writes=["cB2"])
            for i in range(4):
                zk = ("ZC", i)
                P.dve(lambda e, i=i: e.tensor_tensor(out=ZC[:, i, :], in0=ZC[:, i, :], in1=MEAN[:], op=ALU.subtract),
                      reads=[zk, "cB1"], writes=[zk])
                P.dve(lambda e, i=i: e.tensor_tensor(out=ZC[:, i, :], in0=ZC[:, i, :], in1=RSTD[:], op=ALU.mult),
                      reads=[zk, "cB2"], writes=[zk])
                P.act(lambda e, i=i: e.activation(out=Y[:, 4 + i, :], in_=ZC[:, i, :], func=AF.Silu,
                                                  scale=LNP[:, 1, i:i + 1], bias=LNP[:, 2, i:i + 1]),
                      reads=[zk, "LNP"], writes=[("Y", t) for t in range(NTILE)])
        P.barrier()


    def load_wgroup(Wt, key, src3, g):
        for k in range(8):
            P.dma(lambda e, k=k: e.dma_start(out=Wt[:, k, :], in_=src3[:, k, g * 512:(g + 1) * 512]),
                  writes=[(key, k)], q="pool")

    def phase_attn(l, U, Y):
        i2 = l // 2
        lam_init = 0.8 - 0.6 * math.exp(-0.3 * l)
        tiles = _tok_tiles()
        wv = w_in_cd_d[i2].rearrange("(k p) n -> p k n", p=128)
        with ExitStack() as es:
            qkT = [sb(es, "qT", [128, 4, TT], BF16), sb(es, "kT", [128, 4, TT], BF16)]
            V = sb(es, "V", [128, NTILE, 512], BF16)
            small = sb(es, "asmall", [128, 8])
            lamb = sb(es, "lamb", [128, 4 * 64])
            GQK = sb(es, "GQK", [128, 2])
            GS = sb(es, "GS", [128, 1])
            with ExitStack() as es2:
                Wt = [sb(es2, "Wt%d" % i, [128, 8, 512], BF16) for i in range(2)]
                COS = sb(es2, "COS", [128, T], BF16)
                SIN = sb(es2, "SIN", [128, T], BF16)
                RT = sb(es2, "RT", [128, 128], BF16)
                BO = sb(es2, "BO", [128, 128])
                QK = sb(es2, "QK", [128, TT])
                SQ = [sb(es2, "aSQ%d" % i, [128, 512]) for i in range(2)]
                RS = [sb(es2, "aRS%d" % i, [128, 512]) for i in range(2)]
                QN = [sb(es2, "aQN%d" % i, [128, 512], BF16) for i in range(2)]
                O1 = [sb(es2, "aO1%d" % i, [128, 512]) for i in range(2)]
                P.dma(lambda e: e.dma_start(out=COS[:], in_=ropec_d), writes=["COS"])
                P.dma(lambda e: e.dma_start(out=SIN[:], in_=ropes_d), writes=["SIN"])
                P.dma(lambda e: e.dma_start(out=RT[:], in_=ropert_d), writes=["RT"])
                P.dma(lambda e: e.dma_start(out=BO[:], in_=blockones_d), writes=["BO"])
                P.dma(lambda e: e.dma_start(out=GQK[:], in_=gqk_d[i2]), writes=["GQK"])
                P.dma(lambda e: e.dma_start(out=GS[:], in_=gsub_d[i2]), writes=["GS"])
                P.dma(lambda e: e.dma_start(out=lamb[:], in_=lamv_d[i2:i2 + 1, :].partition_broadcast(128)),
                      writes=["lamb"])
                for j in range(2):
                    P.dve(lambda e, j=j: e.tensor_tensor(out=lamb[:, j * 128:j * 128 + 64], in0=lamb[:, j * 128:j * 128 + 64],
                                                          in1=lamb[:, j * 128 + 64:j * 128 + 128], op=ALU.mult),
                          reads=["lamb"], writes=["lamb"])
                    P.dve(lambda e, j=j: e.reduce_sum(out=small[:, j:j + 1], in_=lamb[:, j * 128:j * 128 + 64], axis=AX.X),
                          reads=["lamb"], writes=["asmall"])
                P.act(lambda e: e.activation(out=small[:, 2:4], in_=small[:, 0:2], func=AF.Exp),
                      reads=["asmall"], writes=["asmall"])
                P.dve(lambda e: e.scalar_tensor_tensor(out=small[:, 4:5], in0=small[:, 3:4], scalar=-lam_init,
                                                        in1=small[:, 2:3], op0=ALU.add, op1=ALU.subtract),
                      reads=["asmall"], writes=["asmall"])
                P.dve(lambda e: e.tensor_scalar(out=GS[:], in0=GS[:], scalar1=1.0 - lam_init, scalar2=None, op0=ALU.mult),
                      reads=["GS"], writes=["GS"])
                cnt = 0
                for g in range(2):
                    wt = Wt[g % 2]
                    wkey = "Wt%d" % (g % 2)
                    load_wgroup(wt, wkey, wv, g)
                    for h in range(4):
                        for (t0, n) in tiles:
                            pb = cnt % 2
                            bb = cnt % 2
                            cnt += 1
                            for k in range(8):
                                P.pe(lambda e, wt=wt, h=h, k=k, t0=t0, n=n, pb=pb: e.matmul(
                                    PS[pb][:, 0:n], wt[:, k, h * 128:(h + 1) * 128], U[:, k, t0:t0 + n],
                                    start=(k == 0), stop=(k == 7)),
                                    reads=[(wkey, k)] + [("U", t) for t in range(t0 // 128, (t0 + n) // 128)],
                                    writes=[PK[pb]])
                            P.act(lambda e, t0=t0, n=n, pb=pb: e.activation(out=QK[:, t0:t0 + n], in_=PS[pb][:, 0:n],
                                                                              func=AF.Copy), reads=[PK[pb]], writes=["QK"])
                            P.act(lambda e, t0=t0, n=n, bb=bb: e.activation(out=SQ[bb][:, 0:n], in_=QK[:, t0:t0 + n],
                                                                              func=AF.Square), reads=["QK"],
                                  writes=["aSQ%d" % bb])
                            p2 = 2 + pb
                            P.pe(lambda e, n=n, bb=bb, p2=p2: e.matmul(PS[p2][:, 0:n], BO[:], SQ[bb][:, 0:n],
                                                                        start=True, stop=True),
                                 reads=["aSQ%d" % bb, "BO"], writes=[PK[p2]])
                            P.act(lambda e, n=n, bb=bb, p2=p2: e.activation(out=RS[bb][:, 0:n], in_=PS[p2][:, 0:n],
                                                                              func=AF.Sqrt, scale=1.0 / 64, bias=EPS),
                                  reads=[PK[p2]], writes=["aRS%d" % bb])
                            P.dve(lambda e, n=n, bb=bb: e.reciprocal(RS[bb][:, 0:n], RS[bb][:, 0:n]),
                                  reads=["aRS%d" % bb], writes=["aRS%d" % bb])
                            dstk = [("qk", g, h, t) for t in range(t0 // 128, (t0 + n) // 128)]
                            if t0 < TC:
                                P.dve(lambda e, g=g, h=h, t0=t0, n=n, bb=bb: e.scalar_tensor_tensor(
                                    out=qkT[g][:, h, t0:t0 + n], in0=QK[:, t0:t0 + n], scalar=GQK[:, g:g + 1],
                                    in1=RS[bb][:, 0:n], op0=ALU.mult, op1=ALU.mult),
                                    reads=["QK", "GQK", "aRS%d" % bb], writes=dstk)
                            else:
                                P.dve(lambda e, g=g, t0=t0, n=n, bb=bb: e.scalar_tensor_tensor(
                                    out=QN[bb][:, 0:n], in0=QK[:, t0:t0 + n], scalar=GQK[:, g:g + 1],
                                    in1=RS[bb][:, 0:n], op0=ALU.mult, op1=ALU.mult),
                                    reads=["QK", "GQK", "aRS%d" % bb], writes=["aQN%d" % bb])
                                p3 = 4 + pb
                                P.pe(lambda e, n=n, bb=bb, p3=p3: e.matmul(PS[p3][:, 0:n], RT[:], QN[bb][:, 0:n],
                                                                            start=True, stop=True),
                                     reads=["aQN%d" % bb, "RT"], writes=[PK[p3]])
                                c0 = t0 - TC
                                P.pool(lambda e, n=n, bb=bb, c0=c0: e.tensor_tensor(
                                    out=O1[bb][:, 0:n], in0=QN[bb][:, 0:n], in1=COS[:, c0:c0 + n], op=ALU.mult),
                                    reads=["aQN%d" % bb, "COS"], writes=["aO1%d" % bb])
                                P.dve(lambda e, n=n, bb=bb, c0=c0, p3=p3: e.tensor_tensor(
                                    out=RS[bb][:, 0:n], in0=PS[p3][:, 0:n], in1=SIN[:, c0:c0 + n], op=ALU.mult),
                                    reads=[PK[p3], "SIN"], writes=["aRS%d" % bb])
                                P.dve(lambda e, g=g, h=h, t0=t0, n=n, bb=bb: e.tensor_tensor(
                                    out=qkT[g][:, h, t0:t0 + n], in0=O1[bb][:, 0:n], in1=RS[bb][:, 0:n], op=ALU.add),
                                    reads=["aO1%d" % bb, "aRS%d" % bb], writes=dstk)
                wt = Wt[0]
                load_wgroup(wt, "Wt0", wv, 2)
                for tt in range(NTILE):
                    pb = 6 + tt % 2
                    for k in range(8):
                        P.pe(lambda e, tt=tt, k=k, pb=pb: e.matmul(PS[pb][:], U[:, k, tt * 128:(tt + 1) * 128], wt[:, k, :],
                                                                    start=(k == 0), stop=(k == 7)),
                             reads=[("U", tt), ("Wt0", k)], writes=[PK[pb]])
                    P.act(lambda e, tt=tt, pb=pb: e.activation(out=V[:, tt, :], in_=PS[pb][:], func=AF.Copy),
                          reads=[PK[pb]], writes=[("V", tt)])
            P.barrier()
            with ExitStack() as es2:
                Eb = [sb(es2, "Eb%d" % i, [128, 2, 512], BF16) for i in range(3)]
                R0 = sb(es2, "aR0", [128, 512])
                R1 = sb(es2, "aR1", [128, 512])
                OO = sb(es2, "aOO", [128, 512])
                SQ2 = sb(es2, "aSQ2", [128, 512])
                ecnt = 0
                groups = [(TC + i * 512, 512, list(range(NTILE))) for i in range(T // 512)] + [(0, TC, [0, 1])]
                SPD = (0, 3)
                for h in range(4):
                    for (q0, n, ktl) in groups:
                        qkeys = [("qk", 0, h, t) for t in range(q0 // 128, (q0 + n) // 128)]
                        npair = len(ktl) // 2
                        its = [(j, pi) for j in range(2) for pi in range(npair)]

                        def emit_s(idx, q0=q0, n=n, h=h, qkeys=qkeys, ktl=ktl):
                            j, pi = its[idx]
                            sd = SPD[(ecnt + idx) % 2]
                            for half in range(2):
                                kt = ktl[2 * pi + half]
                                P.pe(lambda e, h=h, j=j, kt=kt, q0=q0, n=n, sd=sd, half=half: e.matmul(
                                    PSD[sd][:, half * 512:half * 512 + n],
                                    qkT[1][j * 64:(j + 1) * 64, h, kt * 128:(kt + 1) * 128],
                                    qkT[0][j * 64:(j + 1) * 64, h, q0:q0 + n], start=True, stop=True),
                                    reads=qkeys + [("qk", 1, h, kt)], writes=[PK[2 * sd], PK[2 * sd + 1]])

                        emit_s(0)
                        for idx, (j, pi) in enumerate(its):
                            po, pz = 2 + 2 * j, 3 + 2 * j
                            sd = SPD[(ecnt + idx) % 2]
                            eb = (ecnt + idx) % 3
                            P.act(lambda e, n=n, sd=sd, eb=eb: e.activation(
                                out=Eb[eb][:, :, 0:n], in_=PSD[sd][:].rearrange("p (a b) -> p a b", a=2)[:, :, 0:n],
                                func=AF.Exp, scale=0.125), reads=[PK[2 * sd], PK[2 * sd + 1]], writes=["Eb%d" % eb])
                            if idx + 1 < len(its):
                                emit_s(idx + 1)
                            for half in range(2):
                                kt = ktl[2 * pi + half]
                                first = (pi == 0 and half == 0)
                                lastk = (pi == npair - 1 and half == 1)
                                P.pe(lambda e, h=h, kt=kt, n=n, eb=eb, po=po, half=half, first=first, lastk=lastk: e.matmul(
                                    PS[po][:, 0:n], V[:, kt, h * 128:(h + 1) * 128], Eb[eb][:, half, 0:n],
                                    start=first, stop=lastk), reads=[("V", kt), "Eb%d" % eb], writes=[PK[po]])
                                P.pe(lambda e, n=n, eb=eb, pz=pz, half=half, first=first, lastk=lastk: e.matmul(
                                    PS[pz][:, 0:n], ones_b[:], Eb[eb][:, half, 0:n], start=first, stop=lastk),
                                    reads=["ones_b", "Eb%d" % eb], writes=[PK[pz]])
                        ecnt += len(its)
                        P.dve(lambda e, n=n: e.reciprocal(R0[:, 0:n], PS[3][:, 0:n]), reads=[PK[3]], writes=["aR0"])
                        P.dve(lambda e, n=n: e.tensor_tensor(out=R0[:, 0:n], in0=PS[2][:, 0:n], in1=R0[:, 0:n], op=ALU.mult),
                              reads=[PK[2], "aR0"], writes=["aR0"])
                        P.dve(lambda e, n=n: e.reciprocal(R1[:, 0:n], PS[5][:, 0:n]), reads=[PK[5]], writes=["aR1"])
                        P.dve(lambda e, n=n: e.tensor_tensor(out=R1[:, 0:n], in0=PS[4][:, 0:n], in1=R1[:, 0:n], op=ALU.mult),
                              reads=[PK[4], "aR1"], writes=["aR1"])
                        P.dve(lambda e, n=n: e.scalar_tensor_tensor(out=OO[:, 0:n], in0=R1[:, 0:n], scalar=small[:, 4:5],
                                                                     in1=R0[:, 0:n], op0=ALU.mult, op1=ALU.add),
                              reads=["aR0", "aR1", "asmall"], writes=["aOO"])
                        P.act(lambda e, n=n: e.activation(out=SQ2[:, 0:n], in_=OO[:, 0:n], func=AF.Square),
                              reads=["aOO"], writes=["aSQ2"])
                        P.pe(lambda e, n=n: e.matmul(PS[0][:, 0:n], ones_f[:], SQ2[:, 0:n], start=True, stop=True),
                             reads=["aSQ2", "ones_f"], writes=[PK[0]])
                        P.act(lambda e, n=n: e.activation(out=SQ2[:, 0:n], in_=PS[0][:, 0:n], func=AF.Sqrt,
                                                          scale=1.0 / 128, bias=1e-5), reads=[PK[0]], writes=["aSQ2"])
                        P.dve(lambda e, n=n: e.reciprocal(SQ2[:, 0:n], SQ2[:, 0:n]), reads=["aSQ2"], writes=["aSQ2"])
                        P.dve(lambda e, h=h, q0=q0, n=n: e.scalar_tensor_tensor(
                            out=Y[:, h, q0:q0 + n], in0=OO[:, 0:n], scalar=GS[:, 0:1], in1=SQ2[:, 0:n],
                            op0=ALU.mult, op1=ALU.mult), reads=["aOO", "GS", "aSQ2"],
                            writes=[("Y", t) for t in range(q0 // 128, (q0 + n) // 128)])
        P.barrier()

    def phase_hyena(l, U, Y):
        i2 = l // 2
        tiles = _tok_tiles()
        wv = w_in_cd_d[i2].rearrange("(k p) n -> p k n", p=128)
        PI = math.pi
        with ExitStack() as es:
            h_tok = sb(es, "h_tok", [128, NTILE, 512], BF16)
            v_tok = sb(es, "v_tok", [128, NTILE, 512], BF16)
            GOc = sb(es, "GOc", [128, 4, TT], BF16)
            with ExitStack() as es2:
                zT = sb(es2, "zT", [33, TT])
                W1 = sb(es2, "hW1", [33, 64])
                W2 = sb(es2, "hW2", [64, 64])
                W3 = sb(es2, "hW3", [64, 512])
                HV = sb(es2, "hHV", [64, 3])
                HB = sb(es2, "hHB", [1, 512])
                HID = [sb(es2, "hHID%d" % i, [64, TT]) for i in range(2)]
                KI = sb(es2, "hKI", [64, 512], I32)
                KF = sb(es2, "hKF", [64, 512])
                AA = sb(es2, "hAA", [64, 512])
                WN = [sb(es2, "hWN%d" % i, [128, 512]) for i in range(2)]
                HT = [sb(es2, "hHT%d" % i, [128, 512]) for i in range(2)]
                P.dma(lambda e: e.dma_start(out=zT[:], in_=zT_d), writes=["zT"])
                P.dma(lambda e: e.dma_start(out=W1[:], in_=hfw1_d[i2]), writes=["hW1"])
                P.dma(lambda e: e.dma_start(out=W2[:], in_=hfw2_d[i2]), writes=["hW2"])
                P.dma(lambda e: e.dma_start(out=W3[:], in_=hfw3_d[i2]), writes=["hW3"])
                P.dma(lambda e: e.dma_start(out=HV[:], in_=hfv_d[i2]), writes=["hHV"])
                P.dma(lambda e: e.dma_start(out=HB[:], in_=hfb_d[i2:i2 + 1, :]), writes=["hHB"])
                for layer in range(2):
                    src = zT if layer == 0 else HID[0]
                    srck = "zT" if layer == 0 else "hHID0"
                    kdim = 33 if layer == 0 else 64
                    wm = W1 if layer == 0 else W2
                    wmk = "hW1" if layer == 0 else "hW2"
                    bcol = 0 if layer == 0 else 2
                    dst = HID[layer]
                    dk = "hHID%d" % layer
                    for ti, (t0, n) in enumerate(tiles):
                        pb = ti % 2
                        P.pe(lambda e, src=src, kdim=kdim, wm=wm, t0=t0, n=n, pb=pb: e.matmul(
                            PS[pb][0:64, 0:n], wm[0:kdim, :], src[0:kdim, t0:t0 + n], start=True, stop=True),
                            reads=[srck, wmk], writes=[PK[pb]])
                        P.dve(lambda e, n=n, pb=pb, bcol=bcol: e.tensor_scalar(
                            out=AA[:, 0:n], in0=PS[pb][0:64, 0:n], scalar1=HV[:, bcol:bcol + 1], scalar2=HV[:, 1:2],
                            op0=ALU.add, op1=ALU.mult), reads=[PK[pb], "hHV"], writes=["hAA"])
                        P.dve(lambda e, n=n: e.tensor_scalar(out=KI[:, 0:n], in0=AA[:, 0:n], scalar1=1.0 / (2 * PI),
                                                             scalar2=None, op0=ALU.mult), reads=["hAA"], writes=["hKI"])
                        P.dve(lambda e, n=n: e.tensor_copy(KF[:, 0:n], KI[:, 0:n]), reads=["hKI"], writes=["hKF"])
                        P.dve(lambda e, n=n: e.scalar_tensor_tensor(out=AA[:, 0:n], in0=KF[:, 0:n], scalar=-2 * PI,
                                                                     in1=AA[:, 0:n], op0=ALU.mult, op1=ALU.add),
                              reads=["hKF", "hAA"], writes=["hAA"])
                        P.dve(lambda e, n=n: e.tensor_scalar(out=KF[:, 0:n], in0=AA[:, 0:n], scalar1=PI, scalar2=-2 * PI,
                                                             op0=ALU.is_gt, op1=ALU.mult), reads=["hAA"], writes=["hKF"])
                        P.dve(lambda e, n=n: e.tensor_tensor(out=AA[:, 0:n], in0=AA[:, 0:n], in1=KF[:, 0:n], op=ALU.add),
                              reads=["hAA", "hKF"], writes=["hAA"])
                        P.dve(lambda e, n=n: e.tensor_scalar(out=KF[:, 0:n], in0=AA[:, 0:n], scalar1=-PI, scalar2=2 * PI,
                                                             op0=ALU.is_lt, op1=ALU.mult), reads=["hAA"], writes=["hKF"])
                        P.dve(lambda e, n=n: e.tensor_tensor(out=AA[:, 0:n], in0=AA[:, 0:n], in1=KF[:, 0:n], op=ALU.add),
                              reads=["hAA", "hKF"], writes=["hAA"])
                        P.dve(lambda e, n=n: e.tensor_scalar(out=AA[:, 0:n], in0=AA[:, 0:n], scalar1=-PI, scalar2=PI,
                                                             op0=ALU.max, op1=ALU.min), reads=["hAA"], writes=["hAA"])
                        P.act(lambda e, dst=dst, t0=t0, n=n: e.activation(out=dst[:, t0:t0 + n], in_=AA[:, 0:n], func=AF.Sin),
                              reads=["hAA"], writes=[dk])
                for tt in range(NTILE):
                    b = tt % 2
                    pb = 2 + b
                    P.dma(lambda e, b=b, tt=tt: e.dma_start(out=WN[b][:], in_=win_d[tt * 128:(tt + 1) * 128, :]),
                          writes=["hWN%d" % b])
                    P.pe(lambda e, tt=tt, pb=pb: e.matmul(PS[pb][:], HID[1][:, tt * 128:(tt + 1) * 128], W3[:],
                                                          start=True, stop=True), reads=["hHID1", "hW3"], writes=[PK[pb]])
                    centre = tt in (1, 2 + 8)
                    if centre:
                        P.dve(lambda e, b=b, pb=pb: e.tensor_tensor(out=HT[b][:], in0=PS[pb][:], in1=WN[b][:], op=ALU.mult),
                              reads=[PK[pb], "hWN%d" % b], writes=["hHT%d" % b])
                        P.dve(lambda e, b=b: e.tensor_tensor(out=HT[b][0:1, :], in0=HT[b][0:1, :], in1=HB[:], op=ALU.add),
                              reads=["hHT%d" % b, "hHB"], writes=["hHT%d" % b])
                        P.act(lambda e, b=b, tt=tt: e.activation(out=h_tok[:, tt, :], in_=HT[b][:], func=AF.Copy),
                              reads=["hHT%d" % b], writes=[("h_tok", tt)])
                    else:
                        P.dve(lambda e, b=b, pb=pb, tt=tt: e.tensor_tensor(out=h_tok[:, tt, :], in0=PS[pb][:], in1=WN[b][:],
                                                                            op=ALU.mult),
                              reads=[PK[pb], "hWN%d" % b], writes=[("h_tok", tt)])
            P.barrier()
            with ExitStack() as es2:
                Wt = [sb(es2, "hWt%d" % i, [128, 8, 512], BF16) for i in range(3)]
                CD = sb(es2, "CD", [128, 12, 3])
                Bf = [sb(es2, "hB%d" % i, [128, TT]) for i in range(3)]
                P.dma(lambda e: e.dma_start(out=CD[:], in_=cd_d[i2]), writes=["CD"])
                for g in range(3):
                    load_wgroup(Wt[g], "hWt%d" % g, wv, 3 + g)
                pcnt = [0]

                def projc(g, c, raw, rawk, dst, dstk):
                    for (t0, n) in tiles:
                        pb = pcnt[0] % 4
                        pcnt[0] += 1
                        for k in range(8):
                            P.pe(lambda e, g=g, c=c, k=k, t0=t0, n=n, pb=pb: e.matmul(
                                PS[pb][:, 0:n], Wt[g][:, k, c * 128:(c + 1) * 128], U[:, k, t0:t0 + n],
                                start=(k == 0), stop=(k == 7)),
                                reads=[("hWt%d" % g, k)] + [("U", t) for t in range(t0 // 128, (t0 + n) // 128)],
                                writes=[PK[pb]])
                        P.act(lambda e, t0=t0, n=n, pb=pb: e.activation(out=raw[:, t0:t0 + n], in_=PS[pb][:, 0:n],
                                                                          func=AF.Copy), reads=[PK[pb]], writes=[rawk])
                    ch = g * 4 + c
                    P.dve(lambda e: e.tensor_scalar(out=dst[:], in0=raw[:], scalar1=CD[:, ch, 1:2], scalar2=None,
                                                    op0=ALU.mult), reads=[rawk, "CD"], writes=[dstk])
                    for (s0, L) in SEQS:
                        P.dve(lambda e, s0=s0, L=L: e.scalar_tensor_tensor(
                            out=dst[:, s0 + 1:s0 + L], in0=raw[:, s0:s0 + L - 1], scalar=CD[:, ch, 0:1],
                            in1=dst[:, s0 + 1:s0 + L], op0=ALU.mult, op1=ALU.add), reads=[rawk, dstk, "CD"], writes=[dstk])
                        P.dve(lambda e, s0=s0, L=L: e.scalar_tensor_tensor(
                            out=dst[:, s0:s0 + L - 1], in0=raw[:, s0 + 1:s0 + L], scalar=CD[:, ch, 2:3],
                            in1=dst[:, s0:s0 + L - 1], op0=ALU.mult, op1=ALU.add), reads=[rawk, dstk, "CD"], writes=[dstk])

                for c in range(4):
                    projc(0, c, Bf[0], "hB0", Bf[1], "hB1")
                    P.act(lambda e, c=c: e.activation(out=GOc[:, c, :], in_=Bf[1][:], func=AF.Copy),
                          reads=["hB1"], writes=[("GOc", c)])
                    projc(1, c, Bf[0], "hB0", Bf[1], "hB1")
                    projc(2, c, Bf[0], "hB0", Bf[2], "hB2")
                    P.pool(lambda e: e.tensor_tensor(out=Bf[1][:], in0=Bf[1][:], in1=Bf[2][:], op=ALU.mult),
                           reads=["hB1", "hB2"], writes=["hB1"])
                    for t4 in range(0, NTILE, 4):
                        nt = min(4, NTILE - t4)
                        pb = 4 + (t4 // 4) % 2
                        for j in range(nt):
                            P.pe(lambda e, t4=t4, j=j, pb=pb: e.transpose(
                                PS[pb][:, j * 128:(j + 1) * 128], Bf[1][:, (t4 + j) * 128:(t4 + j + 1) * 128], ident_f[:]),
                                reads=["hB1", "ident_f"], writes=[PK[pb]])
                        P.act(lambda e, t4=t4, nt=nt, pb=pb, c=c: e.activation(
                            out=v_tok[:, t4:t4 + nt, c * 128:(c + 1) * 128],
                            in_=PS[pb][:, 0:nt * 128].rearrange("p (j t) -> p j t", j=nt), func=AF.Copy),
                            reads=[PK[pb]], writes=[("v_tok", c)])
            P.barrier()
            with ExitStack() as es2:
                YS = sb(es2, "YS", [128, 24, 512], BF16)
                YSc = sb(es2, "YSc", [128, 4, 512], BF16)
                es3 = ExitStack()
                FW = [sb(es3, "FW%d" % i, [128, 16, 256], BF16) for i in range(2)]
                HH = [sb(es3, "HH%d" % i, [128, 2, 512]) for i in range(2)]
                T1 = sb(es3, "hT1", [128, 512])
                T2 = sb(es3, "hT2", [128, 512])
                T3 = sb(es3, "hT3", [128, 512])
                T4 = sb(es3, "hT4", [128, 512])
                fcnt = 0
                for (npair, tt0, ntt, fwd, ys, ysk) in ((12, 2, 16, fwl_d, YS, "YS"), (2, 0, 2, fwc_d, YSc, "YSc")):
                    for i in range(npair):
                        fb = fcnt % 2
                        fcnt += 1
                        fw = FW[fb]
                        fwk = "FW%d" % fb
                        hh = HH[fb]
                        hhk = "HH%d" % fb
                        P.dma(lambda e, fw=fw, fwd=fwd, i=i, ntt=ntt: e.dma_start(out=fw[:, 0:ntt, :], in_=fwd[i]),
                              writes=[fwk])
                        pbase = 4 * fb
                        for (srct, srck, pr, pi) in ((h_tok, "h_tok", pbase, pbase + 1), (v_tok, "v_tok", pbase + 2, pbase + 3)):
                            for part, pb in ((0, pr), (1, pi)):
                                for tt in range(ntt):
                                    rk = [("h_tok", tt0 + tt)] if srct is h_tok else [("v_tok", c) for c in range(4)]
                                    P.pe(lambda e, fw=fw, tt=tt, part=part, pb=pb, srct=srct, tt0=tt0, ntt=ntt: e.matmul(
                                        PS[pb][:], fw[:, tt, part * 128:(part + 1) * 128], srct[:, tt0 + tt, :],
                                        start=(tt == 0), stop=(tt == ntt - 1)), reads=[fwk] + rk, writes=[PK[pb]])
                            if srct is h_tok:
                                P.act(lambda e, hh=hh, pbase=pbase: e.activation(out=hh[:, 0, :], in_=PS[pbase][:], func=AF.Copy),
                                      reads=[PK[pbase]], writes=[hhk])
                                P.act(lambda e, hh=hh, pbase=pbase: e.activation(out=hh[:, 1, :], in_=PS[pbase + 1][:], func=AF.Copy),
                                      reads=[PK[pbase + 1]], writes=[hhk])
                        nch = npair
                        pu, pv = pbase + 2, pbase + 3
                        P.dve(lambda e, hh=hh, pu=pu: e.tensor_tensor(out=T1[:], in0=PS[pu][:], in1=hh[:, 0, :], op=ALU.mult),
                              reads=[PK[pu], hhk], writes=["hT1"])
                        P.dve(lambda e, hh=hh, pv=pv: e.tensor_tensor(out=T2[:], in0=PS[pv][:], in1=hh[:, 1, :], op=ALU.mult),
                              reads=[PK[pv], hhk], writes=["hT2"])
                        P.pool(lambda e, ys=ys, i=i: e.tensor_tensor(out=ys[:, i, :], in0=T1[:], in1=T2[:], op=ALU.subtract),
                               reads=["hT1", "hT2"], writes=[(ysk, i)])
                        P.dve(lambda e, hh=hh, pu=pu: e.tensor_tensor(out=T3[:], in0=PS[pu][:], in1=hh[:, 1, :], op=ALU.mult),
                              reads=[PK[pu], hhk], writes=["hT3"])
                        P.dve(lambda e, hh=hh, pv=pv: e.tensor_tensor(out=T4[:], in0=PS[pv][:], in1=hh[:, 0, :], op=ALU.mult),
                              reads=[PK[pv], hhk], writes=["hT4"])
                        P.pool(lambda e, ys=ys, i=i, nch=nch: e.tensor_tensor(out=ys[:, nch + i, :], in0=T3[:], in1=T4[:],
                                                                               op=ALU.add),
                               reads=["hT3", "hT4"], writes=[(ysk, nch + i)])
                P.barrier()
                es3.close()
                IV = [sb(es2, "IV%d" % i, [128, 12, 512], BF16) for i in range(2)]
                icnt = 0
                for j in range(4):
                    for half in range(2):
                        ib = icnt % 2
                        icnt += 1
                        P.dma(lambda e, ib=ib, j=j, half=half: e.dma_start(out=IV[ib][:], in_=invl_d[j, half]),
                              writes=["IV%d" % ib])
                        for c in range(4):
                            pc = 4 * (j % 2) + c
                            for fc in range(12):
                                P.pe(lambda e, ib=ib, half=half, c=c, fc=fc, pc=pc: e.matmul(
                                    PS[pc][:], YS[:, half * 12 + fc, c * 128:(c + 1) * 128], IV[ib][:, fc, :],
                                    start=(half == 0 and fc == 0), stop=(half == 1 and fc == 11)),
                                    reads=[("YS", half * 12 + fc), "IV%d" % ib], writes=[PK[pc]])
                    t0 = TC + j * 512
                    for c in range(4):
                        pc = 4 * (j % 2) + c
                        P.dve(lambda e, c=c, t0=t0, pc=pc: e.tensor_tensor(out=Y[:, 4 + c, t0:t0 + 512], in0=PS[pc][:],
                                                                           in1=GOc[:, c, t0:t0 + 512], op=ALU.mult),
                              reads=[PK[pc], ("GOc", c)], writes=[("Y", t) for t in range(t0 // 128, t0 // 128 + 4)])
                IVc = [sb(es2, "IVc%d" % i, [128, 2, 256], BF16) for i in range(2)]
                for half in range(2):
                    P.dma(lambda e, half=half: e.dma_start(out=IVc[half][:], in_=invc_d[half]), writes=["IVc%d" % half])
                for c in range(4):
                    for half in range(2):
                        for fc in range(2):
                            P.pe(lambda e, half=half, c=c, fc=fc: e.matmul(
                                PS[4 + c][:, 0:TC], YSc[:, half * 2 + fc, c * 128:(c + 1) * 128], IVc[half][:, fc, :],
                                start=(half == 0 and fc == 0), stop=(half == 1 and fc == 1)),
                                reads=[("YSc", half * 2 + fc), "IVc%d" % half], writes=[PK[4 + c]])
                    P.dve(lambda e, c=c: e.tensor_tensor(out=Y[:, 4 + c, 0:TC], in0=PS[4 + c][:, 0:TC], in1=GOc[:, c, 0:TC],
                                                         op=ALU.mult),
                          reads=[PK[4 + c], ("GOc", c)], writes=[("Y", 0), ("Y", 1)])
        P.barrier()

    NWB = 9

    def make_loader(WB):
        cnt = [0]

        def load_w(src_ap):
            i = cnt[0] % NWB
            cnt[0] += 1
            wv = src_ap.rearrange("(k p) n -> p k n", p=128)
            for k in range(8):
                P.dma(lambda e, i=i, k=k: e.dma_start(out=WB[i][:, k, :], in_=wv[:, k, :]),
                      writes=[("WB", i, k)], q="pool")
            return i

        def load_expert(l, ex):
            return (load_w(w_gate_d[l, ex]), load_w(w_up_d[l, ex]), load_w(w_down_d[l, ex]))

        return load_expert

    def phase_moe(l, AFF, last, WB, load_expert, wq):
        outs = []
        with ExitStack() as es:
            gP = sb(es, "gP", [128, 3, NE])
            iP = sb(es, "iP", [128, 3, NE], I32)
            P.dve(lambda e: e.memset(iP[:], 0), writes=["iP"])
            for tt in range(NTILE):
                P.dma(lambda e, tt=tt: e.dma_start(out=M_d[tt * 128:(tt + 1) * 128, :], in_=zero_f[:]),
                      reads=["zero_f"], writes=["Macc"])
            with ExitStack() as es1:
                affT = sb(es1, "affT", [NE, TT])
                work = sb(es1, "mwork", [NE, TT])
                GV = sb(es1, "GV", [NE, NSLOT])
                IXu = sb(es1, "IXu", [NE, NSLOT], U32)
                IXf = sb(es1, "IXf", [NE, NSLOT])
                for t4 in range(0, NTILE, 4):
                    nt = min(4, NTILE - t4)
                    pb = (t4 // 4) % 2
                    for j in range(nt):
                        P.pe(lambda e, t4=t4, j=j, pb=pb: e.transpose(PS[pb][0:NE, j * 128:(j + 1) * 128],
                                                                       AFF[:, t4 + j, :], ident_f[:]),
                             reads=[("AFF", t4 + j), "ident_f"], writes=[PK[pb]])
                    P.dve(lambda e, t4=t4, nt=nt, pb=pb: e.tensor_copy(affT[:, t4 * 128:(t4 + nt) * 128],
                                                                        PS[pb][0:NE, 0:nt * 128]),
                          reads=[PK[pb]], writes=["affT"])
                P.dve(lambda e: e.tensor_copy(work[:], affT[:]), reads=["affT"], writes=["mwork"])
                for (s0, L, cap, c0) in ((TC, T, CAP_L, 0), (0, TC, CAP_C, CAP_L)):
                    for rd in range(cap // 8):
                        sl = slice(c0 + rd * 8, c0 + rd * 8 + 8)
                        P.dve(lambda e, s0=s0, L=L, sl=sl: e.max(out=GV[:, sl], in_=work[:, s0:s0 + L]),
                              reads=["mwork"], writes=["GV"])
                        P.dve(lambda e, s0=s0, L=L, sl=sl: e.max_index(out=IXu[:, sl], in_max=GV[:, sl],
                                                                       in_values=work[:, s0:s0 + L]),
                              reads=["mwork", "GV"], writes=["IXu"])
                        P.dve(lambda e, s0=s0, L=L, sl=sl: e.match_replace(out=work[:, s0:s0 + L], in_to_replace=GV[:, sl],
                                                                           in_values=work[:, s0:s0 + L], imm_value=-1.0),
                              reads=["mwork", "GV"], writes=["mwork"])
                P.dve(lambda e: e.tensor_copy(IXf[:], IXu[:]), reads=["IXu"], writes=["IXf"])
                P.dve(lambda e: e.tensor_scalar(out=IXf[:, 0:CAP_L], in0=IXf[:, 0:CAP_L], scalar1=float(TC), scalar2=None,
                                                op0=ALU.add), reads=["IXf"], writes=["IXf"])
                for (src, dst, dk, pb) in ((IXf, iP, "iP", 2), (GV, gP, "gP", 3)):
                    sk = "IXf" if src is IXf else "GV"
                    for s in range(3):
                        n = 128 if s < 2 else CAP_C
                        P.pe(lambda e, src=src, s=s, n=n, pb=pb: e.transpose(PS[pb][0:n, s * NE:(s + 1) * NE],
                                                                              src[:, s * 128:s * 128 + n], ident_f[0:NE, 0:NE]),
                             reads=[sk, "ident_f"], writes=[PK[pb]])
                    P.dve(lambda e, dst=dst, pb=pb: e.tensor_copy(dst[:, 0:2, :],
                                                                   PS[pb][:, 0:2 * NE].rearrange("p (s e) -> p s e", s=2)),
                          reads=[PK[pb]], writes=[dk])
                    P.dve(lambda e, dst=dst, pb=pb: e.tensor_copy(dst[0:CAP_C, 2, :], PS[pb][0:CAP_C, 2 * NE:3 * NE]),
                          reads=[PK[pb]], writes=[dk])
            P.barrier()
            with ExitStack() as es2:
                xe = [sb(es2, "xe%d" % i, [128, 3, D], BF16) for i in range(2)]
                xeT = [sb(es2, "xeT%d" % i, [128, 8, NSLOT], BF16) for i in range(2)]
                hT = [sb(es2, "hT%d" % i, [128, 8, NSLOT], BF16) for i in range(2)]
                sg = [sb(es2, "sg%d" % i, [128, NSLOT]) for i in range(2)]
                yb = [sb(es2, "yb%d" % i, [128, 3, D]) for i in range(2)]

                def gather(ex_i):
                    b = ex_i % 2
                    for s in range(3):
                        n = 128 if s < 2 else CAP_C
                        P.dma(lambda e, b=b, s=s, n=n, ex_i=ex_i: e.indirect_dma_start(
                            out=xe[b][0:n, s, :], out_offset=None, in_=u2tok_d,
                            in_offset=bass.IndirectOffsetOnAxis(ap=iP[0:n, s, ex_i:ex_i + 1], axis=0)),
                            reads=["iP"] + [("u2tok", t) for t in range(NTILE)], writes=[("xe%d" % b, s)], q="pool")

                gather(0)
                for ex_i in range(NE):
                    b = ex_i % 2
                    kxe, kxT, khT, ksg, kyb = "xe%d" % b, "xeT%d" % b, "hT%d" % b, "sg%d" % b, "yb%d" % b
                    wg, wu, wd = wq[ex_i]
                    for s in range(3):
                        n = 128 if s < 2 else CAP_C
                        pb = 4 + (s % 2)
                        pv = PS[pb][:].bitcast(BF16)
                        for k in range(8):
                            P.pe(lambda e, b=b, s=s, n=n, k=k, pv=pv: e.transpose(
                                pv[:, k * 128:k * 128 + n], xe[b][0:n, s, k * 128:(k + 1) * 128], ident_b[0:n, 0:n]),
                                reads=[(kxe, s), "ident_b"], writes=[PK[pb]])
                        P.act(lambda e, b=b, s=s, n=n, pv=pv: e.activation(
                            out=xeT[b][:, :, s * 128:s * 128 + n],
                            in_=pv.rearrange("p (k t) -> p k t", k=8)[:, :, 0:n], func=AF.Copy),
                            reads=[PK[pb]], writes=[kxT])
                    if ex_i + 1 < NE:
                        gather(ex_i + 1)
                    for f in range(8):
                        pa, pu = 0 + (f % 2) * 2, 1 + (f % 2) * 2
                        for k in range(8):
                            P.pe(lambda e, b=b, f=f, k=k, pa=pa, wg=wg: e.matmul(
                                PS[pa][:, 0:NSLOT], WB[wg][:, k, f * 128:(f + 1) * 128], xeT[b][:, k, :],
                                start=(k == 0), stop=(k == 7)), reads=[("WB", wg, k), kxT], writes=[PK[pa]])
                        for k in range(8):
                            P.pe(lambda e, b=b, f=f, k=k, pu=pu, wu=wu: e.matmul(
                                PS[pu][:, 0:NSLOT], WB[wu][:, k, f * 128:(f + 1) * 128], xeT[b][:, k, :],
                                start=(k == 0), stop=(k == 7)), reads=[("WB", wu, k), kxT], writes=[PK[pu]])
                        P.act(lambda e, b=b, pa=pa: e.activation(out=sg[b][:], in_=PS[pa][:, 0:NSLOT], func=AF.Silu),
                              reads=[PK[pa]], writes=[ksg])
                        P.dve(lambda e, b=b, f=f, pu=pu: e.tensor_tensor(out=hT[b][:, f, :], in0=sg[b][:],
                                                                          in1=PS[pu][:, 0:NSLOT], op=ALU.mult),
                              reads=[ksg, PK[pu]], writes=[khT])
                    for s in range(3):
                        n = 128 if s < 2 else CAP_C
                        for hf in range(2):
                            pb = 6 + hf
                            for k in range(8):
                                P.pe(lambda e, b=b, s=s, n=n, hf=hf, k=k, pb=pb, wd=wd: e.matmul(
                                    PS[pb][0:n, :], hT[b][:, k, s * 128:s * 128 + n], WB[wd][:, k, hf * 512:(hf + 1) * 512],
                                    start=(k == 0), stop=(k == 7)), reads=[khT, ("WB", wd, k)], writes=[PK[pb]])
                            P.dve(lambda e, b=b, s=s, n=n, hf=hf, pb=pb, ex_i=ex_i: e.tensor_scalar(
                                out=yb[b][0:n, s, hf * 512:(hf + 1) * 512], in0=PS[pb][0:n, :],
                                scalar1=gP[0:n, s, ex_i:ex_i + 1], scalar2=None, op0=ALU.mult),
                                reads=[PK[pb], "gP"], writes=[(kyb, s)])
                        P.dma(lambda e, b=b, s=s, n=n, ex_i=ex_i: e.indirect_dma_start(
                            out=M_d, out_offset=bass.IndirectOffsetOnAxis(ap=iP[0:n, s, ex_i:ex_i + 1], axis=0),
                            in_=yb[b][0:n, s, :], in_offset=None, compute_op=ALU.add),
                            reads=[(kyb, s), "iP"], writes=["Macc"], q="pool")
                    if ex_i + 3 < NE:
                        wq.append(load_expert(l, ex_i + 3))
            P.barrier()
            with ExitStack() as es3:
                G = [sb(es3, "mG%d" % r, [128, D]) for r in range(2)]
                for r in range(2):
                    load_mod(G[r], "mG%d" % r, r, 5)
                xt = [sb(es3, "mx%d" % i, [128, D]) for i in range(2)]
                mt = [sb(es3, "mm%d" % i, [128, D]) for i in range(2)]
                for tt in range(NTILE):
                    b = tt % 2
                    r = 1 if tt < 2 else 0
                    kx, km = "mx%d" % b, "mm%d" % b
                    P.dma(lambda e, b=b, tt=tt: e.dma_start(out=xt[b][:], in_=H_d[tt * 128:(tt + 1) * 128, :]),
                          reads=[hkey(tt)], writes=[kx])
                    P.dma(lambda e, b=b, tt=tt: e.dma_start(out=mt[b][:], in_=M_d[tt * 128:(tt + 1) * 128, :]),
                          reads=["Macc"], writes=[km])
                    P.dve(lambda e, b=b, r=r: e.tensor_tensor(out=mt[b][:], in0=mt[b][:], in1=G[r][:], op=ALU.mult),
                          reads=[km, "mG%d" % r], writes=[km])
                    P.pool(lambda e, b=b: e.tensor_tensor(out=xt[b][:], in0=xt[b][:], in1=mt[b][:], op=ALU.add),
                           reads=[kx, km], writes=[kx])
                    if last:
                        if tt >= 2:
                            outs.append(P.dma(lambda e, b=b, tt=tt: e.dma_start(
                                out=out_d[(tt - 2) * 128:(tt - 1) * 128, :], in_=xt[b][:]), reads=[kx], writes=[("out", tt)]))
                    else:
                        P.dma(lambda e, b=b, tt=tt: e.dma_start(out=H_d[tt * 128:(tt + 1) * 128, :], in_=xt[b][:]),
                              reads=[kx], writes=[hkey(tt)])
        P.barrier()
        return outs

    finals = []
    for l in range(n_layers):
        last = (l == n_layers - 1)
        phase_mod(l)
        with ExitStack() as es:
            U = sb(es, "U", [128, 8, TT], BF16)
            Y = sb(es, "Y", [128, 8, TT], BF16)
            phase_norm(l, l, 1, U)
            if l % 2 == 0:
                phase_conv(l, U, Y)
                phase_outproj(l, Y, w_out_ab_d[l // 2])
            else:
                phase_attn(l, U, Y)
                phase_hyena(l, U, Y)
                phase_outproj(l, Y, w_out_cd_d[l // 2])
        with ExitStack() as es:
            AFF = sb(es, "AFF", [128, NTILE, NE])
            WB = [sb(es, "WB%d" % i, [128, 8, D], BF16) for i in range(NWB)]
            load_expert = make_loader(WB)
            wq = [load_expert(l, ex) for ex in range(3)]
            phase_norm(l, 1, 2, None, AFF=AFF)
            finals = phase_moe(l, AFF, last, WB, load_expert, wq)
    P.finalize(finals)
    top.close()
    return nc


def _host_inputs(inp):
    f = lambda a: np.ascontiguousarray(np.asarray(a, dtype=np.float32))
    sh = {}
    sh["w_ada"] = f(inp["w_ada"])
    sh["b_ada"] = f(inp["b_ada"])
    sh["g_mix"] = f(inp["g_mix"])
    sh["g_ffn"] = f(inp["g_ffn"])
    sh["w_in_ab"] = f(inp["w_in_ab"])
    sh["ca"] = f(np.asarray(inp["conv_a"]).reshape(2, 3, 4, 128).transpose(0, 3, 2, 1))
    sh["cb"] = f(np.asarray(inp["conv_b"]).reshape(2, 31, 4, 128).transpose(0, 3, 2, 1))
    lnp = np.stack([np.asarray(inp["conv_b_bias"]), np.asarray(inp["ln_b_g"]), np.asarray(inp["ln_b_b"])], axis=1)
    sh["lnp"] = f(lnp.reshape(2, 3, 4, 128).transpose(0, 3, 1, 2))
    sh["w_out_ab"] = f(inp["w_out_ab"])
    sh["w_in_cd"] = f(inp["w_in_cd"])
    sh["w_out_cd"] = f(inp["w_out_cd"])
    gq = np.asarray(inp["g_q"]); gk = np.asarray(inp["g_k"])
    sh["gqk"] = f(np.stack([np.tile(gq, (1, 2)), np.tile(gk, (1, 2))], axis=-1))
    sh["lamv"] = f(np.concatenate([np.asarray(inp[k]) for k in ("lam_q1", "lam_k1", "lam_q2", "lam_k2")], axis=1))
    sh["gsub"] = f(np.asarray(inp["g_subln"]).reshape(2, 128, 1))
    sh["cd"] = f(np.asarray(inp["conv_d"]).reshape(2, 3, 12, 128).transpose(0, 3, 2, 1))
    sh["hf_w1"] = f(inp["hf_w1"])
    sh["hfv"] = f(np.stack([np.asarray(inp["hf_b1"]), np.asarray(inp["hf_freq"]), np.asarray(inp["hf_b2"])], axis=-1))
    sh["hf_w2"] = f(inp["hf_w2"])
    sh["hf_w3"] = f(inp["hf_w3"])
    sh["hf_bias"] = f(inp["hf_bias"])
    sh["wr"] = f(np.asarray(inp["w_router"]).reshape(DEPTH, 8, 128, NE).transpose(0, 2, 1, 3))
    sh["w_gate"] = f(inp["w_gate"])
    sh["w_up"] = f(inp["w_up"])
    sh["w_down"] = f(inp["w_down"])
    sh.update(host_constants())
    return sh


def _core_inputs(inp, shared, b):
    m = dict(shared)
    m["x"] = np.ascontiguousarray(np.asarray(inp["x"][b], dtype=np.float32))
    m["ctx"] = np.ascontiguousarray(np.asarray(inp["ctx"][b], dtype=np.float32))
    cv = np.stack([np.asarray(inp["c"][b]), np.asarray(inp["c_ctx"])], axis=0)
    m["cT"] = np.ascontiguousarray(cv.reshape(2, 8, 128).transpose(2, 1, 0).astype(np.float32))
    return m


_NC_CACHE = {}


def kernel(**inputs):
    n = 8
    if "nc" not in _NC_CACHE:
        _NC_CACHE["nc"] = build(DEPTH)
    nc = _NC_CACHE["nc"]
    shared = _host_inputs(inputs)
    in_maps = [_core_inputs(inputs, shared, b) for b in range(n)]
    res = run_bass_kernel_spmd(nc, in_maps, core_ids=list(range(n)))
    return np.stack([np.asarray(r["out"]) for r in res.results], axis=0).astype(np.float32)
```
